# Optimizing a Trainium2 kernel written in Bass

```python
import jax
import jax.numpy as jnp
from jax import lax
import numpy as np

D_MODEL = 1024
BATCH = 2
SEQ = 8192
DEPTH = 2

GRID_W = 64
CTX_LEN = 256
N_AB_LAYERS = (DEPTH + 1) // 2
N_C_LAYERS = DEPTH // 2

RW_HEADS = 8
RW_HEAD_DIM = 64
RW_DIM = RW_HEADS * RW_HEAD_DIM
DECAY_LORA = 64
ICLR_LORA = 64
GATE_LORA = 128
RW_COLS = 3 * RW_DIM + 2 * DECAY_LORA + 2 * ICLR_LORA + GATE_LORA
RW_SPLITS = (RW_DIM, 2 * RW_DIM, 3 * RW_DIM, 3 * RW_DIM + DECAY_LORA, 3 * RW_DIM + 2 * DECAY_LORA,
             3 * RW_DIM + 2 * DECAY_LORA + ICLR_LORA, 3 * RW_DIM + 2 * DECAY_LORA + 2 * ICLR_LORA)
LN_X_EPS = 64e-5

MLA_HEADS = 8
MLA_NOPE = 64
MLA_ROPE = 32
MLA_V = 64
MLA_QK = MLA_NOPE + MLA_ROPE
MLA_Q_RANK = 384
MLA_KV_RANK = 256
MLA_COLS = MLA_Q_RANK + MLA_KV_RANK + MLA_ROPE
AB_IN = RW_COLS + MLA_COLS
AB_OUT = RW_DIM + MLA_HEADS * MLA_V

GQA_HEADS = 16
GQA_KV_HEADS = 4
GQA_HEAD_DIM = 64
GQA_IN = (GQA_HEADS + 2 * GQA_KV_HEADS) * GQA_HEAD_DIM

N_EXPERTS = 16
N_GROUPS = 4
EXPERTS_PER_GROUP = N_EXPERTS // N_GROUPS
TOP_K = 2
EXPERT_FF = 256
SHARED_FF = 256

Q_BLOCK = 128
ROPE_THETA = 10000.0
NORM_EPS = 1e-6

kernel_name = 'hybrid_rwkv7_mla_gqa_moe_dit'


def rmsnorm(x, g):
    xf = x.astype(jnp.float32)
    y = xf * lax.rsqrt(jnp.mean(xf * xf, axis=-1, keepdims=True) + NORM_EPS)
    return y.astype(x.dtype) * g


def ada_mods(cvec, w, b):
    m = jax.nn.silu(cvec) @ w + b
    return jnp.split(m[:, None, :], 6, axis=-1)


def modulate(h, shift, scale):
    return h * (1.0 + scale) + shift


def rope_1d(x, pos):
    half = x.shape[-1] // 2
    inv = ROPE_THETA ** (-jnp.arange(half, dtype=jnp.float32) / half)
    ang = pos[:, None] * inv[None, :]
    cos = jnp.cos(ang)[None, :, None, :].astype(x.dtype)
    sin = jnp.sin(ang)[None, :, None, :].astype(x.dtype)
    x1, x2 = x[..., :half], x[..., half:]
    return jnp.concatenate([x1 * cos - x2 * sin, x2 * cos + x1 * sin], axis=-1)


def axial_rope(x, row, col):
    h = x.shape[-1] // 2
    return jnp.concatenate([rope_1d(x[..., :h], row), rope_1d(x[..., h:], col)], axis=-1)


def ctx_attention(q, k, v, scale):
    B, L, H, d = q.shape
    Hk = k.shape[2]
    qg = q.reshape(B, L, Hk, H // Hk, d)
    s = jnp.einsum('bqkgd,bskd->bkgqs', qg, k).astype(jnp.float32) * scale
    p = jax.nn.softmax(s, axis=-1).astype(v.dtype)
    o = jnp.einsum('bkgqs,bskd->bqkgd', p, v)
    return o.reshape(B, L, H, v.shape[-1])


def latent_attention(q, k_lat, v_lat, k_ctx, v_ctx, scale):
    B, S, H, d = q.shape
    Hk = k_lat.shape[2]
    G = H // Hk
    dv = v_lat.shape[-1]
    n_ctx = k_ctx.shape[1]
    n_blk = S // Q_BLOCK
    qb = q.reshape(B, n_blk, Q_BLOCK, Hk, G, d).transpose(1, 0, 2, 3, 4, 5)

    def one_block(qblk):
        s_ctx = jnp.einsum('bqkgd,bskd->bkgqs', qblk, k_ctx)
        s_lat = jnp.einsum('bqkgd,bskd->bkgqs', qblk, k_lat)
        s = jnp.concatenate([s_ctx, s_lat], axis=-1).astype(jnp.float32) * scale
        p = jax.nn.softmax(s, axis=-1).astype(v_lat.dtype)
        return (jnp.einsum('bkgqs,bskd->bqkgd', p[..., :n_ctx], v_ctx)
                + jnp.einsum('bkgqs,bskd->bqkgd', p[..., n_ctx:], v_lat))

    o = lax.map(one_block, qb)
    return o.transpose(1, 0, 2, 3, 4, 5).reshape(B, S, H, dv)


def centred_shift_mix(p, mu):
    prev = jnp.pad(p[:, :-1], ((0, 0), (1, 0), (0, 0)))
    nxt = jnp.pad(p[:, 1:], ((0, 0), (0, 1), (0, 0)))
    return p + mu * (0.5 * (prev + nxt) - p)


def wkv7_scan(state0, r, w, k, v, a_vec, b_vec, reverse):
    xs = tuple(t.transpose(1, 0, 2, 3) for t in (r, w, k, v, a_vec, b_vec))

    def step(S, inp):
        r_t, w_t, k_t, v_t, a_t, b_t = inp
        sa = jnp.einsum('bhij,bhj->bhi', S, a_t)
        S = (S * w_t[:, :, None, :] + sa[..., None] * b_t[:, :, None, :]
             + v_t[..., None] * k_t[:, :, None, :])
        return S, jnp.einsum('bhij,bhj->bhi', S, r_t)

    s_fin, ys = lax.scan(step, state0, xs, reverse=reverse)
    return s_fin, ys.transpose(1, 0, 2, 3)


def rwkv_prep(p, prm):
    p = centred_shift_mix(p, prm['mu'])
    r, k, v, w1f, w1b, a1f, a1b, g1 = jnp.split(p, RW_SPLITS, axis=-1)
    B, L = p.shape[:2]

    def heads(t):
        return t.astype(jnp.float32).reshape(B, L, RW_HEADS, RW_HEAD_DIM)

    kk = heads(k * prm['k_k'])
    kk = kk * lax.rsqrt(jnp.sum(kk * kk, axis=-1, keepdims=True) + 1e-12)
    kf = k.astype(jnp.float32)
    dirs = []
    for d, (w1, a1) in enumerate(((w1f, a1f), (w1b, a1b))):
        z = (prm['w0'][d] + jnp.tanh(w1) @ prm['w2'][d]).astype(jnp.float32)
        decay = jnp.exp(-jnp.exp(-jax.nn.softplus(-z) - 0.5))
        a = jax.nn.sigmoid((prm['a0'][d] + a1 @ prm['a2'][d]).astype(jnp.float32))
        k_d = kf * (1.0 + (a - 1.0) * prm['k_a'])
        dirs.append((heads(decay), heads(k_d), -kk, kk * heads(a)))
    g = jax.nn.sigmoid(g1) @ prm['g2']
    return heads(r), heads(v), dirs, g


def rwkv_scans(r, v, dirs, states0):
    ys, finals = [], []
    for d, (decay, k_d, a_vec, b_vec) in enumerate(dirs):
        s_fin, y_d = wkv7_scan(states0[d], r, decay, k_d, v, a_vec, b_vec, reverse=(d == 1))
        ys.append(y_d)
        finals.append(s_fin)
    return ys[0] + ys[1], (finals[0], finals[1])


def rwkv_out(y, r, v, dirs, g, prm):
    B, L = y.shape[:2]
    mean = jnp.mean(y, axis=-1, keepdims=True)
    var = jnp.mean(jnp.square(y - mean), axis=-1, keepdims=True)
    yn = ((y - mean) * lax.rsqrt(var + LN_X_EPS)).reshape(B, L, RW_DIM) * prm['ln_w'] + prm['ln_b']
    k_sum = dirs[0][1] + dirs[1][1]
    bonus = jnp.sum(r * k_sum * prm['r_k'], axis=-1, keepdims=True) * v
    return ((yn + bonus.reshape(B, L, RW_DIM)) * g).astype(g.dtype)


def mla_qkv(p, prm, pos):
    B, L = p.shape[:2]
    q_a, kv_a, k_rope = jnp.split(p, [MLA_Q_RANK, MLA_Q_RANK + MLA_KV_RANK], axis=-1)
    q = (rmsnorm(q_a, prm['g_qa']) @ prm['w_q_up']).reshape(B, L, MLA_HEADS, MLA_QK)
    kv = (rmsnorm(kv_a, prm['g_kva']) @ prm['w_kv_up']).reshape(B, L, MLA_HEADS, MLA_NOPE + MLA_V)
    k_nope, v = kv[..., :MLA_NOPE], kv[..., MLA_NOPE:]
    k_rope = jnp.broadcast_to(k_rope[:, :, None, :], (B, L, MLA_HEADS, MLA_ROPE))
    k = jnp.concatenate([k_nope, k_rope], axis=-1)
    q = rmsnorm(q, prm['g_q'])
    k = rmsnorm(k, prm['g_k'])
    if pos is not None:
        row, col = pos
        q = jnp.concatenate([q[..., :MLA_NOPE], axial_rope(q[..., MLA_NOPE:], row, col)], axis=-1)
        k = jnp.concatenate([k[..., :MLA_NOPE], axial_rope(k[..., MLA_NOPE:], row, col)], axis=-1)
    return q, k, v


def mixer_ab(hc, hl, row, col, need_ctx, prm):
    B, S = hl.shape[:2]
    pc = hc @ prm['w_in']
    pl = hl @ prm['w_in']
    r_c, v_c, dirs_c, g_c = rwkv_prep(pc[..., :RW_COLS], prm)
    r_l, v_l, dirs_l, g_l = rwkv_prep(pl[..., :RW_COLS], prm)
    zero = jnp.zeros((B, RW_HEADS, RW_HEAD_DIM, RW_HEAD_DIM), jnp.float32)
    y_c, states_c = rwkv_scans(r_c, v_c, dirs_c, (zero, zero))
    y_l, _ = rwkv_scans(r_l, v_l, dirs_l, states_c)
    q_c, k_c, vm_c = mla_qkv(pc[..., RW_COLS:], prm, None)
    q_l, k_l, vm_l = mla_qkv(pl[..., RW_COLS:], prm, (row, col))
    scale = MLA_QK ** -0.5
    o_l = latent_attention(q_l, k_l, vm_l, k_c, vm_c, scale).reshape(B, S, MLA_HEADS * MLA_V)
    out_l = jnp.concatenate([rwkv_out(y_l, r_l, v_l, dirs_l, g_l, prm), o_l], axis=-1) @ prm['w_out']
    if not need_ctx:
        return None, out_l
    C = hc.shape[1]
    o_c = ctx_attention(q_c, k_c, vm_c, scale).reshape(B, C, MLA_HEADS * MLA_V)
    out_c = jnp.concatenate([rwkv_out(y_c, r_c, v_c, dirs_c, g_c, prm), o_c], axis=-1) @ prm['w_out']
    return out_c, out_l


def gqa_qkv(h, prm, pos):
    B, L = h.shape[:2]
    p = h @ prm['w_in']
    q, k, v = jnp.split(p, [GQA_HEADS * GQA_HEAD_DIM, (GQA_HEADS + GQA_KV_HEADS) * GQA_HEAD_DIM], axis=-1)
    q = rmsnorm(q.reshape(B, L, GQA_HEADS, GQA_HEAD_DIM), prm['g_q'])
    k = rmsnorm(k.reshape(B, L, GQA_KV_HEADS, GQA_HEAD_DIM), prm['g_k'])
    v = v.reshape(B, L, GQA_KV_HEADS, GQA_HEAD_DIM)
    if pos is not None:
        q = axial_rope(q, pos[0], pos[1])
        k = axial_rope(k, pos[0], pos[1])
    return q, k, v


def mixer_gqa(hc, hl, row, col, need_ctx, prm):
    B, S = hl.shape[:2]
    q_c, k_c, v_c = gqa_qkv(hc, prm, None)
    q_l, k_l, v_l = gqa_qkv(hl, prm, (row, col))
    scale = GQA_HEAD_DIM ** -0.5
    out_l = latent_attention(q_l, k_l, v_l, k_c, v_c, scale).reshape(B, S, GQA_HEADS * GQA_HEAD_DIM) @ prm['w_out']
    if not need_ctx:
        return None, out_l
    C = hc.shape[1]
    out_c = ctx_attention(q_c, k_c, v_c, scale).reshape(B, C, GQA_HEADS * GQA_HEAD_DIM) @ prm['w_out']
    return out_c, out_l


def moe_ffn(h, prm):
    B, L, D = h.shape
    t = h.reshape(B * L, D)
    scores = jax.nn.sigmoid((t @ prm['router_w']).astype(jnp.float32))
    grouped = (scores + prm['router_b']).reshape(-1, N_GROUPS, EXPERTS_PER_GROUP)
    group_score = jnp.sum(lax.top_k(grouped, 2)[0], axis=-1)
    grp = jnp.argmax(group_score, axis=-1)
    in_group = jnp.take_along_axis(grouped, grp[:, None, None], axis=1)[:, 0]
    _, local = lax.top_k(in_group, TOP_K)
    idx = grp[:, None] * EXPERTS_PER_GROUP + local
    wts = jnp.take_along_axis(scores, idx, axis=-1)
    wts = wts / jnp.sum(wts, axis=-1, keepdims=True)
    combine = jnp.einsum('tk,tke->te', wts, jax.nn.one_hot(idx, N_EXPERTS, dtype=jnp.float32)).astype(h.dtype)
    g = jnp.einsum('td,edf->tef', t, prm['w_gate'])
    u = jnp.einsum('td,edf->tef', t, prm['w_up'])
    y = jnp.einsum('tef,efd->td', jax.nn.silu(g) * u * combine[..., None], prm['w_down'])
    y = y + (jax.nn.silu(t @ prm['sh_gate']) * (t @ prm['sh_up'])) @ prm['sh_down']
    return y.reshape(B, L, D)


def setup_inputs(seed: int = 0) -> dict:
    key = jax.random.key(seed)
    ks = iter(jax.random.split(key, 64))

    def nrm(shape, scale):
        return jax.random.normal(next(ks), shape, jnp.float32) * scale

    def unif(shape, lo, hi):
        return jax.random.uniform(next(ks), shape, jnp.float32, lo, hi)

    def gain(shape):
        return 1.0 + nrm(shape, 0.05)

    D = D_MODEL
    A, Cn = N_AB_LAYERS, N_C_LAYERS
    return {
        'x': nrm((BATCH, SEQ, D), 1.0),
        'c': nrm((BATCH, D), 1.0),
        'ctx': nrm((BATCH, CTX_LEN, D), 1.0),
        'c_ctx': nrm((D,), 1.0),
        'ada_w': nrm((DEPTH, D, 6 * D), 0.5 * D ** -0.5),
        'ada_b': nrm((DEPTH, 6 * D), 0.02),
        'norm1_g': gain((DEPTH, D)),
        'norm2_g': gain((DEPTH, D)),
        'ab_w_in': nrm((A, D, AB_IN), D ** -0.5),
        'ab_w_out': nrm((A, AB_OUT, D), AB_OUT ** -0.5),
        'rw_mu': unif((A, RW_COLS), 0.0, 1.0),
        'rw_w0': unif((A, 2, RW_DIM), -6.0, -1.0),
        'rw_w2': nrm((A, 2, DECAY_LORA, RW_DIM), 0.5 * DECAY_LORA ** -0.5),
        'rw_a0': nrm((A, 2, RW_DIM), 0.5),
        'rw_a2': nrm((A, 2, ICLR_LORA, RW_DIM), 0.5 * ICLR_LORA ** -0.5),
        'rw_k_k': 0.85 + nrm((A, RW_DIM), 0.05),
        'rw_k_a': gain((A, RW_DIM)),
        'rw_r_k': nrm((A, RW_HEADS, RW_HEAD_DIM), 0.1),
        'rw_g2': nrm((A, GATE_LORA, RW_DIM), GATE_LORA ** -0.5),
        'rw_ln_w': gain((A, RW_DIM)),
        'rw_ln_b': nrm((A, RW_DIM), 0.02),
        'mla_g_qa': gain((A, MLA_Q_RANK)),
        'mla_w_q_up': nrm((A, MLA_Q_RANK, MLA_HEADS * MLA_QK), MLA_Q_RANK ** -0.5),
        'mla_g_kva': gain((A, MLA_KV_RANK)),
        'mla_w_kv_up': nrm((A, MLA_KV_RANK, MLA_HEADS * (MLA_NOPE + MLA_V)), MLA_KV_RANK ** -0.5),
        'mla_g_q': gain((A, MLA_QK)),
        'mla_g_k': gain((A, MLA_QK)),
        'gqa_w_in': nrm((Cn, D, GQA_IN), D ** -0.5),
        'gqa_w_out': nrm((Cn, GQA_HEADS * GQA_HEAD_DIM, D), (GQA_HEADS * GQA_HEAD_DIM) ** -0.5),
        'gqa_g_q': gain((Cn, GQA_HEAD_DIM)),
        'gqa_g_k': gain((Cn, GQA_HEAD_DIM)),
        'router_w': nrm((D, N_EXPERTS), D ** -0.5),
        'router_b': nrm((N_EXPERTS,), 0.01),
        'moe_w_gate': nrm((DEPTH, N_EXPERTS, D, EXPERT_FF), D ** -0.5),
        'moe_w_up': nrm((DEPTH, N_EXPERTS, D, EXPERT_FF), D ** -0.5),
        'moe_w_down': nrm((DEPTH, N_EXPERTS, EXPERT_FF, D), EXPERT_FF ** -0.5),
        'shared_w_gate': nrm((DEPTH, D, SHARED_FF), D ** -0.5),
        'shared_w_up': nrm((DEPTH, D, SHARED_FF), D ** -0.5),
        'shared_w_down': nrm((DEPTH, SHARED_FF, D), SHARED_FF ** -0.5),
    }


def reference(x, c, ctx, c_ctx, ada_w, ada_b, norm1_g, norm2_g, ab_w_in, ab_w_out, rw_mu, rw_w0,
              rw_w2, rw_a0, rw_a2, rw_k_k, rw_k_a, rw_r_k, rw_g2, rw_ln_w, rw_ln_b, mla_g_qa,
              mla_w_q_up, mla_g_kva, mla_w_kv_up, mla_g_q, mla_g_k, gqa_w_in, gqa_w_out, gqa_g_q,
              gqa_g_k, router_w, router_b, moe_w_gate, moe_w_up, moe_w_down, shared_w_gate,
              shared_w_up, shared_w_down):
    seq = x.shape[1]
    rows = seq // GRID_W
    row = jnp.repeat(jnp.arange(rows, dtype=jnp.float32), GRID_W)
    col = jnp.tile(jnp.arange(GRID_W, dtype=jnp.float32), rows)
    xl, xc = x, ctx
    for l in range(DEPTH):
        need_ctx = l < DEPTH - 1
        i = l // 2
        sh1_l, sc1_l, ga1_l, sh2_l, sc2_l, ga2_l = ada_mods(c, ada_w[l], ada_b[l])
        sh1_c, sc1_c, ga1_c, sh2_c, sc2_c, ga2_c = ada_mods(c_ctx[None, :], ada_w[l], ada_b[l])
        hl = modulate(rmsnorm(xl, norm1_g[l]), sh1_l, sc1_l)
        hc = modulate(rmsnorm(xc, norm1_g[l]), sh1_c, sc1_c)
        if l % 2 == 0:
            prm = dict(w_in=ab_w_in[i], w_out=ab_w_out[i], mu=rw_mu[i], w0=rw_w0[i], w2=rw_w2[i],
                       a0=rw_a0[i], a2=rw_a2[i], k_k=rw_k_k[i], k_a=rw_k_a[i], r_k=rw_r_k[i],
                       g2=rw_g2[i], ln_w=rw_ln_w[i], ln_b=rw_ln_b[i], g_qa=mla_g_qa[i],
                       w_q_up=mla_w_q_up[i], g_kva=mla_g_kva[i], w_kv_up=mla_w_kv_up[i],
                       g_q=mla_g_q[i], g_k=mla_g_k[i])
            oc, ol = mixer_ab(hc, hl, row, col, need_ctx, prm)
        else:
            prm = dict(w_in=gqa_w_in[i], w_out=gqa_w_out[i], g_q=gqa_g_q[i], g_k=gqa_g_k[i])
            oc, ol = mixer_gqa(hc, hl, row, col, need_ctx, prm)
        moe_prm = dict(router_w=router_w, router_b=router_b, w_gate=moe_w_gate[l], w_up=moe_w_up[l],
                       w_down=moe_w_down[l], sh_gate=shared_w_gate[l], sh_up=shared_w_up[l],
                       sh_down=shared_w_down[l])
        xl = xl + ga1_l * ol
        hl = modulate(rmsnorm(xl, norm2_g[l]), sh2_l, sc2_l)
        xl = xl + ga2_l * moe_ffn(hl, moe_prm)
        if need_ctx:
            xc = xc + ga1_c * oc
            hc = modulate(rmsnorm(xc, norm2_g[l]), sh2_c, sc2_c)
            xc = xc + ga2_c * moe_ffn(hc, moe_prm)
    return xl
```

```python
import numpy as np
import concourse.bass as bass
import concourse.mybir as mybir
from concourse.bass_utils import run_bass_kernel_spmd

F32 = mybir.dt.float32
BF16 = mybir.dt.bfloat16
I32 = mybir.dt.int32
AF = mybir.ActivationFunctionType
ALU = mybir.AluOpType
AX = mybir.AxisListType

EPOCH = 30000
NSLOT = {"sp": 24, "pool": 12, "act": 8}


class Buf:
    __slots__ = ("name", "lw", "rd", "excl")

    def __init__(self, name, excl=False):
        self.name = name
        self.lw = None
        self.rd = []
        self.excl = excl


class V:
    __slots__ = ("buf", "ap")

    def __init__(self, buf, ap):
        self.buf = buf
        self.ap = ap

    def __getitem__(self, idx):
        return V(self.buf, self.ap[idx])

    def rr(self, pat, **kw):
        return V(self.buf, self.ap.rearrange(pat, **kw))

    def bc(self, shape):
        return V(self.buf, self.ap.broadcast_to(shape))

    def un(self, axis):
        return V(self.buf, self.ap.unsqueeze(axis))

    def bitcast(self, dt):
        return V(self.buf, self.ap.bitcast(dt))

    @property
    def shape(self):
        return self.ap.shape


class Op:
    __slots__ = ("eng", "fn", "deps", "sig", "cnt", "isdma", "slot", "slotval", "prevval")

    def __init__(self, eng, fn, isdma):
        self.eng = eng
        self.fn = fn
        self.deps = set()
        self.sig = False
        self.cnt = 0
        self.isdma = isdma
        self.slot = None
        self.slotval = 0
        self.prevval = 0


class _Scope:
    def __init__(self, p):
        self.p = p

    def __enter__(self):
        self.p._scopes.append([])
        return self

    def __exit__(self, *a):
        p = self.p
        items = p._scopes.pop()
        ops = set(p._fence)
        for cm, b in items:
            if b.lw is not None:
                ops.add(b.lw)
            ops.update(b.rd)
        best = {}
        keep = []
        for i in ops:
            o = p.ops[i]
            if o.isdma:
                if o.slot not in best or best[o.slot] < i:
                    best[o.slot] = i
            else:
                if o.eng not in best or best[o.eng] < i:
                    best[o.eng] = i
        p._fence = keep + list(best.values())
        for cm, b in reversed(items):
            cm.__exit__(None, None, None)
        return False


class Prog:
    ENGS = ("pe", "act", "dve", "pool", "sp")

    def __init__(self):
        self.nc = bass.Bass("TRN2", target_bir_lowering=False)
        self.ops = []
        self._ctx = []
        self.ntile = 0
        self._scopes = []
        self._fence = []
        self._dcount = {q: 0 for q in NSLOT}

    def dram(self, name, shape, dt, kind):
        t = self.nc.dram_tensor(name, list(shape), dt, kind=kind)
        return V(Buf(name), t.ap())

    def sb(self, shape, dt, name=None):
        self.ntile += 1
        name = name or f"t{self.ntile}"
        cm = self.nc.sbuf_tensor(f"{name}_{self.ntile}", list(shape), dt)
        h = cm.__enter__()
        b = Buf(name)
        b.rd = list(self._fence)
        if self._scopes:
            self._scopes[-1].append((cm, b))
        else:
            self._ctx.append(cm)
        return V(b, h[:])

    def scope(self):
        return _Scope(self)

    def ps(self, shape, dt, name=None):
        self.ntile += 1
        name = name or f"p{self.ntile}"
        cm = self.nc.psum_tensor(f"{name}_{self.ntile}", list(shape), dt)
        h = cm.__enter__()
        self._ctx.append(cm)
        return V(Buf(name, excl=True), h[:])

    def op(self, eng, fn, reads, writes, isdma=False):
        i = len(self.ops)
        o = Op(eng, fn, isdma)
        rb = {id(v.buf): v.buf for v in reads if v is not None}
        wb = {id(v.buf): v.buf for v in writes if v is not None}
        for b in rb.values():
            if b.lw is not None:
                o.deps.add(b.lw)
            if b.excl:
                for r in b.rd:
                    if self.ops[r].eng != eng:
                        o.deps.add(r)
        for b in wb.values():
            if b.lw is not None:
                o.deps.add(b.lw)
            for r in b.rd:
                o.deps.add(r)
        o.deps.discard(i)
        for b in rb.values():
            if id(b) not in wb:
                b.rd.append(i)
        for b in wb.values():
            b.lw = i
            b.rd = []
        if isdma:
            k = self._dcount[eng]
            self._dcount[eng] += 1
            n = NSLOT[eng]
            o.slot = (eng, k % n)
            o.slotval = 16 * (k // n + 1)
            o.prevval = 16 * (k // n)
        self.ops.append(o)
        return i

    def dma(self, out, in_, q="sp", **kw):
        self.op(q, lambda e: e.dma_start(out=out.ap, in_=in_.ap, **kw), [in_], [out], isdma=True)

    def mm(self, out, lhsT, rhs, start=True, stop=True, **kw):
        self.op("pe", lambda e: e.matmul(out.ap, lhsT.ap, rhs.ap, start=start, stop=stop, **kw),
                [lhsT, rhs], [out])

    def tr(self, out, in_, ident):
        self.op("pe", lambda e: e.transpose(out.ap, in_.ap, ident.ap), [in_, ident], [out])

    def act(self, out, in_, func, bias=None, scale=None, accum=None):
        kw = {}
        rd = [in_]
        if bias is not None:
            if isinstance(bias, V):
                kw["bias"] = bias.ap
                rd.append(bias)
            else:
                kw["bias"] = bias
        if scale is not None:
            if isinstance(scale, V):
                kw["scale"] = scale.ap
                rd.append(scale)
            else:
                kw["scale"] = scale
        wr = [out]
        if accum is not None:
            kw["accum_out"] = accum.ap
            wr.append(accum)
        self.op("act", lambda e: e.activation(out.ap, in_.ap, func, **kw), rd, wr)

    def tt(self, out, a, b, op, eng="dve"):
        self.op(eng, lambda e: e.tensor_tensor(out.ap, a.ap, b.ap, op), [a, b], [out])

    def ts(self, out, a, s1, op0, s2=None, op1=None, eng="dve", accum=None):
        rd = [a]
        x1 = s1.ap if isinstance(s1, V) else s1
        x2 = s2.ap if isinstance(s2, V) else s2
        if isinstance(s1, V):
            rd.append(s1)
        if isinstance(s2, V):
            rd.append(s2)
        kw = {}
        wr = [out]
        if op1 is not None:
            kw["op1"] = op1
        if accum is not None:
            kw["accum_out"] = accum.ap
            wr.append(accum)
        self.op(eng, lambda e: e.tensor_scalar(out.ap, a.ap, x1, x2, op0, **kw), rd, wr)

    def stt(self, out, a, s, b, op0, op1, eng="dve"):
        rd = [a, b]
        x = s.ap if isinstance(s, V) else s
        if isinstance(s, V):
            rd.append(s)
        self.op(eng, lambda e: e.scalar_tensor_tensor(out.ap, a.ap, x, b.ap, op0, op1), rd, [out])

    def cp(self, out, in_, eng="dve"):
        if eng == "act":
            self.op("act", lambda e: e.copy(out.ap, in_.ap), [in_], [out])
        else:
            self.op(eng, lambda e: e.tensor_copy(out.ap, in_.ap), [in_], [out])

    def red(self, out, in_, op=ALU.add, axis=AX.X, eng="dve"):
        self.op(eng, lambda e: e.tensor_reduce(out.ap, in_.ap, axis, op), [in_], [out])

    def memset(self, out, val, eng="pool"):
        self.op(eng, lambda e: e.memset(out.ap, val), [], [out])

    def iota(self, out, pattern, base=0, cm=0):
        self.op("pool", lambda e: e.iota(out.ap, pattern, base=base, channel_multiplier=cm,
                                         allow_small_or_imprecise_dtypes=True), [], [out])

    def finish(self):
        nc = self.nc
        ops = self.ops
        last = {}
        for i, o in enumerate(ops):
            last[o.eng] = i
        fin = Op("sp", None, False)
        for e, i in last.items():
            fin.deps.add(i)
        lastslot = {}
        for i, o in enumerate(ops):
            if o.isdma:
                lastslot[o.slot] = i
        for i in lastslot.values():
            fin.deps.add(i)
        ops.append(fin)
        for o in ops:
            for j in o.deps:
                d = ops[j]
                if d.eng == "pe" and o.eng == "pe" and not o.isdma:
                    continue
                d.sig = True
        ccount = {e: 0 for e in self.ENGS}
        dcount = self._dcount
        for o in ops:
            if o.isdma:
                pass
            elif o.sig:
                ccount[o.eng] += 1
                o.cnt = ccount[o.eng]
        sems = {}
        cms = []

        def getsem(key):
            if key not in sems:
                cm = nc.semaphore(f"s_{key[0]}_{key[1]}")
                sems[key] = cm.__enter__()
                cms.append(cm)
            return sems[key]

        for e in self.ENGS:
            for ep in range(ccount[e] // EPOCH + 1):
                getsem((e, "c%d" % ep))
        for q, n in NSLOT.items():
            for s in range(min(n, dcount[q])):
                getsem((q, s))

        def compkey(o):
            ep = (o.cnt - 1) // EPOCH
            return (o.eng, "c%d" % ep), o.cnt - ep * EPOCH

        with nc.Block() as block:
            def emit_engine(ename, eng):
                known = {}
                for i, o in enumerate(ops):
                    if o.eng != ename:
                        continue
                    waits = {}
                    for j in o.deps:
                        d = ops[j]
                        if d.isdma:
                            key, val = d.slot, d.slotval
                        else:
                            if d.eng == "pe" and ename == "pe" and not o.isdma:
                                continue
                            key, val = compkey(d)
                        if waits.get(key, 0) < val:
                            waits[key] = val
                    if o.isdma and o.prevval > 0:
                        key = o.slot
                        if waits.get(key, 0) < o.prevval:
                            waits[key] = o.prevval
                    for key, val in waits.items():
                        if known.get(key, 0) >= val:
                            continue
                        known[key] = val
                        eng.wait_ge(getsem(key), val)
                    if o.fn is None:
                        continue
                    ins = o.fn(eng)
                    if o.isdma:
                        ins.then_inc(getsem(o.slot), 16)
                    elif o.sig:
                        key, _ = compkey(o)
                        ins.then_inc(getsem(key), 1)

            @block.tensor
            def _(e):
                emit_engine("pe", e)

            @block.scalar
            def _(e):
                emit_engine("act", e)

            @block.vector
            def _(e):
                emit_engine("dve", e)

            @block.gpsimd
            def _(e):
                emit_engine("pool", e)

            @block.sync
            def _(e):
                emit_engine("sp", e)
        self._cms = cms
        return nc


D = 1024
CTX = 256
NORM_EPS = 1e-6
THETA = 10000.0
import math


class Ctx:
    def __init__(self, p):
        self.p = p
        self.banks = [p.ps([128, 512], F32, f"bank{i}") for i in range(8)]
        self.bi = 0
        io = p.sb([128, 128], F32, "io")
        pi = p.sb([128, 1], F32, "pi")
        p.iota(io, [[1, 128]], base=0, cm=0)
        p.iota(pi, [[0, 1]], base=0, cm=1)
        self.pidx = pi
        self.identf = p.sb([128, 128], F32, "identf")
        p.ts(self.identf, io, pi[:, 0:1], ALU.is_equal)
        self.identb = p.sb([128, 128], BF16, "identb")
        p.cp(self.identb, self.identf)
        self.iof = io

    def bank(self):
        b = self.banks[self.bi % 8]
        self.bi += 1
        return b

    def load_fm(self, rows_v, n, eng_out="dve"):
        p = self.p
        tmp = p.sb([n, 128], F32, "lfm_tmp")
        p.dma(tmp, rows_v)
        bk = self.bank()
        p.tr(bk[:, 0:n], tmp, self.identf[0:n, 0:n])
        out = p.sb([128, n], F32, "lfm_out")
        p.cp(out, bk[:, 0:n], eng=eng_out)
        return out

    def rstd(self, ss, n, inv_d, eps):
        p = self.p
        p.ts(ss, ss, inv_d, ALU.mult, eps, ALU.add)
        p.act(ss, ss, AF.Ln)
        p.act(ss, ss, AF.Exp, scale=-0.5)


def run_prog(p, in_maps):
    nc = p.finish()
    res = run_bass_kernel_spmd(nc, in_maps, core_ids=list(range(len(in_maps))))
    return res.results


def build_phase0():
    p = Prog()
    cvec = p.dram("cvec", [2, D], F32, "ExternalInput")
    ada_w = p.dram("ada_w", [2, D, 6 * D], F32, "ExternalInput")
    ada_b = p.dram("ada_b", [2, 6 * D], F32, "ExternalInput")
    mods_tm = p.dram("mods_tm", [2, 2, 6 * D], F32, "ExternalOutput")
    mods_fm = p.dram("mods_fm", [2, 128, 48, 2], F32, "ExternalOutput")
    c = Ctx(p)
    cT = c.load_fm(cvec.rr("j (c p) -> (j c) p", p=128), 16)
    sT = p.sb([128, 16], F32, "sT")
    p.act(sT, cT, AF.Silu)
    sTv = sT.rr("p (j c) -> p c j", j=2)
    wbuf = [p.sb([128, 8, 1536], F32, f"adaw{i}") for i in range(2)]
    it = 0
    for l in range(2):
        bfm = c.load_fm(ada_b[l].rr("(c p) -> c p", p=128), 48)
        btm = p.sb([2, 6 * D], F32, "btm")
        p.dma(btm, ada_b[l:l + 1, :].bc([2, 6 * D]))
        otm = p.sb([2, 6 * D], F32, "otm")
        ofm = p.sb([128, 48, 2], F32, "ofm")
        for qd in range(4):
            w = wbuf[it % 2]
            it += 1
            for kc in range(8):
                p.dma(w[:, kc, :], ada_w[l, kc * 128:(kc + 1) * 128, qd * 1536:(qd + 1) * 1536])
            for blk in range(3):
                bk = c.bank()
                for kc in range(8):
                    p.mm(bk[0:2, :], sTv[:, kc, :], w[:, kc, blk * 512:(blk + 1) * 512],
                         start=(kc == 0), stop=(kc == 7))
                col = qd * 1536 + blk * 512
                p.tt(otm[:, col:col + 512], bk[0:2, :], btm[:, col:col + 512], ALU.add)
            bk = c.bank()
            for cc in range(12):
                for kc in range(8):
                    p.mm(bk[:, cc * 2:cc * 2 + 2], w[:, kc, cc * 128:(cc + 1) * 128], sTv[:, kc, :],
                         start=(kc == 0), stop=(kc == 7))
            p.tt(ofm[:, qd * 12:(qd + 1) * 12, :], bk[:, 0:24].rr("p (c j) -> p c j", j=2),
                 bfm[:, qd * 12:(qd + 1) * 12].un(2).bc([128, 12, 2]), ALU.add)
        p.dma(mods_tm[l], otm)
        p.dma(mods_fm[l], ofm)
    return p


def build_post(S, has_ctx, stop=99):
    TL = S // 4
    NTL = TL // 128
    NT = NTL + (2 if has_ctx else 0)
    TOK = NT * 128
    p = Prog()
    x_in = p.dram("x", [TOK, D], F32, "ExternalInput")
    attT = p.dram("attT", [8, 128, TOK], BF16, "ExternalInput")
    mods_tm = p.dram("mods_tm", [2, 6 * D], F32, "ExternalInput")
    mods_fm = p.dram("mods_fm", [128, 48, 2], F32, "ExternalInput")
    w_out = p.dram("w_out", [D, D], F32, "ExternalInput")
    n2g = p.dram("norm2_g", [D], F32, "ExternalInput")
    router_w = p.dram("router_w", [D, 16], F32, "ExternalInput")
    router_b = p.dram("router_b", [1, 16], F32, "ExternalInput")
    wg_all = p.dram("wg_all", [17, D, 256], F32, "ExternalInput")
    wu_all = p.dram("wu_all", [17, D, 256], F32, "ExternalInput")
    wd_all = p.dram("wd_all", [17, 256, D], F32, "ExternalInput")
    x_out = p.dram("x_out", [TOK, D], F32, "ExternalOutput")
    c = Ctx(p)
    B = c.banks
    nvar = 2 if has_ctx else 1
    blocks = []
    t = 0
    while t < NTL:
        n = min(4, NTL - t)
        blocks.append((0, t, n))
        t += n
    if has_ctx:
        blocks.append((1, NTL, 2))

    mfm = p.sb([128, 48, 2], F32, "mfm")
    p.dma(mfm, mods_fm)
    g2fm = c.load_fm(n2g.rr("(c p) -> c p", p=128), 8)
    A2 = p.sb([128, 8, 2], F32, "A2")
    p.ts(A2, mfm[:, 32:40, :], 1.0, ALU.add)
    p.tt(A2, A2, g2fm.un(2).bc([128, 8, 2]), ALU.mult)
    SH2 = mfm[:, 24:32, :]
    rw = p.sb([128, 8, 16], F32, "rw")
    p.dma(rw, router_w.rr("(kc p) e -> p kc e", p=128))
    rb = p.sb([128, 16], F32, "rb")
    p.dma(rb, router_b.bc([128, 16]))
    sel = p.sb([16, 16, 128], F32, "sel")
    p.iota(sel, [[1, 16], [0, 128]], base=0, cm=0)
    p.ts(sel, sel, c.pidx[0:16, 0:1], ALU.is_equal)
    xs = p.sb([128, NT, D], F32, "xs")
    for t in range(NT):
        p.dma(xs[:, t, :], x_in[t * 128:(t + 1) * 128, :])
    h2T = p.sb([128, 8, TOK], BF16, "h2T")
    combT = p.sb([16, TOK], F32, "combT")
    lg_all = p.sb([128, NT, 16], F32, "lg_all")
    gabc = [p.sb([128, D], F32, f"gabc{i}") for i in range(2)]

    def load_ga(which, var, dst):
        p.dma(dst, mods_tm[var:var + 1, which * D:(which + 1) * D].bc([128, D]))

    with p.scope():
        if stop >= 2:
            wo = p.sb([128, 8, D], BF16, "wo")
            atb = [p.sb([128, 8, 128], BF16, f"atb{i}") for i in range(2)]
            for var in range(nvar):
                p.dma(wo, w_out.rr("(kc p) d -> p kc d", p=128), q="pool")
                load_ga(2, var, gabc[0])
                p.tt(wo, wo, gabc[0].un(1).bc([128, 8, D]), ALU.mult, eng="pool")
                tiles = range(NTL) if var == 0 else range(NTL, NT)
                for t in tiles:
                    a = atb[t % 2]
                    p.dma(a, attT[:, :, t * 128:(t + 1) * 128].rr("c p t -> p c t"))
                    for half in range(2):
                        bk = B[(t % 2) * 2 + half]
                        for kc in range(8):
                            p.mm(bk[:, :], a[:, kc, :], wo[:, kc, half * 512:(half + 1) * 512],
                                 start=(kc == 0), stop=(kc == 7))
                        xv = xs[:, t, half * 512:(half + 1) * 512]
                        p.tt(xv, xv, bk, ALU.add)

    with p.scope():
        if stop >= 3:
            junk = p.sb([128, D], BF16, "junk")
            xn = [p.sb([128, D], F32, f"xn{i}") for i in range(2)]
            h2f = [p.sb([128, 8, 128], F32, f"h2f{i}") for i in range(2)]
            ssq = p.sb([128, NT], F32, "ssq")
            for t in range(NT):
                var = 0 if t < NTL else 1
                p.act(junk, xs[:, t, :], AF.Square, accum=ssq[:, t:t + 1])
            c.rstd(ssq, NT, 1.0 / D, NORM_EPS)
            for t in range(NT):
                var = 0 if t < NTL else 1
                x_n = xn[t % 2]
                hf = h2f[t % 2]
                p.ts(x_n, xs[:, t, :], ssq[:, t:t + 1], ALU.mult)
                for hb in range(2):
                    bk = B[4 + (t % 2) * 2 + hb]
                    for k4 in range(4):
                        kc = hb * 4 + k4
                        p.tr(bk[:, k4 * 128:(k4 + 1) * 128], x_n[:, kc * 128:(kc + 1) * 128], c.identf)
                    for k4 in range(4):
                        kc = hb * 4 + k4
                        p.act(hf[:, kc, :], bk[:, k4 * 128:(k4 + 1) * 128], AF.Identity,
                              bias=SH2[:, kc, var:var + 1], scale=A2[:, kc, var:var + 1])
                p.cp(h2T[:, :, t * 128:(t + 1) * 128], hf, eng="pool")
                bk = B[t % 2]
                for kc in range(8):
                    p.mm(bk[:, 0:16], hf[:, kc, :], rw[:, kc, :], start=(kc == 0), stop=(kc == 7))
                p.cp(lg_all[:, t, :], bk[:, 0:16])
            N16 = NT * 16
            sc = p.sb([128, NT, 16], F32, "sc")
            bs = p.sb([128, NT, 16], F32, "bs")
            p.act(sc, lg_all, AF.Sigmoid)
            p.tt(bs, sc, rb.un(1).bc([128, NT, 16]), ALU.add)
            bsv = bs.rr("p t (g e) -> p (t g) e", g=4)
            G = NT * 4
            m1 = p.sb([128, G], F32, "m1")
            m2 = p.sb([128, G], F32, "m2")
            p.red(m1, bsv, op=ALU.max)
            eq1 = p.sb([128, G, 4], F32, "eq1")
            p.tt(eq1, bsv, m1.un(2).bc([128, G, 4]), ALU.is_equal)
            p.stt(eq1, eq1, -1e9, bsv, ALU.mult, ALU.add)
            p.red(m2, eq1, op=ALU.max)
            gs = p.sb([128, NT, 4], F32, "gs")
            p.tt(gs.rr("p t g -> p (t g)"), m1, m2, ALU.add)
            gmax = p.sb([128, NT], F32, "gmax")
            p.red(gmax, gs, op=ALU.max)
            gsel = p.sb([128, NT, 4], F32, "gsel")
            p.tt(gsel, gs, gmax.un(2).bc([128, NT, 4]), ALU.is_equal)
            ge2 = p.sb([128, G, 4], F32, "ge2")
            p.tt(ge2, bsv, m2.un(2).bc([128, G, 4]), ALU.is_ge)
            p.tt(ge2, ge2, gsel.rr("p t g -> p (t g)").un(2).bc([128, G, 4]), ALU.mult)
            p.tt(ge2, ge2, sc.rr("p t (g e) -> p (t g) e", g=4), ALU.mult)
            den = p.sb([128, NT], F32, "den")
            p.red(den, ge2.rr("p (t g) e -> p t (g e)", g=4))
            p.op("dve", lambda e: e.reciprocal(den.ap, den.ap), [den], [den])
            comb = p.sb([128, NT, 16], F32, "comb")
            p.tt(comb, ge2.rr("p (t g) e -> p t (g e)", g=4), den.un(2).bc([128, NT, 16]), ALU.mult)
            for t in range(NT):
                bk = B[t % 2]
                p.tr(bk[0:16, 0:128], comb[:, t, :], c.identf)
                p.cp(combT[:, t * 128:(t + 1) * 128], bk[0:16, 0:128], eng="act")

    with p.scope():
        if stop >= 4:
            wg = [p.sb([128, 8, 256], BF16, f"wg{i}") for i in range(2)]
            wu = [p.sb([128, 8, 256], BF16, f"wu{i}") for i in range(2)]
            wd = [p.sb([128, 2, D], BF16, f"wd{i}") for i in range(2)]
            wds = [p.sb([128, 2, D], BF16, f"wds{i}") for i in range(2)]
            comb_sb = [p.sb([128, 512], F32, f"comb_sb{i}") for i in range(2)]
            sg = [p.sb([128, 512], F32, f"sg{i}") for i in range(2)]
            tg = [p.sb([128, 512], F32, f"tg{i}") for i in range(2)]
            actb = [p.sb([128, 2, 512], BF16, f"actb{i}") for i in range(2)]
            for var in range(nvar):
                load_ga(5, var, gabc[var])
            it = 0
            for e in range(17 if stop < 40 or stop == 99 else stop - 40):
                eb = e % 2
                p.dma(wg[eb], wg_all[e].rr("(kc p) f -> p kc f", p=128), q="pool")
                p.dma(wu[eb], wu_all[e].rr("(kc p) f -> p kc f", p=128), q="pool")
                p.dma(wd[eb], wd_all[e].rr("(fc p) d -> p fc d", p=128), q="pool")
                for var in range(nvar):
                    p.tt(wds[eb], wd[eb], gabc[var].un(1).bc([128, 2, D]), ALU.mult, eng="pool")
                    for (bv, t0, nt) in blocks:
                        if bv != var:
                            continue
                        n = nt * 128
                        tok = slice(t0 * 128, t0 * 128 + n)
                        ib = it % 2
                        it += 1
                        if e < 16:
                            p.mm(B[4][:, 0:n], sel[:, e, :], combT[:, tok])
                            p.cp(comb_sb[ib][:, 0:n], B[4][:, 0:n], eng="act")
                        for fc in range(2):
                            gb = B[fc * 2]
                            ub = B[fc * 2 + 1]
                            for kc in range(8):
                                p.mm(gb[:, 0:n], wg[eb][:, kc, fc * 128:(fc + 1) * 128], h2T[:, kc, tok],
                                     start=(kc == 0), stop=(kc == 7))
                            for kc in range(8):
                                p.mm(ub[:, 0:n], wu[eb][:, kc, fc * 128:(fc + 1) * 128], h2T[:, kc, tok],
                                     start=(kc == 0), stop=(kc == 7))
                            p.act(sg[fc][:, 0:n], gb[:, 0:n], AF.Silu)
                            if e < 16:
                                p.tt(tg[fc][:, 0:n], ub[:, 0:n], comb_sb[ib][:, 0:n], ALU.mult)
                                p.tt(actb[ib][:, fc, 0:n], sg[fc][:, 0:n], tg[fc][:, 0:n], ALU.mult, eng="pool")
                            else:
                                p.tt(actb[ib][:, fc, 0:n], sg[fc][:, 0:n], ub[:, 0:n], ALU.mult)
                        for ti in range(nt):
                            t = t0 + ti
                            for half in range(2):
                                db = B[5 + (ti * 2 + half) % 3]
                                for fc in range(2):
                                    p.mm(db, actb[ib][:, fc, ti * 128:(ti + 1) * 128],
                                         wds[eb][:, fc, half * 512:(half + 1) * 512],
                                         start=(fc == 0), stop=(fc == 1))
                                xv = xs[:, t, half * 512:(half + 1) * 512]
                                p.tt(xv, xv, db, ALU.add)
    for t in range(NT):
        p.dma(x_out[t * 128:(t + 1) * 128, :], xs[:, t, :])
    return p


def rope_tables(p, c, pos, NTL, half):
    inv = p.sb([128, half], F32, "inv")
    for i in range(half):
        p.memset(inv[:, i:i + 1], float(THETA ** (-i / half)) / (2.0 * math.pi))
    Y = p.sb([128, NTL, 2, half], F32, "ropeY")
    p.tt(Y.rr("p t r h -> p (t r) h"), pos.rr("p t r -> p (t r)").un(2).bc([128, NTL * 2, half]),
         inv.un(1).bc([128, NTL * 2, half]), ALU.mult)
    COS = p.sb([128, NTL, 2, 2 * half], F32, "COS")
    SINS = p.sb([128, NTL, 2, 2 * half], F32, "SINS")
    Yi = p.sb([128, NTL, 2, half], I32, "ropeYi")
    Yf = p.sb([128, NTL, 2, half], F32, "ropeYf")
    T = p.sb([128, NTL, 2, half], F32, "ropeT")
    R = p.sb([128, NTL, 2, half], F32, "ropeR")
    for which in range(2):
        if which == 1:
            p.ts(Y, Y, 0.25, ALU.add)
        p.cp(Yi, Y)
        p.cp(Yf, Yi)
        p.tt(R, Y, Yf, ALU.subtract)
        p.ts(T, R, 0.5, ALU.is_gt)
        p.tt(R, R, T, ALU.subtract)
        p.ts(T, R, -0.5, ALU.is_lt)
        p.tt(R, R, T, ALU.add)
        if which == 0:
            p.act(SINS[:, :, :, half:2 * half], R, AF.Sin, scale=2.0 * math.pi * (1 - 1e-6))
            p.ts(SINS[:, :, :, 0:half], SINS[:, :, :, half:2 * half], -1.0, ALU.mult)
        else:
            p.act(COS[:, :, :, 0:half], R, AF.Sin, scale=2.0 * math.pi * (1 - 1e-6))
            p.cp(COS[:, :, :, half:2 * half], COS[:, :, :, 0:half])
    return COS, SINS


def qk_post(p, c, pieces, nh, hd, gains_bc, dst, scratch, rope=None):
    sq, qk, ss = scratch["sq"], scratch["qk"], scratch["ss"]
    off = 0
    for (v, n) in pieces:
        p.act(sq[:, off:off + n * hd], v, AF.Square)
        off += n * hd
    p.red(ss, sq.rr("p (h d) -> p h d", d=hd))
    c.rstd(ss, nh, 1.0 / hd, NORM_EPS)
    off = 0
    h0 = 0
    for (v, n) in pieces:
        p.tt(qk[:, off:off + n * hd].rr("p (h d) -> p h d", d=hd), v.rr("p (h d) -> p h d", d=hd),
             ss[:, h0:h0 + n].un(2).bc([128, n, hd]), ALU.mult)
        off += n * hd
        h0 += n
    if rope is None:
        p.tt(dst, qk, gains_bc, ALU.mult, eng="pool")
        return
    p.tt(qk, qk, gains_bc, ALU.mult, eng="pool")
    roff, half, COS_t, SINS_t = rope
    qv = qk.rr("p (h d) -> p h d", d=hd)
    dv = dst.rr("p (h d) -> p h d", d=hd)
    if roff > 0:
        p.cp(dv[:, :, 0:roff], qv[:, :, 0:roff], eng="pool")
    t1, t2 = scratch["t1"], scratch["t2"]
    w = 2 * half
    for rc in range(2):
        xs = qv[:, :, roff + rc * w: roff + (rc + 1) * w]
        a = t1[:, 0:nh * w].rr("p (h d) -> p h d", d=w)
        b = t2[:, 0:nh * w].rr("p (h d) -> p h d", d=w)
        eng = "dve" if rc == 0 else "pool"
        p.tt(a, xs, COS_t[:, rc, :].un(1).bc([128, nh, w]), ALU.mult, eng=eng)
        p.tt(b[:, :, 0:half], xs[:, :, half:w], SINS_t[:, rc, 0:half].un(1).bc([128, nh, half]), ALU.mult, eng=eng)
        p.tt(b[:, :, half:w], xs[:, :, 0:half], SINS_t[:, rc, half:w].un(1).bc([128, nh, half]), ALU.mult, eng=eng)
        p.tt(dv[:, :, roff + rc * w: roff + (rc + 1) * w], a, b, ALU.add, eng=eng)


def norm1_hT(p, c, xt, A, SH, var, hT_dst, xnb, junk, ssq, banks):
    p.act(junk, xt, AF.Square, accum=ssq)
    c.rstd(ssq, 1, 1.0 / D, NORM_EPS)
    p.ts(xnb, xt, ssq[:, 0:1], ALU.mult)
    for hb in range(2):
        bk = banks[hb].bitcast(BF16)
        for k4 in range(4):
            kc = hb * 4 + k4
            p.tr(bk[:, k4 * 128:(k4 + 1) * 128], xnb[:, kc * 128:(kc + 1) * 128], c.identb)
        for k4 in range(4):
            kc = hb * 4 + k4
            p.act(hT_dst[:, kc, :], bk[:, k4 * 128:(k4 + 1) * 128], AF.Identity,
                  bias=SH[:, kc, var:var + 1], scale=A[:, kc, var:var + 1])


def mod_consts(p, c, mods_fm, norm_g, which_sh, which_sc):
    mfm = p.sb([128, 48, 2], F32, "mfm")
    p.dma(mfm, mods_fm)
    gfm = c.load_fm(norm_g.rr("(c p) -> c p", p=128), 8)
    A = p.sb([128, 8, 2], F32, "Amod")
    p.ts(A, mfm[:, which_sc * 8:(which_sc + 1) * 8, :], 1.0, ALU.add)
    p.tt(A, A, gfm.un(2).bc([128, 8, 2]), ALU.mult)
    SH = mfm[:, which_sh * 8:(which_sh + 1) * 8, :]
    return A, SH


def build_pre1(S):
    TL = S // 4
    NTL = TL // 128
    NT = NTL + 2
    TOK = NT * 128
    p = Prog()
    x_in = p.dram("x", [TOK, D], F32, "ExternalInput")
    mods_fm = p.dram("mods_fm", [128, 48, 2], F32, "ExternalInput")
    n1g = p.dram("norm1_g", [D], F32, "ExternalInput")
    w_in = p.dram("w_in", [D, 1536], F32, "ExternalInput")
    g_q = p.dram("g_q", [1, 64], F32, "ExternalInput")
    g_k = p.dram("g_k", [1, 64], F32, "ExternalInput")
    pos_in = p.dram("pos", [128, NTL, 2], F32, "ExternalInput")
    qkT = p.dram("qkT", [10, 128, TOK], BF16, "ExternalOutput")
    v_out = p.dram("v", [TOK, 256], BF16, "ExternalOutput")
    c = Ctx(p)
    B = c.banks
    A, SH = mod_consts(p, c, mods_fm, n1g, 0, 1)
    pos = p.sb([128, NTL, 2], F32, "pos")
    p.dma(pos, pos_in)
    COS, SINS = rope_tables(p, c, pos, NTL, 16)
    gains = p.sb([128, 20, 64], F32, "gains")
    gq = p.sb([128, 64], F32, "gq")
    gk = p.sb([128, 64], F32, "gk")
    p.dma(gq, g_q.bc([128, 64]))
    p.dma(gk, g_k.bc([128, 64]))
    p.cp(gains[:, 0:16, :], gq.un(1).bc([128, 16, 64]))
    p.cp(gains[:, 16:20, :], gk.un(1).bc([128, 4, 64]))
    gains_f = gains.rr("p h d -> p (h d)")
    wi = p.sb([128, 8, 1536], BF16, "wi")
    p.dma(wi, w_in.rr("(kc p) n -> p kc n", p=128), q="pool")
    xt = [p.sb([128, D], F32, f"xt{i}") for i in range(2)]
    xnb = [p.sb([128, D], BF16, f"xnb{i}") for i in range(2)]
    junk = p.sb([128, D], BF16, "junk")
    hT = [p.sb([128, 8, 128], BF16, f"hT{i}") for i in range(2)]
    ssq = [p.sb([128, 1], F32, f"ssq{i}") for i in range(2)]
    scr = dict(sq=p.sb([128, 1280], F32, "sq"), qk=p.sb([128, 1280], F32, "qk"), ss=p.sb([128, 20], F32, "ss"),
               t1=p.sb([128, 1280], F32, "t1"), t2=p.sb([128, 1280], F32, "t2"))
    qkb = [p.sb([128, 1280], BF16, f"qkb{i}") for i in range(2)]
    vb = [p.sb([128, 256], BF16, f"vb{i}") for i in range(2)]
    qkTs = [p.sb([128, 10, 128], BF16, f"qkTs{i}") for i in range(2)]
    for t in range(NT):
        i2 = t % 2
        var = 0 if t < NTL else 1
        p.dma(xt[i2], x_in[t * 128:(t + 1) * 128, :])
        norm1_hT(p, c, xt[i2], A, SH, var, hT[i2], xnb[i2], junk, ssq[i2], (B[0], B[1]))
        for blk in range(3):
            bk = B[2 + blk]
            for kc in range(8):
                p.mm(bk, hT[i2][:, kc, :], wi[:, kc, blk * 512:(blk + 1) * 512], start=(kc == 0), stop=(kc == 7))
        rope = None if var == 1 else (0, 16, COS[:, t], SINS[:, t])
        qk_post(p, c, [(B[2], 8), (B[3], 8), (B[4][:, 0:256], 4)], 20, 64, gains_f, qkb[i2], scr, rope)
        p.cp(vb[i2], B[4][:, 256:512], eng="act")
        p.dma(v_out[t * 128:(t + 1) * 128, :], vb[i2])
        bA = B[5].bitcast(BF16)
        bB = B[6].bitcast(BF16)
        for pr in range(10):
            dstb = bA[:, pr * 128:(pr + 1) * 128] if pr < 8 else bB[:, (pr - 8) * 128:(pr - 7) * 128]
            p.tr(dstb, qkb[i2][:, pr * 128:(pr + 1) * 128], c.identb)
        p.cp(qkTs[i2][:, 0:8, :].rr("p a t -> p (a t)"), bA, eng="act")
        p.cp(qkTs[i2][:, 8:10, :].rr("p a t -> p (a t)"), bB[:, 0:256])
        p.dma(qkT[:, :, t * 128:(t + 1) * 128].rr("a p t -> p a t"), qkTs[i2])
    return p


def attention_block(p, c, qT_d, kT_d, v_d, oT_d, NH, NKV, dk, dv, S, scale, qcT_d=None, ocT_d=None):
    LK = CTX + S
    NKT = LK // 128
    B = c.banks
    kT = p.sb([dk, NKV, LK], BF16, "kT")
    for kv in range(NKV):
        p.dma(kT[:, kv, :], kT_d[kv])
    vp = p.sb([128, NKV, NKT, dv + 1], BF16, "vp")
    p.memset(vp[:, :, :, dv:dv + 1], 1.0)
    for kv in range(NKV):
        p.dma(vp[:, kv, :, 0:dv], v_d[kv].rr("(t p) d -> p t d", p=128))
    selden = p.sb([dv + 1, dv], F32, "selden")
    p.memset(selden, 0.0)
    p.memset(selden[dv:dv + 1, :], 1.0)
    qTs = [p.sb([dk, NH, 512], BF16, f"qTs{i}") for i in range(2)]
    pT = [p.sb([128, 512], BF16, f"pT{i}") for i in range(4)]
    accs = [p.sb([dv + 1, 512], F32, f"accs{i}") for i in range(2)]
    rec = [p.sb([dv, 512], F32, f"rec{i}") for i in range(2)]
    ob = [p.sb([dv, 512], BF16, f"ob{i}") for i in range(2)]
    jobs = []
    for qb in range(S // 512):
        jobs.append((qT_d, oT_d, qb * 512, 512, NKT))
    if qcT_d is not None:
        jobs.append((qcT_d, ocT_d, 0, CTX, CTX // 128))
    it = 0
    ih = 0
    for ji, (qd, od, q0, n, nkt) in enumerate(jobs):
        qs = qTs[ji % 2]
        for h in range(NH):
            p.dma(qs[:, h, 0:n], qd[h, :, q0:q0 + n])
        for h in range(NH):
            kv = h * NKV // NH
            acc = B[4 + ih % 2]
            for kt in range(nkt):
                sb_ = B[it % 4]
                pt = pT[it % 4]
                it += 1
                p.mm(sb_[:, 0:n], kT[:, kv, kt * 128:(kt + 1) * 128], qs[:, h, 0:n])
                p.act(pt[:, 0:n], sb_[:, 0:n], AF.Exp, scale=scale)
                p.mm(acc[0:dv + 1, 0:n], vp[:, kv, kt, :], pt[:, 0:n], start=(kt == 0), stop=(kt == nkt - 1))
            a = accs[ih % 2]
            p.cp(a[:, 0:n], acc[0:dv + 1, 0:n])
            p.mm(B[6][0:dv, 0:n], selden, a[:, 0:n])
            r = rec[ih % 2]
            p.op("dve", lambda e, r=r, n=n: e.reciprocal(r.ap[:, 0:n], B[6].ap[0:dv, 0:n]), [B[6]], [r])
            o = ob[ih % 2]
            p.tt(o[:, 0:n], a[0:dv, 0:n], r[:, 0:n], ALU.mult, eng="pool")
            p.dma(od[h, :, q0:q0 + n], o[:, 0:n])
            ih += 1


def build_attn1(S):
    p = Prog()
    LK = CTX + S
    qT = p.dram("qT", [4, 64, S], BF16, "ExternalInput")
    kT = p.dram("kT", [1, 64, LK], BF16, "ExternalInput")
    v = p.dram("v", [1, LK, 64], BF16, "ExternalInput")
    oT = p.dram("oT", [4, 64, S], BF16, "ExternalOutput")
    c = Ctx(p)
    attention_block(p, c, qT, kT, v, oT, 4, 1, 64, 64, S, 64 ** -0.5)
    return p


RW_COLS = 1920


def build_pre0(S, dbg=0):
    TL = S // 4
    NTL = TL // 128
    NTM = NTL + 2
    NTA = NTL + 3
    TOKM = NTM * 128
    p = Prog()
    x_in = p.dram("x", [NTA * 128, D], F32, "ExternalInput")
    mods_fm = p.dram("mods_fm", [128, 48, 2], F32, "ExternalInput")
    n1g = p.dram("norm1_g", [D], F32, "ExternalInput")
    w_in = p.dram("w_in", [D, 2592], F32, "ExternalInput")
    mu_d = p.dram("mu", [RW_COLS], F32, "ExternalInput")
    flags_d = p.dram("flags", [128, 2], F32, "ExternalInput")
    g_qa = p.dram("g_qa", [1, 384], F32, "ExternalInput")
    g_kva = p.dram("g_kva", [1, 256], F32, "ExternalInput")
    w_q_up = p.dram("w_q_up", [384, 768], F32, "ExternalInput")
    w_kv_up = p.dram("w_kv_up", [256, 1024], F32, "ExternalInput")
    g_q = p.dram("g_q", [1, 96], F32, "ExternalInput")
    g_k = p.dram("g_k", [1, 96], F32, "ExternalInput")
    pos_in = p.dram("pos", [128, NTL, 2], F32, "ExternalInput")
    rwT = p.dram("rwT", [15, 128, TL + CTX], F32, "ExternalOutput")
    qT_o = p.dram("qT", [8, 96, TOKM], BF16, "ExternalOutput")
    kT_o = p.dram("kT", [8, 96, TOKM], BF16, "ExternalOutput")
    v_o = p.dram("v", [TOKM, 512], BF16, "ExternalOutput")
    c = Ctx(p)
    B = c.banks
    A, SH = mod_consts(p, c, mods_fm, n1g, 0, 1)
    pos = p.sb([128, NTL, 2], F32, "pos")
    p.dma(pos, pos_in)
    COS, SINS = rope_tables(p, c, pos, NTL, 8)
    hT_all = p.sb([128, 8, NTA * 128], BF16, "hT_all")

    with p.scope():
        gq_bc = p.sb([128, 8, 96], F32, "gq_bc")
        gk_bc = p.sb([128, 8, 96], F32, "gk_bc")
        g96 = p.sb([128, 96], F32, "g96")
        p.dma(g96, g_q.bc([128, 96]))
        p.cp(gq_bc, g96.un(1).bc([128, 8, 96]))
        g96b = p.sb([128, 96], F32, "g96b")
        p.dma(g96b, g_k.bc([128, 96]))
        p.cp(gk_bc, g96b.un(1).bc([128, 8, 96]))
        gqa_bc = p.sb([128, 384], F32, "gqa_bc")
        gkva_bc = p.sb([128, 256], F32, "gkva_bc")
        p.dma(gqa_bc, g_qa.bc([128, 384]))
        p.dma(gkva_bc, g_kva.bc([128, 256]))
        wim = p.sb([128, 8, 672], BF16, "wim")
        p.dma(wim, w_in[:, RW_COLS:2592].rr("(kc p) n -> p kc n", p=128), q="pool")
        wq = p.sb([128, 3, 768], BF16, "wq")
        p.dma(wq, w_q_up.rr("(kc p) n -> p kc n", p=128), q="pool")
        wkv = p.sb([128, 2, 1024], BF16, "wkv")
        p.dma(wkv, w_kv_up.rr("(kc p) n -> p kc n", p=128), q="pool")
        xt = [p.sb([128, D], F32, f"xt{i}") for i in range(2)]
        xnb = [p.sb([128, D], BF16, f"xnb{i}") for i in range(2)]
        junk = p.sb([128, D], BF16, "junk")
        ssq = [p.sb([128, 1], F32, f"ssq{i}") for i in range(2)]
        ssa = [p.sb([128, 2], F32, f"ssa{i}") for i in range(2)]
        anb = [p.sb([128, 640], BF16, f"anb{i}") for i in range(2)]
        anT = [p.sb([128, 5, 128], BF16, f"anT{i}") for i in range(2)]
        krs = [p.sb([128, 32], F32, f"krs{i}") for i in range(2)]
        Ksb = [p.sb([128, 8, 96], F32, f"Ksb{i}") for i in range(2)]
        vb = [p.sb([128, 8, 64], BF16, f"vb{i}") for i in range(2)]
        scr = dict(sq=p.sb([128, 768], F32, "sq"), qk=p.sb([128, 768], F32, "qk"), ss=p.sb([128, 8], F32, "ss"),
                   t1=p.sb([128, 768], F32, "t1"), t2=p.sb([128, 768], F32, "t2"))
        qb_ = [p.sb([128, 768], BF16, f"qb{i}") for i in range(2)]
        kb_ = [p.sb([128, 768], BF16, f"kb{i}") for i in range(2)]
        qTs = [p.sb([96, 8, 128], BF16, f"qTs{i}") for i in range(2)]
        kTs = [p.sb([96, 8, 128], BF16, f"kTs{i}") for i in range(2)]
        for t in range(NTA):
            i2 = t % 2
            var = 1 if (NTL <= t < NTL + 2) else 0
            p.dma(xt[i2], x_in[t * 128:(t + 1) * 128, :])
            hT = hT_all[:, :, t * 128:(t + 1) * 128]
            norm1_hT(p, c, xt[i2], A, SH, var, hT, xnb[i2], junk, ssq[i2], (B[0], B[1]))
            if t >= NTM or dbg == 1:
                continue
            for kc in range(8):
                p.mm(B[2][:, 0:384], hT[:, kc, :], wim[:, kc, 0:384], start=(kc == 0), stop=(kc == 7))
            for kc in range(8):
                p.mm(B[3][:, 0:288], hT[:, kc, :], wim[:, kc, 384:672], start=(kc == 0), stop=(kc == 7))
            sa = ssa[i2]
            p.act(junk[:, 0:384], B[2][:, 0:384], AF.Square, accum=sa[:, 0:1])
            p.act(junk[:, 384:640], B[3][:, 0:256], AF.Square, accum=sa[:, 1:2])
            p.ts(sa[:, 0:1], sa[:, 0:1], 256.0 / 384.0, ALU.mult)
            c.rstd(sa, 2, 1.0 / 256.0, NORM_EPS)
            p.stt(anb[i2][:, 0:384], B[2][:, 0:384], sa[:, 0:1], gqa_bc, ALU.mult, ALU.mult)
            p.stt(anb[i2][:, 384:640], B[3][:, 0:256], sa[:, 1:2], gkva_bc, ALU.mult, ALU.mult)
            p.cp(krs[i2], B[3][:, 256:288], eng="act")
            if dbg == 3:
                continue
            b4 = B[4].bitcast(BF16)
            for k5 in range(5):
                p.tr(b4[:, k5 * 128:(k5 + 1) * 128], anb[i2][:, k5 * 128:(k5 + 1) * 128], c.identb)
            p.cp(anT[i2].rr("p a t -> p (a t)"), b4[:, 0:640], eng="act")
            if dbg == 4:
                continue
            for (bk, c0, c1) in ((B[5], 0, 480), (B[6], 480, 768)):
                for kc in range(3):
                    p.mm(bk[:, 0:c1 - c0], anT[i2][:, kc, :], wq[:, kc, c0:c1], start=(kc == 0), stop=(kc == 2))
            if dbg == 51:
                continue
            for (bk, c0) in ((B[7], 0), (B[2], 512)):
                for kc in range(2):
                    p.mm(bk, anT[i2][:, 3 + kc, :], wkv[:, kc, c0:c0 + 512], start=(kc == 0), stop=(kc == 1))
            if dbg == 52:
                continue
            K = Ksb[i2]
            for (bk, h0) in ((B[7], 0), (B[2], 4)):
                kvv = bk.rr("p (h d) -> p h d", d=128)
                p.cp(K[:, h0:h0 + 4, 0:64], kvv[:, :, 0:64])
                if dbg != 531:
                    p.cp(vb[i2][:, h0:h0 + 4, :], kvv[:, :, 64:128], eng=("dve" if dbg == 532 else "act"))
            if dbg != 533:
                p.cp(K[:, :, 64:96], krs[i2].un(1).bc([128, 8, 32]), eng="pool")
            if dbg in (53, 531, 532, 533):
                continue
            p.dma(v_o[t * 128:(t + 1) * 128, :], vb[i2].rr("p h d -> p (h d)"))
            if dbg == 5:
                continue
            rope = None if var == 1 else (64, 8, COS[:, t], SINS[:, t])
            qk_post(p, c, [(B[5][:, 0:480], 5), (B[6][:, 0:288], 3)], 8, 96, gq_bc.rr("p h d -> p (h d)"),
                    qb_[i2], scr, rope)
            if dbg == 6:
                continue
            qk_post(p, c, [(K.rr("p h d -> p (h d)"), 8)], 8, 96, gk_bc.rr("p h d -> p (h d)"), kb_[i2], scr, rope)
            if dbg == 7:
                continue
            for (src, dstT, bk, out_d, eng) in ((qb_[i2], qTs[i2], B[0], qT_o, "act"), (kb_[i2], kTs[i2], B[1], kT_o, "dve")):
                bb = bk.bitcast(BF16)
                for h in range(8):
                    p.tr(bb[0:96, h * 128:(h + 1) * 128], src[:, h * 96:(h + 1) * 96], c.identb)
                p.cp(dstT.rr("p a t -> p (a t)"), bb[0:96, :], eng=eng)
                p.dma(out_d[:, :, t * 128:(t + 1) * 128].rr("a p t -> p a t"), dstT)

    with p.scope():
        mufm = c.load_fm(mu_d.rr("(c p) -> c p", p=128), 15)
        om = p.sb([128, 15], F32, "om")
        hm = p.sb([128, 15], F32, "hm")
        p.ts(om, mufm, -1.0, ALU.mult, 1.0, ALU.add)
        p.ts(hm, mufm, 0.5, ALU.mult)
        flags = p.sb([128, 2], F32, "flags")
        p.dma(flags, flags_d)
        W = TL + CTX + 4
        E = [p.sb([128, W], F32, f"E{i}") for i in range(2)]
        for i in range(2):
            p.memset(E[i][:, TL + 2:TL + 3], 0.0)
            p.memset(E[i][:, W - 1:W], 0.0)
        hal = [p.sb([128, 2], F32, f"hal{i}") for i in range(2)]
        sm = [p.sb([128, TL + CTX], F32, f"sm{i}") for i in range(2)]
        tm = [p.sb([128, TL + CTX], F32, f"tm{i}") for i in range(2)]
        wc = [p.sb([128, 8, 128], BF16, f"wc{i}") for i in range(3)]
        blks = []
        t0 = 0
        while t0 < TL:
            n = min(512, TL - t0)
            blks.append((t0, n, 1 + t0))
            t0 += n
        blks.append((TL, CTX, TL + 3))
        ib = 0
        for cc in range(15 if dbg < 2 else 0):
            w = wc[cc % 3]
            e = E[cc % 2]
            p.dma(w, w_in[:, cc * 128:(cc + 1) * 128].rr("(kc p) n -> p kc n", p=128), q="pool")
            for (t0, n, e0) in blks:
                bk = B[ib % 6]
                ib += 1
                for kc in range(8):
                    p.mm(bk[:, 0:n], w[:, kc, :], hT_all[:, kc, t0:t0 + n], start=(kc == 0), stop=(kc == 7))
                p.cp(e[:, e0:e0 + n], bk[:, 0:n], eng=("act" if ib % 2 else "dve"))
            bk = B[6 + cc % 2]
            hoff = (NTL + 2) * 128
            for kc in range(8):
                p.mm(bk[:, 0:2], w[:, kc, :], hT_all[:, kc, hoff:hoff + 2], start=(kc == 0), stop=(kc == 7))
            p.tt(hal[cc % 2], bk[:, 0:2], flags, ALU.mult)
            p.cp(e[:, 0:1], hal[cc % 2][:, 0:1], eng="pool")
            p.cp(e[:, TL + 1:TL + 2], hal[cc % 2][:, 1:2], eng="pool")
            s_, t_ = sm[cc % 2], tm[cc % 2]
            for (o0, n, e0) in ((0, TL, 1), (TL, CTX, TL + 3)):
                p.tt(s_[:, o0:o0 + n], e[:, e0 - 1:e0 - 1 + n], e[:, e0 + 1:e0 + 1 + n], ALU.add, eng="pool")
                p.ts(t_[:, o0:o0 + n], e[:, e0:e0 + n], om[:, cc:cc + 1], ALU.mult)
                p.stt(s_[:, o0:o0 + n], s_[:, o0:o0 + n], hm[:, cc:cc + 1], t_[:, o0:o0 + n], ALU.mult, ALU.add)
            p.dma(rwT[cc], s_)
    return p


LN_X_EPS = 64e-5
LAM = math.exp(-0.5)


def build_mix0(S, do_attn=True, RW=F32):
    LT = CTX + S
    NPAIR = LT // 128
    p = Prog()
    rw_r = p.dram("rw_r", [128, LT], F32, "ExternalInput")
    rw_k = p.dram("rw_k", [128, LT], F32, "ExternalInput")
    rw_v = p.dram("rw_v", [128, LT], F32, "ExternalInput")
    lo_w = p.dram("lo_w", [128, LT], F32, "ExternalInput")
    lo_a = p.dram("lo_a", [128, LT], F32, "ExternalInput")
    lo_g = p.dram("lo_g", [128, LT], F32, "ExternalInput")
    w2_d = p.dram("w2", [128, 128], F32, "ExternalInput")
    a2_d = p.dram("a2", [128, 128], F32, "ExternalInput")
    g2_d = p.dram("g2", [128, 128], F32, "ExternalInput")
    pv_d = p.dram("pv", [9, 128], F32, "ExternalInput")
    yT = p.dram("yT", [2, 128, LT], F32, "ExternalOutput")
    rwoT = p.dram("rwoT", [128, LT], BF16, "ExternalOutput")
    if do_attn:
        qT = p.dram("qT", [2, 96, S], BF16, "ExternalInput")
        qcT = p.dram("qcT", [2, 96, CTX], BF16, "ExternalInput")
        kT = p.dram("kT", [2, 96, LT], BF16, "ExternalInput")
        v_d = p.dram("v", [2, LT, 64], BF16, "ExternalInput")
        oT = p.dram("oT", [2, 64, S], BF16, "ExternalOutput")
        ocT = p.dram("ocT", [2, 64, CTX], BF16, "ExternalOutput")
    c = Ctx(p)
    B = c.banks
    pv = c.load_fm(pv_d, 9)
    W0 = [pv[:, 0:1], pv[:, 1:2]]
    A0 = [pv[:, 2:3], pv[:, 3:4]]
    KK_, KA_, RK_, LNW, LNB = pv[:, 4:5], pv[:, 5:6], pv[:, 6:7], pv[:, 7:8], pv[:, 8:9]
    omka = p.sb([128, 2], F32, "omka")
    p.ts(omka[:, 0:1], KA_, -1.0, ALU.mult, 1.0, ALU.add)
    p.ts(omka[:, 1:2], KA_, -2.0, ALU.mult, 2.0, ALU.add)
    w2s = p.sb([128, 128], F32, "w2s")
    a2s = p.sb([128, 128], F32, "a2s")
    g2s = p.sb([128, 128], F32, "g2s")
    p.dma(w2s, w2_d)
    p.dma(a2s, a2_d)
    p.dma(g2s, g2_d)
    p64 = p.sb([128, 1], F32, "p64")
    p.ts(p64, c.pidx, 63.5, ALU.is_gt)
    c64 = p.sb([128, 128], F32, "c64")
    p.ts(c64, c.iof, 63.5, ALU.is_gt)
    same = p.sb([128, 128], F32, "same")
    p.ts(same, c64, p64[:, 0:1], ALU.is_equal)
    masks = {}
    for nm, op_ in (("SU", ALU.is_gt), ("SL", ALU.is_lt), ("IU", ALU.is_ge), ("IL", ALU.is_le)):
        m = p.sb([128, 2, 128], F32, "mask" + nm)
        p.ts(m[:, 0, :], c.iof, c.pidx[:, 0:1], op_)
        p.tt(m[:, 0, :], m[:, 0, :], same, ALU.mult)
        p.cp(m[:, 1, :], m[:, 0, :])
        masks[nm] = m.rr("p a t -> p (a t)")
    ones64 = p.sb([128, 128], F32, "ones64")
    p.ts(ones64, same, 1.0 / 64.0, ALU.mult)
    ident2 = p.sb([128, 2, 128], F32, "ident2")
    p.cp(ident2[:, 0, :], c.identf)
    p.cp(ident2[:, 1, :], c.identf)
    ident2 = ident2.rr("p a t -> p (a t)")
    rmask = p.sb([128, 512], F32, "rmask")
    p.memset(rmask, 1.0)
    p.memset(rmask.rr("p (a t) -> p a t", t=64)[:, :, 0:1], 0.0)

    blocks = [(0, CTX)]
    t0 = CTX
    while t0 < LT:
        blocks.append((t0, 512))
        t0 += 512
    NB = len(blocks)
    order = [list(range(NB)), [0] + list(range(NB - 1, 0, -1))]

    with p.scope():
        def T(shape, nm, dt=F32):
            return p.sb(shape, dt, nm)
        ops_ = []
        for d in range(2):
            two = []
            for i in range(2):
                two.append(dict(Rt=T([128, 512], "Rt"), Kt=T([128, 512], "Kt"), Bt=T([128, 512], "Bt"), At=T([128, 512], "At"),
                                A_tm=T([128, 4, 128], "A_tm"), K_tm=T([128, 4, 128], "K_tm"), B_tm=T([128, 4, 128], "B_tm"),
                                V_tm=T([128, 4, 128], "V_tm"), etot=T([128, 8], "etot"), yb=T([128, 512], "yb")))
            ops_.append(two)
        tmp = {k: T([128, 512], k) for k in ("r", "k", "v", "lw", "la", "th", "sg", "a", "kk", "t1", "t2", "c", "E", "kd", "b", "Kh", "Bh")}
        tot = T([128, 8], "tot")
        Hbd = [[T([128, 128], f"H{d}{i}") for i in range(2)] for d in range(2)]
        for d in range(2):
            for i in range(2):
                p.memset(Hbd[d][i], 0.0)
        hcur = [0, 0]
        pairbuf = []
        for d in range(2):
            pairbuf.append({k: T([128, 256], f"{k}{d}") for k in ("X", "XT", "X2", "XT2", "P", "P2", "MAK", "MRK", "MRB", "WT")}
                           | {"AT": T([128, 128], f"AT{d}"), "U": T([128, 128], f"U{d}")})
            p.memset(pairbuf[d]["U"], 0.0)

        def prep(d, bi, ob):
            t0, n = blocks[bi]
            tok = slice(t0, t0 + n)
            nch = n // 64
            dp = slice(64 * d, 64 * d + 64)
            x = tmp
            p.dma(x["r"][:, 0:n], rw_r[:, tok])
            p.dma(x["k"][:, 0:n], rw_k[:, tok])
            p.dma(x["v"][:, 0:n], rw_v[:, tok])
            p.dma(x["lw"][:, 0:n], lo_w[:, tok])
            p.dma(x["la"][:, 0:n], lo_a[:, tok])
            N = slice(0, n)
            p.act(x["th"][dp, N], x["lw"][dp, N], AF.Tanh)
            p.mm(B[7][:, N], w2s[dp, :], x["th"][dp, N])
            p.act(x["sg"][:, N], B[7][:, N], AF.Sigmoid, bias=W0[d])
            p.mm(B[7][:, N], a2s[dp, :], x["la"][dp, N])
            p.act(x["a"][:, N], B[7][:, N], AF.Sigmoid, bias=A0[d])
            p.ts(x["kk"][:, N], x["k"][:, N], KK_, ALU.mult)
            p.tt(x["t1"][:, N], x["kk"][:, N], x["kk"][:, N], ALU.mult, eng="pool")
            p.ts(x["t1"][:, N], x["t1"][:, N], 64.0, ALU.mult, eng="pool")
            p.mm(B[7][:, N], ones64, x["t1"][:, N])
            p.ts(x["t2"][:, N], B[7][:, N], 1e-12, ALU.add)
            p.act(x["t2"][:, N], x["t2"][:, N], AF.Ln)
            p.act(x["t2"][:, N], x["t2"][:, N], AF.Exp, scale=-0.5)
            p.tt(x["kk"][:, N], x["kk"][:, N], x["t2"][:, N], ALU.mult)
            p.ts(x["t1"][:, N], x["a"][:, N], KA_, ALU.mult, omka[:, 0:1], ALU.add)
            p.tt(x["kd"][:, N], x["k"][:, N], x["t1"][:, N], ALU.mult, eng="pool")
            p.tt(x["b"][:, N], x["kk"][:, N], x["a"][:, N], ALU.mult, eng="pool")
            p.op("dve", lambda e: e.tensor_tensor_scan(x["c"].ap[:, N], rmask.ap[:, N], x["sg"].ap[:, N], 0.0,
                                                       ALU.mult, ALU.add), [rmask, x["sg"]], [x["c"]])
            cv = x["c"][:, N].rr("p (a t) -> p a t", t=64)
            p.cp(tot[:, 0:nch], cv[:, :, 63])
            if d == 1:
                p.tt(x["c"][:, N], x["sg"][:, N], x["c"][:, N], ALU.subtract)
                p.tt(cv, cv, tot[:, 0:nch].un(2).bc([128, nch, 64]), ALU.add)
            p.act(ob["etot"][:, 0:nch], tot[:, 0:nch], AF.Exp, scale=-LAM)
            p.act(x["E"][:, N], x["c"][:, N], AF.Exp, scale=-LAM)
            p.tt(ob["Rt"][:, N], x["r"][:, N], x["E"][:, N], ALU.mult)
            p.act(x["E"][:, N], x["c"][:, N], AF.Exp, scale=LAM)
            p.tt(ob["Kt"][:, N], x["kd"][:, N], x["E"][:, N], ALU.mult)
            p.tt(ob["Bt"][:, N], x["b"][:, N], x["E"][:, N], ALU.mult, eng="pool")
            p.tt(x["t1"][:, N], x["c"][:, N], x["sg"][:, N], ALU.subtract)
            p.act(x["E"][:, N], x["t1"][:, N], AF.Exp, scale=-LAM)
            p.stt(ob["At"][:, N], x["kk"][:, N], -1.0, x["E"][:, N], ALU.mult, ALU.mult)
            p.tt(x["t2"][:, N].rr("p (a t) -> p a t", t=64), tot[:, 0:nch].un(2).bc([128, nch, 64]), cv, ALU.subtract)
            p.act(x["E"][:, N], x["t2"][:, N], AF.Exp, scale=-LAM)
            p.tt(x["Kh"][:, N], x["kd"][:, N], x["E"][:, N], ALU.mult)
            p.tt(x["Bh"][:, N], x["b"][:, N], x["E"][:, N], ALU.mult, eng="pool")
            npair = n // 128
            for (src, dst, bk, eng) in ((ob["At"], ob["A_tm"], B[0], "act"), (x["Kh"], ob["K_tm"], B[1], "dve"),
                                        (x["Bh"], ob["B_tm"], B[0], "act"), (x["v"], ob["V_tm"], B[1], "dve")):
                for pr in range(npair):
                    p.tr(bk[:, pr * 128:(pr + 1) * 128], src[:, pr * 128:(pr + 1) * 128], c.identf)
                p.cp(dst.rr("p a t -> p (a t)")[:, 0:n], bk[:, 0:n], eng=eng)

        def pair_level(d, ob, pr):
            pb = pairbuf[d]
            Tk = slice(pr * 128, (pr + 1) * 128)
            mN, mM, mI = (("SU", "SL", "IU") if d == 0 else ("SL", "SU", "IL"))
            bk0, bk1 = B[2], B[3]
            HB = ((B[0], B[2]), (B[1], B[3]))

            def prod(slot, lhs, rhs):
                for h in range(2):
                    hp = slice(64 * h, 64 * h + 64)
                    p.mm(HB[h][slot][:, 0:128], ob[lhs][hp, Tk], ob[rhs][hp, Tk])

            def evac(slot, dst, mk):
                for h in range(2):
                    hc = slice(128 * h, 128 * h + 128)
                    p.tt(pb[dst][:, hc], HB[h][slot][:, 0:128], masks[mk][:, 0:128], ALU.mult)

            prod(0, "Bt", "At")
            prod(1, "At", "Bt")
            evac(0, "X", mN)
            evac(1, "XT", mM)
            prod(0, "At", "Kt")
            prod(1, "Kt", "Rt")
            evac(0, "MAK", mM)
            evac(1, "MRK", mI)
            prod(0, "Bt", "Rt")
            evac(0, "MRB", mI)
            p.tt(pb["P"], pb["X"], ident2, ALU.add, eng="pool")
            X, XT, Pc = pb["X"], pb["XT"], pb["P"]
            X2, XT2, P2 = pb["X2"], pb["XT2"], pb["P2"]
            for k in range(1, 6):
                for h in range(2):
                    hc = slice(128 * h, 128 * h + 128)
                    p.mm(bk1[:, hc], X[:, hc], XT[:, hc])
                    if k < 5:
                        p.mm(bk0[:, hc], XT[:, hc], X[:, hc])
                p.cp(XT2, bk1[:, 0:256], eng="act")
                if k < 5:
                    p.cp(X2, bk0[:, 0:256])
                for h in range(2):
                    hc = slice(128 * h, 128 * h + 128)
                    p.mm(bk0[:, hc], XT2[:, hc], Pc[:, hc])
                p.tt(P2, bk0[:, 0:256], Pc, ALU.add)
                X, X2 = X2, X
                XT, XT2 = XT2, XT
                Pc, P2 = P2, Pc
            for h in range(2):
                hc = slice(128 * h, 128 * h + 128)
                p.mm(bk1[:, hc], ob["A_tm"][:, pr, :], Pc[:, hc])
                p.mm(bk0[:, hc], pb["MAK"][:, hc], Pc[:, hc])
            p.cp(pb["AT"][0:64, :], bk1[0:64, 0:128], eng="act")
            p.cp(pb["AT"][64:128, :], bk1[64:128, 128:256], eng="act")
            p.cp(pb["WT"], bk0[:, 0:256])

        def chunk_level(d, ob, pr, pi):
            pb = pairbuf[d]
            cp_ = slice(64 * pi, 64 * pi + 64)
            ch = pr * 2 + pi
            tcol = slice(pr * 128 + 64 * pi, pr * 128 + 64 * pi + 64)
            Hold = Hbd[d][hcur[d]]
            Hnew = Hbd[d][1 - hcur[d]]
            hcur[d] = 1 - hcur[d]
            bu, bh, by = B[4], B[5], B[6]
            for h in range(2):
                hc = slice(128 * h, 128 * h + 128)
                ic = slice(64 * h, 64 * h + 64)
                p.mm(bu[:, ic], pb["WT"][:, hc], ob["V_tm"][:, pr, ic], start=True, stop=False)
                p.mm(bu[:, ic], pb["AT"], Hold[:, ic], start=False, stop=True)
            p.cp(pb["U"][cp_, :], bu[cp_, 0:128], eng="act")
            for h in range(2):
                ic = slice(64 * h, 64 * h + 64)
                p.mm(bh[:, ic], ob["K_tm"][cp_, pr, :], ob["V_tm"][cp_, pr, ic], start=True, stop=False)
                p.mm(bh[:, ic], ob["B_tm"][cp_, pr, :], pb["U"][cp_, ic], start=False, stop=True)
            for h in range(2):
                hp = slice(64 * h, 64 * h + 64)
                ic = slice(64 * h, 64 * h + 64)
                p.stt(Hnew[hp, ic], Hold[hp, ic], ob["etot"][hp, ch:ch + 1], bh[hp, ic], ALU.mult, ALU.add)
            for h in range(2):
                ys = slice(64 * h, 64 * h + 64)
                mcol = slice(128 * h + 64 * pi, 128 * h + 64 * pi + 64)
                p.mm(by[:, ys], Hold, ob["Rt"][:, tcol], start=True, stop=False)
                p.mm(by[:, ys], ob["V_tm"][:, pr, :], pb["MRK"][:, mcol], start=False, stop=False)
                p.mm(by[:, ys], pb["U"], pb["MRB"][:, mcol], start=False, stop=True)
            p.cp(ob["yb"][0:64, tcol], by[0:64, 0:64], eng="act")
            p.cp(ob["yb"][64:128, tcol], by[64:128, 64:128], eng="act")

        for step in range(NB):
            for d in range(2):
                bi = order[d][step]
                t0, n = blocks[bi]
                ob = ops_[d][step % 2]
                prep(d, bi, ob)
                npair = n // 128
                prs = range(npair) if d == 0 else range(npair - 1, -1, -1)
                for pr in prs:
                    pair_level(d, ob, pr)
                    for pi in ((0, 1) if d == 0 else (1, 0)):
                        chunk_level(d, ob, pr, pi)
                p.dma(yT[d][:, t0:t0 + n], ob["yb"][:, 0:n])

    with p.scope():
        x = {k: p.sb([128, 512], F32, k) for k in ("y0", "y1", "r", "k", "v", "la", "lg", "a0", "a1", "t1", "t2", "t3", "g")}
        ob_ = [p.sb([128, 512], BF16, f"rwo{i}") for i in range(2)]
        for bi, (t0, n) in enumerate(blocks):
            tok = slice(t0, t0 + n)
            N = slice(0, n)
            p.dma(x["y0"][:, N], yT[0][:, tok])
            p.dma(x["y1"][:, N], yT[1][:, tok])
            p.dma(x["r"][:, N], rw_r[:, tok])
            p.dma(x["k"][:, N], rw_k[:, tok])
            p.dma(x["v"][:, N], rw_v[:, tok])
            p.dma(x["la"][:, N], lo_a[:, tok])
            p.dma(x["lg"][:, N], lo_g[:, tok])
            p.tt(x["y0"][:, N], x["y0"][:, N], x["y1"][:, N], ALU.add)
            p.mm(B[0][:, N], ones64, x["y0"][:, N])
            p.tt(x["y0"][:, N], x["y0"][:, N], B[0][:, N], ALU.subtract)
            p.tt(x["t1"][:, N], x["y0"][:, N], x["y0"][:, N], ALU.mult, eng="pool")
            p.mm(B[1][:, N], ones64, x["t1"][:, N])
            p.ts(x["t1"][:, N], B[1][:, N], LN_X_EPS, ALU.add)
            p.act(x["t1"][:, N], x["t1"][:, N], AF.Ln)
            p.act(x["t1"][:, N], x["t1"][:, N], AF.Exp, scale=-0.5)
            p.tt(x["y0"][:, N], x["y0"][:, N], x["t1"][:, N], ALU.mult)
            p.ts(x["y0"][:, N], x["y0"][:, N], LNW, ALU.mult, LNB, ALU.add)
            for d in range(2):
                dp = slice(64 * d, 64 * d + 64)
                p.mm(B[2 + d][:, N], a2s[dp, :], x["la"][dp, N])
                p.act(x["a%d" % d][:, N], B[2 + d][:, N], AF.Sigmoid, bias=A0[d])
            p.tt(x["a0"][:, N], x["a0"][:, N], x["a1"][:, N], ALU.add, eng="pool")
            p.ts(x["a0"][:, N], x["a0"][:, N], KA_, ALU.mult, omka[:, 1:2], ALU.add)
            p.tt(x["t2"][:, N], x["k"][:, N], x["a0"][:, N], ALU.mult, eng="pool")
            p.stt(x["t2"][:, N], x["t2"][:, N], RK_, x["r"][:, N], ALU.mult, ALU.mult)
            p.ts(x["t2"][:, N], x["t2"][:, N], 64.0, ALU.mult, eng="pool")
            p.mm(B[4][:, N], ones64, x["t2"][:, N])
            p.tt(x["t3"][:, N], B[4][:, N], x["v"][:, N], ALU.mult)
            p.tt(x["y0"][:, N], x["y0"][:, N], x["t3"][:, N], ALU.add, eng="pool")
            p.act(x["lg"][:, N], x["lg"][:, N], AF.Sigmoid)
            p.mm(B[5][:, N], g2s, x["lg"][:, N])
            o = ob_[bi % 2]
            p.tt(o[:, N], x["y0"][:, N], B[5][:, N], ALU.mult)
            p.dma(rwoT[:, tok], o[:, N])

    if do_attn:
        with p.scope():
            attention_block(p, c, qT, kT, v_d, oT, 2, 2, 96, 64, S, 96 ** -0.5, qcT_d=qcT, ocT_d=ocT)
    return p


def _make_pos(q, TL):
    NTL = TL // 128
    t = q * TL + np.arange(TL)
    pos = np.stack([t // 64, t % 64], -1).astype(np.float32)
    return np.ascontiguousarray(pos.reshape(NTL, 128, 2).transpose(1, 0, 2))


def _cat(xs, axis):
    return np.ascontiguousarray(np.concatenate(xs, axis=axis))


def kernel(x, c, ctx, c_ctx, ada_w, ada_b, norm1_g, norm2_g, ab_w_in, ab_w_out, rw_mu, rw_w0,
           rw_w2, rw_a0, rw_a2, rw_k_k, rw_k_a, rw_r_k, rw_g2, rw_ln_w, rw_ln_b, mla_g_qa,
           mla_w_q_up, mla_g_kva, mla_w_kv_up, mla_g_q, mla_g_k, gqa_w_in, gqa_w_out, gqa_g_q,
           gqa_g_k, router_w, router_b, moe_w_gate, moe_w_up, moe_w_down, shared_w_gate,
           shared_w_up, shared_w_down):
    f = lambda a: np.ascontiguousarray(np.asarray(a, dtype=np.float32))
    x, c, ctx, c_ctx = f(x), f(c), f(ctx), f(c_ctx)
    S = x.shape[1]
    TL = S // 4
    LT = CTX + S
    R8 = range(8)
    maps = [{"cvec": np.stack([c[r // 4], c_ctx]), "ada_w": f(ada_w), "ada_b": f(ada_b)} for r in R8]
    r0 = run_prog(build_phase0(), maps)
    mods_tm = [f(r0[r]["mods_tm"]) for r in R8]
    mods_fm = [f(r0[r]["mods_fm"]) for r in R8]
    maps = []
    for r in R8:
        b, q = r // 4, r % 4
        halo = np.zeros((128, D), np.float32)
        fl = np.zeros((128, 2), np.float32)
        if q > 0:
            halo[0] = x[b, q * TL - 1]
            fl[:, 0] = 1
        if q < 3:
            halo[1] = x[b, (q + 1) * TL]
            fl[:, 1] = 1
        maps.append({"x": _cat([x[b, q * TL:(q + 1) * TL], ctx[b], halo], 0), "mods_fm": mods_fm[r][0],
                     "norm1_g": f(norm1_g)[0], "w_in": f(ab_w_in)[0], "mu": f(rw_mu)[0], "flags": fl,
                     "g_qa": f(mla_g_qa), "g_kva": f(mla_g_kva), "w_q_up": f(mla_w_q_up)[0],
                     "w_kv_up": f(mla_w_kv_up)[0], "g_q": f(mla_g_q), "g_k": f(mla_g_k), "pos": _make_pos(q, TL)})
    rA = run_prog(build_pre0(S), maps)
    maps = []
    rw_r_k_flat = f(rw_r_k)[0].reshape(512)
    for r in R8:
        b, j = r // 4, r % 4
        cores = [4 * b + qq for qq in range(4)]
        rwT = _cat([rA[4 * b]["rwT"][:, :, TL:]] + [rA[cc]["rwT"][:, :, :TL] for cc in cores], 2)
        cs = slice(128 * j, 128 * j + 128)
        pv = np.stack([f(rw_w0)[0, 0, cs], f(rw_w0)[0, 1, cs], f(rw_a0)[0, 0, cs], f(rw_a0)[0, 1, cs],
                       f(rw_k_k)[0, cs], f(rw_k_a)[0, cs], rw_r_k_flat[cs], f(rw_ln_w)[0, cs], f(rw_ln_b)[0, cs]])
        hs = slice(2 * j, 2 * j + 2)
        qT = _cat([rA[cc]["qT"][hs, :, :TL] for cc in cores], 2)
        qcT = np.ascontiguousarray(rA[4 * b]["qT"][hs, :, TL:])
        kT = _cat([rA[4 * b]["kT"][hs, :, TL:]] + [rA[cc]["kT"][hs, :, :TL] for cc in cores], 2)
        vv = _cat([rA[4 * b]["v"][TL:]] + [rA[cc]["v"][:TL] for cc in cores], 0)
        vv = np.ascontiguousarray(vv.reshape(LT, 8, 64)[:, hs].transpose(1, 0, 2))
        maps.append({"rw_r": np.ascontiguousarray(rwT[j]), "rw_k": np.ascontiguousarray(rwT[4 + j]),
                     "rw_v": np.ascontiguousarray(rwT[8 + j]), "lo_w": np.ascontiguousarray(rwT[12]),
                     "lo_a": np.ascontiguousarray(rwT[13]), "lo_g": np.ascontiguousarray(rwT[14]),
                     "w2": np.ascontiguousarray(f(rw_w2)[0][:, :, cs].reshape(128, 128)),
                     "a2": np.ascontiguousarray(f(rw_a2)[0][:, :, cs].reshape(128, 128)),
                     "g2": np.ascontiguousarray(f(rw_g2)[0][:, cs]), "pv": np.ascontiguousarray(pv),
                     "qT": qT, "qcT": qcT, "kT": kT, "v": vv})
    rB = run_prog(build_mix0(S), maps)
    del rA
    wg0 = _cat([f(moe_w_gate)[0], f(shared_w_gate)[0][None]], 0)
    wu0 = _cat([f(moe_w_up)[0], f(shared_w_up)[0][None]], 0)
    wd0 = _cat([f(moe_w_down)[0], f(shared_w_down)[0][None]], 0)
    maps = []
    for r in R8:
        b, q = r // 4, r % 4
        chunks = []
        for kc in range(4):
            rw = rB[4 * b + kc]["rwoT"]
            chunks.append(_cat([rw[:, CTX + q * TL:CTX + (q + 1) * TL], rw[:, :CTX]], 1))
        for j in range(4):
            o = rB[4 * b + j]["oT"].reshape(128, S)
            oc = rB[4 * b + j]["ocT"].reshape(128, CTX)
            chunks.append(_cat([o[:, q * TL:(q + 1) * TL], oc], 1))
        maps.append({"x": _cat([x[b, q * TL:(q + 1) * TL], ctx[b]], 0), "attT": np.ascontiguousarray(np.stack(chunks, 0)),
                     "mods_tm": mods_tm[r][0], "mods_fm": mods_fm[r][0], "w_out": f(ab_w_out)[0],
                     "norm2_g": f(norm2_g)[0], "router_w": f(router_w), "router_b": f(router_b)[None, :],
                     "wg_all": wg0, "wu_all": wu0, "wd_all": wd0})
    rC = run_prog(build_post(S, True), maps)
    del rB
    x1 = [f(rC[r]["x_out"]) for r in R8]
    maps = []
    for r in R8:
        q = r % 4
        maps.append({"x": x1[r], "mods_fm": mods_fm[r][1], "norm1_g": f(norm1_g)[1], "w_in": f(gqa_w_in)[0],
                     "g_q": f(gqa_g_q), "g_k": f(gqa_g_k), "pos": _make_pos(q, TL)})
    rD = run_prog(build_pre1(S), maps)
    maps = []
    for r in R8:
        b, kvh = r // 4, r % 4
        cores = [4 * b + qq for qq in range(4)]
        qk = _cat([rD[cc]["qkT"][:, :, :TL] for cc in cores], 2).reshape(20, 64, S)
        kc_ = rD[4 * b]["qkT"][:, :, TL:].reshape(20, 64, CTX)
        qT = np.ascontiguousarray(qk[4 * kvh:4 * kvh + 4])
        kT = _cat([kc_[16 + kvh], qk[16 + kvh]], 1)[None]
        vv = _cat([rD[4 * b]["v"][TL:]] + [rD[cc]["v"][:TL] for cc in cores], 0)
        vv = np.ascontiguousarray(vv[:, kvh * 64:(kvh + 1) * 64])[None]
        maps.append({"qT": qT, "kT": np.ascontiguousarray(kT), "v": vv})
    rE = run_prog(build_attn1(S), maps)
    del rD
    wg1 = _cat([f(moe_w_gate)[1], f(shared_w_gate)[1][None]], 0)
    wu1 = _cat([f(moe_w_up)[1], f(shared_w_up)[1][None]], 0)
    wd1 = _cat([f(moe_w_down)[1], f(shared_w_down)[1][None]], 0)
    maps = []
    for r in R8:
        b, q = r // 4, r % 4
        chunks = []
        for kc in range(8):
            o = rE[4 * b + kc // 2]["oT"]
            i0 = (kc % 2) * 2
            chunks.append(o[i0:i0 + 2].reshape(128, S)[:, q * TL:(q + 1) * TL])
        maps.append({"x": np.ascontiguousarray(x1[r][:TL]), "attT": np.ascontiguousarray(np.stack(chunks, 0)),
                     "mods_tm": mods_tm[r][1], "mods_fm": mods_fm[r][1], "w_out": f(gqa_w_out)[0],
                     "norm2_g": f(norm2_g)[1], "router_w": f(router_w), "router_b": f(router_b)[None, :],
                     "wg_all": wg1, "wu_all": wu1, "wd_all": wd1})
    rF = run_prog(build_post(S, False), maps)
    out = np.zeros((2, S, D), np.float32)
    for r in R8:
        b, q = r // 4, r % 4
        out[b, q * TL:(q + 1) * TL] = rF[r]["x_out"]
    return out
```

```python
import numpy as np
import concourse.bass as bass
import concourse.mybir as mybir
from concourse.bass_utils import run_bass_kernel_spmd

F32 = mybir.dt.float32
BF16 = mybir.dt.bfloat16
I32 = mybir.dt.int32
AF = mybir.ActivationFunctionType
ALU = mybir.AluOpType
AX = mybir.AxisListType

EPOCH = 30000
NSLOT = {"sp": 24, "pool": 12, "act": 8}


class Buf:
    __slots__ = ("name", "lw", "rd", "excl")

    def __init__(self, name, excl=False):
        self.name = name
        self.lw = None
        self.rd = []
        self.excl = excl


class V:
    __slots__ = ("buf", "ap")

    def __init__(self, buf, ap):
        self.buf = buf
        self.ap = ap

    def __getitem__(self, idx):
        return V(self.buf, self.ap[idx])

    def rr(self, pat, **kw):
        return V(self.buf, self.ap.rearrange(pat, **kw))

    def bc(self, shape):
        return V(self.buf, self.ap.broadcast_to(shape))

    def un(self, axis):
        return V(self.buf, self.ap.unsqueeze(axis))

    def bitcast(self, dt):
        return V(self.buf, self.ap.bitcast(dt))

    @property
    def shape(self):
        return self.ap.shape


class Op:
    __slots__ = ("eng", "fn", "deps", "sig", "cnt", "isdma", "slot", "slotval", "prevval")

    def __init__(self, eng, fn, isdma):
        self.eng = eng
        self.fn = fn
        self.deps = set()
        self.sig = False
        self.cnt = 0
        self.isdma = isdma
        self.slot = None
        self.slotval = 0
        self.prevval = 0


class _Scope:
    def __init__(self, p):
        self.p = p

    def __enter__(self):
        self.p._scopes.append([])
        return self

    def __exit__(self, *a):
        p = self.p
        items = p._scopes.pop()
        ops = set(p._fence)
        for cm, b in items:
            if b.lw is not None:
                ops.add(b.lw)
            ops.update(b.rd)
        best = {}
        keep = []
        for i in ops:
            o = p.ops[i]
            if o.isdma:
                if o.slot not in best or best[o.slot] < i:
                    best[o.slot] = i
            else:
                if o.eng not in best or best[o.eng] < i:
                    best[o.eng] = i
        p._fence = keep + list(best.values())
        for cm, b in reversed(items):
            cm.__exit__(None, None, None)
        return False


class Prog:
    ENGS = ("pe", "act", "dve", "pool", "sp")

    def __init__(self):
        self.nc = bass.Bass("TRN2", target_bir_lowering=False)
        self.ops = []
        self._ctx = []
        self.ntile = 0
        self._scopes = []
        self._fence = []
        self._dcount = {q: 0 for q in NSLOT}

    def dram(self, name, shape, dt, kind):
        t = self.nc.dram_tensor(name, list(shape), dt, kind=kind)
        return V(Buf(name), t.ap())

    def sb(self, shape, dt, name=None):
        self.ntile += 1
        name = name or f"t{self.ntile}"
        cm = self.nc.sbuf_tensor(f"{name}_{self.ntile}", list(shape), dt)
        h = cm.__enter__()
        b = Buf(name)
        b.rd = list(self._fence)
        if self._scopes:
            self._scopes[-1].append((cm, b))
        else:
            self._ctx.append(cm)
        return V(b, h[:])

    def scope(self):
        return _Scope(self)

    def ps(self, shape, dt, name=None):
        self.ntile += 1
        name = name or f"p{self.ntile}"
        cm = self.nc.psum_tensor(f"{name}_{self.ntile}", list(shape), dt)
        h = cm.__enter__()
        self._ctx.append(cm)
        return V(Buf(name, excl=True), h[:])

    def op(self, eng, fn, reads, writes, isdma=False):
        i = len(self.ops)
        o = Op(eng, fn, isdma)
        rb = {id(v.buf): v.buf for v in reads if v is not None}
        wb = {id(v.buf): v.buf for v in writes if v is not None}
        for b in rb.values():
            if b.lw is not None:
                o.deps.add(b.lw)
            if b.excl:
                for r in b.rd:
                    if self.ops[r].eng != eng:
                        o.deps.add(r)
        for b in wb.values():
            if b.lw is not None:
                o.deps.add(b.lw)
            for r in b.rd:
                o.deps.add(r)
        o.deps.discard(i)
        for b in rb.values():
            if id(b) not in wb:
                b.rd.append(i)
        for b in wb.values():
            b.lw = i
            b.rd = []
        if isdma:
            k = self._dcount[eng]
            self._dcount[eng] += 1
            n = NSLOT[eng]
            o.slot = (eng, k % n)
            o.slotval = 16 * (k // n + 1)
            o.prevval = 16 * (k // n)
        self.ops.append(o)
        return i

    def dma(self, out, in_, q="sp", **kw):
        self.op(q, lambda e: e.dma_start(out=out.ap, in_=in_.ap, **kw), [in_], [out], isdma=True)

    def mm(self, out, lhsT, rhs, start=True, stop=True, **kw):
        self.op("pe", lambda e: e.matmul(out.ap, lhsT.ap, rhs.ap, start=start, stop=stop, **kw),
                [lhsT, rhs], [out])

    def tr(self, out, in_, ident):
        self.op("pe", lambda e: e.transpose(out.ap, in_.ap, ident.ap), [in_, ident], [out])

    def act(self, out, in_, func, bias=None, scale=None, accum=None):
        kw = {}
        rd = [in_]
        if bias is not None:
            if isinstance(bias, V):
                kw["bias"] = bias.ap
                rd.append(bias)
            else:
                kw["bias"] = bias
        if scale is not None:
            if isinstance(scale, V):
                kw["scale"] = scale.ap
                rd.append(scale)
            else:
                kw["scale"] = scale
        wr = [out]
        if accum is not None:
            kw["accum_out"] = accum.ap
            wr.append(accum)
        self.op("act", lambda e: e.activation(out.ap, in_.ap, func, **kw), rd, wr)

    def tt(self, out, a, b, op, eng="dve"):
        self.op(eng, lambda e: e.tensor_tensor(out.ap, a.ap, b.ap, op), [a, b], [out])

    def ts(self, out, a, s1, op0, s2=None, op1=None, eng="dve", accum=None):
        rd = [a]
        x1 = s1.ap if isinstance(s1, V) else s1
        x2 = s2.ap if isinstance(s2, V) else s2
        if isinstance(s1, V):
            rd.append(s1)
        if isinstance(s2, V):
            rd.append(s2)
        kw = {}
        wr = [out]
        if op1 is not None:
            kw["op1"] = op1
        if accum is not None:
            kw["accum_out"] = accum.ap
            wr.append(accum)
        self.op(eng, lambda e: e.tensor_scalar(out.ap, a.ap, x1, x2, op0, **kw), rd, wr)

    def stt(self, out, a, s, b, op0, op1, eng="dve"):
        rd = [a, b]
        x = s.ap if isinstance(s, V) else s
        if isinstance(s, V):
            rd.append(s)
        self.op(eng, lambda e: e.scalar_tensor_tensor(out.ap, a.ap, x, b.ap, op0, op1), rd, [out])

    def cp(self, out, in_, eng="dve"):
        if eng == "act":
            self.op("act", lambda e: e.copy(out.ap, in_.ap), [in_], [out])
        else:
            self.op(eng, lambda e: e.tensor_copy(out.ap, in_.ap), [in_], [out])

    def red(self, out, in_, op=ALU.add, axis=AX.X, eng="dve"):
        self.op(eng, lambda e: e.tensor_reduce(out.ap, in_.ap, axis, op), [in_], [out])

    def memset(self, out, val, eng="pool"):
        self.op(eng, lambda e: e.memset(out.ap, val), [], [out])

    def iota(self, out, pattern, base=0, cm=0):
        self.op("pool", lambda e: e.iota(out.ap, pattern, base=base, channel_multiplier=cm,
                                         allow_small_or_imprecise_dtypes=True), [], [out])

    def finish(self):
        nc = self.nc
        ops = self.ops
        last = {}
        for i, o in enumerate(ops):
            last[o.eng] = i
        fin = Op("sp", None, False)
        for e, i in last.items():
            fin.deps.add(i)
        lastslot = {}
        for i, o in enumerate(ops):
            if o.isdma:
                lastslot[o.slot] = i
        for i in lastslot.values():
            fin.deps.add(i)
        ops.append(fin)
        for o in ops:
            for j in o.deps:
                d = ops[j]
                if d.eng == "pe" and o.eng == "pe" and not o.isdma:
                    continue
                d.sig = True
        ccount = {e: 0 for e in self.ENGS}
        dcount = self._dcount
        for o in ops:
            if o.isdma:
                pass
            elif o.sig:
                ccount[o.eng] += 1
                o.cnt = ccount[o.eng]
        sems = {}
        cms = []

        def getsem(key):
            if key not in sems:
                cm = nc.semaphore(f"s_{key[0]}_{key[1]}")
                sems[key] = cm.__enter__()
                cms.append(cm)
            return sems[key]

        for e in self.ENGS:
            for ep in range(ccount[e] // EPOCH + 1):
                getsem((e, "c%d" % ep))
        for q, n in NSLOT.items():
            for s in range(min(n, dcount[q])):
                getsem((q, s))

        def compkey(o):
            ep = (o.cnt - 1) // EPOCH
            return (o.eng, "c%d" % ep), o.cnt - ep * EPOCH

        with nc.Block() as block:
            def emit_engine(ename, eng):
                known = {}
                for i, o in enumerate(ops):
                    if o.eng != ename:
                        continue
                    waits = {}
                    for j in o.deps:
                        d = ops[j]
                        if d.isdma:
                            key, val = d.slot, d.slotval
                        else:
                            if d.eng == "pe" and ename == "pe" and not o.isdma:
                                continue
                            key, val = compkey(d)
                        if waits.get(key, 0) < val:
                            waits[key] = val
                    if o.isdma and o.prevval > 0:
                        key = o.slot
                        if waits.get(key, 0) < o.prevval:
                            waits[key] = o.prevval
                    for key, val in waits.items():
                        if known.get(key, 0) >= val:
                            continue
                        known[key] = val
                        eng.wait_ge(getsem(key), val)
                    if o.fn is None:
                        continue
                    ins = o.fn(eng)
                    if o.isdma:
                        ins.then_inc(getsem(o.slot), 16)
                    elif o.sig:
                        key, _ = compkey(o)
                        ins.then_inc(getsem(key), 1)

            @block.tensor
            def _(e):
                emit_engine("pe", e)

            @block.scalar
            def _(e):
                emit_engine("act", e)

            @block.vector
            def _(e):
                emit_engine("dve", e)

            @block.gpsimd
            def _(e):
                emit_engine("pool", e)

            @block.sync
            def _(e):
                emit_engine("sp", e)
        self._cms = cms
        return nc


D = 1024
CTX = 256
NORM_EPS = 1e-6
THETA = 10000.0
import math


class Ctx:
    def __init__(self, p):
        self.p = p
        self.banks = [p.ps([128, 512], F32, f"bank{i}") for i in range(8)]
        self.bi = 0
        io = p.sb([128, 128], F32, "io")
        pi = p.sb([128, 1], F32, "pi")
        p.iota(io, [[1, 128]], base=0, cm=0)
        p.iota(pi, [[0, 1]], base=0, cm=1)
        self.pidx = pi
        self.identf = p.sb([128, 128], F32, "identf")
        p.ts(self.identf, io, pi[:, 0:1], ALU.is_equal)
        self.identb = p.sb([128, 128], BF16, "identb")
        p.cp(self.identb, self.identf)
        self.iof = io

    def bank(self):
        b = self.banks[self.bi % 8]
        self.bi += 1
        return b

    def load_fm(self, rows_v, n, eng_out="dve"):
        p = self.p
        tmp = p.sb([n, 128], F32, "lfm_tmp")
        p.dma(tmp, rows_v)
        bk = self.bank()
        p.tr(bk[:, 0:n], tmp, self.identf[0:n, 0:n])
        out = p.sb([128, n], F32, "lfm_out")
        p.cp(out, bk[:, 0:n], eng=eng_out)
        return out

    def rstd(self, ss, n, inv_d, eps):
        p = self.p
        p.ts(ss, ss, inv_d, ALU.mult, eps, ALU.add)
        p.act(ss, ss, AF.Ln)
        p.act(ss, ss, AF.Exp, scale=-0.5)


def run_prog(p, in_maps):
    nc = p.finish()
    res = run_bass_kernel_spmd(nc, in_maps, core_ids=list(range(len(in_maps))))
    return res.results


def build_phase0():
    p = Prog()
    cvec = p.dram("cvec", [2, D], F32, "ExternalInput")
    ada_w = p.dram("ada_w", [2, D, 6 * D], F32, "ExternalInput")
    ada_b = p.dram("ada_b", [2, 6 * D], F32, "ExternalInput")
    mods_tm = p.dram("mods_tm", [2, 2, 6 * D], F32, "ExternalOutput")
    mods_fm = p.dram("mods_fm", [2, 128, 48, 2], F32, "ExternalOutput")
    c = Ctx(p)
    cT = c.load_fm(cvec.rr("j (c p) -> (j c) p", p=128), 16)
    sT = p.sb([128, 16], F32, "sT")
    p.act(sT, cT, AF.Silu)
    sTv = sT.rr("p (j c) -> p c j", j=2)
    wbuf = [p.sb([128, 8, 1536], F32, f"adaw{i}") for i in range(2)]
    it = 0
    for l in range(2):
        bfm = c.load_fm(ada_b[l].rr("(c p) -> c p", p=128), 48)
        btm = p.sb([2, 6 * D], F32, "btm")
        p.dma(btm, ada_b[l:l + 1, :].bc([2, 6 * D]))
        otm = p.sb([2, 6 * D], F32, "otm")
        ofm = p.sb([128, 48, 2], F32, "ofm")
        for qd in range(4):
            w = wbuf[it % 2]
            it += 1
            for kc in range(8):
                p.dma(w[:, kc, :], ada_w[l, kc * 128:(kc + 1) * 128, qd * 1536:(qd + 1) * 1536])
            for blk in range(3):
                bk = c.bank()
                for kc in range(8):
                    p.mm(bk[0:2, :], sTv[:, kc, :], w[:, kc, blk * 512:(blk + 1) * 512],
                         start=(kc == 0), stop=(kc == 7))
                col = qd * 1536 + blk * 512
                p.tt(otm[:, col:col + 512], bk[0:2, :], btm[:, col:col + 512], ALU.add)
            bk = c.bank()
            for cc in range(12):
                for kc in range(8):
                    p.mm(bk[:, cc * 2:cc * 2 + 2], w[:, kc, cc * 128:(cc + 1) * 128], sTv[:, kc, :],
                         start=(kc == 0), stop=(kc == 7))
            p.tt(ofm[:, qd * 12:(qd + 1) * 12, :], bk[:, 0:24].rr("p (c j) -> p c j", j=2),
                 bfm[:, qd * 12:(qd + 1) * 12].un(2).bc([128, 12, 2]), ALU.add)
        p.dma(mods_tm[l], otm)
        p.dma(mods_fm[l], ofm)
    return p


def build_post(S, has_ctx, stop=99):
    TL = S // 4
    NTL = TL // 128
    NT = NTL + (2 if has_ctx else 0)
    TOK = NT * 128
    p = Prog()
    x_in = p.dram("x", [TOK, D], F32, "ExternalInput")
    attT = p.dram("attT", [8, 128, TOK], BF16, "ExternalInput")
    mods_tm = p.dram("mods_tm", [2, 6 * D], F32, "ExternalInput")
    mods_fm = p.dram("mods_fm", [128, 48, 2], F32, "ExternalInput")
    w_out = p.dram("w_out", [D, D], F32, "ExternalInput")
    n2g = p.dram("norm2_g", [D], F32, "ExternalInput")
    router_w = p.dram("router_w", [D, 16], F32, "ExternalInput")
    router_b = p.dram("router_b", [1, 16], F32, "ExternalInput")
    wg_all = p.dram("wg_all", [17, D, 256], F32, "ExternalInput")
    wu_all = p.dram("wu_all", [17, D, 256], F32, "ExternalInput")
    wd_all = p.dram("wd_all", [17, 256, D], F32, "ExternalInput")
    x_out = p.dram("x_out", [TOK, D], F32, "ExternalOutput")
    c = Ctx(p)
    B = c.banks
    nvar = 2 if has_ctx else 1
    blocks = []
    t = 0
    while t < NTL:
        n = min(4, NTL - t)
        blocks.append((0, t, n))
        t += n
    if has_ctx:
        blocks.append((1, NTL, 2))

    mfm = p.sb([128, 48, 2], F32, "mfm")
    p.dma(mfm, mods_fm)
    g2fm = c.load_fm(n2g.rr("(c p) -> c p", p=128), 8)
    A2 = p.sb([128, 8, 2], F32, "A2")
    p.ts(A2, mfm[:, 32:40, :], 1.0, ALU.add)
    p.tt(A2, A2, g2fm.un(2).bc([128, 8, 2]), ALU.mult)
    SH2 = mfm[:, 24:32, :]
    rw = p.sb([128, 8, 16], F32, "rw")
    p.dma(rw, router_w.rr("(kc p) e -> p kc e", p=128))
    rb = p.sb([128, 16], F32, "rb")
    p.dma(rb, router_b.bc([128, 16]))
    sel = p.sb([16, 16, 128], F32, "sel")
    p.iota(sel, [[1, 16], [0, 128]], base=0, cm=0)
    p.ts(sel, sel, c.pidx[0:16, 0:1], ALU.is_equal)
    xs = p.sb([128, NT, D], F32, "xs")
    for t in range(NT):
        p.dma(xs[:, t, :], x_in[t * 128:(t + 1) * 128, :])
    h2T = p.sb([128, 8, TOK], BF16, "h2T")
    combT = p.sb([16, TOK], F32, "combT")
    lg_all = p.sb([128, NT, 16], F32, "lg_all")
    gabc = [p.sb([128, D], F32, f"gabc{i}") for i in range(2)]

    def load_ga(which, var, dst):
        p.dma(dst, mods_tm[var:var + 1, which * D:(which + 1) * D].bc([128, D]))

    with p.scope():
        if stop >= 2:
            wo = p.sb([128, 8, D], BF16, "wo")
            atb = [p.sb([128, 8, 128], BF16, f"atb{i}") for i in range(2)]
            for var in range(nvar):
                p.dma(wo, w_out.rr("(kc p) d -> p kc d", p=128), q="pool")
                load_ga(2, var, gabc[0])
                p.tt(wo, wo, gabc[0].un(1).bc([128, 8, D]), ALU.mult, eng="pool")
                tiles = range(NTL) if var == 0 else range(NTL, NT)
                for t in tiles:
                    a = atb[t % 2]
                    p.dma(a, attT[:, :, t * 128:(t + 1) * 128].rr("c p t -> p c t"))
                    for half in range(2):
                        bk = B[(t % 2) * 2 + half]
                        for kc in range(8):
                            p.mm(bk[:, :], a[:, kc, :], wo[:, kc, half * 512:(half + 1) * 512],
                                 start=(kc == 0), stop=(kc == 7))
                        xv = xs[:, t, half * 512:(half + 1) * 512]
                        p.tt(xv, xv, bk, ALU.add)

    with p.scope():
        if stop >= 3:
            junk = p.sb([128, D], BF16, "junk")
            xn = [p.sb([128, D], F32, f"xn{i}") for i in range(2)]
            h2f = [p.sb([128, 8, 128], F32, f"h2f{i}") for i in range(2)]
            ssq = p.sb([128, NT], F32, "ssq")
            for t in range(NT):
                var = 0 if t < NTL else 1
                p.act(junk, xs[:, t, :], AF.Square, accum=ssq[:, t:t + 1])
            c.rstd(ssq, NT, 1.0 / D, NORM_EPS)
            for t in range(NT):
                var = 0 if t < NTL else 1
                x_n = xn[t % 2]
                hf = h2f[t % 2]
                p.ts(x_n, xs[:, t, :], ssq[:, t:t + 1], ALU.mult)
                for hb in range(2):
                    bk = B[4 + (t % 2) * 2 + hb]
                    for k4 in range(4):
                        kc = hb * 4 + k4
                        p.tr(bk[:, k4 * 128:(k4 + 1) * 128], x_n[:, kc * 128:(kc + 1) * 128], c.identf)
                    for k4 in range(4):
                        kc = hb * 4 + k4
                        p.act(hf[:, kc, :], bk[:, k4 * 128:(k4 + 1) * 128], AF.Identity,
                              bias=SH2[:, kc, var:var + 1], scale=A2[:, kc, var:var + 1])
                p.cp(h2T[:, :, t * 128:(t + 1) * 128], hf, eng="pool")
                bk = B[t % 2]
                for kc in range(8):
                    p.mm(bk[:, 0:16], hf[:, kc, :], rw[:, kc, :], start=(kc == 0), stop=(kc == 7))
                p.cp(lg_all[:, t, :], bk[:, 0:16])
            N16 = NT * 16
            sc = p.sb([128, NT, 16], F32, "sc")
            bs = p.sb([128, NT, 16], F32, "bs")
            p.act(sc, lg_all, AF.Sigmoid)
            p.tt(bs, sc, rb.un(1).bc([128, NT, 16]), ALU.add)
            bsv = bs.rr("p t (g e) -> p (t g) e", g=4)
            G = NT * 4
            m1 = p.sb([128, G], F32, "m1")
            m2 = p.sb([128, G], F32, "m2")
            p.red(m1, bsv, op=ALU.max)
            eq1 = p.sb([128, G, 4], F32, "eq1")
            p.tt(eq1, bsv, m1.un(2).bc([128, G, 4]), ALU.is_equal)
            p.stt(eq1, eq1, -1e9, bsv, ALU.mult, ALU.add)
            p.red(m2, eq1, op=ALU.max)
            gs = p.sb([128, NT, 4], F32, "gs")
            p.tt(gs.rr("p t g -> p (t g)"), m1, m2, ALU.add)
            gmax = p.sb([128, NT], F32, "gmax")
            p.red(gmax, gs, op=ALU.max)
            gsel = p.sb([128, NT, 4], F32, "gsel")
            p.tt(gsel, gs, gmax.un(2).bc([128, NT, 4]), ALU.is_equal)
            ge2 = p.sb([128, G, 4], F32, "ge2")
            p.tt(ge2, bsv, m2.un(2).bc([128, G, 4]), ALU.is_ge)
            p.tt(ge2, ge2, gsel.rr("p t g -> p (t g)").un(2).bc([128, G, 4]), ALU.mult)
            p.tt(ge2, ge2, sc.rr("p t (g e) -> p (t g) e", g=4), ALU.mult)
            den = p.sb([128, NT], F32, "den")
            p.red(den, ge2.rr("p (t g) e -> p t (g e)", g=4))
            p.op("dve", lambda e: e.reciprocal(den.ap, den.ap), [den], [den])
            comb = p.sb([128, NT, 16], F32, "comb")
            p.tt(comb, ge2.rr("p (t g) e -> p t (g e)", g=4), den.un(2).bc([128, NT, 16]), ALU.mult)
            for t in range(NT):
                bk = B[t % 2]
                p.tr(bk[0:16, 0:128], comb[:, t, :], c.identf)
                p.cp(combT[:, t * 128:(t + 1) * 128], bk[0:16, 0:128], eng="act")

    with p.scope():
        if stop >= 4:
            wg = [p.sb([128, 8, 256], BF16, f"wg{i}") for i in range(2)]
            wu = [p.sb([128, 8, 256], BF16, f"wu{i}") for i in range(2)]
            wd = [p.sb([128, 2, D], BF16, f"wd{i}") for i in range(2)]
            wds = [p.sb([128, 2, D], BF16, f"wds{i}") for i in range(2)]
            comb_sb = [p.sb([128, 512], F32, f"comb_sb{i}") for i in range(2)]
            sg = [p.sb([128, 512], F32, f"sg{i}") for i in range(2)]
            tg = [p.sb([128, 512], F32, f"tg{i}") for i in range(2)]
            actb = [p.sb([128, 2, 512], BF16, f"actb{i}") for i in range(2)]
            for var in range(nvar):
                load_ga(5, var, gabc[var])
            it = 0
            for e in range(17 if stop < 40 or stop == 99 else stop - 40):
                eb = e % 2
                p.dma(wg[eb], wg_all[e].rr("(kc p) f -> p kc f", p=128), q="pool")
                p.dma(wu[eb], wu_all[e].rr("(kc p) f -> p kc f", p=128), q="pool")
                p.dma(wd[eb], wd_all[e].rr("(fc p) d -> p fc d", p=128), q="pool")
                for var in range(nvar):
                    p.tt(wds[eb], wd[eb], gabc[var].un(1).bc([128, 2, D]), ALU.mult, eng="pool")
                    for (bv, t0, nt) in blocks:
                        if bv != var:
                            continue
                        n = nt * 128
                        tok = slice(t0 * 128, t0 * 128 + n)
                        ib = it % 2
                        it += 1
                        if e < 16:
                            p.mm(B[4][:, 0:n], sel[:, e, :], combT[:, tok])
                            p.cp(comb_sb[ib][:, 0:n], B[4][:, 0:n], eng="act")
                        for fc in range(2):
                            gb = B[fc * 2]
                            ub = B[fc * 2 + 1]
                            for kc in range(8):
                                p.mm(gb[:, 0:n], wg[eb][:, kc, fc * 128:(fc + 1) * 128], h2T[:, kc, tok],
                                     start=(kc == 0), stop=(kc == 7))
                            for kc in range(8):
                                p.mm(ub[:, 0:n], wu[eb][:, kc, fc * 128:(fc + 1) * 128], h2T[:, kc, tok],
                                     start=(kc == 0), stop=(kc == 7))
                            p.act(sg[fc][:, 0:n], gb[:, 0:n], AF.Silu)
                            if e < 16:
                                p.tt(tg[fc][:, 0:n], ub[:, 0:n], comb_sb[ib][:, 0:n], ALU.mult)
                                p.tt(actb[ib][:, fc, 0:n], sg[fc][:, 0:n], tg[fc][:, 0:n], ALU.mult, eng="pool")
                            else:
                                p.tt(actb[ib][:, fc, 0:n], sg[fc][:, 0:n], ub[:, 0:n], ALU.mult)
                        for ti in range(nt):
                            t = t0 + ti
                            for half in range(2):
                                db = B[5 + (ti * 2 + half) % 3]
                                for fc in range(2):
                                    p.mm(db, actb[ib][:, fc, ti * 128:(ti + 1) * 128],
                                         wds[eb][:, fc, half * 512:(half + 1) * 512],
                                         start=(fc == 0), stop=(fc == 1))
                                xv = xs[:, t, half * 512:(half + 1) * 512]
                                p.tt(xv, xv, db, ALU.add)
    for t in range(NT):
        p.dma(x_out[t * 128:(t + 1) * 128, :], xs[:, t, :])
    return p


def rope_tables(p, c, pos, NTL, half):
    inv = p.sb([128, half], F32, "inv")
    for i in range(half):
        p.memset(inv[:, i:i + 1], float(THETA ** (-i / half)) / (2.0 * math.pi))
    Y = p.sb([128, NTL, 2, half], F32, "ropeY")
    p.tt(Y.rr("p t r h -> p (t r) h"), pos.rr("p t r -> p (t r)").un(2).bc([128, NTL * 2, half]),
         inv.un(1).bc([128, NTL * 2, half]), ALU.mult)
    COS = p.sb([128, NTL, 2, 2 * half], F32, "COS")
    SINS = p.sb([128, NTL, 2, 2 * half], F32, "SINS")
    Yi = p.sb([128, NTL, 2, half], I32, "ropeYi")
    Yf = p.sb([128, NTL, 2, half], F32, "ropeYf")
    T = p.sb([128, NTL, 2, half], F32, "ropeT")
    R = p.sb([128, NTL, 2, half], F32, "ropeR")
    for which in range(2):
        if which == 1:
            p.ts(Y, Y, 0.25, ALU.add)
        p.cp(Yi, Y)
        p.cp(Yf, Yi)
        p.tt(R, Y, Yf, ALU.subtract)
        p.ts(T, R, 0.5, ALU.is_gt)
        p.tt(R, R, T, ALU.subtract)
        p.ts(T, R, -0.5, ALU.is_lt)
        p.tt(R, R, T, ALU.add)
        if which == 0:
            p.act(SINS[:, :, :, half:2 * half], R, AF.Sin, scale=2.0 * math.pi * (1 - 1e-6))
            p.ts(SINS[:, :, :, 0:half], SINS[:, :, :, half:2 * half], -1.0, ALU.mult)
        else:
            p.act(COS[:, :, :, 0:half], R, AF.Sin, scale=2.0 * math.pi * (1 - 1e-6))
            p.cp(COS[:, :, :, half:2 * half], COS[:, :, :, 0:half])
    return COS, SINS


def qk_post(p, c, pieces, nh, hd, gains_bc, dst, scratch, rope=None):
    sq, qk, ss = scratch["sq"], scratch["qk"], scratch["ss"]
    off = 0
    for (v, n) in pieces:
        p.act(sq[:, off:off + n * hd], v, AF.Square)
        off += n * hd
    p.red(ss, sq.rr("p (h d) -> p h d", d=hd))
    c.rstd(ss, nh, 1.0 / hd, NORM_EPS)
    off = 0
    h0 = 0
    for (v, n) in pieces:
        p.tt(qk[:, off:off + n * hd].rr("p (h d) -> p h d", d=hd), v.rr("p (h d) -> p h d", d=hd),
             ss[:, h0:h0 + n].un(2).bc([128, n, hd]), ALU.mult)
        off += n * hd
        h0 += n
    if rope is None:
        p.tt(dst, qk, gains_bc, ALU.mult, eng="pool")
        return
    p.tt(qk, qk, gains_bc, ALU.mult, eng="pool")
    roff, half, COS_t, SINS_t = rope
    qv = qk.rr("p (h d) -> p h d", d=hd)
    dv = dst.rr("p (h d) -> p h d", d=hd)
    if roff > 0:
        p.cp(dv[:, :, 0:roff], qv[:, :, 0:roff], eng="pool")
    t1, t2 = scratch["t1"], scratch["t2"]
    w = 2 * half
    for rc in range(2):
        xs = qv[:, :, roff + rc * w: roff + (rc + 1) * w]
        a = t1[:, 0:nh * w].rr("p (h d) -> p h d", d=w)
        b = t2[:, 0:nh * w].rr("p (h d) -> p h d", d=w)
        eng = "dve" if rc == 0 else "pool"
        p.tt(a, xs, COS_t[:, rc, :].un(1).bc([128, nh, w]), ALU.mult, eng=eng)
        p.tt(b[:, :, 0:half], xs[:, :, half:w], SINS_t[:, rc, 0:half].un(1).bc([128, nh, half]), ALU.mult, eng=eng)
        p.tt(b[:, :, half:w], xs[:, :, 0:half], SINS_t[:, rc, half:w].un(1).bc([128, nh, half]), ALU.mult, eng=eng)
        p.tt(dv[:, :, roff + rc * w: roff + (rc + 1) * w], a, b, ALU.add, eng=eng)


def norm1_hT(p, c, xt, A, SH, var, hT_dst, xnb, junk, ssq, banks):
    p.act(junk, xt, AF.Square, accum=ssq)
    c.rstd(ssq, 1, 1.0 / D, NORM_EPS)
    p.ts(xnb, xt, ssq[:, 0:1], ALU.mult)
    for hb in range(2):
        bk = banks[hb].bitcast(BF16)
        for k4 in range(4):
            kc = hb * 4 + k4
            p.tr(bk[:, k4 * 128:(k4 + 1) * 128], xnb[:, kc * 128:(kc + 1) * 128], c.identb)
        for k4 in range(4):
            kc = hb * 4 + k4
            p.act(hT_dst[:, kc, :], bk[:, k4 * 128:(k4 + 1) * 128], AF.Identity,
                  bias=SH[:, kc, var:var + 1], scale=A[:, kc, var:var + 1])


def mod_consts(p, c, mods_fm, norm_g, which_sh, which_sc):
    mfm = p.sb([128, 48, 2], F32, "mfm")
    p.dma(mfm, mods_fm)
    gfm = c.load_fm(norm_g.rr("(c p) -> c p", p=128), 8)
    A = p.sb([128, 8, 2], F32, "Amod")
    p.ts(A, mfm[:, which_sc * 8:(which_sc + 1) * 8, :], 1.0, ALU.add)
    p.tt(A, A, gfm.un(2).bc([128, 8, 2]), ALU.mult)
    SH = mfm[:, which_sh * 8:(which_sh + 1) * 8, :]
    return A, SH


def build_pre1(S):
    TL = S // 4
    NTL = TL // 128
    NT = NTL + 2
    TOK = NT * 128
    p = Prog()
    x_in = p.dram("x", [TOK, D], F32, "ExternalInput")
    mods_fm = p.dram("mods_fm", [128, 48, 2], F32, "ExternalInput")
    n1g = p.dram("norm1_g", [D], F32, "ExternalInput")
    w_in = p.dram("w_in", [D, 1536], F32, "ExternalInput")
    g_q = p.dram("g_q", [1, 64], F32, "ExternalInput")
    g_k = p.dram("g_k", [1, 64], F32, "ExternalInput")
    pos_in = p.dram("pos", [128, NTL, 2], F32, "ExternalInput")
    qkT = p.dram("qkT", [10, 128, TOK], BF16, "ExternalOutput")
    v_out = p.dram("v", [TOK, 256], BF16, "ExternalOutput")
    c = Ctx(p)
    B = c.banks
    A, SH = mod_consts(p, c, mods_fm, n1g, 0, 1)
    pos = p.sb([128, NTL, 2], F32, "pos")
    p.dma(pos, pos_in)
    COS, SINS = rope_tables(p, c, pos, NTL, 16)
    gains = p.sb([128, 20, 64], F32, "gains")
    gq = p.sb([128, 64], F32, "gq")
    gk = p.sb([128, 64], F32, "gk")
    p.dma(gq, g_q.bc([128, 64]))
    p.dma(gk, g_k.bc([128, 64]))
    p.cp(gains[:, 0:16, :], gq.un(1).bc([128, 16, 64]))
    p.cp(gains[:, 16:20, :], gk.un(1).bc([128, 4, 64]))
    gains_f = gains.rr("p h d -> p (h d)")
    wi = p.sb([128, 8, 1536], BF16, "wi")
    p.dma(wi, w_in.rr("(kc p) n -> p kc n", p=128), q="pool")
    xt = [p.sb([128, D], F32, f"xt{i}") for i in range(2)]
    xnb = [p.sb([128, D], BF16, f"xnb{i}") for i in range(2)]
    junk = p.sb([128, D], BF16, "junk")
    hT = [p.sb([128, 8, 128], BF16, f"hT{i}") for i in range(2)]
    ssq = [p.sb([128, 1], F32, f"ssq{i}") for i in range(2)]
    scr = dict(sq=p.sb([128, 1280], F32, "sq"), qk=p.sb([128, 1280], F32, "qk"), ss=p.sb([128, 20], F32, "ss"),
               t1=p.sb([128, 1280], F32, "t1"), t2=p.sb([128, 1280], F32, "t2"))
    qkb = [p.sb([128, 1280], BF16, f"qkb{i}") for i in range(2)]
    vb = [p.sb([128, 256], BF16, f"vb{i}") for i in range(2)]
    qkTs = [p.sb([128, 10, 128], BF16, f"qkTs{i}") for i in range(2)]
    for t in range(NT):
        i2 = t % 2
        var = 0 if t < NTL else 1
        p.dma(xt[i2], x_in[t * 128:(t + 1) * 128, :])
        norm1_hT(p, c, xt[i2], A, SH, var, hT[i2], xnb[i2], junk, ssq[i2], (B[0], B[1]))
        for blk in range(3):
            bk = B[2 + blk]
            for kc in range(8):
                p.mm(bk, hT[i2][:, kc, :], wi[:, kc, blk * 512:(blk + 1) * 512], start=(kc == 0), stop=(kc == 7))
        rope = None if var == 1 else (0, 16, COS[:, t], SINS[:, t])
        qk_post(p, c, [(B[2], 8), (B[3], 8), (B[4][:, 0:256], 4)], 20, 64, gains_f, qkb[i2], scr, rope)
        p.cp(vb[i2], B[4][:, 256:512], eng="act")
        p.dma(v_out[t * 128:(t + 1) * 128, :], vb[i2])
        bA = B[5].bitcast(BF16)
        bB = B[6].bitcast(BF16)
        for pr in range(10):
            dstb = bA[:, pr * 128:(pr + 1) * 128] if pr < 8 else bB[:, (pr - 8) * 128:(pr - 7) * 128]
            p.tr(dstb, qkb[i2][:, pr * 128:(pr + 1) * 128], c.identb)
        p.cp(qkTs[i2][:, 0:8, :].rr("p a t -> p (a t)"), bA, eng="act")
        p.cp(qkTs[i2][:, 8:10, :].rr("p a t -> p (a t)"), bB[:, 0:256])
        p.dma(qkT[:, :, t * 128:(t + 1) * 128].rr("a p t -> p a t"), qkTs[i2])
    return p


def attention_block(p, c, qT_d, kT_d, v_d, oT_d, NH, NKV, dk, dv, S, scale, qcT_d=None, ocT_d=None):
    LK = CTX + S
    NKT = LK // 128
    B = c.banks
    kT = p.sb([dk, NKV, LK], BF16, "kT")
    for kv in range(NKV):
        p.dma(kT[:, kv, :], kT_d[kv])
    vp = p.sb([128, NKV, NKT, dv + 1], BF16, "vp")
    p.memset(vp[:, :, :, dv:dv + 1], 1.0)
    for kv in range(NKV):
        p.dma(vp[:, kv, :, 0:dv], v_d[kv].rr("(t p) d -> p t d", p=128))
    selden = p.sb([dv + 1, dv], F32, "selden")
    p.memset(selden, 0.0)
    p.memset(selden[dv:dv + 1, :], 1.0)
    qTs = [p.sb([dk, NH, 512], BF16, f"qTs{i}") for i in range(2)]
    pT = [p.sb([128, 512], BF16, f"pT{i}") for i in range(4)]
    accs = [p.sb([dv + 1, 512], F32, f"accs{i}") for i in range(2)]
    rec = [p.sb([dv, 512], F32, f"rec{i}") for i in range(2)]
    ob = [p.sb([dv, 512], BF16, f"ob{i}") for i in range(2)]
    jobs = []
    for qb in range(S // 512):
        jobs.append((qT_d, oT_d, qb * 512, 512, NKT))
    if qcT_d is not None:
        jobs.append((qcT_d, ocT_d, 0, CTX, CTX // 128))
    items = []
    for ji, (qd, od, q0, n, nkt) in enumerate(jobs):
        for h in range(NH):
            for kt in range(nkt):
                items.append((ji, h, kt))
    LA = 2
    NS = 4
    loaded = set()

    def load_q(ji):
        if ji in loaded or ji >= len(jobs):
            return
        loaded.add(ji)
        qd, od, q0, n, nkt = jobs[ji]
        qs = qTs[ji % 2]
        for h in range(NH):
            p.dma(qs[:, h, 0:n], qd[h, :, q0:q0 + n])

    def issue_s(i):
        ji, h, kt = items[i]
        qd, od, q0, n, nkt = jobs[ji]
        load_q(ji)
        kv = h * NKV // NH
        p.mm(B[i % NS][:, 0:n], kT[:, kv, kt * 128:(kt + 1) * 128], qTs[ji % 2][:, h, 0:n])

    for i in range(min(LA, len(items))):
        issue_s(i)
    ih = 0
    for i, (ji, h, kt) in enumerate(items):
        qd, od, q0, n, nkt = jobs[ji]
        kv = h * NKV // NH
        if h == 0 and kt == 0:
            load_q(ji + 1)
        if i + LA < len(items):
            issue_s(i + LA)
        acc = B[4 + ih % 2]
        pt = pT[i % 4]
        p.act(pt[:, 0:n], B[i % NS][:, 0:n], AF.Exp, scale=scale)
        p.mm(acc[0:dv + 1, 0:n], vp[:, kv, kt, :], pt[:, 0:n], start=(kt == 0), stop=(kt == nkt - 1))
        if kt == nkt - 1:
            a = accs[ih % 2]
            p.cp(a[:, 0:n], acc[0:dv + 1, 0:n])
            p.mm(B[6][0:dv, 0:n], selden, a[:, 0:n])
            r = rec[ih % 2]
            p.op("dve", lambda e, r=r, n=n: e.reciprocal(r.ap[:, 0:n], B[6].ap[0:dv, 0:n]), [B[6]], [r])
            o = ob[ih % 2]
            p.tt(o[:, 0:n], a[0:dv, 0:n], r[:, 0:n], ALU.mult, eng="pool")
            p.dma(od[h, :, q0:q0 + n], o[:, 0:n])
            ih += 1


def build_attn1(S):
    p = Prog()
    LK = CTX + S
    qT = p.dram("qT", [4, 64, S], BF16, "ExternalInput")
    kT = p.dram("kT", [1, 64, LK], BF16, "ExternalInput")
    v = p.dram("v", [1, LK, 64], BF16, "ExternalInput")
    oT = p.dram("oT", [4, 64, S], BF16, "ExternalOutput")
    c = Ctx(p)
    attention_block(p, c, qT, kT, v, oT, 4, 1, 64, 64, S, 64 ** -0.5)
    return p


RW_COLS = 1920


def build_pre0(S, dbg=0):
    TL = S // 4
    NTL = TL // 128
    NTM = NTL + 2
    NTA = NTL + 3
    TOKM = NTM * 128
    p = Prog()
    x_in = p.dram("x", [NTA * 128, D], F32, "ExternalInput")
    mods_fm = p.dram("mods_fm", [128, 48, 2], F32, "ExternalInput")
    n1g = p.dram("norm1_g", [D], F32, "ExternalInput")
    w_in = p.dram("w_in", [D, 2592], F32, "ExternalInput")
    mu_d = p.dram("mu", [RW_COLS], F32, "ExternalInput")
    flags_d = p.dram("flags", [128, 2], F32, "ExternalInput")
    g_qa = p.dram("g_qa", [1, 384], F32, "ExternalInput")
    g_kva = p.dram("g_kva", [1, 256], F32, "ExternalInput")
    w_q_up = p.dram("w_q_up", [384, 768], F32, "ExternalInput")
    w_kv_up = p.dram("w_kv_up", [256, 1024], F32, "ExternalInput")
    g_q = p.dram("g_q", [1, 96], F32, "ExternalInput")
    g_k = p.dram("g_k", [1, 96], F32, "ExternalInput")
    pos_in = p.dram("pos", [128, NTL, 2], F32, "ExternalInput")
    rwT = p.dram("rwT", [15, 128, TL + CTX], F32, "ExternalOutput")
    qT_o = p.dram("qT", [8, 96, TOKM], BF16, "ExternalOutput")
    kT_o = p.dram("kT", [8, 96, TOKM], BF16, "ExternalOutput")
    v_o = p.dram("v", [TOKM, 512], BF16, "ExternalOutput")
    c = Ctx(p)
    B = c.banks
    A, SH = mod_consts(p, c, mods_fm, n1g, 0, 1)
    pos = p.sb([128, NTL, 2], F32, "pos")
    p.dma(pos, pos_in)
    COS, SINS = rope_tables(p, c, pos, NTL, 8)
    hT_all = p.sb([128, 8, NTA * 128], BF16, "hT_all")

    with p.scope():
        gq_bc = p.sb([128, 8, 96], F32, "gq_bc")
        gk_bc = p.sb([128, 8, 96], F32, "gk_bc")
        g96 = p.sb([128, 96], F32, "g96")
        p.dma(g96, g_q.bc([128, 96]))
        p.cp(gq_bc, g96.un(1).bc([128, 8, 96]))
        g96b = p.sb([128, 96], F32, "g96b")
        p.dma(g96b, g_k.bc([128, 96]))
        p.cp(gk_bc, g96b.un(1).bc([128, 8, 96]))
        gqa_bc = p.sb([128, 384], F32, "gqa_bc")
        gkva_bc = p.sb([128, 256], F32, "gkva_bc")
        p.dma(gqa_bc, g_qa.bc([128, 384]))
        p.dma(gkva_bc, g_kva.bc([128, 256]))
        wim = p.sb([128, 8, 672], BF16, "wim")
        p.dma(wim, w_in[:, RW_COLS:2592].rr("(kc p) n -> p kc n", p=128), q="pool")
        wq = p.sb([128, 3, 768], BF16, "wq")
        p.dma(wq, w_q_up.rr("(kc p) n -> p kc n", p=128), q="pool")
        wkv = p.sb([128, 2, 1024], BF16, "wkv")
        p.dma(wkv, w_kv_up.rr("(kc p) n -> p kc n", p=128), q="pool")
        xt = [p.sb([128, D], F32, f"xt{i}") for i in range(2)]
        xnb = [p.sb([128, D], BF16, f"xnb{i}") for i in range(2)]
        junk = p.sb([128, D], BF16, "junk")
        ssq = [p.sb([128, 1], F32, f"ssq{i}") for i in range(2)]
        ssa = [p.sb([128, 2], F32, f"ssa{i}") for i in range(2)]
        anb = [p.sb([128, 640], BF16, f"anb{i}") for i in range(2)]
        anT = [p.sb([128, 5, 128], BF16, f"anT{i}") for i in range(2)]
        krs = [p.sb([128, 32], F32, f"krs{i}") for i in range(2)]
        Ksb = [p.sb([128, 8, 96], F32, f"Ksb{i}") for i in range(2)]
        vb = [p.sb([128, 8, 64], BF16, f"vb{i}") for i in range(2)]
        scr = dict(sq=p.sb([128, 768], F32, "sq"), qk=p.sb([128, 768], F32, "qk"), ss=p.sb([128, 8], F32, "ss"),
                   t1=p.sb([128, 768], F32, "t1"), t2=p.sb([128, 768], F32, "t2"))
        qb_ = [p.sb([128, 768], BF16, f"qb{i}") for i in range(2)]
        kb_ = [p.sb([128, 768], BF16, f"kb{i}") for i in range(2)]
        qTs = [p.sb([96, 8, 128], BF16, f"qTs{i}") for i in range(2)]
        kTs = [p.sb([96, 8, 128], BF16, f"kTs{i}") for i in range(2)]
        for t in range(NTA):
            i2 = t % 2
            var = 1 if (NTL <= t < NTL + 2) else 0
            p.dma(xt[i2], x_in[t * 128:(t + 1) * 128, :])
            hT = hT_all[:, :, t * 128:(t + 1) * 128]
            norm1_hT(p, c, xt[i2], A, SH, var, hT, xnb[i2], junk, ssq[i2], (B[0], B[1]))
            if t >= NTM or dbg == 1:
                continue
            for kc in range(8):
                p.mm(B[2][:, 0:384], hT[:, kc, :], wim[:, kc, 0:384], start=(kc == 0), stop=(kc == 7))
            for kc in range(8):
                p.mm(B[3][:, 0:288], hT[:, kc, :], wim[:, kc, 384:672], start=(kc == 0), stop=(kc == 7))
            sa = ssa[i2]
            p.act(junk[:, 0:384], B[2][:, 0:384], AF.Square, accum=sa[:, 0:1])
            p.act(junk[:, 384:640], B[3][:, 0:256], AF.Square, accum=sa[:, 1:2])
            p.ts(sa[:, 0:1], sa[:, 0:1], 256.0 / 384.0, ALU.mult)
            c.rstd(sa, 2, 1.0 / 256.0, NORM_EPS)
            p.stt(anb[i2][:, 0:384], B[2][:, 0:384], sa[:, 0:1], gqa_bc, ALU.mult, ALU.mult)
            p.stt(anb[i2][:, 384:640], B[3][:, 0:256], sa[:, 1:2], gkva_bc, ALU.mult, ALU.mult)
            p.cp(krs[i2], B[3][:, 256:288], eng="act")
            if dbg == 3:
                continue
            b4 = B[4].bitcast(BF16)
            for k5 in range(5):
                p.tr(b4[:, k5 * 128:(k5 + 1) * 128], anb[i2][:, k5 * 128:(k5 + 1) * 128], c.identb)
            p.cp(anT[i2].rr("p a t -> p (a t)"), b4[:, 0:640], eng="act")
            if dbg == 4:
                continue
            for (bk, c0, c1) in ((B[5], 0, 480), (B[6], 480, 768)):
                for kc in range(3):
                    p.mm(bk[:, 0:c1 - c0], anT[i2][:, kc, :], wq[:, kc, c0:c1], start=(kc == 0), stop=(kc == 2))
            if dbg == 51:
                continue
            for (bk, c0) in ((B[7], 0), (B[2], 512)):
                for kc in range(2):
                    p.mm(bk, anT[i2][:, 3 + kc, :], wkv[:, kc, c0:c0 + 512], start=(kc == 0), stop=(kc == 1))
            if dbg == 52:
                continue
            K = Ksb[i2]
            for (bk, h0) in ((B[7], 0), (B[2], 4)):
                kvv = bk.rr("p (h d) -> p h d", d=128)
                p.cp(K[:, h0:h0 + 4, 0:64], kvv[:, :, 0:64])
                if dbg != 531:
                    p.cp(vb[i2][:, h0:h0 + 4, :], kvv[:, :, 64:128], eng=("dve" if dbg == 532 else "act"))
            if dbg != 533:
                p.cp(K[:, :, 64:96], krs[i2].un(1).bc([128, 8, 32]), eng="pool")
            if dbg in (53, 531, 532, 533):
                continue
            p.dma(v_o[t * 128:(t + 1) * 128, :], vb[i2].rr("p h d -> p (h d)"))
            if dbg == 5:
                continue
            rope = None if var == 1 else (64, 8, COS[:, t], SINS[:, t])
            qk_post(p, c, [(B[5][:, 0:480], 5), (B[6][:, 0:288], 3)], 8, 96, gq_bc.rr("p h d -> p (h d)"),
                    qb_[i2], scr, rope)
            if dbg == 6:
                continue
            qk_post(p, c, [(K.rr("p h d -> p (h d)"), 8)], 8, 96, gk_bc.rr("p h d -> p (h d)"), kb_[i2], scr, rope)
            if dbg == 7:
                continue
            for (src, dstT, bk, out_d, eng) in ((qb_[i2], qTs[i2], B[0], qT_o, "act"), (kb_[i2], kTs[i2], B[1], kT_o, "dve")):
                bb = bk.bitcast(BF16)
                for h in range(8):
                    p.tr(bb[0:96, h * 128:(h + 1) * 128], src[:, h * 96:(h + 1) * 96], c.identb)
                p.cp(dstT.rr("p a t -> p (a t)"), bb[0:96, :], eng=eng)
                p.dma(out_d[:, :, t * 128:(t + 1) * 128].rr("a p t -> p a t"), dstT)

    with p.scope():
        mufm = c.load_fm(mu_d.rr("(c p) -> c p", p=128), 15)
        om = p.sb([128, 15], F32, "om")
        hm = p.sb([128, 15], F32, "hm")
        p.ts(om, mufm, -1.0, ALU.mult, 1.0, ALU.add)
        p.ts(hm, mufm, 0.5, ALU.mult)
        flags = p.sb([128, 2], F32, "flags")
        p.dma(flags, flags_d)
        W = TL + CTX + 4
        E = [p.sb([128, W], F32, f"E{i}") for i in range(2)]
        for i in range(2):
            p.memset(E[i][:, TL + 2:TL + 3], 0.0)
            p.memset(E[i][:, W - 1:W], 0.0)
        hal = [p.sb([128, 2], F32, f"hal{i}") for i in range(2)]
        sm = [p.sb([128, TL + CTX], F32, f"sm{i}") for i in range(2)]
        tm = [p.sb([128, TL + CTX], F32, f"tm{i}") for i in range(2)]
        wc = [p.sb([128, 8, 128], BF16, f"wc{i}") for i in range(3)]
        blks = []
        t0 = 0
        while t0 < TL:
            n = min(512, TL - t0)
            blks.append((t0, n, 1 + t0))
            t0 += n
        blks.append((TL, CTX, TL + 3))
        ib = 0
        for cc in range(15 if dbg < 2 else 0):
            w = wc[cc % 3]
            e = E[cc % 2]
            p.dma(w, w_in[:, cc * 128:(cc + 1) * 128].rr("(kc p) n -> p kc n", p=128), q="pool")
            for (t0, n, e0) in blks:
                bk = B[ib % 6]
                ib += 1
                for kc in range(8):
                    p.mm(bk[:, 0:n], w[:, kc, :], hT_all[:, kc, t0:t0 + n], start=(kc == 0), stop=(kc == 7))
                p.cp(e[:, e0:e0 + n], bk[:, 0:n], eng=("act" if ib % 2 else "dve"))
            bk = B[6 + cc % 2]
            hoff = (NTL + 2) * 128
            for kc in range(8):
                p.mm(bk[:, 0:2], w[:, kc, :], hT_all[:, kc, hoff:hoff + 2], start=(kc == 0), stop=(kc == 7))
            p.tt(hal[cc % 2], bk[:, 0:2], flags, ALU.mult)
            p.cp(e[:, 0:1], hal[cc % 2][:, 0:1], eng="pool")
            p.cp(e[:, TL + 1:TL + 2], hal[cc % 2][:, 1:2], eng="pool")
            s_, t_ = sm[cc % 2], tm[cc % 2]
            for (o0, n, e0) in ((0, TL, 1), (TL, CTX, TL + 3)):
                p.tt(s_[:, o0:o0 + n], e[:, e0 - 1:e0 - 1 + n], e[:, e0 + 1:e0 + 1 + n], ALU.add, eng="pool")
                p.ts(t_[:, o0:o0 + n], e[:, e0:e0 + n], om[:, cc:cc + 1], ALU.mult)
                p.stt(s_[:, o0:o0 + n], s_[:, o0:o0 + n], hm[:, cc:cc + 1], t_[:, o0:o0 + n], ALU.mult, ALU.add)
            p.dma(rwT[cc], s_)
    return p


LN_X_EPS = 64e-5
LAM = math.exp(-0.5)


def build_mix0(S, do_attn=True, RW=F32):
    LT = CTX + S
    NPAIR = LT // 128
    p = Prog()
    rw_r = p.dram("rw_r", [128, LT], F32, "ExternalInput")
    rw_k = p.dram("rw_k", [128, LT], F32, "ExternalInput")
    rw_v = p.dram("rw_v", [128, LT], F32, "ExternalInput")
    lo_w = p.dram("lo_w", [128, LT], F32, "ExternalInput")
    lo_a = p.dram("lo_a", [128, LT], F32, "ExternalInput")
    lo_g = p.dram("lo_g", [128, LT], F32, "ExternalInput")
    w2_d = p.dram("w2", [128, 128], F32, "ExternalInput")
    a2_d = p.dram("a2", [128, 128], F32, "ExternalInput")
    g2_d = p.dram("g2", [128, 128], F32, "ExternalInput")
    pv_d = p.dram("pv", [9, 128], F32, "ExternalInput")
    yT = p.dram("yT", [2, 128, LT], F32, "ExternalOutput")
    rwoT = p.dram("rwoT", [128, LT], BF16, "ExternalOutput")
    if do_attn:
        qT = p.dram("qT", [2, 96, S], BF16, "ExternalInput")
        qcT = p.dram("qcT", [2, 96, CTX], BF16, "ExternalInput")
        kT = p.dram("kT", [2, 96, LT], BF16, "ExternalInput")
        v_d = p.dram("v", [2, LT, 64], BF16, "ExternalInput")
        oT = p.dram("oT", [2, 64, S], BF16, "ExternalOutput")
        ocT = p.dram("ocT", [2, 64, CTX], BF16, "ExternalOutput")
    c = Ctx(p)
    B = c.banks
    pv = c.load_fm(pv_d, 9)
    W0 = [pv[:, 0:1], pv[:, 1:2]]
    A0 = [pv[:, 2:3], pv[:, 3:4]]
    KK_, KA_, RK_, LNW, LNB = pv[:, 4:5], pv[:, 5:6], pv[:, 6:7], pv[:, 7:8], pv[:, 8:9]
    omka = p.sb([128, 2], F32, "omka")
    p.ts(omka[:, 0:1], KA_, -1.0, ALU.mult, 1.0, ALU.add)
    p.ts(omka[:, 1:2], KA_, -2.0, ALU.mult, 2.0, ALU.add)
    w2s = p.sb([128, 128], F32, "w2s")
    a2s = p.sb([128, 128], F32, "a2s")
    g2s = p.sb([128, 128], F32, "g2s")
    p.dma(w2s, w2_d)
    p.dma(a2s, a2_d)
    p.dma(g2s, g2_d)
    p64 = p.sb([128, 1], F32, "p64")
    p.ts(p64, c.pidx, 63.5, ALU.is_gt)
    c64 = p.sb([128, 128], F32, "c64")
    p.ts(c64, c.iof, 63.5, ALU.is_gt)
    same = p.sb([128, 128], F32, "same")
    p.ts(same, c64, p64[:, 0:1], ALU.is_equal)
    masks = {}
    for nm, op_ in (("SU", ALU.is_gt), ("SL", ALU.is_lt), ("IU", ALU.is_ge), ("IL", ALU.is_le)):
        m = p.sb([128, 2, 128], F32, "mask" + nm)
        p.ts(m[:, 0, :], c.iof, c.pidx[:, 0:1], op_)
        p.tt(m[:, 0, :], m[:, 0, :], same, ALU.mult)
        p.cp(m[:, 1, :], m[:, 0, :])
        masks[nm] = m.rr("p a t -> p (a t)")
    ones64 = p.sb([128, 128], F32, "ones64")
    p.ts(ones64, same, 1.0 / 64.0, ALU.mult)
    ident2 = p.sb([128, 2, 128], F32, "ident2")
    p.cp(ident2[:, 0, :], c.identf)
    p.cp(ident2[:, 1, :], c.identf)
    ident2 = ident2.rr("p a t -> p (a t)")
    rmask = p.sb([128, 512], F32, "rmask")
    p.memset(rmask, 1.0)
    p.memset(rmask.rr("p (a t) -> p a t", t=64)[:, :, 0:1], 0.0)

    blocks = [(0, CTX)]
    t0 = CTX
    while t0 < LT:
        blocks.append((t0, 512))
        t0 += 512
    NB = len(blocks)
    order = [list(range(NB)), [0] + list(range(NB - 1, 0, -1))]

    with p.scope():
        def T(shape, nm, dt=F32):
            return p.sb(shape, dt, nm)
        ops_ = []
        for d in range(2):
            two = []
            for i in range(2):
                two.append(dict(Rt=T([128, 512], "Rt"), Kt=T([128, 512], "Kt"), Bt=T([128, 512], "Bt"), At=T([128, 512], "At"),
                                A_tm=T([128, 4, 128], "A_tm"), K_tm=T([128, 4, 128], "K_tm"), B_tm=T([128, 4, 128], "B_tm"),
                                V_tm=T([128, 4, 128], "V_tm"), etot=T([128, 8], "etot"), yb=T([128, 512], "yb")))
            ops_.append(two)
        tmp = {k: T([128, 512], k) for k in ("r", "k", "v", "lw", "la", "th", "sg", "a", "kk", "t1", "t2", "c", "E", "kd", "b", "Kh", "Bh")}
        tot = T([128, 8], "tot")
        Hbd = [[T([128, 128], f"H{d}{i}") for i in range(2)] for d in range(2)]
        for d in range(2):
            for i in range(2):
                p.memset(Hbd[d][i], 0.0)
        hcur = [0, 0]
        pairbuf = []
        for d in range(2):
            pairbuf.append({k: T([128, 256], f"{k}{d}") for k in ("X", "XT", "X2", "XT2", "P", "P2", "MAK", "MRK", "MRB", "WT")}
                           | {"AT": T([128, 128], f"AT{d}"), "U": T([128, 128], f"U{d}")})
            p.memset(pairbuf[d]["U"], 0.0)

        def prep(d, bi, ob):
            t0, n = blocks[bi]
            tok = slice(t0, t0 + n)
            nch = n // 64
            dp = slice(64 * d, 64 * d + 64)
            x = tmp
            p.dma(x["r"][:, 0:n], rw_r[:, tok])
            p.dma(x["k"][:, 0:n], rw_k[:, tok])
            p.dma(x["v"][:, 0:n], rw_v[:, tok])
            p.dma(x["lw"][:, 0:n], lo_w[:, tok])
            p.dma(x["la"][:, 0:n], lo_a[:, tok])
            N = slice(0, n)
            p.act(x["th"][dp, N], x["lw"][dp, N], AF.Tanh)
            p.mm(B[7][:, N], w2s[dp, :], x["th"][dp, N])
            p.act(x["sg"][:, N], B[7][:, N], AF.Sigmoid, bias=W0[d])
            p.mm(B[7][:, N], a2s[dp, :], x["la"][dp, N])
            p.act(x["a"][:, N], B[7][:, N], AF.Sigmoid, bias=A0[d])
            p.ts(x["kk"][:, N], x["k"][:, N], KK_, ALU.mult)
            p.tt(x["t1"][:, N], x["kk"][:, N], x["kk"][:, N], ALU.mult, eng="pool")
            p.ts(x["t1"][:, N], x["t1"][:, N], 64.0, ALU.mult, eng="pool")
            p.mm(B[7][:, N], ones64, x["t1"][:, N])
            p.ts(x["t2"][:, N], B[7][:, N], 1e-12, ALU.add)
            p.act(x["t2"][:, N], x["t2"][:, N], AF.Ln)
            p.act(x["t2"][:, N], x["t2"][:, N], AF.Exp, scale=-0.5)
            p.tt(x["kk"][:, N], x["kk"][:, N], x["t2"][:, N], ALU.mult)
            p.ts(x["t1"][:, N], x["a"][:, N], KA_, ALU.mult, omka[:, 0:1], ALU.add)
            p.tt(x["kd"][:, N], x["k"][:, N], x["t1"][:, N], ALU.mult, eng="pool")
            p.tt(x["b"][:, N], x["kk"][:, N], x["a"][:, N], ALU.mult, eng="pool")
            p.op("dve", lambda e: e.tensor_tensor_scan(x["c"].ap[:, N], rmask.ap[:, N], x["sg"].ap[:, N], 0.0,
                                                       ALU.mult, ALU.add), [rmask, x["sg"]], [x["c"]])
            cv = x["c"][:, N].rr("p (a t) -> p a t", t=64)
            p.cp(tot[:, 0:nch], cv[:, :, 63])
            if d == 1:
                p.tt(x["c"][:, N], x["sg"][:, N], x["c"][:, N], ALU.subtract)
                p.tt(cv, cv, tot[:, 0:nch].un(2).bc([128, nch, 64]), ALU.add)
            p.act(ob["etot"][:, 0:nch], tot[:, 0:nch], AF.Exp, scale=-LAM)
            p.act(x["E"][:, N], x["c"][:, N], AF.Exp, scale=-LAM)
            p.tt(ob["Rt"][:, N], x["r"][:, N], x["E"][:, N], ALU.mult)
            p.act(x["E"][:, N], x["c"][:, N], AF.Exp, scale=LAM)
            p.tt(ob["Kt"][:, N], x["kd"][:, N], x["E"][:, N], ALU.mult)
            p.tt(ob["Bt"][:, N], x["b"][:, N], x["E"][:, N], ALU.mult, eng="pool")
            p.tt(x["t1"][:, N], x["c"][:, N], x["sg"][:, N], ALU.subtract)
            p.act(x["E"][:, N], x["t1"][:, N], AF.Exp, scale=-LAM)
            p.stt(ob["At"][:, N], x["kk"][:, N], -1.0, x["E"][:, N], ALU.mult, ALU.mult)
            p.tt(x["t2"][:, N].rr("p (a t) -> p a t", t=64), tot[:, 0:nch].un(2).bc([128, nch, 64]), cv, ALU.subtract)
            p.act(x["E"][:, N], x["t2"][:, N], AF.Exp, scale=-LAM)
            p.tt(x["Kh"][:, N], x["kd"][:, N], x["E"][:, N], ALU.mult)
            p.tt(x["Bh"][:, N], x["b"][:, N], x["E"][:, N], ALU.mult, eng="pool")
            npair = n // 128
            for (src, dst, bk, eng) in ((ob["At"], ob["A_tm"], B[0], "act"), (x["Kh"], ob["K_tm"], B[1], "dve"),
                                        (x["Bh"], ob["B_tm"], B[0], "act"), (x["v"], ob["V_tm"], B[1], "dve")):
                for pr in range(npair):
                    p.tr(bk[:, pr * 128:(pr + 1) * 128], src[:, pr * 128:(pr + 1) * 128], c.identf)
                p.cp(dst.rr("p a t -> p (a t)")[:, 0:n], bk[:, 0:n], eng=eng)

        def pair_level(d, ob, pr):
            pb = pairbuf[d]
            Tk = slice(pr * 128, (pr + 1) * 128)
            mN, mM, mI = (("SU", "SL", "IU") if d == 0 else ("SL", "SU", "IL"))
            bk0, bk1 = B[2], B[3]
            HB = ((B[0], B[2]), (B[1], B[3]))

            def prod(slot, lhs, rhs):
                for h in range(2):
                    hp = slice(64 * h, 64 * h + 64)
                    p.mm(HB[h][slot][:, 0:128], ob[lhs][hp, Tk], ob[rhs][hp, Tk])

            def evac(slot, dst, mk):
                for h in range(2):
                    hc = slice(128 * h, 128 * h + 128)
                    p.tt(pb[dst][:, hc], HB[h][slot][:, 0:128], masks[mk][:, 0:128], ALU.mult)

            prod(0, "Bt", "At")
            prod(1, "At", "Bt")
            evac(0, "X", mN)
            evac(1, "XT", mM)
            prod(0, "At", "Kt")
            prod(1, "Kt", "Rt")
            evac(0, "MAK", mM)
            evac(1, "MRK", mI)
            prod(0, "Bt", "Rt")
            evac(0, "MRB", mI)
            p.tt(pb["P"], pb["X"], ident2, ALU.add, eng="pool")
            X, XT, Pc = pb["X"], pb["XT"], pb["P"]
            X2, XT2, P2 = pb["X2"], pb["XT2"], pb["P2"]
            for k in range(1, 6):
                for h in range(2):
                    hc = slice(128 * h, 128 * h + 128)
                    p.mm(bk1[:, hc], X[:, hc], XT[:, hc])
                    if k < 5:
                        p.mm(bk0[:, hc], XT[:, hc], X[:, hc])
                p.cp(XT2, bk1[:, 0:256], eng="act")
                if k < 5:
                    p.cp(X2, bk0[:, 0:256])
                for h in range(2):
                    hc = slice(128 * h, 128 * h + 128)
                    p.mm(bk0[:, hc], XT2[:, hc], Pc[:, hc])
                p.tt(P2, bk0[:, 0:256], Pc, ALU.add)
                X, X2 = X2, X
                XT, XT2 = XT2, XT
                Pc, P2 = P2, Pc
            for h in range(2):
                hc = slice(128 * h, 128 * h + 128)
                p.mm(bk1[:, hc], ob["A_tm"][:, pr, :], Pc[:, hc])
                p.mm(bk0[:, hc], pb["MAK"][:, hc], Pc[:, hc])
            p.cp(pb["AT"][0:64, :], bk1[0:64, 0:128], eng="act")
            p.cp(pb["AT"][64:128, :], bk1[64:128, 128:256], eng="act")
            p.cp(pb["WT"], bk0[:, 0:256])

        def chunk_level(d, ob, pr, pi):
            pb = pairbuf[d]
            cp_ = slice(64 * pi, 64 * pi + 64)
            ch = pr * 2 + pi
            tcol = slice(pr * 128 + 64 * pi, pr * 128 + 64 * pi + 64)
            Hold = Hbd[d][hcur[d]]
            Hnew = Hbd[d][1 - hcur[d]]
            hcur[d] = 1 - hcur[d]
            bu, bh, by = B[4], B[5], B[6]
            for h in range(2):
                hc = slice(128 * h, 128 * h + 128)
                ic = slice(64 * h, 64 * h + 64)
                p.mm(bu[:, ic], pb["WT"][:, hc], ob["V_tm"][:, pr, ic], start=True, stop=False)
                p.mm(bu[:, ic], pb["AT"], Hold[:, ic], start=False, stop=True)
            p.cp(pb["U"][cp_, :], bu[cp_, 0:128], eng="act")
            for h in range(2):
                ic = slice(64 * h, 64 * h + 64)
                p.mm(bh[:, ic], ob["K_tm"][cp_, pr, :], ob["V_tm"][cp_, pr, ic], start=True, stop=False)
                p.mm(bh[:, ic], ob["B_tm"][cp_, pr, :], pb["U"][cp_, ic], start=False, stop=True)
            for h in range(2):
                hp = slice(64 * h, 64 * h + 64)
                ic = slice(64 * h, 64 * h + 64)
                p.stt(Hnew[hp, ic], Hold[hp, ic], ob["etot"][hp, ch:ch + 1], bh[hp, ic], ALU.mult, ALU.add)
            for h in range(2):
                ys = slice(64 * h, 64 * h + 64)
                mcol = slice(128 * h + 64 * pi, 128 * h + 64 * pi + 64)
                p.mm(by[:, ys], Hold, ob["Rt"][:, tcol], start=True, stop=False)
                p.mm(by[:, ys], ob["V_tm"][:, pr, :], pb["MRK"][:, mcol], start=False, stop=False)
                p.mm(by[:, ys], pb["U"], pb["MRB"][:, mcol], start=False, stop=True)
            p.cp(ob["yb"][0:64, tcol], by[0:64, 0:64], eng="act")
            p.cp(ob["yb"][64:128, tcol], by[64:128, 64:128], eng="act")

        for step in range(NB):
            for d in range(2):
                bi = order[d][step]
                t0, n = blocks[bi]
                ob = ops_[d][step % 2]
                prep(d, bi, ob)
                npair = n // 128
                prs = range(npair) if d == 0 else range(npair - 1, -1, -1)
                for pr in prs:
                    pair_level(d, ob, pr)
                    for pi in ((0, 1) if d == 0 else (1, 0)):
                        chunk_level(d, ob, pr, pi)
                p.dma(yT[d][:, t0:t0 + n], ob["yb"][:, 0:n])

    with p.scope():
        x = {k: p.sb([128, 512], F32, k) for k in ("y0", "y1", "r", "k", "v", "la", "lg", "a0", "a1", "t1", "t2", "t3", "g")}
        ob_ = [p.sb([128, 512], BF16, f"rwo{i}") for i in range(2)]
        for bi, (t0, n) in enumerate(blocks):
            tok = slice(t0, t0 + n)
            N = slice(0, n)
            p.dma(x["y0"][:, N], yT[0][:, tok])
            p.dma(x["y1"][:, N], yT[1][:, tok])
            p.dma(x["r"][:, N], rw_r[:, tok])
            p.dma(x["k"][:, N], rw_k[:, tok])
            p.dma(x["v"][:, N], rw_v[:, tok])
            p.dma(x["la"][:, N], lo_a[:, tok])
            p.dma(x["lg"][:, N], lo_g[:, tok])
            p.tt(x["y0"][:, N], x["y0"][:, N], x["y1"][:, N], ALU.add)
            p.mm(B[0][:, N], ones64, x["y0"][:, N])
            p.tt(x["y0"][:, N], x["y0"][:, N], B[0][:, N], ALU.subtract)
            p.tt(x["t1"][:, N], x["y0"][:, N], x["y0"][:, N], ALU.mult, eng="pool")
            p.mm(B[1][:, N], ones64, x["t1"][:, N])
            p.ts(x["t1"][:, N], B[1][:, N], LN_X_EPS, ALU.add)
            p.act(x["t1"][:, N], x["t1"][:, N], AF.Ln)
            p.act(x["t1"][:, N], x["t1"][:, N], AF.Exp, scale=-0.5)
            p.tt(x["y0"][:, N], x["y0"][:, N], x["t1"][:, N], ALU.mult)
            p.ts(x["y0"][:, N], x["y0"][:, N], LNW, ALU.mult, LNB, ALU.add)
            for d in range(2):
                dp = slice(64 * d, 64 * d + 64)
                p.mm(B[2 + d][:, N], a2s[dp, :], x["la"][dp, N])
                p.act(x["a%d" % d][:, N], B[2 + d][:, N], AF.Sigmoid, bias=A0[d])
            p.tt(x["a0"][:, N], x["a0"][:, N], x["a1"][:, N], ALU.add, eng="pool")
            p.ts(x["a0"][:, N], x["a0"][:, N], KA_, ALU.mult, omka[:, 1:2], ALU.add)
            p.tt(x["t2"][:, N], x["k"][:, N], x["a0"][:, N], ALU.mult, eng="pool")
            p.stt(x["t2"][:, N], x["t2"][:, N], RK_, x["r"][:, N], ALU.mult, ALU.mult)
            p.ts(x["t2"][:, N], x["t2"][:, N], 64.0, ALU.mult, eng="pool")
            p.mm(B[4][:, N], ones64, x["t2"][:, N])
            p.tt(x["t3"][:, N], B[4][:, N], x["v"][:, N], ALU.mult)
            p.tt(x["y0"][:, N], x["y0"][:, N], x["t3"][:, N], ALU.add, eng="pool")
            p.act(x["lg"][:, N], x["lg"][:, N], AF.Sigmoid)
            p.mm(B[5][:, N], g2s, x["lg"][:, N])
            o = ob_[bi % 2]
            p.tt(o[:, N], x["y0"][:, N], B[5][:, N], ALU.mult)
            p.dma(rwoT[:, tok], o[:, N])

    if do_attn:
        with p.scope():
            attention_block(p, c, qT, kT, v_d, oT, 2, 2, 96, 64, S, 96 ** -0.5, qcT_d=qcT, ocT_d=ocT)
    return p


def _make_pos(q, TL):
    NTL = TL // 128
    t = q * TL + np.arange(TL)
    pos = np.stack([t // 64, t % 64], -1).astype(np.float32)
    return np.ascontiguousarray(pos.reshape(NTL, 128, 2).transpose(1, 0, 2))


def _cat(xs, axis):
    return np.ascontiguousarray(np.concatenate(xs, axis=axis))


def kernel(x, c, ctx, c_ctx, ada_w, ada_b, norm1_g, norm2_g, ab_w_in, ab_w_out, rw_mu, rw_w0,
           rw_w2, rw_a0, rw_a2, rw_k_k, rw_k_a, rw_r_k, rw_g2, rw_ln_w, rw_ln_b, mla_g_qa,
           mla_w_q_up, mla_g_kva, mla_w_kv_up, mla_g_q, mla_g_k, gqa_w_in, gqa_w_out, gqa_g_q,
           gqa_g_k, router_w, router_b, moe_w_gate, moe_w_up, moe_w_down, shared_w_gate,
           shared_w_up, shared_w_down):
    f = lambda a: np.ascontiguousarray(np.asarray(a, dtype=np.float32))
    x, c, ctx, c_ctx = f(x), f(c), f(ctx), f(c_ctx)
    S = x.shape[1]
    TL = S // 4
    LT = CTX + S
    R8 = range(8)
    maps = [{"cvec": np.stack([c[r // 4], c_ctx]), "ada_w": f(ada_w), "ada_b": f(ada_b)} for r in R8]
    r0 = run_prog(build_phase0(), maps)
    mods_tm = [f(r0[r]["mods_tm"]) for r in R8]
    mods_fm = [f(r0[r]["mods_fm"]) for r in R8]
    maps = []
    for r in R8:
        b, q = r // 4, r % 4
        halo = np.zeros((128, D), np.float32)
        fl = np.zeros((128, 2), np.float32)
        if q > 0:
            halo[0] = x[b, q * TL - 1]
            fl[:, 0] = 1
        if q < 3:
            halo[1] = x[b, (q + 1) * TL]
            fl[:, 1] = 1
        maps.append({"x": _cat([x[b, q * TL:(q + 1) * TL], ctx[b], halo], 0), "mods_fm": mods_fm[r][0],
                     "norm1_g": f(norm1_g)[0], "w_in": f(ab_w_in)[0], "mu": f(rw_mu)[0], "flags": fl,
                     "g_qa": f(mla_g_qa), "g_kva": f(mla_g_kva), "w_q_up": f(mla_w_q_up)[0],
                     "w_kv_up": f(mla_w_kv_up)[0], "g_q": f(mla_g_q), "g_k": f(mla_g_k), "pos": _make_pos(q, TL)})
    rA = run_prog(build_pre0(S), maps)
    maps = []
    rw_r_k_flat = f(rw_r_k)[0].reshape(512)
    for r in R8:
        b, j = r // 4, r % 4
        cores = [4 * b + qq for qq in range(4)]
        rwT = _cat([rA[4 * b]["rwT"][:, :, TL:]] + [rA[cc]["rwT"][:, :, :TL] for cc in cores], 2)
        cs = slice(128 * j, 128 * j + 128)
        pv = np.stack([f(rw_w0)[0, 0, cs], f(rw_w0)[0, 1, cs], f(rw_a0)[0, 0, cs], f(rw_a0)[0, 1, cs],
                       f(rw_k_k)[0, cs], f(rw_k_a)[0, cs], rw_r_k_flat[cs], f(rw_ln_w)[0, cs], f(rw_ln_b)[0, cs]])
        hs = slice(2 * j, 2 * j + 2)
        qT = _cat([rA[cc]["qT"][hs, :, :TL] for cc in cores], 2)
        qcT = np.ascontiguousarray(rA[4 * b]["qT"][hs, :, TL:])
        kT = _cat([rA[4 * b]["kT"][hs, :, TL:]] + [rA[cc]["kT"][hs, :, :TL] for cc in cores], 2)
        vv = _cat([rA[4 * b]["v"][TL:]] + [rA[cc]["v"][:TL] for cc in cores], 0)
        vv = np.ascontiguousarray(vv.reshape(LT, 8, 64)[:, hs].transpose(1, 0, 2))
        maps.append({"rw_r": np.ascontiguousarray(rwT[j]), "rw_k": np.ascontiguousarray(rwT[4 + j]),
                     "rw_v": np.ascontiguousarray(rwT[8 + j]), "lo_w": np.ascontiguousarray(rwT[12]),
                     "lo_a": np.ascontiguousarray(rwT[13]), "lo_g": np.ascontiguousarray(rwT[14]),
                     "w2": np.ascontiguousarray(f(rw_w2)[0][:, :, cs].reshape(128, 128)),
                     "a2": np.ascontiguousarray(f(rw_a2)[0][:, :, cs].reshape(128, 128)),
                     "g2": np.ascontiguousarray(f(rw_g2)[0][:, cs]), "pv": np.ascontiguousarray(pv),
                     "qT": qT, "qcT": qcT, "kT": kT, "v": vv})
    rB = run_prog(build_mix0(S), maps)
    del rA
    wg0 = _cat([f(moe_w_gate)[0], f(shared_w_gate)[0][None]], 0)
    wu0 = _cat([f(moe_w_up)[0], f(shared_w_up)[0][None]], 0)
    wd0 = _cat([f(moe_w_down)[0], f(shared_w_down)[0][None]], 0)
    maps = []
    for r in R8:
        b, q = r // 4, r % 4
        chunks = []
        for kc in range(4):
            rw = rB[4 * b + kc]["rwoT"]
            chunks.append(_cat([rw[:, CTX + q * TL:CTX + (q + 1) * TL], rw[:, :CTX]], 1))
        for j in range(4):
            o = rB[4 * b + j]["oT"].reshape(128, S)
            oc = rB[4 * b + j]["ocT"].reshape(128, CTX)
            chunks.append(_cat([o[:, q * TL:(q + 1) * TL], oc], 1))
        maps.append({"x": _cat([x[b, q * TL:(q + 1) * TL], ctx[b]], 0), "attT": np.ascontiguousarray(np.stack(chunks, 0)),
                     "mods_tm": mods_tm[r][0], "mods_fm": mods_fm[r][0], "w_out": f(ab_w_out)[0],
                     "norm2_g": f(norm2_g)[0], "router_w": f(router_w), "router_b": f(router_b)[None, :],
                     "wg_all": wg0, "wu_all": wu0, "wd_all": wd0})
    rC = run_prog(build_post(S, True), maps)
    del rB
    x1 = [f(rC[r]["x_out"]) for r in R8]
    maps = []
    for r in R8:
        q = r % 4
        maps.append({"x": x1[r], "mods_fm": mods_fm[r][1], "norm1_g": f(norm1_g)[1], "w_in": f(gqa_w_in)[0],
                     "g_q": f(gqa_g_q), "g_k": f(gqa_g_k), "pos": _make_pos(q, TL)})
    rD = run_prog(build_pre1(S), maps)
    maps = []
    for r in R8:
        b, kvh = r // 4, r % 4
        cores = [4 * b + qq for qq in range(4)]
        qk = _cat([rD[cc]["qkT"][:, :, :TL] for cc in cores], 2).reshape(20, 64, S)
        kc_ = rD[4 * b]["qkT"][:, :, TL:].reshape(20, 64, CTX)
        qT = np.ascontiguousarray(qk[4 * kvh:4 * kvh + 4])
        kT = _cat([kc_[16 + kvh], qk[16 + kvh]], 1)[None]
        vv = _cat([rD[4 * b]["v"][TL:]] + [rD[cc]["v"][:TL] for cc in cores], 0)
        vv = np.ascontiguousarray(vv[:, kvh * 64:(kvh + 1) * 64])[None]
        maps.append({"qT": qT, "kT": np.ascontiguousarray(kT), "v": vv})
    rE = run_prog(build_attn1(S), maps)
    del rD
    wg1 = _cat([f(moe_w_gate)[1], f(shared_w_gate)[1][None]], 0)
    wu1 = _cat([f(moe_w_up)[1], f(shared_w_up)[1][None]], 0)
    wd1 = _cat([f(moe_w_down)[1], f(shared_w_down)[1][None]], 0)
    maps = []
    for r in R8:
        b, q = r // 4, r % 4
        chunks = []
        for kc in range(8):
            o = rE[4 * b + kc // 2]["oT"]
            i0 = (kc % 2) * 2
            chunks.append(o[i0:i0 + 2].reshape(128, S)[:, q * TL:(q + 1) * TL])
        maps.append({"x": np.ascontiguousarray(x1[r][:TL]), "attT": np.ascontiguousarray(np.stack(chunks, 0)),
                     "mods_tm": mods_tm[r][1], "mods_fm": mods_fm[r][1], "w_out": f(gqa_w_out)[0],
                     "norm2_g": f(norm2_g)[1], "router_w": f(router_w), "router_b": f(router_b)[None, :],
                     "wg_all": wg1, "wu_all": wu1, "wd_all": wd1})
    rF = run_prog(build_post(S, False), maps)
    out = np.zeros((2, S, D), np.float32)
    for r in R8:
        b, q = r // 4, r % 4
        out[b, q * TL:(q + 1) * TL] = rF[r]["x_out"]
    return out
```

```python
import numpy as np
import concourse.bass as bass
import concourse.mybir as mybir
from concourse.bass_utils import run_bass_kernel_spmd

F32 = mybir.dt.float32
BF16 = mybir.dt.bfloat16
I32 = mybir.dt.int32
AF = mybir.ActivationFunctionType
ALU = mybir.AluOpType
AX = mybir.AxisListType

EPOCH = 30000
SCHED_WINDOW = 40
NSLOT = {"sp": 24, "pool": 12, "act": 8}


class Buf:
    __slots__ = ("name", "lw", "rd", "excl")

    def __init__(self, name, excl=False):
        self.name = name
        self.lw = None
        self.rd = []
        self.excl = excl


class V:
    __slots__ = ("buf", "ap")

    def __init__(self, buf, ap):
        self.buf = buf
        self.ap = ap

    def __getitem__(self, idx):
        return V(self.buf, self.ap[idx])

    def rr(self, pat, **kw):
        return V(self.buf, self.ap.rearrange(pat, **kw))

    def bc(self, shape):
        return V(self.buf, self.ap.broadcast_to(shape))

    def un(self, axis):
        return V(self.buf, self.ap.unsqueeze(axis))

    def bitcast(self, dt):
        return V(self.buf, self.ap.bitcast(dt))

    @property
    def shape(self):
        return self.ap.shape


class Op:
    __slots__ = ("eng", "fn", "deps", "sig", "cnt", "isdma", "slot", "slotval", "prevval", "cost")

    def __init__(self, eng, fn, isdma):
        self.cost = 300.0
        self.eng = eng
        self.fn = fn
        self.deps = set()
        self.sig = False
        self.cnt = 0
        self.isdma = isdma
        self.slot = None
        self.slotval = 0
        self.prevval = 0


class _Scope:
    def __init__(self, p):
        self.p = p

    def __enter__(self):
        self.p._scopes.append([])
        return self

    def __exit__(self, *a):
        p = self.p
        items = p._scopes.pop()
        ops = set(p._fence)
        for cm, b in items:
            if b.lw is not None:
                ops.add(b.lw)
            ops.update(b.rd)
        best = {}
        keep = []
        for i in ops:
            o = p.ops[i]
            if o.isdma:
                if o.slot not in best or best[o.slot] < i:
                    best[o.slot] = i
            else:
                if o.eng not in best or best[o.eng] < i:
                    best[o.eng] = i
        p._fence = keep + list(best.values())
        for cm, b in reversed(items):
            cm.__exit__(None, None, None)
        return False


class Prog:
    ENGS = ("pe", "act", "dve", "pool", "sp")

    def __init__(self):
        self.nc = bass.Bass("TRN2", target_bir_lowering=False)
        self.ops = []
        self._ctx = []
        self.ntile = 0
        self._scopes = []
        self._fence = []
        self._dcount = {q: 0 for q in NSLOT}

    def dram(self, name, shape, dt, kind):
        t = self.nc.dram_tensor(name, list(shape), dt, kind=kind)
        return V(Buf(name), t.ap())

    def sb(self, shape, dt, name=None):
        self.ntile += 1
        name = name or f"t{self.ntile}"
        cm = self.nc.sbuf_tensor(f"{name}_{self.ntile}", list(shape), dt)
        h = cm.__enter__()
        b = Buf(name)
        b.rd = list(self._fence)
        if self._scopes:
            self._scopes[-1].append((cm, b))
        else:
            self._ctx.append(cm)
        return V(b, h[:])

    def scope(self):
        return _Scope(self)

    def ps(self, shape, dt, name=None):
        self.ntile += 1
        name = name or f"p{self.ntile}"
        cm = self.nc.psum_tensor(f"{name}_{self.ntile}", list(shape), dt)
        h = cm.__enter__()
        b = Buf(name, excl=True)
        b.rd = list(self._fence)
        if self._scopes:
            self._scopes[-1].append((cm, b))
        else:
            self._ctx.append(cm)
        return V(b, h[:])

    def op(self, eng, fn, reads, writes, isdma=False, cost=None):
        i = len(self.ops)
        o = Op(eng, fn, isdma)
        if cost is None:
            w0 = next((v for v in writes if v is not None), None)
            n = 1
            if w0 is not None:
                for d_ in w0.ap.shape[1:]:
                    n *= d_
            if isdma:
                cost = 2000.0 + n * 128 * 4 / 150.0
            elif eng == "act":
                cost = 230.0 + n / 1.2
            elif eng == "dve":
                cost = 70.0 + n / 0.96
            elif eng == "pool":
                cost = 150.0 + n / 0.96
            else:
                cost = 300.0
        o.cost = cost
        rb = {id(v.buf): v.buf for v in reads if v is not None}
        wb = {id(v.buf): v.buf for v in writes if v is not None}
        for b in rb.values():
            if b.lw is not None:
                o.deps.add(b.lw)
            if b.excl:
                for r in b.rd:
                    if self.ops[r].eng != eng:
                        o.deps.add(r)
        for b in wb.values():
            if b.lw is not None:
                o.deps.add(b.lw)
            for r in b.rd:
                o.deps.add(r)
        o.deps.discard(i)
        for b in rb.values():
            if id(b) not in wb:
                b.rd.append(i)
        for b in wb.values():
            b.lw = i
            b.rd = []
        if isdma:
            k = self._dcount[eng]
            self._dcount[eng] += 1
            n = NSLOT[eng]
            o.slot = (eng, k % n)
            o.slotval = 16 * (k // n + 1)
            o.prevval = 16 * (k // n)
        self.ops.append(o)
        return i

    def dma(self, out, in_, q="sp", **kw):
        self.op(q, lambda e: e.dma_start(out=out.ap, in_=in_.ap, **kw), [in_], [out], isdma=True)

    def mm(self, out, lhsT, rhs, start=True, stop=True, **kw):
        n = 1
        for d_ in rhs.ap.shape[1:]:
            n *= d_
        passes = 4 if rhs.ap.dtype == F32 else 1
        self.op("pe", lambda e: e.matmul(out.ap, lhsT.ap, rhs.ap, start=start, stop=stop, **kw),
                [lhsT, rhs], [out], cost=30.0 + max(n, 64) * passes * 0.45)

    def tr(self, out, in_, ident):
        passes = 4 if in_.ap.dtype == F32 else 1
        self.op("pe", lambda e: e.transpose(out.ap, in_.ap, ident.ap), [in_, ident], [out],
                cost=60.0 + 128 * passes * 0.45)

    def act(self, out, in_, func, bias=None, scale=None, accum=None):
        kw = {}
        rd = [in_]
        if bias is not None:
            if isinstance(bias, V):
                kw["bias"] = bias.ap
                rd.append(bias)
            else:
                kw["bias"] = bias
        if scale is not None:
            if isinstance(scale, V):
                kw["scale"] = scale.ap
                rd.append(scale)
            else:
                kw["scale"] = scale
        wr = [out]
        if accum is not None:
            kw["accum_out"] = accum.ap
            wr.append(accum)
        self.op("act", lambda e: e.activation(out.ap, in_.ap, func, **kw), rd, wr)

    def tt(self, out, a, b, op, eng="dve"):
        self.op(eng, lambda e: e.tensor_tensor(out.ap, a.ap, b.ap, op), [a, b], [out])

    def ts(self, out, a, s1, op0, s2=None, op1=None, eng="dve", accum=None):
        rd = [a]
        x1 = s1.ap if isinstance(s1, V) else s1
        x2 = s2.ap if isinstance(s2, V) else s2
        if isinstance(s1, V):
            rd.append(s1)
        if isinstance(s2, V):
            rd.append(s2)
        kw = {}
        wr = [out]
        if op1 is not None:
            kw["op1"] = op1
        if accum is not None:
            kw["accum_out"] = accum.ap
            wr.append(accum)
        self.op(eng, lambda e: e.tensor_scalar(out.ap, a.ap, x1, x2, op0, **kw), rd, wr)

    def stt(self, out, a, s, b, op0, op1, eng="dve"):
        rd = [a, b]
        x = s.ap if isinstance(s, V) else s
        if isinstance(s, V):
            rd.append(s)
        self.op(eng, lambda e: e.scalar_tensor_tensor(out.ap, a.ap, x, b.ap, op0, op1), rd, [out])

    def cp(self, out, in_, eng="dve"):
        if eng == "act":
            self.op("act", lambda e: e.copy(out.ap, in_.ap), [in_], [out])
        else:
            self.op(eng, lambda e: e.tensor_copy(out.ap, in_.ap), [in_], [out])

    def red(self, out, in_, op=ALU.add, axis=AX.X, eng="dve"):
        self.op(eng, lambda e: e.tensor_reduce(out.ap, in_.ap, axis, op), [in_], [out])

    def memset(self, out, val, eng="pool"):
        self.op(eng, lambda e: e.memset(out.ap, val), [], [out])

    def iota(self, out, pattern, base=0, cm=0):
        self.op("pool", lambda e: e.iota(out.ap, pattern, base=base, channel_multiplier=cm,
                                         allow_small_or_imprecise_dtypes=True), [], [out])

    def _schedule(self, ops):
        n = len(ops)
        W = SCHED_WINDOW
        SEM = 120.0
        users = [[] for _ in range(n)]
        nleft = [0] * n
        for i, o in enumerate(ops):
            nleft[i] = len(o.deps)
            for j in o.deps:
                users[j].append(i)
        pend = {e: [i for i, o in enumerate(ops) if o.eng == e] for e in self.ENGS}
        head = {e: 0 for e in self.ENGS}
        done = [False] * n
        finish = [0.0] * n
        ready = [0.0] * n
        tfree = {e: 0.0 for e in self.ENGS}
        order = {e: [] for e in self.ENGS}
        remaining = n
        while remaining:
            best = None
            for e in self.ENGS:
                lst = pend[e]
                h = head[e]
                while h < len(lst) and done[lst[h]]:
                    h += 1
                head[e] = h
                seen_dma = False
                cnt = 0
                k = h
                while k < len(lst) and cnt < W:
                    i = lst[k]
                    k += 1
                    if done[i]:
                        continue
                    cnt += 1
                    o = ops[i]
                    if o.isdma:
                        if seen_dma:
                            continue
                        seen_dma = True
                    if nleft[i] > 0:
                        continue
                    st = max(tfree[e], ready[i])
                    key = (st + 0.5 * (cnt - 1), i)
                    if best is None or key < best[0]:
                        best = (key, e, i, st)
            if best is None:
                raise RuntimeError("scheduler deadlock")
            _, e, i, st = best
            o = ops[i]
            done[i] = True
            remaining -= 1
            order[e].append(i)
            if o.isdma:
                tfree[e] = st + 60.0
                finish[i] = st + o.cost
            else:
                tfree[e] = st + o.cost
                finish[i] = st + o.cost
            for u in users[i]:
                nleft[u] -= 1
                lat = 0.0 if (o.eng == "pe" and ops[u].eng == "pe" and not o.isdma) else SEM
                r = finish[i] + lat
                if r > ready[u]:
                    ready[u] = r
        self.est_ns = max(finish) if finish else 0.0
        return order

    def finish(self):
        nc = self.nc
        ops = self.ops
        last = {}
        for i, o in enumerate(ops):
            last[o.eng] = i
        fin = Op("sp", None, False)
        for e, i in last.items():
            fin.deps.add(i)
        lastslot = {}
        for i, o in enumerate(ops):
            if o.isdma:
                lastslot[o.slot] = i
        for i in lastslot.values():
            fin.deps.add(i)
        ops.append(fin)
        order = self._schedule(ops) if SCHED_WINDOW > 0 else {e: [i for i, o in enumerate(ops) if o.eng == e] for e in self.ENGS}
        self._order = order
        for o in ops:
            for j in o.deps:
                d = ops[j]
                if d.eng == "pe" and o.eng == "pe" and not o.isdma:
                    continue
                d.sig = True
        ccount = {e: 0 for e in self.ENGS}
        dcount = self._dcount
        for e_ in self.ENGS:
            for i_ in order[e_]:
                o = ops[i_]
                if o.isdma:
                    pass
                elif o.sig:
                    ccount[o.eng] += 1
                    o.cnt = ccount[o.eng]
        sems = {}
        cms = []

        def getsem(key):
            if key not in sems:
                cm = nc.semaphore(f"s_{key[0]}_{key[1]}")
                sems[key] = cm.__enter__()
                cms.append(cm)
            return sems[key]

        for e in self.ENGS:
            for ep in range(ccount[e] // EPOCH + 1):
                getsem((e, "c%d" % ep))
        for q, n in NSLOT.items():
            for s in range(min(n, dcount[q])):
                getsem((q, s))

        def compkey(o):
            ep = (o.cnt - 1) // EPOCH
            return (o.eng, "c%d" % ep), o.cnt - ep * EPOCH

        with nc.Block() as block:
            def emit_engine(ename, eng):
                known = {}
                for i in order[ename]:
                    o = ops[i]
                    waits = {}
                    for j in o.deps:
                        d = ops[j]
                        if d.isdma:
                            key, val = d.slot, d.slotval
                        else:
                            if d.eng == "pe" and ename == "pe" and not o.isdma:
                                continue
                            key, val = compkey(d)
                        if waits.get(key, 0) < val:
                            waits[key] = val
                    if o.isdma and o.prevval > 0:
                        key = o.slot
                        if waits.get(key, 0) < o.prevval:
                            waits[key] = o.prevval
                    for key, val in waits.items():
                        if known.get(key, 0) >= val:
                            continue
                        known[key] = val
                        eng.wait_ge(getsem(key), val)
                    if o.fn is None:
                        continue
                    ins = o.fn(eng)
                    if o.isdma:
                        ins.then_inc(getsem(o.slot), 16)
                    elif o.sig:
                        key, _ = compkey(o)
                        ins.then_inc(getsem(key), 1)

            @block.tensor
            def _(e):
                emit_engine("pe", e)

            @block.scalar
            def _(e):
                emit_engine("act", e)

            @block.vector
            def _(e):
                emit_engine("dve", e)

            @block.gpsimd
            def _(e):
                emit_engine("pool", e)

            @block.sync
            def _(e):
                emit_engine("sp", e)
        self._cms = cms
        return nc


D = 1024
CTX = 256
NORM_EPS = 1e-6
THETA = 10000.0
import math


class Ctx:
    def __init__(self, p, banks=True):
        self.p = p
        self.bi = 0
        self.dbanks = []
        if banks:
            self.set_banks(False)
        io = p.sb([128, 128], F32, "io")
        pi = p.sb([128, 1], F32, "pi")
        p.iota(io, [[1, 128]], base=0, cm=0)
        p.iota(pi, [[0, 1]], base=0, cm=1)
        self.pidx = pi
        self.identf = p.sb([128, 128], F32, "identf")
        p.ts(self.identf, io, pi[:, 0:1], ALU.is_equal)
        self.identb = p.sb([128, 128], BF16, "identb")
        p.cp(self.identb, self.identf)
        self.iof = io

    def set_banks(self, dbl):
        p = self.p
        if dbl:
            self.dbanks = [p.ps([128, 1024], F32, f"dbank{i}") for i in range(2)]
            self.banks = [self.dbanks[0][:, 0:512], self.dbanks[0][:, 512:1024],
                          self.dbanks[1][:, 0:512], self.dbanks[1][:, 512:1024]]
            self.banks += [p.ps([128, 512], F32, f"bank{i}") for i in range(4, 8)]
        else:
            self.banks = [p.ps([128, 512], F32, f"bank{i}") for i in range(8)]

    def bank(self):
        b = self.banks[self.bi % 8]
        self.bi += 1
        return b

    def load_fm(self, rows_v, n, eng_out="dve"):
        p = self.p
        tmp = p.sb([n, 128], F32, "lfm_tmp")
        p.dma(tmp, rows_v)
        bk = self.bank()
        p.tr(bk[:, 0:n], tmp, self.identf[0:n, 0:n])
        out = p.sb([128, n], F32, "lfm_out")
        p.cp(out, bk[:, 0:n], eng=eng_out)
        return out

    def rstd(self, ss, n, inv_d, eps):
        p = self.p
        p.ts(ss, ss, inv_d, ALU.mult, eps, ALU.add)
        p.act(ss, ss, AF.Ln)
        p.act(ss, ss, AF.Exp, scale=-0.5)


def run_prog(p, in_maps):
    nc = p.finish()
    res = run_bass_kernel_spmd(nc, in_maps, core_ids=list(range(len(in_maps))))
    return res.results


def build_phase0():
    p = Prog()
    cvec = p.dram("cvec", [2, D], F32, "ExternalInput")
    ada_w = p.dram("ada_w", [2, D, 6 * D], F32, "ExternalInput")
    ada_b = p.dram("ada_b", [2, 6 * D], F32, "ExternalInput")
    mods_tm = p.dram("mods_tm", [2, 2, 6 * D], F32, "ExternalOutput")
    mods_fm = p.dram("mods_fm", [2, 128, 48, 2], F32, "ExternalOutput")
    c = Ctx(p)
    cT = c.load_fm(cvec.rr("j (c p) -> (j c) p", p=128), 16)
    sT = p.sb([128, 16], F32, "sT")
    p.act(sT, cT, AF.Silu)
    sTv = sT.rr("p (j c) -> p c j", j=2)
    wbuf = [p.sb([128, 8, 1536], F32, f"adaw{i}") for i in range(2)]
    it = 0
    for l in range(2):
        bfm = c.load_fm(ada_b[l].rr("(c p) -> c p", p=128), 48)
        btm = p.sb([2, 6 * D], F32, "btm")
        p.dma(btm, ada_b[l:l + 1, :].bc([2, 6 * D]))
        otm = p.sb([2, 6 * D], F32, "otm")
        ofm = p.sb([128, 48, 2], F32, "ofm")
        for qd in range(4):
            w = wbuf[it % 2]
            it += 1
            for kc in range(8):
                p.dma(w[:, kc, :], ada_w[l, kc * 128:(kc + 1) * 128, qd * 1536:(qd + 1) * 1536])
            for blk in range(3):
                bk = c.bank()
                for kc in range(8):
                    p.mm(bk[0:2, :], sTv[:, kc, :], w[:, kc, blk * 512:(blk + 1) * 512],
                         start=(kc == 0), stop=(kc == 7))
                col = qd * 1536 + blk * 512
                p.tt(otm[:, col:col + 512], bk[0:2, :], btm[:, col:col + 512], ALU.add)
            bk = c.bank()
            for cc in range(12):
                for kc in range(8):
                    p.mm(bk[:, cc * 2:cc * 2 + 2], w[:, kc, cc * 128:(cc + 1) * 128], sTv[:, kc, :],
                         start=(kc == 0), stop=(kc == 7))
            p.tt(ofm[:, qd * 12:(qd + 1) * 12, :], bk[:, 0:24].rr("p (c j) -> p c j", j=2),
                 bfm[:, qd * 12:(qd + 1) * 12].un(2).bc([128, 12, 2]), ALU.add)
        p.dma(mods_tm[l], otm)
        p.dma(mods_fm[l], ofm)
    return p


def build_post(S, has_ctx, stop=99):
    TL = S // 4
    NTL = TL // 128
    NT = NTL + (2 if has_ctx else 0)
    TOK = NT * 128
    p = Prog()
    x_in = p.dram("x", [TOK, D], F32, "ExternalInput")
    attT = p.dram("attT", [8, 128, TOK], BF16, "ExternalInput")
    mods_tm = p.dram("mods_tm", [2, 6 * D], F32, "ExternalInput")
    mods_fm = p.dram("mods_fm", [128, 48, 2], F32, "ExternalInput")
    w_out = p.dram("w_out", [D, D], F32, "ExternalInput")
    n2g = p.dram("norm2_g", [D], F32, "ExternalInput")
    router_w = p.dram("router_w", [D, 16], F32, "ExternalInput")
    router_b = p.dram("router_b", [1, 16], F32, "ExternalInput")
    wg_all = p.dram("wg_all", [17, D, 256], F32, "ExternalInput")
    wu_all = p.dram("wu_all", [17, D, 256], F32, "ExternalInput")
    wd_all = p.dram("wd_all", [17, 256, D], F32, "ExternalInput")
    x_out = p.dram("x_out", [TOK, D], F32, "ExternalOutput")
    c = Ctx(p)
    B = c.banks
    nvar = 2 if has_ctx else 1
    blocks = []
    t = 0
    while t < NTL:
        n = min(4, NTL - t)
        blocks.append((0, t, n))
        t += n
    if has_ctx:
        blocks.append((1, NTL, 2))

    mfm = p.sb([128, 48, 2], F32, "mfm")
    p.dma(mfm, mods_fm)
    g2fm = c.load_fm(n2g.rr("(c p) -> c p", p=128), 8)
    A2 = p.sb([128, 8, 2], F32, "A2")
    p.ts(A2, mfm[:, 32:40, :], 1.0, ALU.add)
    p.tt(A2, A2, g2fm.un(2).bc([128, 8, 2]), ALU.mult)
    SH2 = mfm[:, 24:32, :]
    rw = p.sb([128, 8, 16], F32, "rw")
    p.dma(rw, router_w.rr("(kc p) e -> p kc e", p=128))
    rb = p.sb([128, 16], F32, "rb")
    p.dma(rb, router_b.bc([128, 16]))
    sel = p.sb([16, 16, 128], F32, "sel")
    p.iota(sel, [[1, 16], [0, 128]], base=0, cm=0)
    p.ts(sel, sel, c.pidx[0:16, 0:1], ALU.is_equal)
    xs = p.sb([128, NT, D], F32, "xs")
    for t in range(NT):
        p.dma(xs[:, t, :], x_in[t * 128:(t + 1) * 128, :])
    h2T = p.sb([128, 8, TOK], BF16, "h2T")
    combT = p.sb([16, TOK], F32, "combT")
    lg_all = p.sb([128, NT, 16], F32, "lg_all")
    gabc = [p.sb([128, D], F32, f"gabc{i}") for i in range(2)]

    def load_ga(which, var, dst):
        p.dma(dst, mods_tm[var:var + 1, which * D:(which + 1) * D].bc([128, D]))

    with p.scope():
        if stop >= 2:
            wo = p.sb([128, 8, D], BF16, "wo")
            atb = [p.sb([128, 8, 128], BF16, f"atb{i}") for i in range(2)]
            for var in range(nvar):
                p.dma(wo, w_out.rr("(kc p) d -> p kc d", p=128), q="pool")
                load_ga(2, var, gabc[0])
                p.tt(wo, wo, gabc[0].un(1).bc([128, 8, D]), ALU.mult, eng="pool")
                tiles = range(NTL) if var == 0 else range(NTL, NT)
                for t in tiles:
                    a = atb[t % 2]
                    p.dma(a, attT[:, :, t * 128:(t + 1) * 128].rr("c p t -> p c t"))
                    for half in range(2):
                        bk = B[(t % 2) * 2 + half]
                        for kc in range(8):
                            p.mm(bk[:, :], a[:, kc, :], wo[:, kc, half * 512:(half + 1) * 512],
                                 start=(kc == 0), stop=(kc == 7))
                        xv = xs[:, t, half * 512:(half + 1) * 512]
                        p.tt(xv, xv, bk, ALU.add)

    with p.scope():
        if stop >= 3:
            junk = p.sb([128, D], BF16, "junk")
            xn = [p.sb([128, D], F32, f"xn{i}") for i in range(2)]
            h2f = [p.sb([128, 8, 128], F32, f"h2f{i}") for i in range(2)]
            ssq = p.sb([128, NT], F32, "ssq")
            for t in range(NT):
                var = 0 if t < NTL else 1
                p.act(junk, xs[:, t, :], AF.Square, accum=ssq[:, t:t + 1])
            c.rstd(ssq, NT, 1.0 / D, NORM_EPS)
            for t in range(NT):
                var = 0 if t < NTL else 1
                x_n = xn[t % 2]
                hf = h2f[t % 2]
                p.ts(x_n, xs[:, t, :], ssq[:, t:t + 1], ALU.mult)
                for hb in range(2):
                    bk = B[4 + (t % 2) * 2 + hb]
                    for k4 in range(4):
                        kc = hb * 4 + k4
                        p.tr(bk[:, k4 * 128:(k4 + 1) * 128], x_n[:, kc * 128:(kc + 1) * 128], c.identf)
                    for k4 in range(4):
                        kc = hb * 4 + k4
                        p.act(hf[:, kc, :], bk[:, k4 * 128:(k4 + 1) * 128], AF.Identity,
                              bias=SH2[:, kc, var:var + 1], scale=A2[:, kc, var:var + 1])
                p.cp(h2T[:, :, t * 128:(t + 1) * 128], hf, eng="pool")
                bk = B[t % 2]
                for kc in range(8):
                    p.mm(bk[:, 0:16], hf[:, kc, :], rw[:, kc, :], start=(kc == 0), stop=(kc == 7))
                p.cp(lg_all[:, t, :], bk[:, 0:16])
            N16 = NT * 16
            sc = p.sb([128, NT, 16], F32, "sc")
            bs = p.sb([128, NT, 16], F32, "bs")
            p.act(sc, lg_all, AF.Sigmoid)
            p.tt(bs, sc, rb.un(1).bc([128, NT, 16]), ALU.add)
            bsv = bs.rr("p t (g e) -> p (t g) e", g=4)
            G = NT * 4
            m1 = p.sb([128, G], F32, "m1")
            m2 = p.sb([128, G], F32, "m2")
            p.red(m1, bsv, op=ALU.max)
            eq1 = p.sb([128, G, 4], F32, "eq1")
            p.tt(eq1, bsv, m1.un(2).bc([128, G, 4]), ALU.is_equal)
            p.stt(eq1, eq1, -1e9, bsv, ALU.mult, ALU.add)
            p.red(m2, eq1, op=ALU.max)
            gs = p.sb([128, NT, 4], F32, "gs")
            p.tt(gs.rr("p t g -> p (t g)"), m1, m2, ALU.add)
            gmax = p.sb([128, NT], F32, "gmax")
            p.red(gmax, gs, op=ALU.max)
            gsel = p.sb([128, NT, 4], F32, "gsel")
            p.tt(gsel, gs, gmax.un(2).bc([128, NT, 4]), ALU.is_equal)
            ge2 = p.sb([128, G, 4], F32, "ge2")
            p.tt(ge2, bsv, m2.un(2).bc([128, G, 4]), ALU.is_ge)
            p.tt(ge2, ge2, gsel.rr("p t g -> p (t g)").un(2).bc([128, G, 4]), ALU.mult)
            p.tt(ge2, ge2, sc.rr("p t (g e) -> p (t g) e", g=4), ALU.mult)
            den = p.sb([128, NT], F32, "den")
            p.red(den, ge2.rr("p (t g) e -> p t (g e)", g=4))
            p.op("dve", lambda e: e.reciprocal(den.ap, den.ap), [den], [den])
            comb = p.sb([128, NT, 16], F32, "comb")
            p.tt(comb, ge2.rr("p (t g) e -> p t (g e)", g=4), den.un(2).bc([128, NT, 16]), ALU.mult)
            for t in range(NT):
                bk = B[t % 2]
                p.tr(bk[0:16, 0:128], comb[:, t, :], c.identf)
                p.cp(combT[:, t * 128:(t + 1) * 128], bk[0:16, 0:128], eng="act")

    with p.scope():
        if stop >= 4:
            wg = [p.sb([128, 8, 256], BF16, f"wg{i}") for i in range(2)]
            wu = [p.sb([128, 8, 256], BF16, f"wu{i}") for i in range(2)]
            wd = [p.sb([128, 2, D], BF16, f"wd{i}") for i in range(2)]
            wds = [p.sb([128, 2, D], BF16, f"wds{i}") for i in range(2)]
            comb_sb = [p.sb([128, 512], F32, f"comb_sb{i}") for i in range(2)]
            sg = [p.sb([128, 512], F32, f"sg{i}") for i in range(2)]
            tg = [p.sb([128, 512], F32, f"tg{i}") for i in range(2)]
            actb = [p.sb([128, 2, 512], BF16, f"actb{i}") for i in range(2)]
            for var in range(nvar):
                load_ga(5, var, gabc[var])
            it = 0
            for e in range(17 if stop < 40 or stop == 99 else stop - 40):
                eb = e % 2
                p.dma(wg[eb], wg_all[e].rr("(kc p) f -> p kc f", p=128), q="pool")
                p.dma(wu[eb], wu_all[e].rr("(kc p) f -> p kc f", p=128), q="pool")
                p.dma(wd[eb], wd_all[e].rr("(fc p) d -> p fc d", p=128), q="pool")
                for var in range(nvar):
                    p.tt(wds[eb], wd[eb], gabc[var].un(1).bc([128, 2, D]), ALU.mult, eng="pool")
                    for (bv, t0, nt) in blocks:
                        if bv != var:
                            continue
                        n = nt * 128
                        tok = slice(t0 * 128, t0 * 128 + n)
                        ib = it % 2
                        it += 1
                        if e < 16:
                            p.mm(B[4][:, 0:n], sel[:, e, :], combT[:, tok])
                            p.cp(comb_sb[ib][:, 0:n], B[4][:, 0:n], eng="act")
                        for fc in range(2):
                            gb = B[fc * 2]
                            ub = B[fc * 2 + 1]
                            for kc in range(8):
                                p.mm(gb[:, 0:n], wg[eb][:, kc, fc * 128:(fc + 1) * 128], h2T[:, kc, tok],
                                     start=(kc == 0), stop=(kc == 7))
                            for kc in range(8):
                                p.mm(ub[:, 0:n], wu[eb][:, kc, fc * 128:(fc + 1) * 128], h2T[:, kc, tok],
                                     start=(kc == 0), stop=(kc == 7))
                            p.act(sg[fc][:, 0:n], gb[:, 0:n], AF.Silu)
                            if e < 16:
                                p.tt(tg[fc][:, 0:n], ub[:, 0:n], comb_sb[ib][:, 0:n], ALU.mult)
                                p.tt(actb[ib][:, fc, 0:n], sg[fc][:, 0:n], tg[fc][:, 0:n], ALU.mult, eng="pool")
                            else:
                                p.tt(actb[ib][:, fc, 0:n], sg[fc][:, 0:n], ub[:, 0:n], ALU.mult)
                        for ti in range(nt):
                            t = t0 + ti
                            for half in range(2):
                                db = B[5 + (ti * 2 + half) % 3]
                                for fc in range(2):
                                    p.mm(db, actb[ib][:, fc, ti * 128:(ti + 1) * 128],
                                         wds[eb][:, fc, half * 512:(half + 1) * 512],
                                         start=(fc == 0), stop=(fc == 1))
                                xv = xs[:, t, half * 512:(half + 1) * 512]
                                p.tt(xv, xv, db, ALU.add)
    for t in range(NT):
        p.dma(x_out[t * 128:(t + 1) * 128, :], xs[:, t, :])
    return p


def rope_tables(p, c, pos, NTL, half):
    inv = p.sb([128, half], F32, "inv")
    for i in range(half):
        p.memset(inv[:, i:i + 1], float(THETA ** (-i / half)) / (2.0 * math.pi))
    Y = p.sb([128, NTL, 2, half], F32, "ropeY")
    p.tt(Y.rr("p t r h -> p (t r) h"), pos.rr("p t r -> p (t r)").un(2).bc([128, NTL * 2, half]),
         inv.un(1).bc([128, NTL * 2, half]), ALU.mult)
    COS = p.sb([128, NTL, 2, 2 * half], F32, "COS")
    SINS = p.sb([128, NTL, 2, 2 * half], F32, "SINS")
    Yi = p.sb([128, NTL, 2, half], I32, "ropeYi")
    Yf = p.sb([128, NTL, 2, half], F32, "ropeYf")
    T = p.sb([128, NTL, 2, half], F32, "ropeT")
    R = p.sb([128, NTL, 2, half], F32, "ropeR")
    for which in range(2):
        if which == 1:
            p.ts(Y, Y, 0.25, ALU.add)
        p.cp(Yi, Y)
        p.cp(Yf, Yi)
        p.tt(R, Y, Yf, ALU.subtract)
        p.ts(T, R, 0.5, ALU.is_gt)
        p.tt(R, R, T, ALU.subtract)
        p.ts(T, R, -0.5, ALU.is_lt)
        p.tt(R, R, T, ALU.add)
        if which == 0:
            p.act(SINS[:, :, :, half:2 * half], R, AF.Sin, scale=2.0 * math.pi * (1 - 1e-6))
            p.ts(SINS[:, :, :, 0:half], SINS[:, :, :, half:2 * half], -1.0, ALU.mult)
        else:
            p.act(COS[:, :, :, 0:half], R, AF.Sin, scale=2.0 * math.pi * (1 - 1e-6))
            p.cp(COS[:, :, :, half:2 * half], COS[:, :, :, 0:half])
    return COS, SINS


def qk_post(p, c, pieces, nh, hd, gains_bc, dst, scratch, rope=None):
    sq, qk, ss = scratch["sq"], scratch["qk"], scratch["ss"]
    off = 0
    for (v, n) in pieces:
        p.act(sq[:, off:off + n * hd], v, AF.Square)
        off += n * hd
    p.red(ss, sq.rr("p (h d) -> p h d", d=hd))
    c.rstd(ss, nh, 1.0 / hd, NORM_EPS)
    off = 0
    h0 = 0
    for (v, n) in pieces:
        p.tt(qk[:, off:off + n * hd].rr("p (h d) -> p h d", d=hd), v.rr("p (h d) -> p h d", d=hd),
             ss[:, h0:h0 + n].un(2).bc([128, n, hd]), ALU.mult)
        off += n * hd
        h0 += n
    if rope is None:
        p.tt(dst, qk, gains_bc, ALU.mult, eng="pool")
        return
    p.tt(qk, qk, gains_bc, ALU.mult, eng="pool")
    roff, half, COS_t, SINS_t = rope
    qv = qk.rr("p (h d) -> p h d", d=hd)
    dv = dst.rr("p (h d) -> p h d", d=hd)
    if roff > 0:
        p.cp(dv[:, :, 0:roff], qv[:, :, 0:roff], eng="pool")
    t1, t2 = scratch["t1"], scratch["t2"]
    w = 2 * half
    for rc in range(2):
        xs = qv[:, :, roff + rc * w: roff + (rc + 1) * w]
        a = t1[:, 0:nh * w].rr("p (h d) -> p h d", d=w)
        b = t2[:, 0:nh * w].rr("p (h d) -> p h d", d=w)
        eng = "dve" if rc == 0 else "pool"
        p.tt(a, xs, COS_t[:, rc, :].un(1).bc([128, nh, w]), ALU.mult, eng=eng)
        p.tt(b[:, :, 0:half], xs[:, :, half:w], SINS_t[:, rc, 0:half].un(1).bc([128, nh, half]), ALU.mult, eng=eng)
        p.tt(b[:, :, half:w], xs[:, :, 0:half], SINS_t[:, rc, half:w].un(1).bc([128, nh, half]), ALU.mult, eng=eng)
        p.tt(dv[:, :, roff + rc * w: roff + (rc + 1) * w], a, b, ALU.add, eng=eng)


def norm1_hT(p, c, xt, A, SH, var, hT_dst, xnb, junk, ssq, banks):
    p.act(junk, xt, AF.Square, accum=ssq)
    c.rstd(ssq, 1, 1.0 / D, NORM_EPS)
    p.ts(xnb, xt, ssq[:, 0:1], ALU.mult)
    for hb in range(2):
        bk = banks[hb].bitcast(BF16)
        for k4 in range(4):
            kc = hb * 4 + k4
            p.tr(bk[:, k4 * 128:(k4 + 1) * 128], xnb[:, kc * 128:(kc + 1) * 128], c.identb)
        for k4 in range(4):
            kc = hb * 4 + k4
            p.act(hT_dst[:, kc, :], bk[:, k4 * 128:(k4 + 1) * 128], AF.Identity,
                  bias=SH[:, kc, var:var + 1], scale=A[:, kc, var:var + 1])


def mod_consts(p, c, mods_fm, norm_g, which_sh, which_sc):
    mfm = p.sb([128, 48, 2], F32, "mfm")
    p.dma(mfm, mods_fm)
    gfm = c.load_fm(norm_g.rr("(c p) -> c p", p=128), 8)
    A = p.sb([128, 8, 2], F32, "Amod")
    p.ts(A, mfm[:, which_sc * 8:(which_sc + 1) * 8, :], 1.0, ALU.add)
    p.tt(A, A, gfm.un(2).bc([128, 8, 2]), ALU.mult)
    SH = mfm[:, which_sh * 8:(which_sh + 1) * 8, :]
    return A, SH


def build_pre1(S):
    TL = S // 4
    NTL = TL // 128
    NT = NTL + 2
    TOK = NT * 128
    p = Prog()
    x_in = p.dram("x", [TOK, D], F32, "ExternalInput")
    mods_fm = p.dram("mods_fm", [128, 48, 2], F32, "ExternalInput")
    n1g = p.dram("norm1_g", [D], F32, "ExternalInput")
    w_in = p.dram("w_in", [D, 1536], F32, "ExternalInput")
    g_q = p.dram("g_q", [1, 64], F32, "ExternalInput")
    g_k = p.dram("g_k", [1, 64], F32, "ExternalInput")
    pos_in = p.dram("pos", [128, NTL, 2], F32, "ExternalInput")
    qkT = p.dram("qkT", [10, 128, TOK], BF16, "ExternalOutput")
    v_out = p.dram("v", [TOK, 256], BF16, "ExternalOutput")
    c = Ctx(p)
    B = c.banks
    A, SH = mod_consts(p, c, mods_fm, n1g, 0, 1)
    pos = p.sb([128, NTL, 2], F32, "pos")
    p.dma(pos, pos_in)
    COS, SINS = rope_tables(p, c, pos, NTL, 16)
    gains = p.sb([128, 20, 64], F32, "gains")
    gq = p.sb([128, 64], F32, "gq")
    gk = p.sb([128, 64], F32, "gk")
    p.dma(gq, g_q.bc([128, 64]))
    p.dma(gk, g_k.bc([128, 64]))
    p.cp(gains[:, 0:16, :], gq.un(1).bc([128, 16, 64]))
    p.cp(gains[:, 16:20, :], gk.un(1).bc([128, 4, 64]))
    gains_f = gains.rr("p h d -> p (h d)")
    wi = p.sb([128, 8, 1536], BF16, "wi")
    p.dma(wi, w_in.rr("(kc p) n -> p kc n", p=128), q="pool")
    xt = [p.sb([128, D], F32, f"xt{i}") for i in range(2)]
    xnb = [p.sb([128, D], BF16, f"xnb{i}") for i in range(2)]
    junk = p.sb([128, D], BF16, "junk")
    hT = [p.sb([128, 8, 128], BF16, f"hT{i}") for i in range(2)]
    ssq = [p.sb([128, 1], F32, f"ssq{i}") for i in range(2)]
    scr = dict(sq=p.sb([128, 1280], F32, "sq"), qk=p.sb([128, 1280], F32, "qk"), ss=p.sb([128, 20], F32, "ss"),
               t1=p.sb([128, 1280], F32, "t1"), t2=p.sb([128, 1280], F32, "t2"))
    qkb = [p.sb([128, 1280], BF16, f"qkb{i}") for i in range(2)]
    vb = [p.sb([128, 256], BF16, f"vb{i}") for i in range(2)]
    qkTs = [p.sb([128, 10, 128], BF16, f"qkTs{i}") for i in range(2)]
    for t in range(NT):
        i2 = t % 2
        var = 0 if t < NTL else 1
        p.dma(xt[i2], x_in[t * 128:(t + 1) * 128, :])
        norm1_hT(p, c, xt[i2], A, SH, var, hT[i2], xnb[i2], junk, ssq[i2], (B[0], B[1]))
        for blk in range(3):
            bk = B[2 + blk]
            for kc in range(8):
                p.mm(bk, hT[i2][:, kc, :], wi[:, kc, blk * 512:(blk + 1) * 512], start=(kc == 0), stop=(kc == 7))
        rope = None if var == 1 else (0, 16, COS[:, t], SINS[:, t])
        qk_post(p, c, [(B[2], 8), (B[3], 8), (B[4][:, 0:256], 4)], 20, 64, gains_f, qkb[i2], scr, rope)
        p.cp(vb[i2], B[4][:, 256:512], eng="act")
        p.dma(v_out[t * 128:(t + 1) * 128, :], vb[i2])
        bA = B[5].bitcast(BF16)
        bB = B[6].bitcast(BF16)
        for pr in range(10):
            dstb = bA[:, pr * 128:(pr + 1) * 128] if pr < 8 else bB[:, (pr - 8) * 128:(pr - 7) * 128]
            p.tr(dstb, qkb[i2][:, pr * 128:(pr + 1) * 128], c.identb)
        p.cp(qkTs[i2][:, 0:8, :].rr("p a t -> p (a t)"), bA, eng="act")
        p.cp(qkTs[i2][:, 8:10, :].rr("p a t -> p (a t)"), bB[:, 0:256])
        p.dma(qkT[:, :, t * 128:(t + 1) * 128].rr("a p t -> p a t"), qkTs[i2])
    return p


def attention_gen(p, c, qT_d, kT_d, v_d, oT_d, NH, NKV, dk, dv, S, scale, qcT_d=None, ocT_d=None, banks=None):
    LK = CTX + S
    NKT = LK // 128
    B = banks if banks is not None else c.banks
    kT = p.sb([128, NKV, LK], BF16, "kT")
    if dk < 128:
        p.memset(kT, 0.0)
    for kv in range(NKV):
        p.dma(kT[0:dk, kv, :], kT_d[kv])
    vp = p.sb([128, NKV, NKT, dv + 1], BF16, "vp")
    p.memset(vp[:, :, :, dv:dv + 1], 1.0)
    for kv in range(NKV):
        p.dma(vp[:, kv, :, 0:dv], v_d[kv].rr("(t p) d -> p t d", p=128))
    selden = p.sb([dv + 1, dv], F32, "selden")
    p.memset(selden, 0.0)
    p.memset(selden[dv:dv + 1, :], 1.0)
    qTs = [p.sb([128, NH, 512], BF16, f"qTs{i}") for i in range(2)]
    if dk < 128:
        for t_ in qTs:
            p.memset(t_, 0.0)
    pT = [p.sb([128, 512], BF16, f"pT{i}") for i in range(4)]
    accs = [p.sb([dv + 1, 512], F32, f"accs{i}") for i in range(2)]
    rec = [p.sb([dv, 512], F32, f"rec{i}") for i in range(2)]
    ob = [p.sb([dv, 512], BF16, f"ob{i}") for i in range(2)]
    jobs = []
    for qb in range(S // 512):
        jobs.append((qT_d, oT_d, qb * 512, 512, NKT))
    if qcT_d is not None:
        jobs.append((qcT_d, ocT_d, 0, CTX, CTX // 128))
    DB = c.dbanks
    items = []
    for ji, (qd, od, q0, n, nkt) in enumerate(jobs):
        for h in range(NH):
            for kp in range(nkt // 2):
                items.append((ji, h, kp))
    pT2 = [p.sb([128, 2, 512], BF16, f"pT2_{i}") for i in range(3)]
    loaded = set()

    def load_q(ji):
        if ji in loaded or ji >= len(jobs):
            return
        loaded.add(ji)
        qd, od, q0, n, nkt = jobs[ji]
        qs = qTs[ji % 2]
        for h in range(NH):
            p.dma(qs[0:dk, h, 0:n], qd[h, :, q0:q0 + n])

    def issue_s(i):
        ji, h, kp = items[i]
        qd, od, q0, n, nkt = jobs[ji]
        load_q(ji)
        kv = h * NKV // NH
        db = DB[i % 2]
        for a_ in range(2):
            kt = 2 * kp + a_
            p.mm(db[:, a_ * 512:a_ * 512 + n], kT[:, kv, kt * 128:(kt + 1) * 128], qTs[ji % 2][:, h, 0:n])

    issue_s(0)
    ih = 0
    for i, (ji, h, kp) in enumerate(items):
        qd, od, q0, n, nkt = jobs[ji]
        kv = h * NKV // NH
        if h == 0 and kp == 0:
            load_q(ji + 1)
        if i + 1 < len(items):
            issue_s(i + 1)
        acc = B[4 + ih % 2]
        pt = pT2[i % 3]
        dbv = DB[i % 2].rr("p (a t) -> p a t", a=2)
        p.act(pt[:, :, 0:n], dbv[:, :, 0:n], AF.Exp, scale=scale)
        for a_ in range(2):
            kt = 2 * kp + a_
            p.mm(acc[0:dv + 1, 0:n], vp[:, kv, kt, :], pt[:, a_, 0:n], start=(kt == 0), stop=(kt == nkt - 1))
        if kp == nkt // 2 - 1:
            a = accs[ih % 2]
            p.cp(a[:, 0:n], acc[0:dv + 1, 0:n])
            p.mm(B[6][0:dv, 0:n], selden, a[:, 0:n])
            r = rec[ih % 2]
            p.op("dve", lambda e, r=r, n=n: e.reciprocal(r.ap[:, 0:n], B[6].ap[0:dv, 0:n]), [B[6]], [r])
            o = ob[ih % 2]
            p.tt(o[:, 0:n], a[0:dv, 0:n], r[:, 0:n], ALU.mult, eng="pool")
            p.dma(od[h, :, q0:q0 + n], o[:, 0:n])
            ih += 1
            yield ih


def attention_block(*a, **kw):
    for _ in attention_gen(*a, **kw):
        pass


def build_attn1(S):
    p = Prog()
    LK = CTX + S
    qT = p.dram("qT", [4, 64, S], BF16, "ExternalInput")
    kT = p.dram("kT", [1, 64, LK], BF16, "ExternalInput")
    v = p.dram("v", [1, LK, 64], BF16, "ExternalInput")
    oT = p.dram("oT", [4, 64, S], BF16, "ExternalOutput")
    c = Ctx(p, banks=False)
    c.set_banks(True)
    attention_block(p, c, qT, kT, v, oT, 4, 1, 64, 64, S, 64 ** -0.5)
    return p


RW_COLS = 1920


def build_pre0(S, dbg=0):
    TL = S // 4
    NTL = TL // 128
    NTM = NTL + 2
    NTA = NTL + 3
    TOKM = NTM * 128
    p = Prog()
    x_in = p.dram("x", [NTA * 128, D], F32, "ExternalInput")
    mods_fm = p.dram("mods_fm", [128, 48, 2], F32, "ExternalInput")
    n1g = p.dram("norm1_g", [D], F32, "ExternalInput")
    w_in = p.dram("w_in", [D, 2592], F32, "ExternalInput")
    mu_d = p.dram("mu", [RW_COLS], F32, "ExternalInput")
    flags_d = p.dram("flags", [128, 2], F32, "ExternalInput")
    g_qa = p.dram("g_qa", [1, 384], F32, "ExternalInput")
    g_kva = p.dram("g_kva", [1, 256], F32, "ExternalInput")
    w_q_up = p.dram("w_q_up", [384, 768], F32, "ExternalInput")
    w_kv_up = p.dram("w_kv_up", [256, 1024], F32, "ExternalInput")
    g_q = p.dram("g_q", [1, 96], F32, "ExternalInput")
    g_k = p.dram("g_k", [1, 96], F32, "ExternalInput")
    pos_in = p.dram("pos", [128, NTL, 2], F32, "ExternalInput")
    rwT = p.dram("rwT", [15, 128, TL + CTX], F32, "ExternalOutput")
    qT_o = p.dram("qT", [8, 96, TOKM], BF16, "ExternalOutput")
    kT_o = p.dram("kT", [8, 96, TOKM], BF16, "ExternalOutput")
    v_o = p.dram("v", [TOKM, 512], BF16, "ExternalOutput")
    c = Ctx(p)
    B = c.banks
    A, SH = mod_consts(p, c, mods_fm, n1g, 0, 1)
    pos = p.sb([128, NTL, 2], F32, "pos")
    p.dma(pos, pos_in)
    COS, SINS = rope_tables(p, c, pos, NTL, 8)
    hT_all = p.sb([128, 8, NTA * 128], BF16, "hT_all")

    with p.scope():
        gq_bc = p.sb([128, 8, 96], F32, "gq_bc")
        gk_bc = p.sb([128, 8, 96], F32, "gk_bc")
        g96 = p.sb([128, 96], F32, "g96")
        p.dma(g96, g_q.bc([128, 96]))
        p.cp(gq_bc, g96.un(1).bc([128, 8, 96]))
        g96b = p.sb([128, 96], F32, "g96b")
        p.dma(g96b, g_k.bc([128, 96]))
        p.cp(gk_bc, g96b.un(1).bc([128, 8, 96]))
        gqa_bc = p.sb([128, 384], F32, "gqa_bc")
        gkva_bc = p.sb([128, 256], F32, "gkva_bc")
        p.dma(gqa_bc, g_qa.bc([128, 384]))
        p.dma(gkva_bc, g_kva.bc([128, 256]))
        wim = p.sb([128, 8, 672], BF16, "wim")
        p.dma(wim, w_in[:, RW_COLS:2592].rr("(kc p) n -> p kc n", p=128), q="pool")
        wq = p.sb([128, 3, 768], BF16, "wq")
        p.dma(wq, w_q_up.rr("(kc p) n -> p kc n", p=128), q="pool")
        wkv = p.sb([128, 2, 1024], BF16, "wkv")
        p.dma(wkv, w_kv_up.rr("(kc p) n -> p kc n", p=128), q="pool")
        xt = [p.sb([128, D], F32, f"xt{i}") for i in range(2)]
        xnb = [p.sb([128, D], BF16, f"xnb{i}") for i in range(2)]
        junk = p.sb([128, D], BF16, "junk")
        ssq = [p.sb([128, 1], F32, f"ssq{i}") for i in range(2)]
        ssa = [p.sb([128, 2], F32, f"ssa{i}") for i in range(2)]
        anb = [p.sb([128, 640], BF16, f"anb{i}") for i in range(2)]
        anT = [p.sb([128, 5, 128], BF16, f"anT{i}") for i in range(2)]
        krs = [p.sb([128, 32], F32, f"krs{i}") for i in range(2)]
        Ksb = [p.sb([128, 8, 96], F32, f"Ksb{i}") for i in range(2)]
        vb = [p.sb([128, 8, 64], BF16, f"vb{i}") for i in range(2)]
        scr = dict(sq=p.sb([128, 768], F32, "sq"), qk=p.sb([128, 768], F32, "qk"), ss=p.sb([128, 8], F32, "ss"),
                   t1=p.sb([128, 768], F32, "t1"), t2=p.sb([128, 768], F32, "t2"))
        qb_ = [p.sb([128, 768], BF16, f"qb{i}") for i in range(2)]
        kb_ = [p.sb([128, 768], BF16, f"kb{i}") for i in range(2)]
        qTs = [p.sb([96, 8, 128], BF16, f"qTs{i}") for i in range(2)]
        kTs = [p.sb([96, 8, 128], BF16, f"kTs{i}") for i in range(2)]
        for t in range(NTA):
            i2 = t % 2
            var = 1 if (NTL <= t < NTL + 2) else 0
            p.dma(xt[i2], x_in[t * 128:(t + 1) * 128, :])
            hT = hT_all[:, :, t * 128:(t + 1) * 128]
            norm1_hT(p, c, xt[i2], A, SH, var, hT, xnb[i2], junk, ssq[i2], (B[0], B[1]))
            if t >= NTM or dbg == 1:
                continue
            for kc in range(8):
                p.mm(B[2][:, 0:384], hT[:, kc, :], wim[:, kc, 0:384], start=(kc == 0), stop=(kc == 7))
            for kc in range(8):
                p.mm(B[3][:, 0:288], hT[:, kc, :], wim[:, kc, 384:672], start=(kc == 0), stop=(kc == 7))
            sa = ssa[i2]
            p.act(junk[:, 0:384], B[2][:, 0:384], AF.Square, accum=sa[:, 0:1])
            p.act(junk[:, 384:640], B[3][:, 0:256], AF.Square, accum=sa[:, 1:2])
            p.ts(sa[:, 0:1], sa[:, 0:1], 256.0 / 384.0, ALU.mult)
            c.rstd(sa, 2, 1.0 / 256.0, NORM_EPS)
            p.stt(anb[i2][:, 0:384], B[2][:, 0:384], sa[:, 0:1], gqa_bc, ALU.mult, ALU.mult)
            p.stt(anb[i2][:, 384:640], B[3][:, 0:256], sa[:, 1:2], gkva_bc, ALU.mult, ALU.mult)
            p.cp(krs[i2], B[3][:, 256:288], eng="act")
            if dbg == 3:
                continue
            b4 = B[4].bitcast(BF16)
            for k5 in range(5):
                p.tr(b4[:, k5 * 128:(k5 + 1) * 128], anb[i2][:, k5 * 128:(k5 + 1) * 128], c.identb)
            p.cp(anT[i2].rr("p a t -> p (a t)"), b4[:, 0:640], eng="act")
            if dbg == 4:
                continue
            for (bk, c0, c1) in ((B[5], 0, 480), (B[6], 480, 768)):
                for kc in range(3):
                    p.mm(bk[:, 0:c1 - c0], anT[i2][:, kc, :], wq[:, kc, c0:c1], start=(kc == 0), stop=(kc == 2))
            if dbg == 51:
                continue
            for (bk, c0) in ((B[7], 0), (B[2], 512)):
                for kc in range(2):
                    p.mm(bk, anT[i2][:, 3 + kc, :], wkv[:, kc, c0:c0 + 512], start=(kc == 0), stop=(kc == 1))
            if dbg == 52:
                continue
            K = Ksb[i2]
            for (bk, h0) in ((B[7], 0), (B[2], 4)):
                kvv = bk.rr("p (h d) -> p h d", d=128)
                p.cp(K[:, h0:h0 + 4, 0:64], kvv[:, :, 0:64])
                if dbg != 531:
                    p.cp(vb[i2][:, h0:h0 + 4, :], kvv[:, :, 64:128], eng=("dve" if dbg == 532 else "act"))
            if dbg != 533:
                p.cp(K[:, :, 64:96], krs[i2].un(1).bc([128, 8, 32]), eng="pool")
            if dbg in (53, 531, 532, 533):
                continue
            p.dma(v_o[t * 128:(t + 1) * 128, :], vb[i2].rr("p h d -> p (h d)"))
            if dbg == 5:
                continue
            rope = None if var == 1 else (64, 8, COS[:, t], SINS[:, t])
            qk_post(p, c, [(B[5][:, 0:480], 5), (B[6][:, 0:288], 3)], 8, 96, gq_bc.rr("p h d -> p (h d)"),
                    qb_[i2], scr, rope)
            if dbg == 6:
                continue
            qk_post(p, c, [(K.rr("p h d -> p (h d)"), 8)], 8, 96, gk_bc.rr("p h d -> p (h d)"), kb_[i2], scr, rope)
            if dbg == 7:
                continue
            for (src, dstT, bk, out_d, eng) in ((qb_[i2], qTs[i2], B[0], qT_o, "act"), (kb_[i2], kTs[i2], B[1], kT_o, "dve")):
                bb = bk.bitcast(BF16)
                for h in range(8):
                    p.tr(bb[0:96, h * 128:(h + 1) * 128], src[:, h * 96:(h + 1) * 96], c.identb)
                p.cp(dstT.rr("p a t -> p (a t)"), bb[0:96, :], eng=eng)
                p.dma(out_d[:, :, t * 128:(t + 1) * 128].rr("a p t -> p a t"), dstT)

    with p.scope():
        mufm = c.load_fm(mu_d.rr("(c p) -> c p", p=128), 15)
        om = p.sb([128, 15], F32, "om")
        hm = p.sb([128, 15], F32, "hm")
        p.ts(om, mufm, -1.0, ALU.mult, 1.0, ALU.add)
        p.ts(hm, mufm, 0.5, ALU.mult)
        flags = p.sb([128, 2], F32, "flags")
        p.dma(flags, flags_d)
        W = TL + CTX + 4
        E = [p.sb([128, W], F32, f"E{i}") for i in range(2)]
        for i in range(2):
            p.memset(E[i][:, TL + 2:TL + 3], 0.0)
            p.memset(E[i][:, W - 1:W], 0.0)
        hal = [p.sb([128, 2], F32, f"hal{i}") for i in range(2)]
        sm = [p.sb([128, TL + CTX], F32, f"sm{i}") for i in range(2)]
        tm = [p.sb([128, TL + CTX], F32, f"tm{i}") for i in range(2)]
        wc = [p.sb([128, 8, 128], BF16, f"wc{i}") for i in range(3)]
        blks = []
        t0 = 0
        while t0 < TL:
            n = min(512, TL - t0)
            blks.append((t0, n, 1 + t0))
            t0 += n
        blks.append((TL, CTX, TL + 3))
        ib = 0
        for cc in range(15 if dbg < 2 else 0):
            w = wc[cc % 3]
            e = E[cc % 2]
            p.dma(w, w_in[:, cc * 128:(cc + 1) * 128].rr("(kc p) n -> p kc n", p=128), q="pool")
            for (t0, n, e0) in blks:
                bk = B[ib % 6]
                ib += 1
                for kc in range(8):
                    p.mm(bk[:, 0:n], w[:, kc, :], hT_all[:, kc, t0:t0 + n], start=(kc == 0), stop=(kc == 7))
                p.cp(e[:, e0:e0 + n], bk[:, 0:n], eng=("act" if ib % 2 else "dve"))
            bk = B[6 + cc % 2]
            hoff = (NTL + 2) * 128
            for kc in range(8):
                p.mm(bk[:, 0:2], w[:, kc, :], hT_all[:, kc, hoff:hoff + 2], start=(kc == 0), stop=(kc == 7))
            p.tt(hal[cc % 2], bk[:, 0:2], flags, ALU.mult)
            p.cp(e[:, 0:1], hal[cc % 2][:, 0:1], eng="pool")
            p.cp(e[:, TL + 1:TL + 2], hal[cc % 2][:, 1:2], eng="pool")
            s_, t_ = sm[cc % 2], tm[cc % 2]
            for (o0, n, e0) in ((0, TL, 1), (TL, CTX, TL + 3)):
                p.tt(s_[:, o0:o0 + n], e[:, e0 - 1:e0 - 1 + n], e[:, e0 + 1:e0 + 1 + n], ALU.add, eng="pool")
                p.ts(t_[:, o0:o0 + n], e[:, e0:e0 + n], om[:, cc:cc + 1], ALU.mult)
                p.stt(s_[:, o0:o0 + n], s_[:, o0:o0 + n], hm[:, cc:cc + 1], t_[:, o0:o0 + n], ALU.mult, ALU.add)
            p.dma(rwT[cc], s_)
    return p


LN_X_EPS = 64e-5
LAM = math.exp(-0.5)


def build_mix0(S, do_attn=True, RWP=BF16):
    LT = CTX + S
    NPAIR = LT // 128
    p = Prog()
    rw_r = p.dram("rw_r", [128, LT], F32, "ExternalInput")
    rw_k = p.dram("rw_k", [128, LT], F32, "ExternalInput")
    rw_v = p.dram("rw_v", [128, LT], F32, "ExternalInput")
    lo_w = p.dram("lo_w", [128, LT], F32, "ExternalInput")
    lo_a = p.dram("lo_a", [128, LT], F32, "ExternalInput")
    lo_g = p.dram("lo_g", [128, LT], F32, "ExternalInput")
    w2_d = p.dram("w2", [128, 128], F32, "ExternalInput")
    a2_d = p.dram("a2", [128, 128], F32, "ExternalInput")
    g2_d = p.dram("g2", [128, 128], F32, "ExternalInput")
    pv_d = p.dram("pv", [9, 128], F32, "ExternalInput")
    yT = p.dram("yT", [2, 128, LT], F32, "ExternalOutput")
    rwoT = p.dram("rwoT", [128, LT], BF16, "ExternalOutput")
    if do_attn:
        qT = p.dram("qT", [2, 96, S], BF16, "ExternalInput")
        qcT = p.dram("qcT", [2, 96, CTX], BF16, "ExternalInput")
        kT = p.dram("kT", [2, 96, LT], BF16, "ExternalInput")
        v_d = p.dram("v", [2, LT, 64], BF16, "ExternalInput")
        oT = p.dram("oT", [2, 64, S], BF16, "ExternalOutput")
        ocT = p.dram("ocT", [2, 64, CTX], BF16, "ExternalOutput")
    c = Ctx(p, banks=False)
    _outer = p.scope()
    _outer.__enter__()
    c.set_banks(False)
    B = c.banks
    pv = c.load_fm(pv_d, 9)
    W0 = [pv[:, 0:1], pv[:, 1:2]]
    A0 = [pv[:, 2:3], pv[:, 3:4]]
    KK_, KA_, RK_, LNW, LNB = pv[:, 4:5], pv[:, 5:6], pv[:, 6:7], pv[:, 7:8], pv[:, 8:9]
    omka = p.sb([128, 2], F32, "omka")
    p.ts(omka[:, 0:1], KA_, -1.0, ALU.mult, 1.0, ALU.add)
    p.ts(omka[:, 1:2], KA_, -2.0, ALU.mult, 2.0, ALU.add)
    w2s = p.sb([128, 128], F32, "w2s")
    a2s = p.sb([128, 128], F32, "a2s")
    g2s = p.sb([128, 128], F32, "g2s")
    p.dma(w2s, w2_d)
    p.dma(a2s, a2_d)
    p.dma(g2s, g2_d)
    w2z, a2z = [], []
    for d in range(2):
        for (src_, lst_) in ((w2s, w2z), (a2s, a2z)):
            z = p.sb([128, 128], F32, "loraz")
            p.memset(z, 0.0)
            dp_ = slice(64 * d, 64 * d + 64)
            p.cp(z[dp_, :], src_[dp_, :], eng="pool")
            lst_.append(z)
    p64 = p.sb([128, 1], F32, "p64")
    p.ts(p64, c.pidx, 63.5, ALU.is_gt)
    c64 = p.sb([128, 128], F32, "c64")
    p.ts(c64, c.iof, 63.5, ALU.is_gt)
    same = p.sb([128, 128], F32, "same")
    p.ts(same, c64, p64[:, 0:1], ALU.is_equal)
    masks = {}
    for nm, op_ in (("SU", ALU.is_gt), ("SL", ALU.is_lt), ("IU", ALU.is_ge), ("IL", ALU.is_le)):
        m = p.sb([128, 2, 128], F32, "mask" + nm)
        p.ts(m[:, 0, :], c.iof, c.pidx[:, 0:1], op_)
        p.tt(m[:, 0, :], m[:, 0, :], same, ALU.mult)
        p.cp(m[:, 1, :], m[:, 0, :])
        masks[nm] = m.rr("p a t -> p (a t)")
    ones64 = p.sb([128, 128], F32, "ones64")
    p.ts(ones64, same, 1.0 / 64.0, ALU.mult)
    ident2 = p.sb([128, 2, 128], F32, "ident2")
    p.cp(ident2[:, 0, :], c.identf)
    p.cp(ident2[:, 1, :], c.identf)
    ident2 = ident2.rr("p a t -> p (a t)")
    rmask = p.sb([128, 512], F32, "rmask")
    p.memset(rmask, 1.0)
    p.memset(rmask.rr("p (a t) -> p a t", t=64)[:, :, 0:1], 0.0)

    blocks = [(0, CTX)]
    t0 = CTX
    while t0 < LT:
        blocks.append((t0, 512))
        t0 += 512
    NB = len(blocks)
    order = [list(range(NB)), [0] + list(range(NB - 1, 0, -1))]

    with p.scope():
        def T(shape, nm, dt=F32):
            return p.sb(shape, dt, nm)
        ops_ = []
        for d in range(2):
            two = []
            for i in range(2):
                two.append(dict(Rt=T([128, 512], "Rt"), Rt16=T([128, 512], "Rt16", RWP), Kt=T([128, 512], "Kt", RWP),
                                Bt=T([128, 512], "Bt", RWP), At=T([128, 512], "At", RWP),
                                A_tm=T([128, 4, 128], "A_tm", RWP), K_tm=T([128, 4, 128], "K_tm", RWP), B_tm=T([128, 4, 128], "B_tm", RWP),
                                V_tm=T([128, 4, 128], "V_tm", RWP), etot=T([128, 8], "etot"), yb=T([128, 512], "yb")))
            ops_.append(two)
        tmp = {k: T([128, 512], k) for k in ("r", "k", "v", "lw", "la", "th", "sg", "a", "kk", "t1", "t2", "c", "E", "kd", "b", "Kh", "Bh")}
        tot = T([128, 8], "tot")
        Hbd = [[T([128, 128], f"H{d}{i}") for i in range(2)] for d in range(2)]
        H16 = [[T([128, 128], f"H16{d}{i}", RWP) for i in range(2)] for d in range(2)]
        for d in range(2):
            for i in range(2):
                p.memset(Hbd[d][i], 0.0)
                p.memset(H16[d][i], 0.0)
        hcur = [0, 0]
        pairbuf = []
        for d in range(2):
            pairbuf.append({k: T([128, 256], f"{k}{d}", RWP) for k in ("X", "XT", "X2", "XT2", "P", "P2", "MAK")}
                           | {k: T([128, 256], f"{k}{d}", RWP) for k in ("MRK", "MRB", "WT")}
                           | {"AT": T([128, 128], f"AT{d}", RWP), "U": T([128, 128], f"U{d}", RWP)})
            p.memset(pairbuf[d]["U"], 0.0)

        def prep(d, bi, ob):
            t0, n = blocks[bi]
            tok = slice(t0, t0 + n)
            nch = n // 64
            dp = slice(64 * d, 64 * d + 64)
            x = tmp
            p.dma(x["r"][:, 0:n], rw_r[:, tok])
            p.dma(x["k"][:, 0:n], rw_k[:, tok])
            p.dma(x["v"][:, 0:n], rw_v[:, tok])
            p.dma(x["lw"][:, 0:n], lo_w[:, tok])
            p.dma(x["la"][:, 0:n], lo_a[:, tok])
            N = slice(0, n)
            p.act(x["th"][:, N], x["lw"][:, N], AF.Tanh)
            p.mm(B[6][:, N], w2z[d], x["th"][:, N])
            p.act(x["sg"][:, N], B[6][:, N], AF.Sigmoid, bias=W0[d])
            p.mm(B[4][:, N], a2z[d], x["la"][:, N])
            p.act(x["a"][:, N], B[4][:, N], AF.Sigmoid, bias=A0[d])
            p.ts(x["kk"][:, N], x["k"][:, N], KK_, ALU.mult)
            p.tt(x["t1"][:, N], x["kk"][:, N], x["kk"][:, N], ALU.mult, eng="pool")
            p.ts(x["t1"][:, N], x["t1"][:, N], 64.0, ALU.mult, eng="pool")
            p.mm(B[6][:, N], ones64, x["t1"][:, N])
            p.ts(x["t2"][:, N], B[6][:, N], 1e-12, ALU.add)
            p.act(x["t2"][:, N], x["t2"][:, N], AF.Ln)
            p.act(x["t2"][:, N], x["t2"][:, N], AF.Exp, scale=-0.5)
            p.tt(x["kk"][:, N], x["kk"][:, N], x["t2"][:, N], ALU.mult)
            p.ts(x["t1"][:, N], x["a"][:, N], KA_, ALU.mult, omka[:, 0:1], ALU.add)
            p.tt(x["kd"][:, N], x["k"][:, N], x["t1"][:, N], ALU.mult, eng="pool")
            p.tt(x["b"][:, N], x["kk"][:, N], x["a"][:, N], ALU.mult, eng="pool")
            p.op("dve", lambda e: e.tensor_tensor_scan(x["c"].ap[:, N], rmask.ap[:, N], x["sg"].ap[:, N], 0.0,
                                                       ALU.mult, ALU.add), [rmask, x["sg"]], [x["c"]])
            cv = x["c"][:, N].rr("p (a t) -> p a t", t=64)
            p.cp(tot[:, 0:nch], cv[:, :, 63])
            if d == 1:
                p.tt(x["c"][:, N], x["sg"][:, N], x["c"][:, N], ALU.subtract)
                p.tt(cv, cv, tot[:, 0:nch].un(2).bc([128, nch, 64]), ALU.add)
            p.act(ob["etot"][:, 0:nch], tot[:, 0:nch], AF.Exp, scale=-LAM)
            p.act(x["E"][:, N], x["c"][:, N], AF.Exp, scale=-LAM)
            p.tt(ob["Rt"][:, N], x["r"][:, N], x["E"][:, N], ALU.mult)
            p.cp(ob["Rt16"][:, N], ob["Rt"][:, N], eng="pool")
            p.act(x["E"][:, N], x["c"][:, N], AF.Exp, scale=LAM)
            p.tt(ob["Kt"][:, N], x["kd"][:, N], x["E"][:, N], ALU.mult)
            p.tt(ob["Bt"][:, N], x["b"][:, N], x["E"][:, N], ALU.mult, eng="pool")
            p.tt(x["t1"][:, N], x["c"][:, N], x["sg"][:, N], ALU.subtract)
            p.act(x["E"][:, N], x["t1"][:, N], AF.Exp, scale=-LAM)
            p.stt(ob["At"][:, N], x["kk"][:, N], -1.0, x["E"][:, N], ALU.mult, ALU.mult)
            p.tt(x["t2"][:, N].rr("p (a t) -> p a t", t=64), tot[:, 0:nch].un(2).bc([128, nch, 64]), cv, ALU.subtract)
            p.act(x["E"][:, N], x["t2"][:, N], AF.Exp, scale=-LAM)
            p.tt(x["Kh"][:, N], x["kd"][:, N], x["E"][:, N], ALU.mult)
            p.tt(x["Bh"][:, N], x["b"][:, N], x["E"][:, N], ALU.mult, eng="pool")
            npair = n // 128
            for (src, dst, bk, eng) in ((ob["At"], ob["A_tm"], B[4], "act"), (x["Kh"], ob["K_tm"], B[6], "dve"),
                                        (x["Bh"], ob["B_tm"], B[4], "act"), (x["v"], ob["V_tm"], B[6], "dve")):
                is16 = (src is ob["At"]) and RWP == BF16
                bkv = bk.bitcast(BF16) if is16 else bk
                for pr in range(npair):
                    p.tr(bkv[:, pr * 128:(pr + 1) * 128], src[:, pr * 128:(pr + 1) * 128], c.identb if is16 else c.identf)
                p.cp(dst.rr("p a t -> p (a t)")[:, 0:n], bkv[:, 0:n], eng=eng)

        def pair_level(d, ob, pr):
            pb = pairbuf[d]
            Tk = slice(pr * 128, (pr + 1) * 128)
            mN, mM, mI = (("SU", "SL", "IU") if d == 0 else ("SL", "SU", "IL"))
            bk0, bk1 = B[2], B[3]
            HB = ((B[0], B[2]), (B[1], B[3]))

            def prod(slot, lhs, rhs):
                for h in range(2):
                    hp = slice(64 * h, 64 * h + 64)
                    p.mm(HB[h][slot][:, 0:128], ob[lhs][hp, Tk], ob[rhs][hp, Tk])

            def evac(slot, dst, mk):
                for h in range(2):
                    hc = slice(128 * h, 128 * h + 128)
                    p.tt(pb[dst][:, hc], HB[h][slot][:, 0:128], masks[mk][:, 0:128], ALU.mult)

            prod(0, "Bt", "At")
            prod(1, "At", "Bt")
            evac(0, "X", mN)
            evac(1, "XT", mM)
            prod(0, "At", "Kt")
            prod(1, "Kt", "Rt16")
            evac(0, "MAK", mM)
            evac(1, "MRK", mI)
            prod(0, "Bt", "Rt16")
            evac(0, "MRB", mI)
            p.tt(pb["P"], pb["X"], ident2, ALU.add, eng="pool")
            X, XT, Pc = pb["X"], pb["XT"], pb["P"]
            X2, XT2, P2 = pb["X2"], pb["XT2"], pb["P2"]
            for k in range(1, 6):
                for h in range(2):
                    hc = slice(128 * h, 128 * h + 128)
                    p.mm(bk1[:, hc], X[:, hc], XT[:, hc])
                    if k < 5:
                        p.mm(bk0[:, hc], XT[:, hc], X[:, hc])
                p.cp(XT2, bk1[:, 0:256], eng="act")
                if k < 5:
                    p.cp(X2, bk0[:, 0:256])
                for h in range(2):
                    hc = slice(128 * h, 128 * h + 128)
                    p.mm(bk0[:, hc], XT2[:, hc], Pc[:, hc])
                p.tt(P2, bk0[:, 0:256], Pc, ALU.add)
                X, X2 = X2, X
                XT, XT2 = XT2, XT
                Pc, P2 = P2, Pc
            for h in range(2):
                hc = slice(128 * h, 128 * h + 128)
                p.mm(bk1[:, hc], ob["A_tm"][:, pr, :], Pc[:, hc])
                p.mm(bk0[:, hc], pb["MAK"][:, hc], Pc[:, hc])
            p.cp(pb["AT"][0:64, :], bk1[0:64, 0:128], eng="act")
            p.cp(pb["AT"][64:128, :], bk1[64:128, 128:256], eng="act")
            p.cp(pb["WT"], bk0[:, 0:256])

        def chunk_level(d, ob, pr, pi):
            pb = pairbuf[d]
            cp_ = slice(64 * pi, 64 * pi + 64)
            ch = pr * 2 + pi
            tcol = slice(pr * 128 + 64 * pi, pr * 128 + 64 * pi + 64)
            Hold = Hbd[d][hcur[d]]
            Hnew = Hbd[d][1 - hcur[d]]
            Hold16 = H16[d][hcur[d]]
            Hnew16 = H16[d][1 - hcur[d]]
            hcur[d] = 1 - hcur[d]
            bu, bh, by = B[4], (B[5] if d == 0 else B[7]), B[6]
            for h in range(2):
                hc = slice(128 * h, 128 * h + 128)
                ic = slice(64 * h, 64 * h + 64)
                p.mm(bu[:, ic], pb["WT"][:, hc], ob["V_tm"][:, pr, ic], start=True, stop=False)
                p.mm(bu[:, ic], pb["AT"], Hold16[:, ic], start=False, stop=True)
            p.cp(pb["U"][cp_, :], bu[cp_, 0:128], eng="act")
            for h in range(2):
                ic = slice(64 * h, 64 * h + 64)
                p.mm(bh[:, ic], ob["K_tm"][cp_, pr, :], ob["V_tm"][cp_, pr, ic], start=True, stop=False)
                p.mm(bh[:, ic], ob["B_tm"][cp_, pr, :], pb["U"][cp_, ic], start=False, stop=True)
            for h in range(2):
                hp = slice(64 * h, 64 * h + 64)
                ic = slice(64 * h, 64 * h + 64)
                p.stt(Hnew[hp, ic], Hold[hp, ic], ob["etot"][hp, ch:ch + 1], bh[hp, ic], ALU.mult, ALU.add)
            p.cp(Hnew16, Hnew, eng="pool")
            for h in range(2):
                ys = slice(64 * h, 64 * h + 64)
                mcol = slice(128 * h + 64 * pi, 128 * h + 64 * pi + 64)
                p.mm(by[:, ys], Hold16, ob["Rt16"][:, tcol], start=True, stop=False)
                p.mm(by[:, ys], ob["V_tm"][:, pr, :], pb["MRK"][:, mcol], start=False, stop=False)
                p.mm(by[:, ys], pb["U"], pb["MRB"][:, mcol], start=False, stop=True)
            p.cp(ob["yb"][0:64, tcol], by[0:64, 0:64], eng="act")
            p.cp(ob["yb"][64:128, tcol], by[64:128, 64:128], eng="act")

        for step in range(NB):
            for d in range(2):
                bi = order[d][step]
                t0, n = blocks[bi]
                ob = ops_[d][step % 2]
                prep(d, bi, ob)
                npair = n // 128
                prs = range(npair) if d == 0 else range(npair - 1, -1, -1)
                for pr in prs:
                    pair_level(d, ob, pr)
                    for pi in ((0, 1) if d == 0 else (1, 0)):
                        chunk_level(d, ob, pr, pi)
                p.dma(yT[d][:, t0:t0 + n], ob["yb"][:, 0:n])

    with p.scope():
        x = {k: p.sb([128, 512], F32, k) for k in ("y0", "y1", "r", "k", "v", "la", "lg", "a0", "a1", "t1", "t2", "t3", "g")}
        ob_ = [p.sb([128, 512], BF16, f"rwo{i}") for i in range(2)]
        for bi, (t0, n) in enumerate(blocks):
            tok = slice(t0, t0 + n)
            N = slice(0, n)
            p.dma(x["y0"][:, N], yT[0][:, tok])
            p.dma(x["y1"][:, N], yT[1][:, tok])
            p.dma(x["r"][:, N], rw_r[:, tok])
            p.dma(x["k"][:, N], rw_k[:, tok])
            p.dma(x["v"][:, N], rw_v[:, tok])
            p.dma(x["la"][:, N], lo_a[:, tok])
            p.dma(x["lg"][:, N], lo_g[:, tok])
            p.tt(x["y0"][:, N], x["y0"][:, N], x["y1"][:, N], ALU.add)
            p.mm(B[0][:, N], ones64, x["y0"][:, N])
            p.tt(x["y0"][:, N], x["y0"][:, N], B[0][:, N], ALU.subtract)
            p.tt(x["t1"][:, N], x["y0"][:, N], x["y0"][:, N], ALU.mult, eng="pool")
            p.mm(B[1][:, N], ones64, x["t1"][:, N])
            p.ts(x["t1"][:, N], B[1][:, N], LN_X_EPS, ALU.add)
            p.act(x["t1"][:, N], x["t1"][:, N], AF.Ln)
            p.act(x["t1"][:, N], x["t1"][:, N], AF.Exp, scale=-0.5)
            p.tt(x["y0"][:, N], x["y0"][:, N], x["t1"][:, N], ALU.mult)
            p.ts(x["y0"][:, N], x["y0"][:, N], LNW, ALU.mult, LNB, ALU.add)
            for d in range(2):
                p.mm(B[2 + d][:, N], a2z[d], x["la"][:, N])
                p.act(x["a%d" % d][:, N], B[2 + d][:, N], AF.Sigmoid, bias=A0[d])
            p.tt(x["a0"][:, N], x["a0"][:, N], x["a1"][:, N], ALU.add, eng="pool")
            p.ts(x["a0"][:, N], x["a0"][:, N], KA_, ALU.mult, omka[:, 1:2], ALU.add)
            p.tt(x["t2"][:, N], x["k"][:, N], x["a0"][:, N], ALU.mult, eng="pool")
            p.stt(x["t2"][:, N], x["t2"][:, N], RK_, x["r"][:, N], ALU.mult, ALU.mult)
            p.ts(x["t2"][:, N], x["t2"][:, N], 64.0, ALU.mult, eng="pool")
            p.mm(B[4][:, N], ones64, x["t2"][:, N])
            p.tt(x["t3"][:, N], B[4][:, N], x["v"][:, N], ALU.mult)
            p.tt(x["y0"][:, N], x["y0"][:, N], x["t3"][:, N], ALU.add, eng="pool")
            p.act(x["lg"][:, N], x["lg"][:, N], AF.Sigmoid)
            p.mm(B[5][:, N], g2s, x["lg"][:, N])
            o = ob_[bi % 2]
            p.tt(o[:, N], x["y0"][:, N], B[5][:, N], ALU.mult)
            p.dma(rwoT[:, tok], o[:, N])

    _outer.__exit__(None, None, None)
    if do_attn:
        with p.scope():
            c.set_banks(True)
            attention_block(p, c, qT, kT, v_d, oT, 2, 2, 96, 64, S, 96 ** -0.5, qcT_d=qcT, ocT_d=ocT)
    return p


def _make_pos(q, TL):
    NTL = TL // 128
    t = q * TL + np.arange(TL)
    pos = np.stack([t // 64, t % 64], -1).astype(np.float32)
    return np.ascontiguousarray(pos.reshape(NTL, 128, 2).transpose(1, 0, 2))


def _cat(xs, axis):
    return np.ascontiguousarray(np.concatenate(xs, axis=axis))


def kernel(x, c, ctx, c_ctx, ada_w, ada_b, norm1_g, norm2_g, ab_w_in, ab_w_out, rw_mu, rw_w0,
           rw_w2, rw_a0, rw_a2, rw_k_k, rw_k_a, rw_r_k, rw_g2, rw_ln_w, rw_ln_b, mla_g_qa,
           mla_w_q_up, mla_g_kva, mla_w_kv_up, mla_g_q, mla_g_k, gqa_w_in, gqa_w_out, gqa_g_q,
           gqa_g_k, router_w, router_b, moe_w_gate, moe_w_up, moe_w_down, shared_w_gate,
           shared_w_up, shared_w_down):
    f = lambda a: np.ascontiguousarray(np.asarray(a, dtype=np.float32))
    x, c, ctx, c_ctx = f(x), f(c), f(ctx), f(c_ctx)
    S = x.shape[1]
    TL = S // 4
    LT = CTX + S
    R8 = range(8)
    maps = [{"cvec": np.stack([c[r // 4], c_ctx]), "ada_w": f(ada_w), "ada_b": f(ada_b)} for r in R8]
    r0 = run_prog(build_phase0(), maps)
    mods_tm = [f(r0[r]["mods_tm"]) for r in R8]
    mods_fm = [f(r0[r]["mods_fm"]) for r in R8]
    maps = []
    for r in R8:
        b, q = r // 4, r % 4
        halo = np.zeros((128, D), np.float32)
        fl = np.zeros((128, 2), np.float32)
        if q > 0:
            halo[0] = x[b, q * TL - 1]
            fl[:, 0] = 1
        if q < 3:
            halo[1] = x[b, (q + 1) * TL]
            fl[:, 1] = 1
        maps.append({"x": _cat([x[b, q * TL:(q + 1) * TL], ctx[b], halo], 0), "mods_fm": mods_fm[r][0],
                     "norm1_g": f(norm1_g)[0], "w_in": f(ab_w_in)[0], "mu": f(rw_mu)[0], "flags": fl,
                     "g_qa": f(mla_g_qa), "g_kva": f(mla_g_kva), "w_q_up": f(mla_w_q_up)[0],
                     "w_kv_up": f(mla_w_kv_up)[0], "g_q": f(mla_g_q), "g_k": f(mla_g_k), "pos": _make_pos(q, TL)})
    rA = run_prog(build_pre0(S), maps)
    maps = []
    rw_r_k_flat = f(rw_r_k)[0].reshape(512)
    for r in R8:
        b, j = r // 4, r % 4
        cores = [4 * b + qq for qq in range(4)]
        rwT = _cat([rA[4 * b]["rwT"][:, :, TL:]] + [rA[cc]["rwT"][:, :, :TL] for cc in cores], 2)
        cs = slice(128 * j, 128 * j + 128)
        pv = np.stack([f(rw_w0)[0, 0, cs], f(rw_w0)[0, 1, cs], f(rw_a0)[0, 0, cs], f(rw_a0)[0, 1, cs],
                       f(rw_k_k)[0, cs], f(rw_k_a)[0, cs], rw_r_k_flat[cs], f(rw_ln_w)[0, cs], f(rw_ln_b)[0, cs]])
        hs = slice(2 * j, 2 * j + 2)
        qT = _cat([rA[cc]["qT"][hs, :, :TL] for cc in cores], 2)
        qcT = np.ascontiguousarray(rA[4 * b]["qT"][hs, :, TL:])
        kT = _cat([rA[4 * b]["kT"][hs, :, TL:]] + [rA[cc]["kT"][hs, :, :TL] for cc in cores], 2)
        vv = _cat([rA[4 * b]["v"][TL:]] + [rA[cc]["v"][:TL] for cc in cores], 0)
        vv = np.ascontiguousarray(vv.reshape(LT, 8, 64)[:, hs].transpose(1, 0, 2))
        maps.append({"rw_r": np.ascontiguousarray(rwT[j]), "rw_k": np.ascontiguousarray(rwT[4 + j]),
                     "rw_v": np.ascontiguousarray(rwT[8 + j]), "lo_w": np.ascontiguousarray(rwT[12]),
                     "lo_a": np.ascontiguousarray(rwT[13]), "lo_g": np.ascontiguousarray(rwT[14]),
                     "w2": np.ascontiguousarray(f(rw_w2)[0][:, :, cs].reshape(128, 128)),
                     "a2": np.ascontiguousarray(f(rw_a2)[0][:, :, cs].reshape(128, 128)),
                     "g2": np.ascontiguousarray(f(rw_g2)[0][:, cs]), "pv": np.ascontiguousarray(pv),
                     "qT": qT, "qcT": qcT, "kT": kT, "v": vv})
    rB = run_prog(build_mix0(S), maps)
    del rA
    wg0 = _cat([f(moe_w_gate)[0], f(shared_w_gate)[0][None]], 0)
    wu0 = _cat([f(moe_w_up)[0], f(shared_w_up)[0][None]], 0)
    wd0 = _cat([f(moe_w_down)[0], f(shared_w_down)[0][None]], 0)
    maps = []
    for r in R8:
        b, q = r // 4, r % 4
        chunks = []
        for kc in range(4):
            rw = rB[4 * b + kc]["rwoT"]
            chunks.append(_cat([rw[:, CTX + q * TL:CTX + (q + 1) * TL], rw[:, :CTX]], 1))
        for j in range(4):
            o = rB[4 * b + j]["oT"].reshape(128, S)
            oc = rB[4 * b + j]["ocT"].reshape(128, CTX)
            chunks.append(_cat([o[:, q * TL:(q + 1) * TL], oc], 1))
        maps.append({"x": _cat([x[b, q * TL:(q + 1) * TL], ctx[b]], 0), "attT": np.ascontiguousarray(np.stack(chunks, 0)),
                     "mods_tm": mods_tm[r][0], "mods_fm": mods_fm[r][0], "w_out": f(ab_w_out)[0],
                     "norm2_g": f(norm2_g)[0], "router_w": f(router_w), "router_b": f(router_b)[None, :],
                     "wg_all": wg0, "wu_all": wu0, "wd_all": wd0})
    rC = run_prog(build_post(S, True), maps)
    del rB
    x1 = [f(rC[r]["x_out"]) for r in R8]
    maps = []
    for r in R8:
        q = r % 4
        maps.append({"x": x1[r], "mods_fm": mods_fm[r][1], "norm1_g": f(norm1_g)[1], "w_in": f(gqa_w_in)[0],
                     "g_q": f(gqa_g_q), "g_k": f(gqa_g_k), "pos": _make_pos(q, TL)})
    rD = run_prog(build_pre1(S), maps)
    maps = []
    for r in R8:
        b, kvh = r // 4, r % 4
        cores = [4 * b + qq for qq in range(4)]
        qk = _cat([rD[cc]["qkT"][:, :, :TL] for cc in cores], 2).reshape(20, 64, S)
        kc_ = rD[4 * b]["qkT"][:, :, TL:].reshape(20, 64, CTX)
        qT = np.ascontiguousarray(qk[4 * kvh:4 * kvh + 4])
        kT = _cat([kc_[16 + kvh], qk[16 + kvh]], 1)[None]
        vv = _cat([rD[4 * b]["v"][TL:]] + [rD[cc]["v"][:TL] for cc in cores], 0)
        vv = np.ascontiguousarray(vv[:, kvh * 64:(kvh + 1) * 64])[None]
        maps.append({"qT": qT, "kT": np.ascontiguousarray(kT), "v": vv})
    rE = run_prog(build_attn1(S), maps)
    del rD
    wg1 = _cat([f(moe_w_gate)[1], f(shared_w_gate)[1][None]], 0)
    wu1 = _cat([f(moe_w_up)[1], f(shared_w_up)[1][None]], 0)
    wd1 = _cat([f(moe_w_down)[1], f(shared_w_down)[1][None]], 0)
    maps = []
    for r in R8:
        b, q = r // 4, r % 4
        chunks = []
        for kc in range(8):
            o = rE[4 * b + kc // 2]["oT"]
            i0 = (kc % 2) * 2
            chunks.append(o[i0:i0 + 2].reshape(128, S)[:, q * TL:(q + 1) * TL])
        maps.append({"x": np.ascontiguousarray(x1[r][:TL]), "attT": np.ascontiguousarray(np.stack(chunks, 0)),
                     "mods_tm": mods_tm[r][1], "mods_fm": mods_fm[r][1], "w_out": f(gqa_w_out)[0],
                     "norm2_g": f(norm2_g)[1], "router_w": f(router_w), "router_b": f(router_b)[None, :],
                     "wg_all": wg1, "wu_all": wu1, "wd_all": wd1})
    rF = run_prog(build_post(S, False), maps)
    out = np.zeros((2, S, D), np.float32)
    for r in R8:
        b, q = r // 4, r % 4
        out[b, q * TL:(q + 1) * TL] = rF[r]["x_out"]
    return out
```

```python
import numpy as np
import concourse.bass as bass
import concourse.mybir as mybir
from concourse.bass_utils import run_bass_kernel_spmd

F32 = mybir.dt.float32
BF16 = mybir.dt.bfloat16
I32 = mybir.dt.int32
AF = mybir.ActivationFunctionType
ALU = mybir.AluOpType
AX = mybir.AxisListType

EPOCH = 30000
SCHED_WINDOW = 40
NSLOT = {"sp": 24, "pool": 12, "act": 8}


class Buf:
    __slots__ = ("name", "lw", "rd", "excl")

    def __init__(self, name, excl=False):
        self.name = name
        self.lw = None
        self.rd = []
        self.excl = excl


class V:
    __slots__ = ("buf", "ap")

    def __init__(self, buf, ap):
        self.buf = buf
        self.ap = ap

    def __getitem__(self, idx):
        return V(self.buf, self.ap[idx])

    def rr(self, pat, **kw):
        return V(self.buf, self.ap.rearrange(pat, **kw))

    def bc(self, shape):
        return V(self.buf, self.ap.broadcast_to(shape))

    def un(self, axis):
        return V(self.buf, self.ap.unsqueeze(axis))

    def bitcast(self, dt):
        return V(self.buf, self.ap.bitcast(dt))

    @property
    def shape(self):
        return self.ap.shape


class Op:
    __slots__ = ("eng", "fn", "deps", "sig", "cnt", "isdma", "slot", "slotval", "prevval", "cost")

    def __init__(self, eng, fn, isdma):
        self.cost = 300.0
        self.eng = eng
        self.fn = fn
        self.deps = set()
        self.sig = False
        self.cnt = 0
        self.isdma = isdma
        self.slot = None
        self.slotval = 0
        self.prevval = 0


class _Scope:
    def __init__(self, p):
        self.p = p

    def __enter__(self):
        self.p._scopes.append([])
        return self

    def __exit__(self, *a):
        p = self.p
        items = p._scopes.pop()
        ops = set(p._fence)
        for cm, b in items:
            if b.lw is not None:
                ops.add(b.lw)
            ops.update(b.rd)
        best = {}
        keep = []
        for i in ops:
            o = p.ops[i]
            if o.isdma:
                if o.slot not in best or best[o.slot] < i:
                    best[o.slot] = i
            else:
                if o.eng not in best or best[o.eng] < i:
                    best[o.eng] = i
        p._fence = keep + list(best.values())
        for cm, b in reversed(items):
            cm.__exit__(None, None, None)
        return False


class Prog:
    ENGS = ("pe", "act", "dve", "pool", "sp")

    def __init__(self):
        self.nc = bass.Bass("TRN2", target_bir_lowering=False)
        self.ops = []
        self._ctx = []
        self.ntile = 0
        self._scopes = []
        self._fence = []
        self._dcount = {q: 0 for q in NSLOT}

    def dram(self, name, shape, dt, kind):
        t = self.nc.dram_tensor(name, list(shape), dt, kind=kind)
        return V(Buf(name), t.ap())

    def sb(self, shape, dt, name=None):
        self.ntile += 1
        name = name or f"t{self.ntile}"
        cm = self.nc.sbuf_tensor(f"{name}_{self.ntile}", list(shape), dt)
        h = cm.__enter__()
        b = Buf(name)
        b.rd = list(self._fence)
        if self._scopes:
            self._scopes[-1].append((cm, b))
        else:
            self._ctx.append(cm)
        return V(b, h[:])

    def scope(self):
        return _Scope(self)

    def ps(self, shape, dt, name=None):
        self.ntile += 1
        name = name or f"p{self.ntile}"
        cm = self.nc.psum_tensor(f"{name}_{self.ntile}", list(shape), dt)
        h = cm.__enter__()
        b = Buf(name, excl=True)
        b.rd = list(self._fence)
        if self._scopes:
            self._scopes[-1].append((cm, b))
        else:
            self._ctx.append(cm)
        return V(b, h[:])

    def op(self, eng, fn, reads, writes, isdma=False, cost=None):
        i = len(self.ops)
        o = Op(eng, fn, isdma)
        if cost is None:
            w0 = next((v for v in writes if v is not None), None)
            n = 1
            if w0 is not None:
                for d_ in w0.ap.shape[1:]:
                    n *= d_
            if isdma:
                cost = 2000.0 + n * 128 * 4 / 150.0
            elif eng == "act":
                cost = 230.0 + n / 1.2
            elif eng == "dve":
                cost = 70.0 + n / 0.96
            elif eng == "pool":
                cost = 150.0 + n / 0.96
            else:
                cost = 300.0
        o.cost = cost
        rb = {id(v.buf): v.buf for v in reads if v is not None}
        wb = {id(v.buf): v.buf for v in writes if v is not None}
        for b in rb.values():
            if b.lw is not None:
                o.deps.add(b.lw)
            if b.excl:
                for r in b.rd:
                    if self.ops[r].eng != eng:
                        o.deps.add(r)
        for b in wb.values():
            if b.lw is not None:
                o.deps.add(b.lw)
            for r in b.rd:
                o.deps.add(r)
        o.deps.discard(i)
        for b in rb.values():
            if id(b) not in wb:
                b.rd.append(i)
        for b in wb.values():
            b.lw = i
            b.rd = []
        if isdma:
            k = self._dcount[eng]
            self._dcount[eng] += 1
            n = NSLOT[eng]
            o.slot = (eng, k % n)
            o.slotval = 16 * (k // n + 1)
            o.prevval = 16 * (k // n)
        self.ops.append(o)
        return i

    def dma(self, out, in_, q="sp", **kw):
        self.op(q, lambda e: e.dma_start(out=out.ap, in_=in_.ap, **kw), [in_], [out], isdma=True)

    def mm(self, out, lhsT, rhs, start=True, stop=True, **kw):
        n = 1
        for d_ in rhs.ap.shape[1:]:
            n *= d_
        passes = 4 if rhs.ap.dtype == F32 else 1
        self.op("pe", lambda e: e.matmul(out.ap, lhsT.ap, rhs.ap, start=start, stop=stop, **kw),
                [lhsT, rhs], [out], cost=30.0 + max(n, 64) * passes * 0.45)

    def tr(self, out, in_, ident):
        passes = 4 if in_.ap.dtype == F32 else 1
        self.op("pe", lambda e: e.transpose(out.ap, in_.ap, ident.ap), [in_, ident], [out],
                cost=60.0 + 128 * passes * 0.45)

    def act(self, out, in_, func, bias=None, scale=None, accum=None):
        kw = {}
        rd = [in_]
        if bias is not None:
            if isinstance(bias, V):
                kw["bias"] = bias.ap
                rd.append(bias)
            else:
                kw["bias"] = bias
        if scale is not None:
            if isinstance(scale, V):
                kw["scale"] = scale.ap
                rd.append(scale)
            else:
                kw["scale"] = scale
        wr = [out]
        if accum is not None:
            kw["accum_out"] = accum.ap
            wr.append(accum)
        self.op("act", lambda e: e.activation(out.ap, in_.ap, func, **kw), rd, wr)

    def tt(self, out, a, b, op, eng="dve"):
        self.op(eng, lambda e: e.tensor_tensor(out.ap, a.ap, b.ap, op), [a, b], [out])

    def ts(self, out, a, s1, op0, s2=None, op1=None, eng="dve", accum=None):
        rd = [a]
        x1 = s1.ap if isinstance(s1, V) else s1
        x2 = s2.ap if isinstance(s2, V) else s2
        if isinstance(s1, V):
            rd.append(s1)
        if isinstance(s2, V):
            rd.append(s2)
        kw = {}
        wr = [out]
        if op1 is not None:
            kw["op1"] = op1
        if accum is not None:
            kw["accum_out"] = accum.ap
            wr.append(accum)
        self.op(eng, lambda e: e.tensor_scalar(out.ap, a.ap, x1, x2, op0, **kw), rd, wr)

    def stt(self, out, a, s, b, op0, op1, eng="dve"):
        rd = [a, b]
        x = s.ap if isinstance(s, V) else s
        if isinstance(s, V):
            rd.append(s)
        self.op(eng, lambda e: e.scalar_tensor_tensor(out.ap, a.ap, x, b.ap, op0, op1), rd, [out])

    def cp(self, out, in_, eng="dve"):
        if eng == "act":
            self.op("act", lambda e: e.copy(out.ap, in_.ap), [in_], [out])
        else:
            self.op(eng, lambda e: e.tensor_copy(out.ap, in_.ap), [in_], [out])

    def red(self, out, in_, op=ALU.add, axis=AX.X, eng="dve"):
        self.op(eng, lambda e: e.tensor_reduce(out.ap, in_.ap, axis, op), [in_], [out])

    def memset(self, out, val, eng="pool"):
        self.op(eng, lambda e: e.memset(out.ap, val), [], [out])

    def iota(self, out, pattern, base=0, cm=0):
        self.op("pool", lambda e: e.iota(out.ap, pattern, base=base, channel_multiplier=cm,
                                         allow_small_or_imprecise_dtypes=True), [], [out])

    def _schedule(self, ops):
        n = len(ops)
        W = SCHED_WINDOW
        SEM = 120.0
        users = [[] for _ in range(n)]
        nleft = [0] * n
        for i, o in enumerate(ops):
            nleft[i] = len(o.deps)
            for j in o.deps:
                users[j].append(i)
        pend = {e: [i for i, o in enumerate(ops) if o.eng == e] for e in self.ENGS}
        head = {e: 0 for e in self.ENGS}
        done = [False] * n
        finish = [0.0] * n
        ready = [0.0] * n
        tfree = {e: 0.0 for e in self.ENGS}
        order = {e: [] for e in self.ENGS}
        remaining = n
        while remaining:
            best = None
            for e in self.ENGS:
                lst = pend[e]
                h = head[e]
                while h < len(lst) and done[lst[h]]:
                    h += 1
                head[e] = h
                seen_dma = False
                cnt = 0
                k = h
                while k < len(lst) and cnt < W:
                    i = lst[k]
                    k += 1
                    if done[i]:
                        continue
                    cnt += 1
                    o = ops[i]
                    if o.isdma:
                        if seen_dma:
                            continue
                        seen_dma = True
                    if nleft[i] > 0:
                        continue
                    st = max(tfree[e], ready[i])
                    key = (st + 0.5 * (cnt - 1), i)
                    if best is None or key < best[0]:
                        best = (key, e, i, st)
            if best is None:
                raise RuntimeError("scheduler deadlock")
            _, e, i, st = best
            o = ops[i]
            done[i] = True
            remaining -= 1
            order[e].append(i)
            if o.isdma:
                tfree[e] = st + 60.0
                finish[i] = st + o.cost
            else:
                tfree[e] = st + o.cost
                finish[i] = st + o.cost
            for u in users[i]:
                nleft[u] -= 1
                lat = 0.0 if (o.eng == "pe" and ops[u].eng == "pe" and not o.isdma) else SEM
                r = finish[i] + lat
                if r > ready[u]:
                    ready[u] = r
        self.est_ns = max(finish) if finish else 0.0
        return order

    def finish(self):
        nc = self.nc
        ops = self.ops
        last = {}
        for i, o in enumerate(ops):
            last[o.eng] = i
        fin = Op("sp", None, False)
        for e, i in last.items():
            fin.deps.add(i)
        lastslot = {}
        for i, o in enumerate(ops):
            if o.isdma:
                lastslot[o.slot] = i
        for i in lastslot.values():
            fin.deps.add(i)
        ops.append(fin)
        order = self._schedule(ops) if SCHED_WINDOW > 0 else {e: [i for i, o in enumerate(ops) if o.eng == e] for e in self.ENGS}
        self._order = order
        for o in ops:
            for j in o.deps:
                d = ops[j]
                if d.eng == "pe" and o.eng == "pe" and not o.isdma:
                    continue
                d.sig = True
        ccount = {e: 0 for e in self.ENGS}
        dcount = self._dcount
        for e_ in self.ENGS:
            for i_ in order[e_]:
                o = ops[i_]
                if o.isdma:
                    pass
                elif o.sig:
                    ccount[o.eng] += 1
                    o.cnt = ccount[o.eng]
        sems = {}
        cms = []

        def getsem(key):
            if key not in sems:
                cm = nc.semaphore(f"s_{key[0]}_{key[1]}")
                sems[key] = cm.__enter__()
                cms.append(cm)
            return sems[key]

        for e in self.ENGS:
            for ep in range(ccount[e] // EPOCH + 1):
                getsem((e, "c%d" % ep))
        for q, n in NSLOT.items():
            for s in range(min(n, dcount[q])):
                getsem((q, s))

        def compkey(o):
            ep = (o.cnt - 1) // EPOCH
            return (o.eng, "c%d" % ep), o.cnt - ep * EPOCH

        with nc.Block() as block:
            def emit_engine(ename, eng):
                known = {}
                for i in order[ename]:
                    o = ops[i]
                    waits = {}
                    for j in o.deps:
                        d = ops[j]
                        if d.isdma:
                            key, val = d.slot, d.slotval
                        else:
                            if d.eng == "pe" and ename == "pe" and not o.isdma:
                                continue
                            key, val = compkey(d)
                        if waits.get(key, 0) < val:
                            waits[key] = val
                    if o.isdma and o.prevval > 0:
                        key = o.slot
                        if waits.get(key, 0) < o.prevval:
                            waits[key] = o.prevval
                    for key, val in waits.items():
                        if known.get(key, 0) >= val:
                            continue
                        known[key] = val
                        eng.wait_ge(getsem(key), val)
                    if o.fn is None:
                        continue
                    ins = o.fn(eng)
                    if o.isdma:
                        ins.then_inc(getsem(o.slot), 16)
                    elif o.sig:
                        key, _ = compkey(o)
                        ins.then_inc(getsem(key), 1)

            @block.tensor
            def _(e):
                emit_engine("pe", e)

            @block.scalar
            def _(e):
                emit_engine("act", e)

            @block.vector
            def _(e):
                emit_engine("dve", e)

            @block.gpsimd
            def _(e):
                emit_engine("pool", e)

            @block.sync
            def _(e):
                emit_engine("sp", e)
        self._cms = cms
        return nc


D = 1024
CTX = 256
NORM_EPS = 1e-6
THETA = 10000.0
import math


class Ctx:
    def __init__(self, p, banks=True):
        self.p = p
        self.bi = 0
        self.dbanks = []
        if banks:
            self.set_banks(False)
        io = p.sb([128, 128], F32, "io")
        pi = p.sb([128, 1], F32, "pi")
        p.iota(io, [[1, 128]], base=0, cm=0)
        p.iota(pi, [[0, 1]], base=0, cm=1)
        self.pidx = pi
        self.identf = p.sb([128, 128], F32, "identf")
        p.ts(self.identf, io, pi[:, 0:1], ALU.is_equal)
        self.identb = p.sb([128, 128], BF16, "identb")
        p.cp(self.identb, self.identf)
        self.iof = io

    def set_banks(self, dbl):
        p = self.p
        if dbl:
            self.dbanks = [p.ps([128, 1024], F32, f"dbank{i}") for i in range(2)]
            self.banks = [self.dbanks[0][:, 0:512], self.dbanks[0][:, 512:1024],
                          self.dbanks[1][:, 0:512], self.dbanks[1][:, 512:1024]]
            self.banks += [p.ps([128, 512], F32, f"bank{i}") for i in range(4, 8)]
        else:
            self.banks = [p.ps([128, 512], F32, f"bank{i}") for i in range(8)]

    def bank(self):
        b = self.banks[self.bi % 8]
        self.bi += 1
        return b

    def load_fm(self, rows_v, n, eng_out="dve"):
        p = self.p
        tmp = p.sb([n, 128], F32, "lfm_tmp")
        p.dma(tmp, rows_v)
        bk = self.bank()
        p.tr(bk[:, 0:n], tmp, self.identf[0:n, 0:n])
        out = p.sb([128, n], F32, "lfm_out")
        p.cp(out, bk[:, 0:n], eng=eng_out)
        return out

    def rstd(self, ss, n, inv_d, eps):
        p = self.p
        p.ts(ss, ss, inv_d, ALU.mult, eps, ALU.add)
        p.act(ss, ss, AF.Ln)
        p.act(ss, ss, AF.Exp, scale=-0.5)


def run_prog(p, in_maps):
    nc = p.finish()
    res = run_bass_kernel_spmd(nc, in_maps, core_ids=list(range(len(in_maps))))
    return res.results


def build_phase0():
    p = Prog()
    cvec = p.dram("cvec", [2, D], F32, "ExternalInput")
    ada_w = p.dram("ada_w", [2, D, 6 * D], F32, "ExternalInput")
    ada_b = p.dram("ada_b", [2, 6 * D], F32, "ExternalInput")
    mods_tm = p.dram("mods_tm", [2, 2, 6 * D], F32, "ExternalOutput")
    mods_fm = p.dram("mods_fm", [2, 128, 48, 2], F32, "ExternalOutput")
    c = Ctx(p)
    cT = c.load_fm(cvec.rr("j (c p) -> (j c) p", p=128), 16)
    sT = p.sb([128, 16], F32, "sT")
    p.act(sT, cT, AF.Silu)
    sTv = sT.rr("p (j c) -> p c j", j=2)
    wbuf = [p.sb([128, 8, 1536], F32, f"adaw{i}") for i in range(2)]
    it = 0
    for l in range(2):
        bfm = c.load_fm(ada_b[l].rr("(c p) -> c p", p=128), 48)
        ofm = p.sb([128, 48, 2], F32, "ofm")
        for qd in range(4):
            w = wbuf[it % 2]
            it += 1
            for kc in range(8):
                p.dma(w[:, kc, :], ada_w[l, kc * 128:(kc + 1) * 128, qd * 1536:(qd + 1) * 1536],
                      q=("sp" if kc % 2 == 0 else "act"))
            bk = c.bank()
            for cc in range(12):
                for kc in range(8):
                    p.mm(bk[:, cc * 2:cc * 2 + 2], w[:, kc, cc * 128:(cc + 1) * 128], sTv[:, kc, :],
                         start=(kc == 0), stop=(kc == 7))
            p.tt(ofm[:, qd * 12:(qd + 1) * 12, :], bk[:, 0:24].rr("p (c j) -> p c j", j=2),
                 bfm[:, qd * 12:(qd + 1) * 12].un(2).bc([128, 12, 2]), ALU.add)
        p.dma(mods_fm[l], ofm)
        bk = c.bank()
        ofm2 = p.sb([128, 2, 48], F32, "ofm2")
        p.cp(ofm2, ofm.rr("p c j -> p j c"))
        p.tr(bk[0:96, 0:128], ofm2.rr("p j c -> p (j c)"), c.identf)
        otm = p.sb([96, 128], F32, "otm")
        p.cp(otm, bk[0:96, 0:128])
        for j in range(2):
            p.dma(mods_tm[l][j].rr("(c p) -> c p", p=128), otm[48 * j:48 * j + 48, :])
    return p


def build_post(S, has_ctx, stop=99):
    TL = S // 4
    NTL = TL // 128
    NT = NTL + (2 if has_ctx else 0)
    TOK = NT * 128
    p = Prog()
    x_in = p.dram("x", [TOK, D], F32, "ExternalInput")
    attT = p.dram("attT", [8, 128, TOK], BF16, "ExternalInput")
    mods_tm = p.dram("mods_tm", [2, 6 * D], F32, "ExternalInput")
    mods_fm = p.dram("mods_fm", [128, 48, 2], F32, "ExternalInput")
    w_out = p.dram("w_out", [D, D], F32, "ExternalInput")
    n2g = p.dram("norm2_g", [D], F32, "ExternalInput")
    router_w = p.dram("router_w", [D, 16], F32, "ExternalInput")
    router_b = p.dram("router_b", [1, 16], F32, "ExternalInput")
    wg_all = p.dram("wg_all", [17, D, 256], F32, "ExternalInput")
    wu_all = p.dram("wu_all", [17, D, 256], F32, "ExternalInput")
    wd_all = p.dram("wd_all", [17, 256, D], F32, "ExternalInput")
    x_out = p.dram("x_out", [TOK, D], F32, "ExternalOutput")
    c = Ctx(p)
    B = c.banks
    nvar = 2 if has_ctx else 1
    blocks = []
    t = 0
    while t < NTL:
        n = min(4, NTL - t)
        blocks.append((0, t, n))
        t += n
    if has_ctx:
        blocks.append((1, NTL, 2))

    mfm = p.sb([128, 48, 2], F32, "mfm")
    p.dma(mfm, mods_fm)
    g2fm = c.load_fm(n2g.rr("(c p) -> c p", p=128), 8)
    A2 = p.sb([128, 8, 2], F32, "A2")
    p.ts(A2, mfm[:, 32:40, :], 1.0, ALU.add)
    p.tt(A2, A2, g2fm.un(2).bc([128, 8, 2]), ALU.mult)
    SH2 = mfm[:, 24:32, :]
    rw = p.sb([128, 8, 16], F32, "rw")
    p.dma(rw, router_w.rr("(kc p) e -> p kc e", p=128))
    rb = p.sb([128, 16], F32, "rb")
    p.dma(rb, router_b.bc([128, 16]))
    sel = p.sb([16, 16, 128], F32, "sel")
    p.iota(sel, [[1, 16], [0, 128]], base=0, cm=0)
    p.ts(sel, sel, c.pidx[0:16, 0:1], ALU.is_equal)
    xs = p.sb([128, NT, D], F32, "xs")
    for t in range(NT):
        p.dma(xs[:, t, :], x_in[t * 128:(t + 1) * 128, :])
    h2T = p.sb([128, 8, TOK], BF16, "h2T")
    combT = p.sb([16, TOK], F32, "combT")
    lg_all = p.sb([128, NT, 16], F32, "lg_all")
    gabc = [p.sb([128, D], F32, f"gabc{i}") for i in range(2)]

    def load_ga(which, var, dst):
        p.dma(dst, mods_tm[var:var + 1, which * D:(which + 1) * D].bc([128, D]))

    with p.scope():
        if stop >= 2:
            wo = p.sb([128, 8, D], BF16, "wo")
            atb = [p.sb([128, 8, 128], BF16, f"atb{i}") for i in range(2)]
            for var in range(nvar):
                p.dma(wo, w_out.rr("(kc p) d -> p kc d", p=128), q="pool")
                load_ga(2, var, gabc[0])
                p.tt(wo, wo, gabc[0].un(1).bc([128, 8, D]), ALU.mult, eng="pool")
                tiles = range(NTL) if var == 0 else range(NTL, NT)
                for t in tiles:
                    a = atb[t % 2]
                    p.dma(a, attT[:, :, t * 128:(t + 1) * 128].rr("c p t -> p c t"))
                    for half in range(2):
                        bk = B[(t % 2) * 2 + half]
                        for kc in range(8):
                            p.mm(bk[:, :], a[:, kc, :], wo[:, kc, half * 512:(half + 1) * 512],
                                 start=(kc == 0), stop=(kc == 7))
                        xv = xs[:, t, half * 512:(half + 1) * 512]
                        p.tt(xv, xv, bk, ALU.add)

    with p.scope():
        if stop >= 3:
            junk = p.sb([128, D], BF16, "junk")
            xn = [p.sb([128, D], F32, f"xn{i}") for i in range(2)]
            h2f = [p.sb([128, 8, 128], F32, f"h2f{i}") for i in range(2)]
            ssq = p.sb([128, NT], F32, "ssq")
            for t in range(NT):
                var = 0 if t < NTL else 1
                p.act(junk, xs[:, t, :], AF.Square, accum=ssq[:, t:t + 1])
            c.rstd(ssq, NT, 1.0 / D, NORM_EPS)
            for t in range(NT):
                var = 0 if t < NTL else 1
                x_n = xn[t % 2]
                hf = h2f[t % 2]
                p.ts(x_n, xs[:, t, :], ssq[:, t:t + 1], ALU.mult)
                for hb in range(2):
                    bk = B[4 + (t % 2) * 2 + hb]
                    for k4 in range(4):
                        kc = hb * 4 + k4
                        p.tr(bk[:, k4 * 128:(k4 + 1) * 128], x_n[:, kc * 128:(kc + 1) * 128], c.identf)
                    for k4 in range(4):
                        kc = hb * 4 + k4
                        p.act(hf[:, kc, :], bk[:, k4 * 128:(k4 + 1) * 128], AF.Identity,
                              bias=SH2[:, kc, var:var + 1], scale=A2[:, kc, var:var + 1])
                p.cp(h2T[:, :, t * 128:(t + 1) * 128], hf, eng="pool")
                bk = B[t % 2]
                for kc in range(8):
                    p.mm(bk[:, 0:16], hf[:, kc, :], rw[:, kc, :], start=(kc == 0), stop=(kc == 7))
                p.cp(lg_all[:, t, :], bk[:, 0:16])
            N16 = NT * 16
            sc = p.sb([128, NT, 16], F32, "sc")
            bs = p.sb([128, NT, 16], F32, "bs")
            p.act(sc, lg_all, AF.Sigmoid)
            p.tt(bs, sc, rb.un(1).bc([128, NT, 16]), ALU.add)
            bsv = bs.rr("p t (g e) -> p (t g) e", g=4)
            G = NT * 4
            m1 = p.sb([128, G], F32, "m1")
            m2 = p.sb([128, G], F32, "m2")
            p.red(m1, bsv, op=ALU.max)
            eq1 = p.sb([128, G, 4], F32, "eq1")
            p.tt(eq1, bsv, m1.un(2).bc([128, G, 4]), ALU.is_equal)
            p.stt(eq1, eq1, -1e9, bsv, ALU.mult, ALU.add)
            p.red(m2, eq1, op=ALU.max)
            gs = p.sb([128, NT, 4], F32, "gs")
            p.tt(gs.rr("p t g -> p (t g)"), m1, m2, ALU.add)
            gmax = p.sb([128, NT], F32, "gmax")
            p.red(gmax, gs, op=ALU.max)
            gsel = p.sb([128, NT, 4], F32, "gsel")
            p.tt(gsel, gs, gmax.un(2).bc([128, NT, 4]), ALU.is_equal)
            ge2 = p.sb([128, G, 4], F32, "ge2")
            p.tt(ge2, bsv, m2.un(2).bc([128, G, 4]), ALU.is_ge)
            p.tt(ge2, ge2, gsel.rr("p t g -> p (t g)").un(2).bc([128, G, 4]), ALU.mult)
            p.tt(ge2, ge2, sc.rr("p t (g e) -> p (t g) e", g=4), ALU.mult)
            den = p.sb([128, NT], F32, "den")
            p.red(den, ge2.rr("p (t g) e -> p t (g e)", g=4))
            p.op("dve", lambda e: e.reciprocal(den.ap, den.ap), [den], [den])
            comb = p.sb([128, NT, 16], F32, "comb")
            p.tt(comb, ge2.rr("p (t g) e -> p t (g e)", g=4), den.un(2).bc([128, NT, 16]), ALU.mult)
            for t in range(NT):
                bk = B[t % 2]
                p.tr(bk[0:16, 0:128], comb[:, t, :], c.identf)
                p.cp(combT[:, t * 128:(t + 1) * 128], bk[0:16, 0:128], eng="act")

    with p.scope():
        if stop >= 4:
            wg = [p.sb([128, 8, 256], BF16, f"wg{i}") for i in range(2)]
            wu = [p.sb([128, 8, 256], BF16, f"wu{i}") for i in range(2)]
            wd = [p.sb([128, 2, D], BF16, f"wd{i}") for i in range(2)]
            wds = [p.sb([128, 2, D], BF16, f"wds{i}") for i in range(2)]
            comb_sb = [p.sb([128, 512], F32, f"comb_sb{i}") for i in range(2)]
            sg = [p.sb([128, 512], F32, f"sg{i}") for i in range(2)]
            tg = [p.sb([128, 512], F32, f"tg{i}") for i in range(2)]
            actb = [p.sb([128, 2, 512], BF16, f"actb{i}") for i in range(2)]
            for var in range(nvar):
                load_ga(5, var, gabc[var])
            it = 0
            for e in range(17 if stop < 40 or stop == 99 else stop - 40):
                eb = e % 2
                p.dma(wg[eb], wg_all[e].rr("(kc p) f -> p kc f", p=128), q="pool")
                p.dma(wu[eb], wu_all[e].rr("(kc p) f -> p kc f", p=128), q="pool")
                p.dma(wd[eb], wd_all[e].rr("(fc p) d -> p fc d", p=128), q="pool")
                for var in range(nvar):
                    p.tt(wds[eb], wd[eb], gabc[var].un(1).bc([128, 2, D]), ALU.mult, eng="pool")
                    for (bv, t0, nt) in blocks:
                        if bv != var:
                            continue
                        n = nt * 128
                        tok = slice(t0 * 128, t0 * 128 + n)
                        ib = it % 2
                        it += 1
                        if e < 16:
                            p.mm(B[4][:, 0:n], sel[:, e, :], combT[:, tok])
                            p.cp(comb_sb[ib][:, 0:n], B[4][:, 0:n], eng="act")
                        for fc in range(2):
                            gb = B[fc * 2]
                            ub = B[fc * 2 + 1]
                            for kc in range(8):
                                p.mm(gb[:, 0:n], wg[eb][:, kc, fc * 128:(fc + 1) * 128], h2T[:, kc, tok],
                                     start=(kc == 0), stop=(kc == 7))
                            for kc in range(8):
                                p.mm(ub[:, 0:n], wu[eb][:, kc, fc * 128:(fc + 1) * 128], h2T[:, kc, tok],
                                     start=(kc == 0), stop=(kc == 7))
                            p.act(sg[fc][:, 0:n], gb[:, 0:n], AF.Silu)
                            if e < 16:
                                p.tt(tg[fc][:, 0:n], ub[:, 0:n], comb_sb[ib][:, 0:n], ALU.mult)
                                p.tt(actb[ib][:, fc, 0:n], sg[fc][:, 0:n], tg[fc][:, 0:n], ALU.mult, eng="pool")
                            else:
                                p.tt(actb[ib][:, fc, 0:n], sg[fc][:, 0:n], ub[:, 0:n], ALU.mult)
                        for ti in range(nt):
                            t = t0 + ti
                            for half in range(2):
                                db = B[5 + (ti * 2 + half) % 3]
                                for fc in range(2):
                                    p.mm(db, actb[ib][:, fc, ti * 128:(ti + 1) * 128],
                                         wds[eb][:, fc, half * 512:(half + 1) * 512],
                                         start=(fc == 0), stop=(fc == 1))
                                xv = xs[:, t, half * 512:(half + 1) * 512]
                                p.tt(xv, xv, db, ALU.add)
    for t in range(NT):
        p.dma(x_out[t * 128:(t + 1) * 128, :], xs[:, t, :])
    return p


def rope_tables(p, c, pos, NTL, half):
    inv = p.sb([128, half], F32, "inv")
    for i in range(half):
        p.memset(inv[:, i:i + 1], float(THETA ** (-i / half)) / (2.0 * math.pi))
    Y = p.sb([128, NTL, 2, half], F32, "ropeY")
    p.tt(Y.rr("p t r h -> p (t r) h"), pos.rr("p t r -> p (t r)").un(2).bc([128, NTL * 2, half]),
         inv.un(1).bc([128, NTL * 2, half]), ALU.mult)
    COS = p.sb([128, NTL, 2, 2 * half], F32, "COS")
    SINS = p.sb([128, NTL, 2, 2 * half], F32, "SINS")
    Yi = p.sb([128, NTL, 2, half], I32, "ropeYi")
    Yf = p.sb([128, NTL, 2, half], F32, "ropeYf")
    T = p.sb([128, NTL, 2, half], F32, "ropeT")
    R = p.sb([128, NTL, 2, half], F32, "ropeR")
    for which in range(2):
        if which == 1:
            p.ts(Y, Y, 0.25, ALU.add)
        p.cp(Yi, Y)
        p.cp(Yf, Yi)
        p.tt(R, Y, Yf, ALU.subtract)
        p.ts(T, R, 0.5, ALU.is_gt)
        p.tt(R, R, T, ALU.subtract)
        p.ts(T, R, -0.5, ALU.is_lt)
        p.tt(R, R, T, ALU.add)
        if which == 0:
            p.act(SINS[:, :, :, half:2 * half], R, AF.Sin, scale=2.0 * math.pi * (1 - 1e-6))
            p.ts(SINS[:, :, :, 0:half], SINS[:, :, :, half:2 * half], -1.0, ALU.mult)
        else:
            p.act(COS[:, :, :, 0:half], R, AF.Sin, scale=2.0 * math.pi * (1 - 1e-6))
            p.cp(COS[:, :, :, half:2 * half], COS[:, :, :, 0:half])
    return COS, SINS


def qk_post(p, c, pieces, nh, hd, gains_bc, dst, scratch, rope=None):
    sq, qk, ss = scratch["sq"], scratch["qk"], scratch["ss"]
    off = 0
    for (v, n) in pieces:
        p.act(sq[:, off:off + n * hd], v, AF.Square)
        off += n * hd
    p.red(ss, sq.rr("p (h d) -> p h d", d=hd))
    c.rstd(ss, nh, 1.0 / hd, NORM_EPS)
    off = 0
    h0 = 0
    for (v, n) in pieces:
        p.tt(qk[:, off:off + n * hd].rr("p (h d) -> p h d", d=hd), v.rr("p (h d) -> p h d", d=hd),
             ss[:, h0:h0 + n].un(2).bc([128, n, hd]), ALU.mult)
        off += n * hd
        h0 += n
    if rope is None:
        p.tt(dst, qk, gains_bc, ALU.mult, eng="pool")
        return
    p.tt(qk, qk, gains_bc, ALU.mult, eng="pool")
    roff, half, COS_t, SINS_t = rope
    qv = qk.rr("p (h d) -> p h d", d=hd)
    dv = dst.rr("p (h d) -> p h d", d=hd)
    if roff > 0:
        p.cp(dv[:, :, 0:roff], qv[:, :, 0:roff], eng="pool")
    t1, t2 = scratch["t1"], scratch["t2"]
    w = 2 * half
    for rc in range(2):
        xs = qv[:, :, roff + rc * w: roff + (rc + 1) * w]
        a = t1[:, 0:nh * w].rr("p (h d) -> p h d", d=w)
        b = t2[:, 0:nh * w].rr("p (h d) -> p h d", d=w)
        eng = "dve" if rc == 0 else "pool"
        p.tt(a, xs, COS_t[:, rc, :].un(1).bc([128, nh, w]), ALU.mult, eng=eng)
        p.tt(b[:, :, 0:half], xs[:, :, half:w], SINS_t[:, rc, 0:half].un(1).bc([128, nh, half]), ALU.mult, eng=eng)
        p.tt(b[:, :, half:w], xs[:, :, 0:half], SINS_t[:, rc, half:w].un(1).bc([128, nh, half]), ALU.mult, eng=eng)
        p.tt(dv[:, :, roff + rc * w: roff + (rc + 1) * w], a, b, ALU.add, eng=eng)


def norm1_hT(p, c, xt, A, SH, var, hT_dst, xnb, junk, ssq, banks):
    p.act(junk, xt, AF.Square, accum=ssq)
    c.rstd(ssq, 1, 1.0 / D, NORM_EPS)
    p.ts(xnb, xt, ssq[:, 0:1], ALU.mult)
    for hb in range(2):
        bk = banks[hb].bitcast(BF16)
        for k4 in range(4):
            kc = hb * 4 + k4
            p.tr(bk[:, k4 * 128:(k4 + 1) * 128], xnb[:, kc * 128:(kc + 1) * 128], c.identb)
        for k4 in range(4):
            kc = hb * 4 + k4
            p.act(hT_dst[:, kc, :], bk[:, k4 * 128:(k4 + 1) * 128], AF.Identity,
                  bias=SH[:, kc, var:var + 1], scale=A[:, kc, var:var + 1])


def mod_consts(p, c, mods_fm, norm_g, which_sh, which_sc):
    mfm = p.sb([128, 48, 2], F32, "mfm")
    p.dma(mfm, mods_fm)
    gfm = c.load_fm(norm_g.rr("(c p) -> c p", p=128), 8)
    A = p.sb([128, 8, 2], F32, "Amod")
    p.ts(A, mfm[:, which_sc * 8:(which_sc + 1) * 8, :], 1.0, ALU.add)
    p.tt(A, A, gfm.un(2).bc([128, 8, 2]), ALU.mult)
    SH = mfm[:, which_sh * 8:(which_sh + 1) * 8, :]
    return A, SH


def build_pre1(S):
    TL = S // 4
    NTL = TL // 128
    NT = NTL + 2
    TOK = NT * 128
    p = Prog()
    x_in = p.dram("x", [TOK, D], F32, "ExternalInput")
    mods_fm = p.dram("mods_fm", [128, 48, 2], F32, "ExternalInput")
    n1g = p.dram("norm1_g", [D], F32, "ExternalInput")
    w_in = p.dram("w_in", [D, 1536], F32, "ExternalInput")
    g_q = p.dram("g_q", [1, 64], F32, "ExternalInput")
    g_k = p.dram("g_k", [1, 64], F32, "ExternalInput")
    pos_in = p.dram("pos", [128, NTL, 2], F32, "ExternalInput")
    qkT = p.dram("qkT", [10, 128, TOK], BF16, "ExternalOutput")
    v_out = p.dram("v", [TOK, 256], BF16, "ExternalOutput")
    c = Ctx(p)
    B = c.banks
    A, SH = mod_consts(p, c, mods_fm, n1g, 0, 1)
    pos = p.sb([128, NTL, 2], F32, "pos")
    p.dma(pos, pos_in)
    COS, SINS = rope_tables(p, c, pos, NTL, 16)
    gains = p.sb([128, 20, 64], F32, "gains")
    gq = p.sb([128, 64], F32, "gq")
    gk = p.sb([128, 64], F32, "gk")
    p.dma(gq, g_q.bc([128, 64]))
    p.dma(gk, g_k.bc([128, 64]))
    p.cp(gains[:, 0:16, :], gq.un(1).bc([128, 16, 64]))
    p.cp(gains[:, 16:20, :], gk.un(1).bc([128, 4, 64]))
    gains_f = gains.rr("p h d -> p (h d)")
    wi = p.sb([128, 8, 1536], BF16, "wi")
    p.dma(wi, w_in.rr("(kc p) n -> p kc n", p=128), q="pool")
    xt = [p.sb([128, D], F32, f"xt{i}") for i in range(2)]
    xnb = [p.sb([128, D], BF16, f"xnb{i}") for i in range(2)]
    junk = p.sb([128, D], BF16, "junk")
    hT = [p.sb([128, 8, 128], BF16, f"hT{i}") for i in range(2)]
    ssq = [p.sb([128, 1], F32, f"ssq{i}") for i in range(2)]
    scrs = [dict(sq=p.sb([128, 1280], F32, "sq"), qk=p.sb([128, 1280], F32, "qk"), ss=p.sb([128, 20], F32, "ss"),
                 t1=p.sb([128, 1280], F32, "t1"), t2=p.sb([128, 1280], F32, "t2")) for _ in range(2)]
    qkb = [p.sb([128, 1280], BF16, f"qkb{i}") for i in range(2)]
    vb = [p.sb([128, 256], BF16, f"vb{i}") for i in range(2)]
    qkTs = [p.sb([128, 10, 128], BF16, f"qkTs{i}") for i in range(2)]
    for t in range(NT):
        i2 = t % 2
        var = 0 if t < NTL else 1
        p.dma(xt[i2], x_in[t * 128:(t + 1) * 128, :])
        norm1_hT(p, c, xt[i2], A, SH, var, hT[i2], xnb[i2], junk, ssq[i2], (B[0], B[1]))
        for blk in range(3):
            bk = B[2 + blk]
            for kc in range(8):
                p.mm(bk, hT[i2][:, kc, :], wi[:, kc, blk * 512:(blk + 1) * 512], start=(kc == 0), stop=(kc == 7))
        rope = None if var == 1 else (0, 16, COS[:, t], SINS[:, t])
        qk_post(p, c, [(B[2], 8), (B[3], 8), (B[4][:, 0:256], 4)], 20, 64, gains_f, qkb[i2], scrs[i2], rope)
        p.cp(vb[i2], B[4][:, 256:512], eng="act")
        p.dma(v_out[t * 128:(t + 1) * 128, :], vb[i2])
        bA = B[5].bitcast(BF16)
        bB = B[6].bitcast(BF16)
        for pr in range(10):
            dstb = bA[:, pr * 128:(pr + 1) * 128] if pr < 8 else bB[:, (pr - 8) * 128:(pr - 7) * 128]
            p.tr(dstb, qkb[i2][:, pr * 128:(pr + 1) * 128], c.identb)
        p.cp(qkTs[i2][:, 0:8, :].rr("p a t -> p (a t)"), bA, eng="act")
        p.cp(qkTs[i2][:, 8:10, :].rr("p a t -> p (a t)"), bB[:, 0:256])
        p.dma(qkT[:, :, t * 128:(t + 1) * 128].rr("a p t -> p a t"), qkTs[i2])
    return p


def attention_gen(p, c, qT_d, kT_d, v_d, oT_d, NH, NKV, dk, dv, S, scale, qcT_d=None, ocT_d=None, banks=None):
    LK = CTX + S
    NKT = LK // 128
    B = banks if banks is not None else c.banks
    kT = p.sb([128, NKV, LK], BF16, "kT")
    if dk < 128:
        p.memset(kT, 0.0)
    for kv in range(NKV):
        p.dma(kT[0:dk, kv, :], kT_d[kv])
    vp = p.sb([128, NKV, NKT, dv + 1], BF16, "vp")
    p.memset(vp[:, :, :, dv:dv + 1], 1.0)
    for kv in range(NKV):
        p.dma(vp[:, kv, :, 0:dv], v_d[kv].rr("(t p) d -> p t d", p=128))
    selden = p.sb([dv + 1, dv], F32, "selden")
    p.memset(selden, 0.0)
    p.memset(selden[dv:dv + 1, :], 1.0)
    qTs = [p.sb([128, NH, 512], BF16, f"qTs{i}") for i in range(2)]
    if dk < 128:
        for t_ in qTs:
            p.memset(t_, 0.0)
    pT = [p.sb([128, 512], BF16, f"pT{i}") for i in range(4)]
    accs = [p.sb([dv + 1, 512], F32, f"accs{i}") for i in range(2)]
    rec = [p.sb([dv, 512], F32, f"rec{i}") for i in range(2)]
    ob = [p.sb([dv, 512], BF16, f"ob{i}") for i in range(2)]
    jobs = []
    for qb in range(S // 512):
        jobs.append((qT_d, oT_d, qb * 512, 512, NKT))
    if qcT_d is not None:
        jobs.append((qcT_d, ocT_d, 0, CTX, CTX // 128))
    DB = c.dbanks
    items = []
    for ji, (qd, od, q0, n, nkt) in enumerate(jobs):
        for h in range(NH):
            for kp in range(nkt // 2):
                items.append((ji, h, kp))
    pT2 = [p.sb([128, 2, 512], BF16, f"pT2_{i}") for i in range(3)]
    loaded = set()

    def load_q(ji):
        if ji in loaded or ji >= len(jobs):
            return
        loaded.add(ji)
        qd, od, q0, n, nkt = jobs[ji]
        qs = qTs[ji % 2]
        for h in range(NH):
            p.dma(qs[0:dk, h, 0:n], qd[h, :, q0:q0 + n])

    def issue_s(i):
        ji, h, kp = items[i]
        qd, od, q0, n, nkt = jobs[ji]
        load_q(ji)
        kv = h * NKV // NH
        db = DB[i % 2]
        for a_ in range(2):
            kt = 2 * kp + a_
            p.mm(db[:, a_ * 512:a_ * 512 + n], kT[:, kv, kt * 128:(kt + 1) * 128], qTs[ji % 2][:, h, 0:n])

    issue_s(0)
    ih = 0
    for i, (ji, h, kp) in enumerate(items):
        qd, od, q0, n, nkt = jobs[ji]
        kv = h * NKV // NH
        if h == 0 and kp == 0:
            load_q(ji + 1)
        if i + 1 < len(items):
            issue_s(i + 1)
        acc = B[4 + ih % 2]
        pt = pT2[i % 3]
        dbv = DB[i % 2].rr("p (a t) -> p a t", a=2)
        p.act(pt[:, :, 0:n], dbv[:, :, 0:n], AF.Exp, scale=scale)
        for a_ in range(2):
            kt = 2 * kp + a_
            p.mm(acc[0:dv + 1, 0:n], vp[:, kv, kt, :], pt[:, a_, 0:n], start=(kt == 0), stop=(kt == nkt - 1))
        if kp == nkt // 2 - 1:
            a = accs[ih % 2]
            p.cp(a[:, 0:n], acc[0:dv + 1, 0:n])
            p.mm(B[6][0:dv, 0:n], selden, a[:, 0:n])
            r = rec[ih % 2]
            p.op("dve", lambda e, r=r, n=n: e.reciprocal(r.ap[:, 0:n], B[6].ap[0:dv, 0:n]), [B[6]], [r])
            o = ob[ih % 2]
            p.tt(o[:, 0:n], a[0:dv, 0:n], r[:, 0:n], ALU.mult, eng="pool")
            p.dma(od[h, :, q0:q0 + n], o[:, 0:n])
            ih += 1
            yield ih


def attention_block(*a, **kw):
    for _ in attention_gen(*a, **kw):
        pass


def build_attn1(S):
    p = Prog()
    LK = CTX + S
    qT = p.dram("qT", [4, 64, S], BF16, "ExternalInput")
    kT = p.dram("kT", [1, 64, LK], BF16, "ExternalInput")
    v = p.dram("v", [1, LK, 64], BF16, "ExternalInput")
    oT = p.dram("oT", [4, 64, S], BF16, "ExternalOutput")
    c = Ctx(p, banks=False)
    c.set_banks(True)
    attention_block(p, c, qT, kT, v, oT, 4, 1, 64, 64, S, 64 ** -0.5)
    return p


RW_COLS = 1920


def build_pre0(S, dbg=0):
    TL = S // 4
    NTL = TL // 128
    NTM = NTL + 2
    NTA = NTL + 3
    TOKM = NTM * 128
    p = Prog()
    x_in = p.dram("x", [NTA * 128, D], F32, "ExternalInput")
    mods_fm = p.dram("mods_fm", [128, 48, 2], F32, "ExternalInput")
    n1g = p.dram("norm1_g", [D], F32, "ExternalInput")
    w_in = p.dram("w_in", [D, 2592], F32, "ExternalInput")
    mu_d = p.dram("mu", [RW_COLS], F32, "ExternalInput")
    flags_d = p.dram("flags", [128, 2], F32, "ExternalInput")
    g_qa = p.dram("g_qa", [1, 384], F32, "ExternalInput")
    g_kva = p.dram("g_kva", [1, 256], F32, "ExternalInput")
    w_q_up = p.dram("w_q_up", [384, 768], F32, "ExternalInput")
    w_kv_up = p.dram("w_kv_up", [256, 1024], F32, "ExternalInput")
    g_q = p.dram("g_q", [1, 96], F32, "ExternalInput")
    g_k = p.dram("g_k", [1, 96], F32, "ExternalInput")
    pos_in = p.dram("pos", [128, NTL, 2], F32, "ExternalInput")
    rwT = p.dram("rwT", [15, 128, TL + CTX], F32, "ExternalOutput")
    qT_o = p.dram("qT", [8, 96, TOKM], BF16, "ExternalOutput")
    kT_o = p.dram("kT", [8, 96, TOKM], BF16, "ExternalOutput")
    v_o = p.dram("v", [TOKM, 512], BF16, "ExternalOutput")
    c = Ctx(p)
    B = c.banks
    A, SH = mod_consts(p, c, mods_fm, n1g, 0, 1)
    pos = p.sb([128, NTL, 2], F32, "pos")
    p.dma(pos, pos_in)
    COS, SINS = rope_tables(p, c, pos, NTL, 8)
    hT_all = p.sb([128, 8, NTA * 128], BF16, "hT_all")

    with p.scope():
        gq_bc = p.sb([128, 8, 96], F32, "gq_bc")
        gk_bc = p.sb([128, 8, 96], F32, "gk_bc")
        g96 = p.sb([128, 96], F32, "g96")
        p.dma(g96, g_q.bc([128, 96]))
        p.cp(gq_bc, g96.un(1).bc([128, 8, 96]))
        g96b = p.sb([128, 96], F32, "g96b")
        p.dma(g96b, g_k.bc([128, 96]))
        p.cp(gk_bc, g96b.un(1).bc([128, 8, 96]))
        gqa_bc = p.sb([128, 384], F32, "gqa_bc")
        gkva_bc = p.sb([128, 256], F32, "gkva_bc")
        p.dma(gqa_bc, g_qa.bc([128, 384]))
        p.dma(gkva_bc, g_kva.bc([128, 256]))
        wim = p.sb([128, 8, 672], BF16, "wim")
        p.dma(wim, w_in[:, RW_COLS:2592].rr("(kc p) n -> p kc n", p=128), q="pool")
        wq = p.sb([128, 3, 768], BF16, "wq")
        p.dma(wq, w_q_up.rr("(kc p) n -> p kc n", p=128), q="pool")
        wkv = p.sb([128, 2, 1024], BF16, "wkv")
        p.dma(wkv, w_kv_up.rr("(kc p) n -> p kc n", p=128), q="pool")
        xt = [p.sb([128, D], F32, f"xt{i}") for i in range(2)]
        xnb = [p.sb([128, D], BF16, f"xnb{i}") for i in range(2)]
        junk = p.sb([128, D], BF16, "junk")
        ssq = [p.sb([128, 1], F32, f"ssq{i}") for i in range(2)]
        ssa = [p.sb([128, 2], F32, f"ssa{i}") for i in range(2)]
        anb = [p.sb([128, 640], BF16, f"anb{i}") for i in range(2)]
        anT = [p.sb([128, 5, 128], BF16, f"anT{i}") for i in range(2)]
        krs = [p.sb([128, 32], F32, f"krs{i}") for i in range(2)]
        Ksb = [p.sb([128, 8, 96], F32, f"Ksb{i}") for i in range(2)]
        vb = [p.sb([128, 8, 64], BF16, f"vb{i}") for i in range(2)]
        scrs = [dict(sq=p.sb([128, 768], F32, "sq"), qk=p.sb([128, 768], F32, "qk"), ss=p.sb([128, 8], F32, "ss"),
                     t1=p.sb([128, 768], F32, "t1"), t2=p.sb([128, 768], F32, "t2")) for _ in range(2)]
        qb_ = [p.sb([128, 768], BF16, f"qb{i}") for i in range(2)]
        kb_ = [p.sb([128, 768], BF16, f"kb{i}") for i in range(2)]
        qTs = [p.sb([96, 8, 128], BF16, f"qTs{i}") for i in range(2)]
        kTs = [p.sb([96, 8, 128], BF16, f"kTs{i}") for i in range(2)]
        for t in range(NTA):
            i2 = t % 2
            var = 1 if (NTL <= t < NTL + 2) else 0
            p.dma(xt[i2], x_in[t * 128:(t + 1) * 128, :])
            hT = hT_all[:, :, t * 128:(t + 1) * 128]
            norm1_hT(p, c, xt[i2], A, SH, var, hT, xnb[i2], junk, ssq[i2], (B[0], B[1]))
            if t >= NTM or dbg == 1:
                continue
            for kc in range(8):
                p.mm(B[2][:, 0:384], hT[:, kc, :], wim[:, kc, 0:384], start=(kc == 0), stop=(kc == 7))
            for kc in range(8):
                p.mm(B[3][:, 0:288], hT[:, kc, :], wim[:, kc, 384:672], start=(kc == 0), stop=(kc == 7))
            sa = ssa[i2]
            p.act(junk[:, 0:384], B[2][:, 0:384], AF.Square, accum=sa[:, 0:1])
            p.act(junk[:, 384:640], B[3][:, 0:256], AF.Square, accum=sa[:, 1:2])
            p.ts(sa[:, 0:1], sa[:, 0:1], 256.0 / 384.0, ALU.mult)
            c.rstd(sa, 2, 1.0 / 256.0, NORM_EPS)
            p.stt(anb[i2][:, 0:384], B[2][:, 0:384], sa[:, 0:1], gqa_bc, ALU.mult, ALU.mult)
            p.stt(anb[i2][:, 384:640], B[3][:, 0:256], sa[:, 1:2], gkva_bc, ALU.mult, ALU.mult)
            p.cp(krs[i2], B[3][:, 256:288], eng="act")
            if dbg == 3:
                continue
            b4 = B[4].bitcast(BF16)
            for k5 in range(5):
                p.tr(b4[:, k5 * 128:(k5 + 1) * 128], anb[i2][:, k5 * 128:(k5 + 1) * 128], c.identb)
            p.cp(anT[i2].rr("p a t -> p (a t)"), b4[:, 0:640], eng="act")
            if dbg == 4:
                continue
            for (bk, c0, c1) in ((B[5], 0, 480), (B[6], 480, 768)):
                for kc in range(3):
                    p.mm(bk[:, 0:c1 - c0], anT[i2][:, kc, :], wq[:, kc, c0:c1], start=(kc == 0), stop=(kc == 2))
            if dbg == 51:
                continue
            for (bk, c0) in ((B[7], 0), (B[2], 512)):
                for kc in range(2):
                    p.mm(bk, anT[i2][:, 3 + kc, :], wkv[:, kc, c0:c0 + 512], start=(kc == 0), stop=(kc == 1))
            if dbg == 52:
                continue
            K = Ksb[i2]
            for (bk, h0) in ((B[7], 0), (B[2], 4)):
                kvv = bk.rr("p (h d) -> p h d", d=128)
                p.cp(K[:, h0:h0 + 4, 0:64], kvv[:, :, 0:64])
                if dbg != 531:
                    p.cp(vb[i2][:, h0:h0 + 4, :], kvv[:, :, 64:128], eng=("dve" if dbg == 532 else "act"))
            if dbg != 533:
                p.cp(K[:, :, 64:96], krs[i2].un(1).bc([128, 8, 32]), eng="pool")
            if dbg in (53, 531, 532, 533):
                continue
            p.dma(v_o[t * 128:(t + 1) * 128, :], vb[i2].rr("p h d -> p (h d)"))
            if dbg == 5:
                continue
            rope = None if var == 1 else (64, 8, COS[:, t], SINS[:, t])
            qk_post(p, c, [(B[5][:, 0:480], 5), (B[6][:, 0:288], 3)], 8, 96, gq_bc.rr("p h d -> p (h d)"),
                    qb_[i2], scrs[0], rope)
            if dbg == 6:
                continue
            qk_post(p, c, [(K.rr("p h d -> p (h d)"), 8)], 8, 96, gk_bc.rr("p h d -> p (h d)"), kb_[i2], scrs[1], rope)
            if dbg == 7:
                continue
            for (src, dstT, bk, out_d, eng) in ((qb_[i2], qTs[i2], B[0], qT_o, "act"), (kb_[i2], kTs[i2], B[1], kT_o, "dve")):
                bb = bk.bitcast(BF16)
                for h in range(8):
                    p.tr(bb[0:96, h * 128:(h + 1) * 128], src[:, h * 96:(h + 1) * 96], c.identb)
                p.cp(dstT.rr("p a t -> p (a t)"), bb[0:96, :], eng=eng)
                p.dma(out_d[:, :, t * 128:(t + 1) * 128].rr("a p t -> p a t"), dstT)

    with p.scope():
        mufm = c.load_fm(mu_d.rr("(c p) -> c p", p=128), 15)
        om = p.sb([128, 15], F32, "om")
        hm = p.sb([128, 15], F32, "hm")
        p.ts(om, mufm, -1.0, ALU.mult, 1.0, ALU.add)
        p.ts(hm, mufm, 0.5, ALU.mult)
        flags = p.sb([128, 2], F32, "flags")
        p.dma(flags, flags_d)
        W = TL + CTX + 4
        E = [p.sb([128, W], F32, f"E{i}") for i in range(2)]
        for i in range(2):
            p.memset(E[i][:, TL + 2:TL + 3], 0.0)
            p.memset(E[i][:, W - 1:W], 0.0)
        hal = [p.sb([128, 2], F32, f"hal{i}") for i in range(2)]
        sm = [p.sb([128, TL + CTX], F32, f"sm{i}") for i in range(2)]
        tm = [p.sb([128, TL + CTX], F32, f"tm{i}") for i in range(2)]
        wc = [p.sb([128, 8, 128], BF16, f"wc{i}") for i in range(3)]
        blks = []
        t0 = 0
        while t0 < TL:
            n = min(512, TL - t0)
            blks.append((t0, n, 1 + t0))
            t0 += n
        blks.append((TL, CTX, TL + 3))
        ib = 0
        for cc in range(15 if dbg < 2 else 0):
            w = wc[cc % 3]
            e = E[cc % 2]
            p.dma(w, w_in[:, cc * 128:(cc + 1) * 128].rr("(kc p) n -> p kc n", p=128), q="pool")
            for (t0, n, e0) in blks:
                bk = B[ib % 6]
                ib += 1
                for kc in range(8):
                    p.mm(bk[:, 0:n], w[:, kc, :], hT_all[:, kc, t0:t0 + n], start=(kc == 0), stop=(kc == 7))
                p.cp(e[:, e0:e0 + n], bk[:, 0:n], eng=("act" if ib % 2 else "dve"))
            bk = B[6 + cc % 2]
            hoff = (NTL + 2) * 128
            for kc in range(8):
                p.mm(bk[:, 0:2], w[:, kc, :], hT_all[:, kc, hoff:hoff + 2], start=(kc == 0), stop=(kc == 7))
            p.tt(hal[cc % 2], bk[:, 0:2], flags, ALU.mult)
            p.cp(e[:, 0:1], hal[cc % 2][:, 0:1], eng="pool")
            p.cp(e[:, TL + 1:TL + 2], hal[cc % 2][:, 1:2], eng="pool")
            s_, t_ = sm[cc % 2], tm[cc % 2]
            for (o0, n, e0) in ((0, TL, 1), (TL, CTX, TL + 3)):
                p.tt(s_[:, o0:o0 + n], e[:, e0 - 1:e0 - 1 + n], e[:, e0 + 1:e0 + 1 + n], ALU.add, eng="pool")
                p.ts(t_[:, o0:o0 + n], e[:, e0:e0 + n], om[:, cc:cc + 1], ALU.mult)
                p.stt(s_[:, o0:o0 + n], s_[:, o0:o0 + n], hm[:, cc:cc + 1], t_[:, o0:o0 + n], ALU.mult, ALU.add)
            p.dma(rwT[cc], s_)
    return p


LN_X_EPS = 64e-5
LAM = math.exp(-0.5)


def build_mix0(S, do_attn=True, RWP=BF16):
    LT = CTX + S
    NPAIR = LT // 128
    p = Prog()
    rw_r = p.dram("rw_r", [128, LT], F32, "ExternalInput")
    rw_k = p.dram("rw_k", [128, LT], F32, "ExternalInput")
    rw_v = p.dram("rw_v", [128, LT], F32, "ExternalInput")
    lo_w = p.dram("lo_w", [128, LT], F32, "ExternalInput")
    lo_a = p.dram("lo_a", [128, LT], F32, "ExternalInput")
    lo_g = p.dram("lo_g", [128, LT], F32, "ExternalInput")
    w2_d = p.dram("w2", [128, 128], F32, "ExternalInput")
    a2_d = p.dram("a2", [128, 128], F32, "ExternalInput")
    g2_d = p.dram("g2", [128, 128], F32, "ExternalInput")
    pv_d = p.dram("pv", [9, 128], F32, "ExternalInput")
    yT = p.dram("yT", [2, 128, LT], F32, "ExternalOutput")
    rwoT = p.dram("rwoT", [128, LT], BF16, "ExternalOutput")
    if do_attn:
        qT = p.dram("qT", [2, 96, S], BF16, "ExternalInput")
        qcT = p.dram("qcT", [2, 96, CTX], BF16, "ExternalInput")
        kT = p.dram("kT", [2, 96, LT], BF16, "ExternalInput")
        v_d = p.dram("v", [2, LT, 64], BF16, "ExternalInput")
        oT = p.dram("oT", [2, 64, S], BF16, "ExternalOutput")
        ocT = p.dram("ocT", [2, 64, CTX], BF16, "ExternalOutput")
    c = Ctx(p, banks=False)
    _outer = p.scope()
    _outer.__enter__()
    c.set_banks(False)
    B = c.banks
    pv = c.load_fm(pv_d, 9)
    W0 = [pv[:, 0:1], pv[:, 1:2]]
    A0 = [pv[:, 2:3], pv[:, 3:4]]
    KK_, KA_, RK_, LNW, LNB = pv[:, 4:5], pv[:, 5:6], pv[:, 6:7], pv[:, 7:8], pv[:, 8:9]
    omka = p.sb([128, 2], F32, "omka")
    p.ts(omka[:, 0:1], KA_, -1.0, ALU.mult, 1.0, ALU.add)
    p.ts(omka[:, 1:2], KA_, -2.0, ALU.mult, 2.0, ALU.add)
    w2s = p.sb([128, 128], F32, "w2s")
    a2s = p.sb([128, 128], F32, "a2s")
    g2s = p.sb([128, 128], F32, "g2s")
    p.dma(w2s, w2_d)
    p.dma(a2s, a2_d)
    p.dma(g2s, g2_d)
    w2z, a2z = [], []
    for d in range(2):
        for (src_, lst_) in ((w2s, w2z), (a2s, a2z)):
            z = p.sb([128, 128], F32, "loraz")
            p.memset(z, 0.0)
            dp_ = slice(64 * d, 64 * d + 64)
            p.cp(z[dp_, :], src_[dp_, :], eng="pool")
            lst_.append(z)
    p64 = p.sb([128, 1], F32, "p64")
    p.ts(p64, c.pidx, 63.5, ALU.is_gt)
    c64 = p.sb([128, 128], F32, "c64")
    p.ts(c64, c.iof, 63.5, ALU.is_gt)
    same = p.sb([128, 128], F32, "same")
    p.ts(same, c64, p64[:, 0:1], ALU.is_equal)
    masks = {}
    for nm, op_ in (("SU", ALU.is_gt), ("SL", ALU.is_lt), ("IU", ALU.is_ge), ("IL", ALU.is_le)):
        m = p.sb([128, 2, 128], F32, "mask" + nm)
        p.ts(m[:, 0, :], c.iof, c.pidx[:, 0:1], op_)
        p.tt(m[:, 0, :], m[:, 0, :], same, ALU.mult)
        p.cp(m[:, 1, :], m[:, 0, :])
        masks[nm] = m.rr("p a t -> p (a t)")
    ones64 = p.sb([128, 128], F32, "ones64")
    p.ts(ones64, same, 1.0 / 64.0, ALU.mult)
    ident2 = p.sb([128, 2, 128], F32, "ident2")
    p.cp(ident2[:, 0, :], c.identf)
    p.cp(ident2[:, 1, :], c.identf)
    ident2 = ident2.rr("p a t -> p (a t)")
    rmask = p.sb([128, 512], F32, "rmask")
    p.memset(rmask, 1.0)
    p.memset(rmask.rr("p (a t) -> p a t", t=64)[:, :, 0:1], 0.0)

    blocks = [(0, CTX)]
    t0 = CTX
    while t0 < LT:
        blocks.append((t0, 512))
        t0 += 512
    NB = len(blocks)
    order = [list(range(NB)), [0] + list(range(NB - 1, 0, -1))]

    with p.scope():
        def T(shape, nm, dt=F32):
            return p.sb(shape, dt, nm)
        ops_ = []
        for d in range(2):
            two = []
            for i in range(2):
                two.append(dict(Rt=T([128, 512], "Rt"), Rt16=T([128, 512], "Rt16", RWP), Kt=T([128, 512], "Kt", RWP),
                                Bt=T([128, 512], "Bt", RWP), At=T([128, 512], "At", RWP),
                                A_tm=T([128, 4, 128], "A_tm", RWP), K_tm=T([128, 4, 128], "K_tm", RWP), B_tm=T([128, 4, 128], "B_tm", RWP),
                                V_tm=T([128, 4, 128], "V_tm", RWP), etot=T([128, 8], "etot"), yb=T([128, 512], "yb")))
            ops_.append(two)
        tmp = {k: T([128, 512], k) for k in ("r", "k", "v", "lw", "la", "th", "sg", "a", "kk", "t1", "t2", "c", "E", "kd", "b", "Kh", "Bh")}
        tot = T([128, 8], "tot")
        Hbd = [[T([128, 128], f"H{d}{i}") for i in range(2)] for d in range(2)]
        H16 = [[T([128, 128], f"H16{d}{i}", RWP) for i in range(2)] for d in range(2)]
        for d in range(2):
            for i in range(2):
                p.memset(Hbd[d][i], 0.0)
                p.memset(H16[d][i], 0.0)
        hcur = [0, 0]
        pairbuf = []
        for d in range(2):
            pairbuf.append({k: T([128, 256], f"{k}{d}", RWP) for k in ("X", "XT", "X2", "XT2", "P", "P2", "MAK")}
                           | {k: T([128, 256], f"{k}{d}", RWP) for k in ("MRK", "MRB", "WT")}
                           | {"AT": T([128, 128], f"AT{d}", RWP), "U": T([128, 128], f"U{d}", RWP)})
            p.memset(pairbuf[d]["U"], 0.0)

        def prep(d, bi, ob):
            t0, n = blocks[bi]
            tok = slice(t0, t0 + n)
            nch = n // 64
            dp = slice(64 * d, 64 * d + 64)
            x = tmp
            p.dma(x["r"][:, 0:n], rw_r[:, tok])
            p.dma(x["k"][:, 0:n], rw_k[:, tok])
            p.dma(x["v"][:, 0:n], rw_v[:, tok])
            p.dma(x["lw"][:, 0:n], lo_w[:, tok])
            p.dma(x["la"][:, 0:n], lo_a[:, tok])
            N = slice(0, n)
            p.act(x["th"][:, N], x["lw"][:, N], AF.Tanh)
            p.mm(B[6][:, N], w2z[d], x["th"][:, N])
            p.act(x["sg"][:, N], B[6][:, N], AF.Sigmoid, bias=W0[d])
            p.mm(B[4][:, N], a2z[d], x["la"][:, N])
            p.act(x["a"][:, N], B[4][:, N], AF.Sigmoid, bias=A0[d])
            p.ts(x["kk"][:, N], x["k"][:, N], KK_, ALU.mult)
            p.tt(x["t1"][:, N], x["kk"][:, N], x["kk"][:, N], ALU.mult, eng="pool")
            p.ts(x["t1"][:, N], x["t1"][:, N], 64.0, ALU.mult, eng="pool")
            p.mm(B[6][:, N], ones64, x["t1"][:, N])
            p.ts(x["t2"][:, N], B[6][:, N], 1e-12, ALU.add)
            p.act(x["t2"][:, N], x["t2"][:, N], AF.Ln)
            p.act(x["t2"][:, N], x["t2"][:, N], AF.Exp, scale=-0.5)
            p.tt(x["kk"][:, N], x["kk"][:, N], x["t2"][:, N], ALU.mult)
            p.ts(x["t1"][:, N], x["a"][:, N], KA_, ALU.mult, omka[:, 0:1], ALU.add)
            p.tt(x["kd"][:, N], x["k"][:, N], x["t1"][:, N], ALU.mult, eng="pool")
            p.tt(x["b"][:, N], x["kk"][:, N], x["a"][:, N], ALU.mult, eng="pool")
            p.op("dve", lambda e: e.tensor_tensor_scan(x["c"].ap[:, N], rmask.ap[:, N], x["sg"].ap[:, N], 0.0,
                                                       ALU.mult, ALU.add), [rmask, x["sg"]], [x["c"]])
            cv = x["c"][:, N].rr("p (a t) -> p a t", t=64)
            p.cp(tot[:, 0:nch], cv[:, :, 63])
            if d == 1:
                p.tt(x["c"][:, N], x["sg"][:, N], x["c"][:, N], ALU.subtract)
                p.tt(cv, cv, tot[:, 0:nch].un(2).bc([128, nch, 64]), ALU.add)
            p.act(ob["etot"][:, 0:nch], tot[:, 0:nch], AF.Exp, scale=-LAM)
            p.act(x["E"][:, N], x["c"][:, N], AF.Exp, scale=-LAM)
            p.tt(ob["Rt"][:, N], x["r"][:, N], x["E"][:, N], ALU.mult)
            p.cp(ob["Rt16"][:, N], ob["Rt"][:, N], eng="pool")
            p.act(x["E"][:, N], x["c"][:, N], AF.Exp, scale=LAM)
            p.tt(ob["Kt"][:, N], x["kd"][:, N], x["E"][:, N], ALU.mult)
            p.tt(ob["Bt"][:, N], x["b"][:, N], x["E"][:, N], ALU.mult, eng="pool")
            p.tt(x["t1"][:, N], x["c"][:, N], x["sg"][:, N], ALU.subtract)
            p.act(x["E"][:, N], x["t1"][:, N], AF.Exp, scale=-LAM)
            p.stt(ob["At"][:, N], x["kk"][:, N], -1.0, x["E"][:, N], ALU.mult, ALU.mult)
            p.tt(x["t2"][:, N].rr("p (a t) -> p a t", t=64), tot[:, 0:nch].un(2).bc([128, nch, 64]), cv, ALU.subtract)
            p.act(x["E"][:, N], x["t2"][:, N], AF.Exp, scale=-LAM)
            p.tt(x["Kh"][:, N], x["kd"][:, N], x["E"][:, N], ALU.mult)
            p.tt(x["Bh"][:, N], x["b"][:, N], x["E"][:, N], ALU.mult, eng="pool")
            npair = n // 128
            for (src, dst, bk, eng) in ((ob["At"], ob["A_tm"], B[4], "act"), (x["Kh"], ob["K_tm"], B[6], "dve"),
                                        (x["Bh"], ob["B_tm"], B[4], "act"), (x["v"], ob["V_tm"], B[6], "dve")):
                is16 = (src is ob["At"]) and RWP == BF16
                bkv = bk.bitcast(BF16) if is16 else bk
                for pr in range(npair):
                    p.tr(bkv[:, pr * 128:(pr + 1) * 128], src[:, pr * 128:(pr + 1) * 128], c.identb if is16 else c.identf)
                p.cp(dst.rr("p a t -> p (a t)")[:, 0:n], bkv[:, 0:n], eng=eng)

        def pair_level(d, ob, pr):
            pb = pairbuf[d]
            Tk = slice(pr * 128, (pr + 1) * 128)
            mN, mM, mI = (("SU", "SL", "IU") if d == 0 else ("SL", "SU", "IL"))
            bk0, bk1 = B[2], B[3]
            HB = ((B[0], B[2]), (B[1], B[3]))

            def prod(slot, lhs, rhs):
                for h in range(2):
                    hp = slice(64 * h, 64 * h + 64)
                    p.mm(HB[h][slot][:, 0:128], ob[lhs][hp, Tk], ob[rhs][hp, Tk])

            def evac(slot, dst, mk):
                for h in range(2):
                    hc = slice(128 * h, 128 * h + 128)
                    p.tt(pb[dst][:, hc], HB[h][slot][:, 0:128], masks[mk][:, 0:128], ALU.mult)

            prod(0, "Bt", "At")
            prod(1, "At", "Bt")
            evac(0, "X", mN)
            evac(1, "XT", mM)
            prod(0, "At", "Kt")
            prod(1, "Kt", "Rt16")
            evac(0, "MAK", mM)
            evac(1, "MRK", mI)
            prod(0, "Bt", "Rt16")
            evac(0, "MRB", mI)
            p.tt(pb["P"], pb["X"], ident2, ALU.add, eng="pool")
            X, XT, Pc = pb["X"], pb["XT"], pb["P"]
            X2, XT2, P2 = pb["X2"], pb["XT2"], pb["P2"]
            for k in range(1, 6):
                for h in range(2):
                    hc = slice(128 * h, 128 * h + 128)
                    p.mm(bk1[:, hc], X[:, hc], XT[:, hc])
                    if k < 5:
                        p.mm(bk0[:, hc], XT[:, hc], X[:, hc])
                p.cp(XT2, bk1[:, 0:256], eng="act")
                if k < 5:
                    p.cp(X2, bk0[:, 0:256])
                for h in range(2):
                    hc = slice(128 * h, 128 * h + 128)
                    p.mm(bk0[:, hc], XT2[:, hc], Pc[:, hc])
                p.tt(P2, bk0[:, 0:256], Pc, ALU.add)
                X, X2 = X2, X
                XT, XT2 = XT2, XT
                Pc, P2 = P2, Pc
            for h in range(2):
                hc = slice(128 * h, 128 * h + 128)
                p.mm(bk1[:, hc], ob["A_tm"][:, pr, :], Pc[:, hc])
                p.mm(bk0[:, hc], pb["MAK"][:, hc], Pc[:, hc])
            p.cp(pb["AT"][0:64, :], bk1[0:64, 0:128], eng="act")
            p.cp(pb["AT"][64:128, :], bk1[64:128, 128:256], eng="act")
            p.cp(pb["WT"], bk0[:, 0:256])

        def chunk_level(d, ob, pr, pi):
            pb = pairbuf[d]
            cp_ = slice(64 * pi, 64 * pi + 64)
            ch = pr * 2 + pi
            tcol = slice(pr * 128 + 64 * pi, pr * 128 + 64 * pi + 64)
            Hold = Hbd[d][hcur[d]]
            Hnew = Hbd[d][1 - hcur[d]]
            Hold16 = H16[d][hcur[d]]
            Hnew16 = H16[d][1 - hcur[d]]
            hcur[d] = 1 - hcur[d]
            bu, bh, by = B[4], (B[5] if d == 0 else B[7]), B[6]
            for h in range(2):
                hc = slice(128 * h, 128 * h + 128)
                ic = slice(64 * h, 64 * h + 64)
                p.mm(bu[:, ic], pb["WT"][:, hc], ob["V_tm"][:, pr, ic], start=True, stop=False)
                p.mm(bu[:, ic], pb["AT"], Hold16[:, ic], start=False, stop=True)
            p.cp(pb["U"][cp_, :], bu[cp_, 0:128], eng="act")
            for h in range(2):
                ic = slice(64 * h, 64 * h + 64)
                p.mm(bh[:, ic], ob["K_tm"][cp_, pr, :], ob["V_tm"][cp_, pr, ic], start=True, stop=False)
                p.mm(bh[:, ic], ob["B_tm"][cp_, pr, :], pb["U"][cp_, ic], start=False, stop=True)
            for h in range(2):
                hp = slice(64 * h, 64 * h + 64)
                ic = slice(64 * h, 64 * h + 64)
                p.stt(Hnew[hp, ic], Hold[hp, ic], ob["etot"][hp, ch:ch + 1], bh[hp, ic], ALU.mult, ALU.add)
            p.cp(Hnew16, Hnew, eng="pool")
            for h in range(2):
                ys = slice(64 * h, 64 * h + 64)
                mcol = slice(128 * h + 64 * pi, 128 * h + 64 * pi + 64)
                p.mm(by[:, ys], Hold16, ob["Rt16"][:, tcol], start=True, stop=False)
                p.mm(by[:, ys], ob["V_tm"][:, pr, :], pb["MRK"][:, mcol], start=False, stop=False)
                p.mm(by[:, ys], pb["U"], pb["MRB"][:, mcol], start=False, stop=True)
            p.cp(ob["yb"][0:64, tcol], by[0:64, 0:64], eng="act")
            p.cp(ob["yb"][64:128, tcol], by[64:128, 64:128], eng="act")

        for step in range(NB):
            for d in range(2):
                bi = order[d][step]
                t0, n = blocks[bi]
                ob = ops_[d][step % 2]
                prep(d, bi, ob)
                npair = n // 128
                prs = range(npair) if d == 0 else range(npair - 1, -1, -1)
                for pr in prs:
                    pair_level(d, ob, pr)
                    for pi in ((0, 1) if d == 0 else (1, 0)):
                        chunk_level(d, ob, pr, pi)
                p.dma(yT[d][:, t0:t0 + n], ob["yb"][:, 0:n])

    with p.scope():
        x = {k: p.sb([128, 512], F32, k) for k in ("y0", "y1", "r", "k", "v", "la", "lg", "a0", "a1", "t1", "t2", "t3", "g")}
        ob_ = [p.sb([128, 512], BF16, f"rwo{i}") for i in range(2)]
        for bi, (t0, n) in enumerate(blocks):
            tok = slice(t0, t0 + n)
            N = slice(0, n)
            p.dma(x["y0"][:, N], yT[0][:, tok])
            p.dma(x["y1"][:, N], yT[1][:, tok])
            p.dma(x["r"][:, N], rw_r[:, tok])
            p.dma(x["k"][:, N], rw_k[:, tok])
            p.dma(x["v"][:, N], rw_v[:, tok])
            p.dma(x["la"][:, N], lo_a[:, tok])
            p.dma(x["lg"][:, N], lo_g[:, tok])
            p.tt(x["y0"][:, N], x["y0"][:, N], x["y1"][:, N], ALU.add)
            p.mm(B[0][:, N], ones64, x["y0"][:, N])
            p.tt(x["y0"][:, N], x["y0"][:, N], B[0][:, N], ALU.subtract)
            p.tt(x["t1"][:, N], x["y0"][:, N], x["y0"][:, N], ALU.mult, eng="pool")
            p.mm(B[1][:, N], ones64, x["t1"][:, N])
            p.ts(x["t1"][:, N], B[1][:, N], LN_X_EPS, ALU.add)
            p.act(x["t1"][:, N], x["t1"][:, N], AF.Ln)
            p.act(x["t1"][:, N], x["t1"][:, N], AF.Exp, scale=-0.5)
            p.tt(x["y0"][:, N], x["y0"][:, N], x["t1"][:, N], ALU.mult)
            p.ts(x["y0"][:, N], x["y0"][:, N], LNW, ALU.mult, LNB, ALU.add)
            for d in range(2):
                p.mm(B[2 + d][:, N], a2z[d], x["la"][:, N])
                p.act(x["a%d" % d][:, N], B[2 + d][:, N], AF.Sigmoid, bias=A0[d])
            p.tt(x["a0"][:, N], x["a0"][:, N], x["a1"][:, N], ALU.add, eng="pool")
            p.ts(x["a0"][:, N], x["a0"][:, N], KA_, ALU.mult, omka[:, 1:2], ALU.add)
            p.tt(x["t2"][:, N], x["k"][:, N], x["a0"][:, N], ALU.mult, eng="pool")
            p.stt(x["t2"][:, N], x["t2"][:, N], RK_, x["r"][:, N], ALU.mult, ALU.mult)
            p.ts(x["t2"][:, N], x["t2"][:, N], 64.0, ALU.mult, eng="pool")
            p.mm(B[4][:, N], ones64, x["t2"][:, N])
            p.tt(x["t3"][:, N], B[4][:, N], x["v"][:, N], ALU.mult)
            p.tt(x["y0"][:, N], x["y0"][:, N], x["t3"][:, N], ALU.add, eng="pool")
            p.act(x["lg"][:, N], x["lg"][:, N], AF.Sigmoid)
            p.mm(B[5][:, N], g2s, x["lg"][:, N])
            o = ob_[bi % 2]
            p.tt(o[:, N], x["y0"][:, N], B[5][:, N], ALU.mult)
            p.dma(rwoT[:, tok], o[:, N])

    _outer.__exit__(None, None, None)
    if do_attn:
        with p.scope():
            c.set_banks(True)
            attention_block(p, c, qT, kT, v_d, oT, 2, 2, 96, 64, S, 96 ** -0.5, qcT_d=qcT, ocT_d=ocT)
    return p


def _make_pos(q, TL):
    NTL = TL // 128
    t = q * TL + np.arange(TL)
    pos = np.stack([t // 64, t % 64], -1).astype(np.float32)
    return np.ascontiguousarray(pos.reshape(NTL, 128, 2).transpose(1, 0, 2))


def _cat(xs, axis):
    return np.ascontiguousarray(np.concatenate(xs, axis=axis))


def kernel(x, c, ctx, c_ctx, ada_w, ada_b, norm1_g, norm2_g, ab_w_in, ab_w_out, rw_mu, rw_w0,
           rw_w2, rw_a0, rw_a2, rw_k_k, rw_k_a, rw_r_k, rw_g2, rw_ln_w, rw_ln_b, mla_g_qa,
           mla_w_q_up, mla_g_kva, mla_w_kv_up, mla_g_q, mla_g_k, gqa_w_in, gqa_w_out, gqa_g_q,
           gqa_g_k, router_w, router_b, moe_w_gate, moe_w_up, moe_w_down, shared_w_gate,
           shared_w_up, shared_w_down):
    f = lambda a: np.ascontiguousarray(np.asarray(a, dtype=np.float32))
    x, c, ctx, c_ctx = f(x), f(c), f(ctx), f(c_ctx)
    S = x.shape[1]
    TL = S // 4
    LT = CTX + S
    R8 = range(8)
    maps = [{"cvec": np.stack([c[r // 4], c_ctx]), "ada_w": f(ada_w), "ada_b": f(ada_b)} for r in R8]
    r0 = run_prog(build_phase0(), maps)
    mods_tm = [f(r0[r]["mods_tm"]) for r in R8]
    mods_fm = [f(r0[r]["mods_fm"]) for r in R8]
    maps = []
    for r in R8:
        b, q = r // 4, r % 4
        halo = np.zeros((128, D), np.float32)
        fl = np.zeros((128, 2), np.float32)
        if q > 0:
            halo[0] = x[b, q * TL - 1]
            fl[:, 0] = 1
        if q < 3:
            halo[1] = x[b, (q + 1) * TL]
            fl[:, 1] = 1
        maps.append({"x": _cat([x[b, q * TL:(q + 1) * TL], ctx[b], halo], 0), "mods_fm": mods_fm[r][0],
                     "norm1_g": f(norm1_g)[0], "w_in": f(ab_w_in)[0], "mu": f(rw_mu)[0], "flags": fl,
                     "g_qa": f(mla_g_qa), "g_kva": f(mla_g_kva), "w_q_up": f(mla_w_q_up)[0],
                     "w_kv_up": f(mla_w_kv_up)[0], "g_q": f(mla_g_q), "g_k": f(mla_g_k), "pos": _make_pos(q, TL)})
    rA = run_prog(build_pre0(S), maps)
    maps = []
    rw_r_k_flat = f(rw_r_k)[0].reshape(512)
    for r in R8:
        b, j = r // 4, r % 4
        cores = [4 * b + qq for qq in range(4)]
        rwT = _cat([rA[4 * b]["rwT"][:, :, TL:]] + [rA[cc]["rwT"][:, :, :TL] for cc in cores], 2)
        cs = slice(128 * j, 128 * j + 128)
        pv = np.stack([f(rw_w0)[0, 0, cs], f(rw_w0)[0, 1, cs], f(rw_a0)[0, 0, cs], f(rw_a0)[0, 1, cs],
                       f(rw_k_k)[0, cs], f(rw_k_a)[0, cs], rw_r_k_flat[cs], f(rw_ln_w)[0, cs], f(rw_ln_b)[0, cs]])
        hs = slice(2 * j, 2 * j + 2)
        qT = _cat([rA[cc]["qT"][hs, :, :TL] for cc in cores], 2)
        qcT = np.ascontiguousarray(rA[4 * b]["qT"][hs, :, TL:])
        kT = _cat([rA[4 * b]["kT"][hs, :, TL:]] + [rA[cc]["kT"][hs, :, :TL] for cc in cores], 2)
        vv = _cat([rA[4 * b]["v"][TL:]] + [rA[cc]["v"][:TL] for cc in cores], 0)
        vv = np.ascontiguousarray(vv.reshape(LT, 8, 64)[:, hs].transpose(1, 0, 2))
        maps.append({"rw_r": np.ascontiguousarray(rwT[j]), "rw_k": np.ascontiguousarray(rwT[4 + j]),
                     "rw_v": np.ascontiguousarray(rwT[8 + j]), "lo_w": np.ascontiguousarray(rwT[12]),
                     "lo_a": np.ascontiguousarray(rwT[13]), "lo_g": np.ascontiguousarray(rwT[14]),
                     "w2": np.ascontiguousarray(f(rw_w2)[0][:, :, cs].reshape(128, 128)),
                     "a2": np.ascontiguousarray(f(rw_a2)[0][:, :, cs].reshape(128, 128)),
                     "g2": np.ascontiguousarray(f(rw_g2)[0][:, cs]), "pv": np.ascontiguousarray(pv),
                     "qT": qT, "qcT": qcT, "kT": kT, "v": vv})
    rB = run_prog(build_mix0(S), maps)
    del rA
    wg0 = _cat([f(moe_w_gate)[0], f(shared_w_gate)[0][None]], 0)
    wu0 = _cat([f(moe_w_up)[0], f(shared_w_up)[0][None]], 0)
    wd0 = _cat([f(moe_w_down)[0], f(shared_w_down)[0][None]], 0)
    maps = []
    for r in R8:
        b, q = r // 4, r % 4
        chunks = []
        for kc in range(4):
            rw = rB[4 * b + kc]["rwoT"]
            chunks.append(_cat([rw[:, CTX + q * TL:CTX + (q + 1) * TL], rw[:, :CTX]], 1))
        for j in range(4):
            o = rB[4 * b + j]["oT"].reshape(128, S)
            oc = rB[4 * b + j]["ocT"].reshape(128, CTX)
            chunks.append(_cat([o[:, q * TL:(q + 1) * TL], oc], 1))
        maps.append({"x": _cat([x[b, q * TL:(q + 1) * TL], ctx[b]], 0), "attT": np.ascontiguousarray(np.stack(chunks, 0)),
                     "mods_tm": mods_tm[r][0], "mods_fm": mods_fm[r][0], "w_out": f(ab_w_out)[0],
                     "norm2_g": f(norm2_g)[0], "router_w": f(router_w), "router_b": f(router_b)[None, :],
                     "wg_all": wg0, "wu_all": wu0, "wd_all": wd0})
    rC = run_prog(build_post(S, True), maps)
    del rB
    x1 = [f(rC[r]["x_out"]) for r in R8]
    maps = []
    for r in R8:
        q = r % 4
        maps.append({"x": x1[r], "mods_fm": mods_fm[r][1], "norm1_g": f(norm1_g)[1], "w_in": f(gqa_w_in)[0],
                     "g_q": f(gqa_g_q), "g_k": f(gqa_g_k), "pos": _make_pos(q, TL)})
    rD = run_prog(build_pre1(S), maps)
    maps = []
    for r in R8:
        b, kvh = r // 4, r % 4
        cores = [4 * b + qq for qq in range(4)]
        qk = _cat([rD[cc]["qkT"][:, :, :TL] for cc in cores], 2).reshape(20, 64, S)
        kc_ = rD[4 * b]["qkT"][:, :, TL:].reshape(20, 64, CTX)
        qT = np.ascontiguousarray(qk[4 * kvh:4 * kvh + 4])
        kT = _cat([kc_[16 + kvh], qk[16 + kvh]], 1)[None]
        vv = _cat([rD[4 * b]["v"][TL:]] + [rD[cc]["v"][:TL] for cc in cores], 0)
        vv = np.ascontiguousarray(vv[:, kvh * 64:(kvh + 1) * 64])[None]
        maps.append({"qT": qT, "kT": np.ascontiguousarray(kT), "v": vv})
    rE = run_prog(build_attn1(S), maps)
    del rD
    wg1 = _cat([f(moe_w_gate)[1], f(shared_w_gate)[1][None]], 0)
    wu1 = _cat([f(moe_w_up)[1], f(shared_w_up)[1][None]], 0)
    wd1 = _cat([f(moe_w_down)[1], f(shared_w_down)[1][None]], 0)
    maps = []
    for r in R8:
        b, q = r // 4, r % 4
        chunks = []
        for kc in range(8):
            o = rE[4 * b + kc // 2]["oT"]
            i0 = (kc % 2) * 2
            chunks.append(o[i0:i0 + 2].reshape(128, S)[:, q * TL:(q + 1) * TL])
        maps.append({"x": np.ascontiguousarray(x1[r][:TL]), "attT": np.ascontiguousarray(np.stack(chunks, 0)),
                     "mods_tm": mods_tm[r][1], "mods_fm": mods_fm[r][1], "w_out": f(gqa_w_out)[0],
                     "norm2_g": f(norm2_g)[1], "router_w": f(router_w), "router_b": f(router_b)[None, :],
                     "wg_all": wg1, "wu_all": wu1, "wd_all": wd1})
    rF = run_prog(build_post(S, False), maps)
    out = np.zeros((2, S, D), np.float32)
    for r in R8:
        b, q = r // 4, r % 4
        out[b, q * TL:(q + 1) * TL] = rF[r]["x_out"]
    return out
```

```python
import numpy as np
import concourse.bass as bass
import concourse.mybir as mybir
from concourse.bass_utils import run_bass_kernel_spmd

F32 = mybir.dt.float32
BF16 = mybir.dt.bfloat16
I32 = mybir.dt.int32
AF = mybir.ActivationFunctionType
ALU = mybir.AluOpType
AX = mybir.AxisListType

EPOCH = 30000
SCHED_WINDOW = 40
NSLOT = {"sp": 24, "pool": 12, "act": 8}


class Buf:
    __slots__ = ("name", "lw", "rd", "excl")

    def __init__(self, name, excl=False):
        self.name = name
        self.lw = None
        self.rd = []
        self.excl = excl


class V:
    __slots__ = ("buf", "ap")

    def __init__(self, buf, ap):
        self.buf = buf
        self.ap = ap

    def __getitem__(self, idx):
        return V(self.buf, self.ap[idx])

    def rr(self, pat, **kw):
        return V(self.buf, self.ap.rearrange(pat, **kw))

    def bc(self, shape):
        return V(self.buf, self.ap.broadcast_to(shape))

    def un(self, axis):
        return V(self.buf, self.ap.unsqueeze(axis))

    def bitcast(self, dt):
        return V(self.buf, self.ap.bitcast(dt))

    @property
    def shape(self):
        return self.ap.shape


class Op:
    __slots__ = ("eng", "fn", "deps", "sig", "cnt", "isdma", "slot", "slotval", "prevval", "cost")

    def __init__(self, eng, fn, isdma):
        self.cost = 300.0
        self.eng = eng
        self.fn = fn
        self.deps = set()
        self.sig = False
        self.cnt = 0
        self.isdma = isdma
        self.slot = None
        self.slotval = 0
        self.prevval = 0


class _Scope:
    def __init__(self, p):
        self.p = p

    def __enter__(self):
        self.p._scopes.append([])
        return self

    def __exit__(self, *a):
        p = self.p
        items = p._scopes.pop()
        ops = set(p._fence)
        for cm, b in items:
            if b.lw is not None:
                ops.add(b.lw)
            ops.update(b.rd)
        best = {}
        keep = []
        for i in ops:
            o = p.ops[i]
            if o.isdma:
                if o.slot not in best or best[o.slot] < i:
                    best[o.slot] = i
            else:
                if o.eng not in best or best[o.eng] < i:
                    best[o.eng] = i
        p._fence = keep + list(best.values())
        for cm, b in reversed(items):
            cm.__exit__(None, None, None)
        return False


class Prog:
    ENGS = ("pe", "act", "dve", "pool", "sp")

    def __init__(self):
        self.nc = bass.Bass("TRN2", target_bir_lowering=False)
        self.ops = []
        self._ctx = []
        self.ntile = 0
        self._scopes = []
        self._fence = []
        self._dcount = {q: 0 for q in NSLOT}

    def dram(self, name, shape, dt, kind):
        t = self.nc.dram_tensor(name, list(shape), dt, kind=kind)
        return V(Buf(name), t.ap())

    def sb(self, shape, dt, name=None):
        self.ntile += 1
        name = name or f"t{self.ntile}"
        cm = self.nc.sbuf_tensor(f"{name}_{self.ntile}", list(shape), dt)
        h = cm.__enter__()
        b = Buf(name)
        b.rd = list(self._fence)
        if self._scopes:
            self._scopes[-1].append((cm, b))
        else:
            self._ctx.append(cm)
        return V(b, h[:])

    def scope(self):
        return _Scope(self)

    def ps(self, shape, dt, name=None):
        self.ntile += 1
        name = name or f"p{self.ntile}"
        cm = self.nc.psum_tensor(f"{name}_{self.ntile}", list(shape), dt)
        h = cm.__enter__()
        b = Buf(name, excl=True)
        b.rd = list(self._fence)
        if self._scopes:
            self._scopes[-1].append((cm, b))
        else:
            self._ctx.append(cm)
        return V(b, h[:])

    def op(self, eng, fn, reads, writes, isdma=False, cost=None):
        i = len(self.ops)
        o = Op(eng, fn, isdma)
        if cost is None:
            w0 = next((v for v in writes if v is not None), None)
            n = 1
            if w0 is not None:
                for d_ in w0.ap.shape[1:]:
                    n *= d_
            if isdma:
                cost = 2000.0 + n * 128 * 4 / 150.0
            elif eng == "act":
                cost = 230.0 + n / 1.2
            elif eng == "dve":
                cost = 70.0 + n / 0.96
            elif eng == "pool":
                cost = 150.0 + n / 0.96
            else:
                cost = 300.0
        o.cost = cost
        rb = {id(v.buf): v.buf for v in reads if v is not None}
        wb = {id(v.buf): v.buf for v in writes if v is not None}
        for b in rb.values():
            if b.lw is not None:
                o.deps.add(b.lw)
            if b.excl:
                for r in b.rd:
                    if self.ops[r].eng != eng:
                        o.deps.add(r)
        for b in wb.values():
            if b.lw is not None:
                o.deps.add(b.lw)
            for r in b.rd:
                o.deps.add(r)
        o.deps.discard(i)
        for b in rb.values():
            if id(b) not in wb:
                b.rd.append(i)
        for b in wb.values():
            b.lw = i
            b.rd = []
        if isdma:
            k = self._dcount[eng]
            self._dcount[eng] += 1
            n = NSLOT[eng]
            o.slot = (eng, k % n)
            o.slotval = 16 * (k // n + 1)
            o.prevval = 16 * (k // n)
        self.ops.append(o)
        return i

    def dma(self, out, in_, q="sp", **kw):
        self.op(q, lambda e: e.dma_start(out=out.ap, in_=in_.ap, **kw), [in_], [out], isdma=True)

    def mm(self, out, lhsT, rhs, start=True, stop=True, **kw):
        n = 1
        for d_ in rhs.ap.shape[1:]:
            n *= d_
        passes = 4 if rhs.ap.dtype == F32 else 1
        self.op("pe", lambda e: e.matmul(out.ap, lhsT.ap, rhs.ap, start=start, stop=stop, **kw),
                [lhsT, rhs], [out], cost=30.0 + max(n, 64) * passes * 0.45)

    def tr(self, out, in_, ident):
        passes = 4 if in_.ap.dtype == F32 else 1
        self.op("pe", lambda e: e.transpose(out.ap, in_.ap, ident.ap), [in_, ident], [out],
                cost=60.0 + 128 * passes * 0.45)

    def act(self, out, in_, func, bias=None, scale=None, accum=None):
        kw = {}
        rd = [in_]
        if bias is not None:
            if isinstance(bias, V):
                kw["bias"] = bias.ap
                rd.append(bias)
            else:
                kw["bias"] = bias
        if scale is not None:
            if isinstance(scale, V):
                kw["scale"] = scale.ap
                rd.append(scale)
            else:
                kw["scale"] = scale
        wr = [out]
        if accum is not None:
            kw["accum_out"] = accum.ap
            wr.append(accum)
        self.op("act", lambda e: e.activation(out.ap, in_.ap, func, **kw), rd, wr)

    def tt(self, out, a, b, op, eng="dve"):
        self.op(eng, lambda e: e.tensor_tensor(out.ap, a.ap, b.ap, op), [a, b], [out])

    def ts(self, out, a, s1, op0, s2=None, op1=None, eng="dve", accum=None):
        rd = [a]
        x1 = s1.ap if isinstance(s1, V) else s1
        x2 = s2.ap if isinstance(s2, V) else s2
        if isinstance(s1, V):
            rd.append(s1)
        if isinstance(s2, V):
            rd.append(s2)
        kw = {}
        wr = [out]
        if op1 is not None:
            kw["op1"] = op1
        if accum is not None:
            kw["accum_out"] = accum.ap
            wr.append(accum)
        self.op(eng, lambda e: e.tensor_scalar(out.ap, a.ap, x1, x2, op0, **kw), rd, wr)

    def stt(self, out, a, s, b, op0, op1, eng="dve"):
        rd = [a, b]
        x = s.ap if isinstance(s, V) else s
        if isinstance(s, V):
            rd.append(s)
        self.op(eng, lambda e: e.scalar_tensor_tensor(out.ap, a.ap, x, b.ap, op0, op1), rd, [out])

    def cp(self, out, in_, eng="dve"):
        if eng == "act":
            self.op("act", lambda e: e.copy(out.ap, in_.ap), [in_], [out])
        else:
            self.op(eng, lambda e: e.tensor_copy(out.ap, in_.ap), [in_], [out])

    def red(self, out, in_, op=ALU.add, axis=AX.X, eng="dve"):
        self.op(eng, lambda e: e.tensor_reduce(out.ap, in_.ap, axis, op), [in_], [out])

    def memset(self, out, val, eng="pool"):
        self.op(eng, lambda e: e.memset(out.ap, val), [], [out])

    def iota(self, out, pattern, base=0, cm=0):
        self.op("pool", lambda e: e.iota(out.ap, pattern, base=base, channel_multiplier=cm,
                                         allow_small_or_imprecise_dtypes=True), [], [out])

    def _schedule(self, ops):
        n = len(ops)
        W = SCHED_WINDOW
        SEM = 120.0
        users = [[] for _ in range(n)]
        nleft = [0] * n
        for i, o in enumerate(ops):
            nleft[i] = len(o.deps)
            for j in o.deps:
                users[j].append(i)
        pend = {e: [i for i, o in enumerate(ops) if o.eng == e] for e in self.ENGS}
        head = {e: 0 for e in self.ENGS}
        done = [False] * n
        finish = [0.0] * n
        ready = [0.0] * n
        tfree = {e: 0.0 for e in self.ENGS}
        order = {e: [] for e in self.ENGS}
        remaining = n
        while remaining:
            best = None
            for e in self.ENGS:
                lst = pend[e]
                h = head[e]
                while h < len(lst) and done[lst[h]]:
                    h += 1
                head[e] = h
                seen_dma = False
                cnt = 0
                k = h
                while k < len(lst) and cnt < W:
                    i = lst[k]
                    k += 1
                    if done[i]:
                        continue
                    cnt += 1
                    o = ops[i]
                    if o.isdma:
                        if seen_dma:
                            continue
                        seen_dma = True
                    if nleft[i] > 0:
                        continue
                    st = max(tfree[e], ready[i])
                    key = (st + 0.5 * (cnt - 1), i)
                    if best is None or key < best[0]:
                        best = (key, e, i, st)
            if best is None:
                raise RuntimeError("scheduler deadlock")
            _, e, i, st = best
            o = ops[i]
            done[i] = True
            remaining -= 1
            order[e].append(i)
            if o.isdma:
                tfree[e] = st + 60.0
                finish[i] = st + o.cost
            else:
                tfree[e] = st + o.cost
                finish[i] = st + o.cost
            for u in users[i]:
                nleft[u] -= 1
                lat = 0.0 if (o.eng == "pe" and ops[u].eng == "pe" and not o.isdma) else SEM
                r = finish[i] + lat
                if r > ready[u]:
                    ready[u] = r
        self.est_ns = max(finish) if finish else 0.0
        return order

    def finish(self):
        nc = self.nc
        ops = self.ops
        last = {}
        for i, o in enumerate(ops):
            last[o.eng] = i
        fin = Op("sp", None, False)
        for e, i in last.items():
            fin.deps.add(i)
        lastslot = {}
        for i, o in enumerate(ops):
            if o.isdma:
                lastslot[o.slot] = i
        for i in lastslot.values():
            fin.deps.add(i)
        ops.append(fin)
        order = self._schedule(ops) if SCHED_WINDOW > 0 else {e: [i for i, o in enumerate(ops) if o.eng == e] for e in self.ENGS}
        self._order = order
        for o in ops:
            for j in o.deps:
                d = ops[j]
                if d.eng == "pe" and o.eng == "pe" and not o.isdma:
                    continue
                d.sig = True
        ccount = {e: 0 for e in self.ENGS}
        dcount = self._dcount
        for e_ in self.ENGS:
            for i_ in order[e_]:
                o = ops[i_]
                if o.isdma:
                    pass
                elif o.sig:
                    ccount[o.eng] += 1
                    o.cnt = ccount[o.eng]
        sems = {}
        cms = []

        def getsem(key):
            if key not in sems:
                cm = nc.semaphore(f"s_{key[0]}_{key[1]}")
                sems[key] = cm.__enter__()
                cms.append(cm)
            return sems[key]

        for e in self.ENGS:
            for ep in range(ccount[e] // EPOCH + 1):
                getsem((e, "c%d" % ep))
        for q, n in NSLOT.items():
            for s in range(min(n, dcount[q])):
                getsem((q, s))

        def compkey(o):
            ep = (o.cnt - 1) // EPOCH
            return (o.eng, "c%d" % ep), o.cnt - ep * EPOCH

        with nc.Block() as block:
            def emit_engine(ename, eng):
                known = {}
                for i in order[ename]:
                    o = ops[i]
                    waits = {}
                    for j in o.deps:
                        d = ops[j]
                        if d.isdma:
                            key, val = d.slot, d.slotval
                        else:
                            if d.eng == "pe" and ename == "pe" and not o.isdma:
                                continue
                            key, val = compkey(d)
                        if waits.get(key, 0) < val:
                            waits[key] = val
                    if o.isdma and o.prevval > 0:
                        key = o.slot
                        if waits.get(key, 0) < o.prevval:
                            waits[key] = o.prevval
                    for key, val in waits.items():
                        if known.get(key, 0) >= val:
                            continue
                        known[key] = val
                        eng.wait_ge(getsem(key), val)
                    if o.fn is None:
                        continue
                    ins = o.fn(eng)
                    if o.isdma:
                        ins.then_inc(getsem(o.slot), 16)
                    elif o.sig:
                        key, _ = compkey(o)
                        ins.then_inc(getsem(key), 1)

            @block.tensor
            def _(e):
                emit_engine("pe", e)

            @block.scalar
            def _(e):
                emit_engine("act", e)

            @block.vector
            def _(e):
                emit_engine("dve", e)

            @block.gpsimd
            def _(e):
                emit_engine("pool", e)

            @block.sync
            def _(e):
                emit_engine("sp", e)
        self._cms = cms
        return nc


D = 1024
CTX = 256
NORM_EPS = 1e-6
THETA = 10000.0
import math


class Ctx:
    def __init__(self, p, banks=True):
        self.p = p
        self.bi = 0
        self.dbanks = []
        if banks:
            self.set_banks(False)
        io = p.sb([128, 128], F32, "io")
        pi = p.sb([128, 1], F32, "pi")
        p.iota(io, [[1, 128]], base=0, cm=0)
        p.iota(pi, [[0, 1]], base=0, cm=1)
        self.pidx = pi
        self.identf = p.sb([128, 128], F32, "identf")
        p.ts(self.identf, io, pi[:, 0:1], ALU.is_equal)
        self.identb = p.sb([128, 128], BF16, "identb")
        p.cp(self.identb, self.identf)
        self.iof = io

    def set_banks(self, dbl):
        p = self.p
        if dbl:
            self.dbanks = [p.ps([128, 1024], F32, f"dbank{i}") for i in range(2)]
            self.banks = [self.dbanks[0][:, 0:512], self.dbanks[0][:, 512:1024],
                          self.dbanks[1][:, 0:512], self.dbanks[1][:, 512:1024]]
            self.banks += [p.ps([128, 512], F32, f"bank{i}") for i in range(4, 8)]
        else:
            self.banks = [p.ps([128, 512], F32, f"bank{i}") for i in range(8)]

    def bank(self):
        b = self.banks[self.bi % 8]
        self.bi += 1
        return b

    def load_fm(self, rows_v, n, eng_out="dve"):
        p = self.p
        tmp = p.sb([n, 128], F32, "lfm_tmp")
        p.dma(tmp, rows_v)
        bk = self.bank()
        p.tr(bk[:, 0:n], tmp, self.identf[0:n, 0:n])
        out = p.sb([128, n], F32, "lfm_out")
        p.cp(out, bk[:, 0:n], eng=eng_out)
        return out

    def rstd(self, ss, n, inv_d, eps):
        p = self.p
        p.ts(ss, ss, inv_d, ALU.mult, eps, ALU.add)
        p.act(ss, ss, AF.Ln)
        p.act(ss, ss, AF.Exp, scale=-0.5)


def run_prog(p, in_maps):
    nc = p.finish()
    res = run_bass_kernel_spmd(nc, in_maps, core_ids=list(range(len(in_maps))))
    return res.results


def build_phase0():
    p = Prog()
    cvec = p.dram("cvec", [2, D], F32, "ExternalInput")
    ada_w = p.dram("ada_w", [2, D, 6 * D], F32, "ExternalInput")
    ada_b = p.dram("ada_b", [2, 6 * D], F32, "ExternalInput")
    mods_tm = p.dram("mods_tm", [2, 2, 6 * D], F32, "ExternalOutput")
    mods_fm = p.dram("mods_fm", [2, 128, 48, 2], F32, "ExternalOutput")
    c = Ctx(p)
    cT = c.load_fm(cvec.rr("j (c p) -> (j c) p", p=128), 16)
    sT = p.sb([128, 16], F32, "sT")
    p.act(sT, cT, AF.Silu)
    sTv = sT.rr("p (j c) -> p c j", j=2)
    wbuf = [p.sb([128, 8, 1536], F32, f"adaw{i}") for i in range(2)]
    it = 0
    for l in range(2):
        bfm = c.load_fm(ada_b[l].rr("(c p) -> c p", p=128), 48)
        ofm = p.sb([128, 48, 2], F32, "ofm")
        for qd in range(4):
            w = wbuf[it % 2]
            it += 1
            for kc in range(8):
                p.dma(w[:, kc, :], ada_w[l, kc * 128:(kc + 1) * 128, qd * 1536:(qd + 1) * 1536],
                      q=("sp" if kc % 2 == 0 else "act"))
            bk = c.bank()
            for cc in range(12):
                for kc in range(8):
                    p.mm(bk[:, cc * 2:cc * 2 + 2], w[:, kc, cc * 128:(cc + 1) * 128], sTv[:, kc, :],
                         start=(kc == 0), stop=(kc == 7))
            p.tt(ofm[:, qd * 12:(qd + 1) * 12, :], bk[:, 0:24].rr("p (c j) -> p c j", j=2),
                 bfm[:, qd * 12:(qd + 1) * 12].un(2).bc([128, 12, 2]), ALU.add)
        p.dma(mods_fm[l], ofm)
        bk = c.bank()
        ofm2 = p.sb([128, 2, 48], F32, "ofm2")
        p.cp(ofm2, ofm.rr("p c j -> p j c"))
        p.tr(bk[0:96, 0:128], ofm2.rr("p j c -> p (j c)"), c.identf)
        otm = p.sb([96, 128], F32, "otm")
        p.cp(otm, bk[0:96, 0:128])
        for j in range(2):
            p.dma(mods_tm[l][j].rr("(c p) -> c p", p=128), otm[48 * j:48 * j + 48, :])
    return p


def build_post(S, has_ctx, stop=99):
    TL = S // 4
    NTL = TL // 128
    NT = NTL + (2 if has_ctx else 0)
    TOK = NT * 128
    p = Prog()
    x_in = p.dram("x", [TOK, D], F32, "ExternalInput")
    attT = p.dram("attT", [8, 128, TOK], BF16, "ExternalInput")
    mods_tm = p.dram("mods_tm", [2, 6 * D], F32, "ExternalInput")
    mods_fm = p.dram("mods_fm", [128, 48, 2], F32, "ExternalInput")
    w_out = p.dram("w_out", [D, D], F32, "ExternalInput")
    n2g = p.dram("norm2_g", [D], F32, "ExternalInput")
    router_w = p.dram("router_w", [D, 16], F32, "ExternalInput")
    router_b = p.dram("router_b", [1, 16], F32, "ExternalInput")
    wg_all = p.dram("wg_all", [17, D, 256], F32, "ExternalInput")
    wu_all = p.dram("wu_all", [17, D, 256], F32, "ExternalInput")
    wd_all = p.dram("wd_all", [17, 256, D], F32, "ExternalInput")
    x_out = p.dram("x_out", [TOK, D], F32, "ExternalOutput")
    c = Ctx(p)
    B = c.banks
    nvar = 2 if has_ctx else 1
    blocks = []
    t = 0
    while t < NTL:
        n = min(4, NTL - t)
        blocks.append((0, t, n))
        t += n
    if has_ctx:
        blocks.append((1, NTL, 2))

    mfm = p.sb([128, 48, 2], F32, "mfm")
    p.dma(mfm, mods_fm)
    g2fm = c.load_fm(n2g.rr("(c p) -> c p", p=128), 8)
    A2 = p.sb([128, 8, 2], F32, "A2")
    p.ts(A2, mfm[:, 32:40, :], 1.0, ALU.add)
    p.tt(A2, A2, g2fm.un(2).bc([128, 8, 2]), ALU.mult)
    SH2 = mfm[:, 24:32, :]
    rw = p.sb([128, 8, 16], F32, "rw")
    p.dma(rw, router_w.rr("(kc p) e -> p kc e", p=128))
    rb = p.sb([128, 16], F32, "rb")
    p.dma(rb, router_b.bc([128, 16]))
    sel = p.sb([16, 16, 128], F32, "sel")
    p.iota(sel, [[1, 16], [0, 128]], base=0, cm=0)
    p.ts(sel, sel, c.pidx[0:16, 0:1], ALU.is_equal)
    xs = p.sb([128, NT, D], F32, "xs")
    for t in range(NT):
        p.dma(xs[:, t, :], x_in[t * 128:(t + 1) * 128, :])
    h2T = p.sb([128, 8, TOK], BF16, "h2T")
    combT = p.sb([16, TOK], F32, "combT")
    lg_all = p.sb([128, NT, 16], F32, "lg_all")
    gabc = [p.sb([128, D], F32, f"gabc{i}") for i in range(2)]

    def load_ga(which, var, dst):
        p.dma(dst, mods_tm[var:var + 1, which * D:(which + 1) * D].bc([128, D]))

    with p.scope():
        if stop >= 2:
            wo = p.sb([128, 8, D], BF16, "wo")
            atb = [p.sb([128, 8, 128], BF16, f"atb{i}") for i in range(2)]
            for var in range(nvar):
                p.dma(wo, w_out.rr("(kc p) d -> p kc d", p=128), q="pool")
                load_ga(2, var, gabc[0])
                p.tt(wo, wo, gabc[0].un(1).bc([128, 8, D]), ALU.mult, eng="pool")
                tiles = range(NTL) if var == 0 else range(NTL, NT)
                for t in tiles:
                    a = atb[t % 2]
                    p.dma(a, attT[:, :, t * 128:(t + 1) * 128].rr("c p t -> p c t"))
                    for half in range(2):
                        bk = B[(t % 2) * 2 + half]
                        for kc in range(8):
                            p.mm(bk[:, :], a[:, kc, :], wo[:, kc, half * 512:(half + 1) * 512],
                                 start=(kc == 0), stop=(kc == 7))
                        xv = xs[:, t, half * 512:(half + 1) * 512]
                        p.tt(xv, xv, bk, ALU.add)

    with p.scope():
        if stop >= 3:
            junk = p.sb([128, D], BF16, "junk")
            xn = [p.sb([128, D], F32, f"xn{i}") for i in range(2)]
            h2f = [p.sb([128, 8, 128], F32, f"h2f{i}") for i in range(2)]
            ssq = p.sb([128, NT], F32, "ssq")
            for t in range(NT):
                var = 0 if t < NTL else 1
                p.act(junk, xs[:, t, :], AF.Square, accum=ssq[:, t:t + 1])
            c.rstd(ssq, NT, 1.0 / D, NORM_EPS)
            for t in range(NT):
                var = 0 if t < NTL else 1
                x_n = xn[t % 2]
                hf = h2f[t % 2]
                p.ts(x_n, xs[:, t, :], ssq[:, t:t + 1], ALU.mult)
                for hb in range(2):
                    bk = B[4 + (t % 2) * 2 + hb]
                    for k4 in range(4):
                        kc = hb * 4 + k4
                        p.tr(bk[:, k4 * 128:(k4 + 1) * 128], x_n[:, kc * 128:(kc + 1) * 128], c.identf)
                    for k4 in range(4):
                        kc = hb * 4 + k4
                        p.act(hf[:, kc, :], bk[:, k4 * 128:(k4 + 1) * 128], AF.Identity,
                              bias=SH2[:, kc, var:var + 1], scale=A2[:, kc, var:var + 1])
                p.cp(h2T[:, :, t * 128:(t + 1) * 128], hf, eng="pool")
                bk = B[t % 2]
                for kc in range(8):
                    p.mm(bk[:, 0:16], hf[:, kc, :], rw[:, kc, :], start=(kc == 0), stop=(kc == 7))
                p.cp(lg_all[:, t, :], bk[:, 0:16])
            N16 = NT * 16
            sc = p.sb([128, NT, 16], F32, "sc")
            bs = p.sb([128, NT, 16], F32, "bs")
            p.act(sc, lg_all, AF.Sigmoid)
            p.tt(bs, sc, rb.un(1).bc([128, NT, 16]), ALU.add)
            bsv = bs.rr("p t (g e) -> p (t g) e", g=4)
            G = NT * 4
            m1 = p.sb([128, G], F32, "m1")
            m2 = p.sb([128, G], F32, "m2")
            p.red(m1, bsv, op=ALU.max)
            eq1 = p.sb([128, G, 4], F32, "eq1")
            p.tt(eq1, bsv, m1.un(2).bc([128, G, 4]), ALU.is_equal)
            p.stt(eq1, eq1, -1e9, bsv, ALU.mult, ALU.add)
            p.red(m2, eq1, op=ALU.max)
            gs = p.sb([128, NT, 4], F32, "gs")
            p.tt(gs.rr("p t g -> p (t g)"), m1, m2, ALU.add)
            gmax = p.sb([128, NT], F32, "gmax")
            p.red(gmax, gs, op=ALU.max)
            gsel = p.sb([128, NT, 4], F32, "gsel")
            p.tt(gsel, gs, gmax.un(2).bc([128, NT, 4]), ALU.is_equal)
            ge2 = p.sb([128, G, 4], F32, "ge2")
            p.tt(ge2, bsv, m2.un(2).bc([128, G, 4]), ALU.is_ge)
            p.tt(ge2, ge2, gsel.rr("p t g -> p (t g)").un(2).bc([128, G, 4]), ALU.mult)
            p.tt(ge2, ge2, sc.rr("p t (g e) -> p (t g) e", g=4), ALU.mult)
            den = p.sb([128, NT], F32, "den")
            p.red(den, ge2.rr("p (t g) e -> p t (g e)", g=4))
            p.op("dve", lambda e: e.reciprocal(den.ap, den.ap), [den], [den])
            comb = p.sb([128, NT, 16], F32, "comb")
            p.tt(comb, ge2.rr("p (t g) e -> p t (g e)", g=4), den.un(2).bc([128, NT, 16]), ALU.mult)
            for t in range(NT):
                bk = B[t % 2]
                p.tr(bk[0:16, 0:128], comb[:, t, :], c.identf)
                p.cp(combT[:, t * 128:(t + 1) * 128], bk[0:16, 0:128], eng="act")

    with p.scope():
        if stop >= 4:
            wg = [p.sb([128, 8, 256], BF16, f"wg{i}") for i in range(2)]
            wu = [p.sb([128, 8, 256], BF16, f"wu{i}") for i in range(2)]
            wd = [p.sb([128, 2, D], BF16, f"wd{i}") for i in range(2)]
            wds = [p.sb([128, 2, D], BF16, f"wds{i}") for i in range(2)]
            comb_sb = [p.sb([128, 512], F32, f"comb_sb{i}") for i in range(2)]
            sg = [p.sb([128, 512], F32, f"sg{i}") for i in range(2)]
            tg = [p.sb([128, 512], F32, f"tg{i}") for i in range(2)]
            actb = [p.sb([128, 2, 512], BF16, f"actb{i}") for i in range(2)]
            for var in range(nvar):
                load_ga(5, var, gabc[var])
            it = 0
            for e in range(17 if stop < 40 or stop == 99 else stop - 40):
                eb = e % 2
                p.dma(wg[eb], wg_all[e].rr("(kc p) f -> p kc f", p=128), q="pool")
                p.dma(wu[eb], wu_all[e].rr("(kc p) f -> p kc f", p=128), q="pool")
                p.dma(wd[eb], wd_all[e].rr("(fc p) d -> p fc d", p=128), q="pool")
                for var in range(nvar):
                    p.tt(wds[eb], wd[eb], gabc[var].un(1).bc([128, 2, D]), ALU.mult, eng="pool")
                    for (bv, t0, nt) in blocks:
                        if bv != var:
                            continue
                        n = nt * 128
                        tok = slice(t0 * 128, t0 * 128 + n)
                        ib = it % 2
                        it += 1
                        if e < 16:
                            p.mm(B[4][:, 0:n], sel[:, e, :], combT[:, tok])
                            p.cp(comb_sb[ib][:, 0:n], B[4][:, 0:n], eng="act")
                        for fc in range(2):
                            gb = B[fc * 2]
                            ub = B[fc * 2 + 1]
                            for kc in range(8):
                                p.mm(gb[:, 0:n], wg[eb][:, kc, fc * 128:(fc + 1) * 128], h2T[:, kc, tok],
                                     start=(kc == 0), stop=(kc == 7))
                            for kc in range(8):
                                p.mm(ub[:, 0:n], wu[eb][:, kc, fc * 128:(fc + 1) * 128], h2T[:, kc, tok],
                                     start=(kc == 0), stop=(kc == 7))
                            p.act(sg[fc][:, 0:n], gb[:, 0:n], AF.Silu)
                            if e < 16:
                                p.tt(tg[fc][:, 0:n], ub[:, 0:n], comb_sb[ib][:, 0:n], ALU.mult)
                                p.tt(actb[ib][:, fc, 0:n], sg[fc][:, 0:n], tg[fc][:, 0:n], ALU.mult, eng="pool")
                            else:
                                p.tt(actb[ib][:, fc, 0:n], sg[fc][:, 0:n], ub[:, 0:n], ALU.mult)
                        for ti in range(nt):
                            t = t0 + ti
                            for half in range(2):
                                db = B[5 + (ti * 2 + half) % 3]
                                for fc in range(2):
                                    p.mm(db, actb[ib][:, fc, ti * 128:(ti + 1) * 128],
                                         wds[eb][:, fc, half * 512:(half + 1) * 512],
                                         start=(fc == 0), stop=(fc == 1))
                                xv = xs[:, t, half * 512:(half + 1) * 512]
                                p.tt(xv, xv, db, ALU.add)
    for t in range(NT):
        p.dma(x_out[t * 128:(t + 1) * 128, :], xs[:, t, :])
    return p


def rope_tables(p, c, pos, NTL, half):
    inv = p.sb([128, half], F32, "inv")
    for i in range(half):
        p.memset(inv[:, i:i + 1], float(THETA ** (-i / half)) / (2.0 * math.pi))
    Y = p.sb([128, NTL, 2, half], F32, "ropeY")
    p.tt(Y.rr("p t r h -> p (t r) h"), pos.rr("p t r -> p (t r)").un(2).bc([128, NTL * 2, half]),
         inv.un(1).bc([128, NTL * 2, half]), ALU.mult)
    COS = p.sb([128, NTL, 2, 2 * half], F32, "COS")
    SINS = p.sb([128, NTL, 2, 2 * half], F32, "SINS")
    Yi = p.sb([128, NTL, 2, half], I32, "ropeYi")
    Yf = p.sb([128, NTL, 2, half], F32, "ropeYf")
    T = p.sb([128, NTL, 2, half], F32, "ropeT")
    R = p.sb([128, NTL, 2, half], F32, "ropeR")
    for which in range(2):
        if which == 1:
            p.ts(Y, Y, 0.25, ALU.add)
        p.cp(Yi, Y)
        p.cp(Yf, Yi)
        p.tt(R, Y, Yf, ALU.subtract)
        p.ts(T, R, 0.5, ALU.is_gt)
        p.tt(R, R, T, ALU.subtract)
        p.ts(T, R, -0.5, ALU.is_lt)
        p.tt(R, R, T, ALU.add)
        if which == 0:
            p.act(SINS[:, :, :, half:2 * half], R, AF.Sin, scale=2.0 * math.pi * (1 - 1e-6))
            p.ts(SINS[:, :, :, 0:half], SINS[:, :, :, half:2 * half], -1.0, ALU.mult)
        else:
            p.act(COS[:, :, :, 0:half], R, AF.Sin, scale=2.0 * math.pi * (1 - 1e-6))
            p.cp(COS[:, :, :, half:2 * half], COS[:, :, :, 0:half])
    return COS, SINS


def qk_post(p, c, pieces, nh, hd, gains_bc, dst, scratch, rope=None):
    sq, qk, ss = scratch["sq"], scratch["qk"], scratch["ss"]
    off = 0
    for (v, n) in pieces:
        p.act(sq[:, off:off + n * hd], v, AF.Square)
        off += n * hd
    p.red(ss, sq.rr("p (h d) -> p h d", d=hd))
    c.rstd(ss, nh, 1.0 / hd, NORM_EPS)
    off = 0
    h0 = 0
    for (v, n) in pieces:
        p.tt(qk[:, off:off + n * hd].rr("p (h d) -> p h d", d=hd), v.rr("p (h d) -> p h d", d=hd),
             ss[:, h0:h0 + n].un(2).bc([128, n, hd]), ALU.mult)
        off += n * hd
        h0 += n
    if rope is None:
        p.tt(dst, qk, gains_bc, ALU.mult, eng="pool")
        return
    p.tt(qk, qk, gains_bc, ALU.mult, eng="pool")
    roff, half, COS_t, SINS_t = rope
    qv = qk.rr("p (h d) -> p h d", d=hd)
    dv = dst.rr("p (h d) -> p h d", d=hd)
    if roff > 0:
        p.cp(dv[:, :, 0:roff], qv[:, :, 0:roff], eng="pool")
    t1, t2 = scratch["t1"], scratch["t2"]
    w = 2 * half
    for rc in range(2):
        xs = qv[:, :, roff + rc * w: roff + (rc + 1) * w]
        a = t1[:, 0:nh * w].rr("p (h d) -> p h d", d=w)
        b = t2[:, 0:nh * w].rr("p (h d) -> p h d", d=w)
        eng = "dve" if rc == 0 else "pool"
        p.tt(a, xs, COS_t[:, rc, :].un(1).bc([128, nh, w]), ALU.mult, eng=eng)
        p.tt(b[:, :, 0:half], xs[:, :, half:w], SINS_t[:, rc, 0:half].un(1).bc([128, nh, half]), ALU.mult, eng=eng)
        p.tt(b[:, :, half:w], xs[:, :, 0:half], SINS_t[:, rc, half:w].un(1).bc([128, nh, half]), ALU.mult, eng=eng)
        p.tt(dv[:, :, roff + rc * w: roff + (rc + 1) * w], a, b, ALU.add, eng=eng)


def norm1_hT(p, c, xt, A, SH, var, hT_dst, xnb, junk, ssq, banks):
    p.act(junk, xt, AF.Square, accum=ssq)
    c.rstd(ssq, 1, 1.0 / D, NORM_EPS)
    p.ts(xnb, xt, ssq[:, 0:1], ALU.mult)
    for hb in range(2):
        bk = banks[hb].bitcast(BF16)
        for k4 in range(4):
            kc = hb * 4 + k4
            p.tr(bk[:, k4 * 128:(k4 + 1) * 128], xnb[:, kc * 128:(kc + 1) * 128], c.identb)
        for k4 in range(4):
            kc = hb * 4 + k4
            p.act(hT_dst[:, kc, :], bk[:, k4 * 128:(k4 + 1) * 128], AF.Identity,
                  bias=SH[:, kc, var:var + 1], scale=A[:, kc, var:var + 1])


def mod_consts(p, c, mods_fm, norm_g, which_sh, which_sc):
    mfm = p.sb([128, 48, 2], F32, "mfm")
    p.dma(mfm, mods_fm)
    gfm = c.load_fm(norm_g.rr("(c p) -> c p", p=128), 8)
    A = p.sb([128, 8, 2], F32, "Amod")
    p.ts(A, mfm[:, which_sc * 8:(which_sc + 1) * 8, :], 1.0, ALU.add)
    p.tt(A, A, gfm.un(2).bc([128, 8, 2]), ALU.mult)
    SH = mfm[:, which_sh * 8:(which_sh + 1) * 8, :]
    return A, SH


def build_pre1(S):
    TL = S // 4
    NTL = TL // 128
    NT = NTL + 2
    TOK = NT * 128
    p = Prog()
    x_in = p.dram("x", [TOK, D], F32, "ExternalInput")
    mods_fm = p.dram("mods_fm", [128, 48, 2], F32, "ExternalInput")
    n1g = p.dram("norm1_g", [D], F32, "ExternalInput")
    w_in = p.dram("w_in", [D, 1536], F32, "ExternalInput")
    g_q = p.dram("g_q", [1, 64], F32, "ExternalInput")
    g_k = p.dram("g_k", [1, 64], F32, "ExternalInput")
    pos_in = p.dram("pos", [128, NTL, 2], F32, "ExternalInput")
    qkT = p.dram("qkT", [10, 128, TOK], BF16, "ExternalOutput")
    v_out = p.dram("v", [TOK, 256], BF16, "ExternalOutput")
    c = Ctx(p)
    B = c.banks
    A, SH = mod_consts(p, c, mods_fm, n1g, 0, 1)
    pos = p.sb([128, NTL, 2], F32, "pos")
    p.dma(pos, pos_in)
    COS, SINS = rope_tables(p, c, pos, NTL, 16)
    gains = p.sb([128, 20, 64], F32, "gains")
    gq = p.sb([128, 64], F32, "gq")
    gk = p.sb([128, 64], F32, "gk")
    p.dma(gq, g_q.bc([128, 64]))
    p.dma(gk, g_k.bc([128, 64]))
    p.cp(gains[:, 0:16, :], gq.un(1).bc([128, 16, 64]))
    p.cp(gains[:, 16:20, :], gk.un(1).bc([128, 4, 64]))
    gains_f = gains.rr("p h d -> p (h d)")
    wi = p.sb([128, 8, 1536], BF16, "wi")
    p.dma(wi, w_in.rr("(kc p) n -> p kc n", p=128), q="pool")
    xt = [p.sb([128, D], F32, f"xt{i}") for i in range(2)]
    xnb = [p.sb([128, D], BF16, f"xnb{i}") for i in range(2)]
    junk = p.sb([128, D], BF16, "junk")
    hT = [p.sb([128, 8, 128], BF16, f"hT{i}") for i in range(2)]
    ssq = [p.sb([128, 1], F32, f"ssq{i}") for i in range(2)]
    scrs = [dict(sq=p.sb([128, 1280], F32, "sq"), qk=p.sb([128, 1280], F32, "qk"), ss=p.sb([128, 20], F32, "ss"),
                 t1=p.sb([128, 1280], F32, "t1"), t2=p.sb([128, 1280], F32, "t2")) for _ in range(2)]
    qkb = [p.sb([128, 1280], BF16, f"qkb{i}") for i in range(2)]
    vb = [p.sb([128, 256], BF16, f"vb{i}") for i in range(2)]
    qkTs = [p.sb([128, 10, 128], BF16, f"qkTs{i}") for i in range(2)]
    for t in range(NT):
        i2 = t % 2
        var = 0 if t < NTL else 1
        p.dma(xt[i2], x_in[t * 128:(t + 1) * 128, :])
        norm1_hT(p, c, xt[i2], A, SH, var, hT[i2], xnb[i2], junk, ssq[i2], (B[0], B[1]))
        for blk in range(3):
            bk = B[2 + blk]
            for kc in range(8):
                p.mm(bk, hT[i2][:, kc, :], wi[:, kc, blk * 512:(blk + 1) * 512], start=(kc == 0), stop=(kc == 7))
        rope = None if var == 1 else (0, 16, COS[:, t], SINS[:, t])
        qk_post(p, c, [(B[2], 8), (B[3], 8), (B[4][:, 0:256], 4)], 20, 64, gains_f, qkb[i2], scrs[i2], rope)
        p.cp(vb[i2], B[4][:, 256:512], eng="act")
        p.dma(v_out[t * 128:(t + 1) * 128, :], vb[i2])
        bA = B[5].bitcast(BF16)
        bB = B[6].bitcast(BF16)
        for pr in range(10):
            dstb = bA[:, pr * 128:(pr + 1) * 128] if pr < 8 else bB[:, (pr - 8) * 128:(pr - 7) * 128]
            p.tr(dstb, qkb[i2][:, pr * 128:(pr + 1) * 128], c.identb)
        p.cp(qkTs[i2][:, 0:8, :].rr("p a t -> p (a t)"), bA, eng="act")
        p.cp(qkTs[i2][:, 8:10, :].rr("p a t -> p (a t)"), bB[:, 0:256])
        p.dma(qkT[:, :, t * 128:(t + 1) * 128].rr("a p t -> p a t"), qkTs[i2])
    return p


def attention_gen(p, c, qT_d, kT_d, v_d, oT_d, NH, NKV, dk, dv, S, scale, qcT_d=None, ocT_d=None, banks=None):
    LK = CTX + S
    NKT = LK // 128
    B = banks if banks is not None else c.banks
    kT = p.sb([128, NKV, LK], BF16, "kT")
    if dk < 128:
        p.memset(kT, 0.0)
    for kv in range(NKV):
        p.dma(kT[0:dk, kv, :], kT_d[kv])
    vp = p.sb([128, NKV, NKT, dv + 1], BF16, "vp")
    p.memset(vp[:, :, :, dv:dv + 1], 1.0)
    for kv in range(NKV):
        p.dma(vp[:, kv, :, 0:dv], v_d[kv].rr("(t p) d -> p t d", p=128))
    selden = p.sb([dv + 1, dv], F32, "selden")
    p.memset(selden, 0.0)
    p.memset(selden[dv:dv + 1, :], 1.0)
    qTs = [p.sb([128, NH, 512], BF16, f"qTs{i}") for i in range(2)]
    if dk < 128:
        for t_ in qTs:
            p.memset(t_, 0.0)
    pT = [p.sb([128, 512], BF16, f"pT{i}") for i in range(4)]
    accs = [p.sb([dv + 1, 512], F32, f"accs{i}") for i in range(2)]
    rec = [p.sb([dv, 512], F32, f"rec{i}") for i in range(2)]
    ob = [p.sb([dv, 512], BF16, f"ob{i}") for i in range(2)]
    jobs = []
    for qb in range(S // 512):
        jobs.append((qT_d, oT_d, qb * 512, 512, NKT))
    if qcT_d is not None:
        jobs.append((qcT_d, ocT_d, 0, CTX, CTX // 128))
    DB = c.dbanks
    items = []
    for ji, (qd, od, q0, n, nkt) in enumerate(jobs):
        for h in range(NH):
            for kp in range(nkt // 2):
                items.append((ji, h, kp))
    pT2 = [p.sb([128, 2, 512], BF16, f"pT2_{i}") for i in range(3)]
    loaded = set()

    def load_q(ji):
        if ji in loaded or ji >= len(jobs):
            return
        loaded.add(ji)
        qd, od, q0, n, nkt = jobs[ji]
        qs = qTs[ji % 2]
        for h in range(NH):
            p.dma(qs[0:dk, h, 0:n], qd[h, :, q0:q0 + n])

    def issue_s(i):
        ji, h, kp = items[i]
        qd, od, q0, n, nkt = jobs[ji]
        load_q(ji)
        kv = h * NKV // NH
        db = DB[i % 2]
        for a_ in range(2):
            kt = 2 * kp + a_
            p.mm(db[:, a_ * 512:a_ * 512 + n], kT[:, kv, kt * 128:(kt + 1) * 128], qTs[ji % 2][:, h, 0:n])

    issue_s(0)
    ih = 0
    for i, (ji, h, kp) in enumerate(items):
        qd, od, q0, n, nkt = jobs[ji]
        kv = h * NKV // NH
        if h == 0 and kp == 0:
            load_q(ji + 1)
        if i + 1 < len(items):
            issue_s(i + 1)
        acc = B[4 + ih % 2]
        pt = pT2[i % 3]
        dbv = DB[i % 2].rr("p (a t) -> p a t", a=2)
        p.act(pt[:, :, 0:n], dbv[:, :, 0:n], AF.Exp, scale=scale)
        for a_ in range(2):
            kt = 2 * kp + a_
            p.mm(acc[0:dv + 1, 0:n], vp[:, kv, kt, :], pt[:, a_, 0:n], start=(kt == 0), stop=(kt == nkt - 1))
        if kp == nkt // 2 - 1:
            a = accs[ih % 2]
            p.cp(a[:, 0:n], acc[0:dv + 1, 0:n])
            p.mm(B[6][0:dv, 0:n], selden, a[:, 0:n])
            r = rec[ih % 2]
            p.op("dve", lambda e, r=r, n=n: e.reciprocal(r.ap[:, 0:n], B[6].ap[0:dv, 0:n]), [B[6]], [r])
            o = ob[ih % 2]
            p.tt(o[:, 0:n], a[0:dv, 0:n], r[:, 0:n], ALU.mult, eng="pool")
            p.dma(od[h, :, q0:q0 + n], o[:, 0:n])
            ih += 1
            yield ih


def attention_block(*a, **kw):
    for _ in attention_gen(*a, **kw):
        pass


def build_attn1(S):
    p = Prog()
    LK = CTX + S
    qT = p.dram("qT", [4, 64, S], BF16, "ExternalInput")
    kT = p.dram("kT", [1, 64, LK], BF16, "ExternalInput")
    v = p.dram("v", [1, LK, 64], BF16, "ExternalInput")
    oT = p.dram("oT", [4, 64, S], BF16, "ExternalOutput")
    c = Ctx(p, banks=False)
    c.set_banks(True)
    attention_block(p, c, qT, kT, v, oT, 4, 1, 64, 64, S, 64 ** -0.5)
    return p


RW_COLS = 1920


def build_pre0(S, dbg=0):
    TL = S // 4
    NTL = TL // 128
    NTM = NTL + 2
    NTA = NTL + 3
    TOKM = NTM * 128
    p = Prog()
    x_in = p.dram("x", [NTA * 128, D], F32, "ExternalInput")
    mods_fm = p.dram("mods_fm", [128, 48, 2], F32, "ExternalInput")
    n1g = p.dram("norm1_g", [D], F32, "ExternalInput")
    w_in = p.dram("w_in", [D, 2592], F32, "ExternalInput")
    mu_d = p.dram("mu", [RW_COLS], F32, "ExternalInput")
    flags_d = p.dram("flags", [128, 2], F32, "ExternalInput")
    g_qa = p.dram("g_qa", [1, 384], F32, "ExternalInput")
    g_kva = p.dram("g_kva", [1, 256], F32, "ExternalInput")
    w_q_up = p.dram("w_q_up", [384, 768], F32, "ExternalInput")
    w_kv_up = p.dram("w_kv_up", [256, 1024], F32, "ExternalInput")
    g_q = p.dram("g_q", [1, 96], F32, "ExternalInput")
    g_k = p.dram("g_k", [1, 96], F32, "ExternalInput")
    pos_in = p.dram("pos", [128, NTL, 2], F32, "ExternalInput")
    rwT = p.dram("rwT", [15, 128, TL + CTX], F32, "ExternalOutput")
    qT_o = p.dram("qT", [8, 96, TOKM], BF16, "ExternalOutput")
    kT_o = p.dram("kT", [8, 96, TOKM], BF16, "ExternalOutput")
    v_o = p.dram("v", [TOKM, 512], BF16, "ExternalOutput")
    c = Ctx(p)
    B = c.banks
    A, SH = mod_consts(p, c, mods_fm, n1g, 0, 1)
    pos = p.sb([128, NTL, 2], F32, "pos")
    p.dma(pos, pos_in)
    COS, SINS = rope_tables(p, c, pos, NTL, 8)
    hT_all = p.sb([128, 8, NTA * 128], BF16, "hT_all")

    with p.scope():
        gq_bc = p.sb([128, 8, 96], F32, "gq_bc")
        gk_bc = p.sb([128, 8, 96], F32, "gk_bc")
        g96 = p.sb([128, 96], F32, "g96")
        p.dma(g96, g_q.bc([128, 96]))
        p.cp(gq_bc, g96.un(1).bc([128, 8, 96]))
        g96b = p.sb([128, 96], F32, "g96b")
        p.dma(g96b, g_k.bc([128, 96]))
        p.cp(gk_bc, g96b.un(1).bc([128, 8, 96]))
        gqa_bc = p.sb([128, 384], F32, "gqa_bc")
        gkva_bc = p.sb([128, 256], F32, "gkva_bc")
        p.dma(gqa_bc, g_qa.bc([128, 384]))
        p.dma(gkva_bc, g_kva.bc([128, 256]))
        wim = p.sb([128, 8, 672], BF16, "wim")
        p.dma(wim, w_in[:, RW_COLS:2592].rr("(kc p) n -> p kc n", p=128), q="pool")
        wq = p.sb([128, 3, 768], BF16, "wq")
        p.dma(wq, w_q_up.rr("(kc p) n -> p kc n", p=128), q="pool")
        wkv = p.sb([128, 2, 1024], BF16, "wkv")
        p.dma(wkv, w_kv_up.rr("(kc p) n -> p kc n", p=128), q="pool")
        xt = [p.sb([128, D], F32, f"xt{i}") for i in range(2)]
        xnb = [p.sb([128, D], BF16, f"xnb{i}") for i in range(2)]
        junk = p.sb([128, D], BF16, "junk")
        ssq = [p.sb([128, 1], F32, f"ssq{i}") for i in range(2)]
        ssa = [p.sb([128, 2], F32, f"ssa{i}") for i in range(2)]
        anb = [p.sb([128, 640], BF16, f"anb{i}") for i in range(2)]
        anT = [p.sb([128, 5, 128], BF16, f"anT{i}") for i in range(2)]
        krs = [p.sb([128, 32], F32, f"krs{i}") for i in range(2)]
        Ksb = [p.sb([128, 8, 96], F32, f"Ksb{i}") for i in range(2)]
        vb = [p.sb([128, 8, 64], BF16, f"vb{i}") for i in range(2)]
        scrs = [dict(sq=p.sb([128, 768], F32, "sq"), qk=p.sb([128, 768], F32, "qk"), ss=p.sb([128, 8], F32, "ss"),
                     t1=p.sb([128, 768], F32, "t1"), t2=p.sb([128, 768], F32, "t2")) for _ in range(2)]
        qb_ = [p.sb([128, 768], BF16, f"qb{i}") for i in range(2)]
        kb_ = [p.sb([128, 768], BF16, f"kb{i}") for i in range(2)]
        qTs = [p.sb([96, 8, 128], BF16, f"qTs{i}") for i in range(2)]
        kTs = [p.sb([96, 8, 128], BF16, f"kTs{i}") for i in range(2)]
        for t in range(NTA):
            i2 = t % 2
            var = 1 if (NTL <= t < NTL + 2) else 0
            p.dma(xt[i2], x_in[t * 128:(t + 1) * 128, :])
            hT = hT_all[:, :, t * 128:(t + 1) * 128]
            norm1_hT(p, c, xt[i2], A, SH, var, hT, xnb[i2], junk, ssq[i2], (B[0], B[1]))
            if t >= NTM or dbg == 1:
                continue
            for kc in range(8):
                p.mm(B[2][:, 0:384], hT[:, kc, :], wim[:, kc, 0:384], start=(kc == 0), stop=(kc == 7))
            for kc in range(8):
                p.mm(B[3][:, 0:288], hT[:, kc, :], wim[:, kc, 384:672], start=(kc == 0), stop=(kc == 7))
            sa = ssa[i2]
            p.act(junk[:, 0:384], B[2][:, 0:384], AF.Square, accum=sa[:, 0:1])
            p.act(junk[:, 384:640], B[3][:, 0:256], AF.Square, accum=sa[:, 1:2])
            p.ts(sa[:, 0:1], sa[:, 0:1], 256.0 / 384.0, ALU.mult)
            c.rstd(sa, 2, 1.0 / 256.0, NORM_EPS)
            p.stt(anb[i2][:, 0:384], B[2][:, 0:384], sa[:, 0:1], gqa_bc, ALU.mult, ALU.mult)
            p.stt(anb[i2][:, 384:640], B[3][:, 0:256], sa[:, 1:2], gkva_bc, ALU.mult, ALU.mult)
            p.cp(krs[i2], B[3][:, 256:288], eng="act")
            if dbg == 3:
                continue
            b4 = B[4].bitcast(BF16)
            for k5 in range(5):
                p.tr(b4[:, k5 * 128:(k5 + 1) * 128], anb[i2][:, k5 * 128:(k5 + 1) * 128], c.identb)
            p.cp(anT[i2].rr("p a t -> p (a t)"), b4[:, 0:640], eng="act")
            if dbg == 4:
                continue
            for (bk, c0, c1) in ((B[5], 0, 480), (B[6], 480, 768)):
                for kc in range(3):
                    p.mm(bk[:, 0:c1 - c0], anT[i2][:, kc, :], wq[:, kc, c0:c1], start=(kc == 0), stop=(kc == 2))
            if dbg == 51:
                continue
            for (bk, c0) in ((B[7], 0), (B[2], 512)):
                for kc in range(2):
                    p.mm(bk, anT[i2][:, 3 + kc, :], wkv[:, kc, c0:c0 + 512], start=(kc == 0), stop=(kc == 1))
            if dbg == 52:
                continue
            K = Ksb[i2]
            for (bk, h0) in ((B[7], 0), (B[2], 4)):
                kvv = bk.rr("p (h d) -> p h d", d=128)
                p.cp(K[:, h0:h0 + 4, 0:64], kvv[:, :, 0:64])
                if dbg != 531:
                    p.cp(vb[i2][:, h0:h0 + 4, :], kvv[:, :, 64:128], eng=("dve" if dbg == 532 else "act"))
            if dbg != 533:
                p.cp(K[:, :, 64:96], krs[i2].un(1).bc([128, 8, 32]), eng="pool")
            if dbg in (53, 531, 532, 533):
                continue
            p.dma(v_o[t * 128:(t + 1) * 128, :], vb[i2].rr("p h d -> p (h d)"))
            if dbg == 5:
                continue
            rope = None if var == 1 else (64, 8, COS[:, t], SINS[:, t])
            qk_post(p, c, [(B[5][:, 0:480], 5), (B[6][:, 0:288], 3)], 8, 96, gq_bc.rr("p h d -> p (h d)"),
                    qb_[i2], scrs[0], rope)
            if dbg == 6:
                continue
            qk_post(p, c, [(K.rr("p h d -> p (h d)"), 8)], 8, 96, gk_bc.rr("p h d -> p (h d)"), kb_[i2], scrs[1], rope)
            if dbg == 7:
                continue
            for (src, dstT, bk, out_d, eng) in ((qb_[i2], qTs[i2], B[0], qT_o, "act"), (kb_[i2], kTs[i2], B[1], kT_o, "dve")):
                bb = bk.bitcast(BF16)
                for h in range(8):
                    p.tr(bb[0:96, h * 128:(h + 1) * 128], src[:, h * 96:(h + 1) * 96], c.identb)
                p.cp(dstT.rr("p a t -> p (a t)"), bb[0:96, :], eng=eng)
                p.dma(out_d[:, :, t * 128:(t + 1) * 128].rr("a p t -> p a t"), dstT)

    with p.scope():
        mufm = c.load_fm(mu_d.rr("(c p) -> c p", p=128), 15)
        om = p.sb([128, 15], F32, "om")
        hm = p.sb([128, 15], F32, "hm")
        p.ts(om, mufm, -1.0, ALU.mult, 1.0, ALU.add)
        p.ts(hm, mufm, 0.5, ALU.mult)
        flags = p.sb([128, 2], F32, "flags")
        p.dma(flags, flags_d)
        W = TL + CTX + 4
        E = [p.sb([128, W], F32, f"E{i}") for i in range(2)]
        for i in range(2):
            p.memset(E[i][:, TL + 2:TL + 3], 0.0)
            p.memset(E[i][:, W - 1:W], 0.0)
        hal = [p.sb([128, 2], F32, f"hal{i}") for i in range(2)]
        sm = [p.sb([128, TL + CTX], F32, f"sm{i}") for i in range(2)]
        tm = [p.sb([128, TL + CTX], F32, f"tm{i}") for i in range(2)]
        wc = [p.sb([128, 8, 128], BF16, f"wc{i}") for i in range(3)]
        blks = []
        t0 = 0
        while t0 < TL:
            n = min(512, TL - t0)
            blks.append((t0, n, 1 + t0))
            t0 += n
        blks.append((TL, CTX, TL + 3))
        ib = 0
        for cc in range(15 if dbg < 2 else 0):
            w = wc[cc % 3]
            e = E[cc % 2]
            p.dma(w, w_in[:, cc * 128:(cc + 1) * 128].rr("(kc p) n -> p kc n", p=128), q="pool")
            for (t0, n, e0) in blks:
                bk = B[ib % 6]
                ib += 1
                for kc in range(8):
                    p.mm(bk[:, 0:n], w[:, kc, :], hT_all[:, kc, t0:t0 + n], start=(kc == 0), stop=(kc == 7))
                p.cp(e[:, e0:e0 + n], bk[:, 0:n], eng=("act" if ib % 2 else "dve"))
            bk = B[6 + cc % 2]
            hoff = (NTL + 2) * 128
            for kc in range(8):
                p.mm(bk[:, 0:2], w[:, kc, :], hT_all[:, kc, hoff:hoff + 2], start=(kc == 0), stop=(kc == 7))
            p.tt(hal[cc % 2], bk[:, 0:2], flags, ALU.mult)
            p.cp(e[:, 0:1], hal[cc % 2][:, 0:1], eng="pool")
            p.cp(e[:, TL + 1:TL + 2], hal[cc % 2][:, 1:2], eng="pool")
            s_, t_ = sm[cc % 2], tm[cc % 2]
            for (o0, n, e0) in ((0, TL, 1), (TL, CTX, TL + 3)):
                p.tt(s_[:, o0:o0 + n], e[:, e0 - 1:e0 - 1 + n], e[:, e0 + 1:e0 + 1 + n], ALU.add, eng="pool")
                p.ts(t_[:, o0:o0 + n], e[:, e0:e0 + n], om[:, cc:cc + 1], ALU.mult)
                p.stt(s_[:, o0:o0 + n], s_[:, o0:o0 + n], hm[:, cc:cc + 1], t_[:, o0:o0 + n], ALU.mult, ALU.add)
            p.dma(rwT[cc], s_)
    return p


LN_X_EPS = 64e-5
LAM = math.exp(-0.5)


def build_mix0(S, do_attn=True, RWP=BF16):
    LT = CTX + S
    NPAIR = LT // 128
    p = Prog()
    rw_r = p.dram("rw_r", [128, LT], F32, "ExternalInput")
    rw_k = p.dram("rw_k", [128, LT], F32, "ExternalInput")
    rw_v = p.dram("rw_v", [128, LT], F32, "ExternalInput")
    lo_w = p.dram("lo_w", [128, LT], F32, "ExternalInput")
    lo_a = p.dram("lo_a", [128, LT], F32, "ExternalInput")
    lo_g = p.dram("lo_g", [128, LT], F32, "ExternalInput")
    w2_d = p.dram("w2", [128, 128], F32, "ExternalInput")
    a2_d = p.dram("a2", [128, 128], F32, "ExternalInput")
    g2_d = p.dram("g2", [128, 128], F32, "ExternalInput")
    pv_d = p.dram("pv", [9, 128], F32, "ExternalInput")
    yT = p.dram("yT", [2, 128, LT], F32, "ExternalOutput")
    rwoT = p.dram("rwoT", [128, LT], BF16, "ExternalOutput")
    if do_attn:
        qT = p.dram("qT", [2, 96, S], BF16, "ExternalInput")
        qcT = p.dram("qcT", [2, 96, CTX], BF16, "ExternalInput")
        kT = p.dram("kT", [2, 96, LT], BF16, "ExternalInput")
        v_d = p.dram("v", [2, LT, 64], BF16, "ExternalInput")
        oT = p.dram("oT", [2, 64, S], BF16, "ExternalOutput")
        ocT = p.dram("ocT", [2, 64, CTX], BF16, "ExternalOutput")
    c = Ctx(p, banks=False)
    _outer = p.scope()
    _outer.__enter__()
    c.set_banks(False)
    B = c.banks
    pv = c.load_fm(pv_d, 9)
    W0 = [pv[:, 0:1], pv[:, 1:2]]
    A0 = [pv[:, 2:3], pv[:, 3:4]]
    KK_, KA_, RK_, LNW, LNB = pv[:, 4:5], pv[:, 5:6], pv[:, 6:7], pv[:, 7:8], pv[:, 8:9]
    omka = p.sb([128, 2], F32, "omka")
    p.ts(omka[:, 0:1], KA_, -1.0, ALU.mult, 1.0, ALU.add)
    p.ts(omka[:, 1:2], KA_, -2.0, ALU.mult, 2.0, ALU.add)
    w2s = p.sb([128, 128], F32, "w2s")
    a2s = p.sb([128, 128], F32, "a2s")
    g2s = p.sb([128, 128], F32, "g2s")
    p.dma(w2s, w2_d)
    p.dma(a2s, a2_d)
    p.dma(g2s, g2_d)
    w2z, a2z = [], []
    for d in range(2):
        for (src_, lst_) in ((w2s, w2z), (a2s, a2z)):
            z = p.sb([128, 128], F32, "loraz")
            p.memset(z, 0.0)
            dp_ = slice(64 * d, 64 * d + 64)
            p.cp(z[dp_, :], src_[dp_, :], eng="pool")
            lst_.append(z)
    p64 = p.sb([128, 1], F32, "p64")
    p.ts(p64, c.pidx, 63.5, ALU.is_gt)
    c64 = p.sb([128, 128], F32, "c64")
    p.ts(c64, c.iof, 63.5, ALU.is_gt)
    same = p.sb([128, 128], F32, "same")
    p.ts(same, c64, p64[:, 0:1], ALU.is_equal)
    masks = {}
    for nm, op_ in (("SU", ALU.is_gt), ("SL", ALU.is_lt), ("IU", ALU.is_ge), ("IL", ALU.is_le)):
        m = p.sb([128, 2, 128], F32, "mask" + nm)
        p.ts(m[:, 0, :], c.iof, c.pidx[:, 0:1], op_)
        p.tt(m[:, 0, :], m[:, 0, :], same, ALU.mult)
        p.cp(m[:, 1, :], m[:, 0, :])
        masks[nm] = m.rr("p a t -> p (a t)")
    ones64 = p.sb([128, 128], F32, "ones64")
    p.ts(ones64, same, 1.0 / 64.0, ALU.mult)
    ident2 = p.sb([128, 2, 128], F32, "ident2")
    p.cp(ident2[:, 0, :], c.identf)
    p.cp(ident2[:, 1, :], c.identf)
    ident2 = ident2.rr("p a t -> p (a t)")
    rmask = p.sb([128, 512], F32, "rmask")
    p.memset(rmask, 1.0)
    p.memset(rmask.rr("p (a t) -> p a t", t=64)[:, :, 0:1], 0.0)

    blocks = [(0, CTX)]
    t0 = CTX
    while t0 < LT:
        blocks.append((t0, 512))
        t0 += 512
    NB = len(blocks)
    order = [list(range(NB)), [0] + list(range(NB - 1, 0, -1))]

    with p.scope():
        def T(shape, nm, dt=F32):
            return p.sb(shape, dt, nm)
        ops_ = []
        for d in range(2):
            two = []
            for i in range(2):
                two.append(dict(Rt=T([128, 512], "Rt"), Rt16=T([128, 512], "Rt16", RWP), Kt=T([128, 512], "Kt", RWP),
                                Bt=T([128, 512], "Bt", RWP), At=T([128, 512], "At", RWP),
                                A_tm=T([128, 4, 128], "A_tm", RWP), K_tm=T([128, 4, 128], "K_tm", RWP), B_tm=T([128, 4, 128], "B_tm", RWP),
                                V_tm=T([128, 4, 128], "V_tm", RWP), etot=T([128, 8], "etot"), yb=T([128, 512], "yb")))
            ops_.append(two)
        tmp = {k: T([128, 512], k) for k in ("r", "k", "v", "lw", "la", "th", "sg", "a", "kk", "t1", "t2", "c", "E", "kd", "b", "Kh", "Bh")}
        tot = T([128, 8], "tot")
        Hbd = [[T([128, 128], f"H{d}{i}") for i in range(2)] for d in range(2)]
        H16 = [[T([128, 128], f"H16{d}{i}", RWP) for i in range(2)] for d in range(2)]
        for d in range(2):
            for i in range(2):
                p.memset(Hbd[d][i], 0.0)
                p.memset(H16[d][i], 0.0)
        hcur = [0, 0]
        pairbuf = []
        for d in range(2):
            pairbuf.append({k: T([128, 256], f"{k}{d}", RWP) for k in ("X", "XT", "X2", "XT2", "P", "P2", "MAK")}
                           | {k: T([128, 256], f"{k}{d}", RWP) for k in ("MRK", "MRB", "WT")}
                           | {"AT": T([128, 128], f"AT{d}", RWP), "U": T([128, 128], f"U{d}", RWP)})
            p.memset(pairbuf[d]["U"], 0.0)

        def prep(d, bi, ob):
            t0, n = blocks[bi]
            tok = slice(t0, t0 + n)
            nch = n // 64
            dp = slice(64 * d, 64 * d + 64)
            x = tmp
            p.dma(x["r"][:, 0:n], rw_r[:, tok])
            p.dma(x["k"][:, 0:n], rw_k[:, tok])
            p.dma(x["v"][:, 0:n], rw_v[:, tok])
            p.dma(x["lw"][:, 0:n], lo_w[:, tok])
            p.dma(x["la"][:, 0:n], lo_a[:, tok])
            N = slice(0, n)
            p.act(x["th"][:, N], x["lw"][:, N], AF.Tanh)
            p.mm(B[6][:, N], w2z[d], x["th"][:, N])
            p.act(x["sg"][:, N], B[6][:, N], AF.Sigmoid, bias=W0[d])
            p.mm(B[4][:, N], a2z[d], x["la"][:, N])
            p.act(x["a"][:, N], B[4][:, N], AF.Sigmoid, bias=A0[d])
            p.ts(x["kk"][:, N], x["k"][:, N], KK_, ALU.mult)
            p.tt(x["t1"][:, N], x["kk"][:, N], x["kk"][:, N], ALU.mult, eng="pool")
            p.mm(B[6][:, N], same, x["t1"][:, N])
            p.ts(x["t2"][:, N], B[6][:, N], 1e-12, ALU.add)
            p.act(x["t2"][:, N], x["t2"][:, N], AF.Ln)
            p.act(x["t2"][:, N], x["t2"][:, N], AF.Exp, scale=-0.5)
            p.tt(x["kk"][:, N], x["kk"][:, N], x["t2"][:, N], ALU.mult)
            p.ts(x["t1"][:, N], x["a"][:, N], KA_, ALU.mult, omka[:, 0:1], ALU.add)
            p.tt(x["kd"][:, N], x["k"][:, N], x["t1"][:, N], ALU.mult, eng="pool")
            p.tt(x["b"][:, N], x["kk"][:, N], x["a"][:, N], ALU.mult, eng="pool")
            p.op("dve", lambda e: e.tensor_tensor_scan(x["c"].ap[:, N], rmask.ap[:, N], x["sg"].ap[:, N], 0.0,
                                                       ALU.mult, ALU.add), [rmask, x["sg"]], [x["c"]])
            cv = x["c"][:, N].rr("p (a t) -> p a t", t=64)
            p.cp(tot[:, 0:nch], cv[:, :, 63])
            if d == 1:
                p.tt(x["c"][:, N], x["sg"][:, N], x["c"][:, N], ALU.subtract)
                p.tt(cv, cv, tot[:, 0:nch].un(2).bc([128, nch, 64]), ALU.add)
            p.act(ob["etot"][:, 0:nch], tot[:, 0:nch], AF.Exp, scale=-LAM)
            p.act(x["E"][:, N], x["c"][:, N], AF.Exp, scale=-LAM)
            p.tt(ob["Rt"][:, N], x["r"][:, N], x["E"][:, N], ALU.mult)
            p.cp(ob["Rt16"][:, N], ob["Rt"][:, N], eng="pool")
            p.act(x["E"][:, N], x["c"][:, N], AF.Exp, scale=LAM)
            p.tt(ob["Kt"][:, N], x["kd"][:, N], x["E"][:, N], ALU.mult)
            p.tt(ob["Bt"][:, N], x["b"][:, N], x["E"][:, N], ALU.mult, eng="pool")
            p.tt(x["t1"][:, N], x["c"][:, N], x["sg"][:, N], ALU.subtract)
            p.act(x["E"][:, N], x["t1"][:, N], AF.Exp, scale=-LAM)
            p.stt(ob["At"][:, N], x["kk"][:, N], -1.0, x["E"][:, N], ALU.mult, ALU.mult)
            p.tt(x["t2"][:, N].rr("p (a t) -> p a t", t=64), tot[:, 0:nch].un(2).bc([128, nch, 64]), cv, ALU.subtract)
            p.act(x["E"][:, N], x["t2"][:, N], AF.Exp, scale=-LAM)
            p.tt(x["Kh"][:, N], x["kd"][:, N], x["E"][:, N], ALU.mult)
            p.tt(x["Bh"][:, N], x["b"][:, N], x["E"][:, N], ALU.mult, eng="pool")
            npair = n // 128
            for (src, dst, bk, eng) in ((ob["At"], ob["A_tm"], B[4], "act"), (x["Kh"], ob["K_tm"], B[6], "dve"),
                                        (x["Bh"], ob["B_tm"], B[4], "act"), (x["v"], ob["V_tm"], B[6], "dve")):
                is16 = (src is ob["At"]) and RWP == BF16
                bkv = bk.bitcast(BF16) if is16 else bk
                for pr in range(npair):
                    p.tr(bkv[:, pr * 128:(pr + 1) * 128], src[:, pr * 128:(pr + 1) * 128], c.identb if is16 else c.identf)
                p.cp(dst.rr("p a t -> p (a t)")[:, 0:n], bkv[:, 0:n], eng=eng)

        def pair_level(d, ob, pr):
            pb = pairbuf[d]
            Tk = slice(pr * 128, (pr + 1) * 128)
            mN, mM, mI = (("SU", "SL", "IU") if d == 0 else ("SL", "SU", "IL"))
            bk0, bk1 = B[2], B[3]
            HB = ((B[0], B[2]), (B[1], B[3]))

            def prod(slot, lhs, rhs):
                for h in range(2):
                    hp = slice(64 * h, 64 * h + 64)
                    p.mm(HB[h][slot][:, 0:128], ob[lhs][hp, Tk], ob[rhs][hp, Tk])

            def evac(slot, dst, mk):
                for h in range(2):
                    hc = slice(128 * h, 128 * h + 128)
                    p.tt(pb[dst][:, hc], HB[h][slot][:, 0:128], masks[mk][:, 0:128], ALU.mult)

            prod(0, "Bt", "At")
            prod(1, "At", "Bt")
            evac(0, "X", mN)
            evac(1, "XT", mM)
            prod(0, "At", "Kt")
            prod(1, "Kt", "Rt16")
            evac(0, "MAK", mM)
            evac(1, "MRK", mI)
            prod(0, "Bt", "Rt16")
            evac(0, "MRB", mI)
            p.tt(pb["P"], pb["X"], ident2, ALU.add, eng="pool")
            X, XT, Pc = pb["X"], pb["XT"], pb["P"]
            X2, XT2, P2 = pb["X2"], pb["XT2"], pb["P2"]
            for k in range(1, 6):
                for h in range(2):
                    hc = slice(128 * h, 128 * h + 128)
                    p.mm(bk1[:, hc], X[:, hc], XT[:, hc])
                    if k < 5:
                        p.mm(bk0[:, hc], XT[:, hc], X[:, hc])
                p.cp(XT2, bk1[:, 0:256], eng="act")
                if k < 5:
                    p.cp(X2, bk0[:, 0:256])
                for h in range(2):
                    hc = slice(128 * h, 128 * h + 128)
                    p.mm(bk0[:, hc], XT2[:, hc], Pc[:, hc])
                p.tt(P2, bk0[:, 0:256], Pc, ALU.add)
                X, X2 = X2, X
                XT, XT2 = XT2, XT
                Pc, P2 = P2, Pc
            for h in range(2):
                hc = slice(128 * h, 128 * h + 128)
                p.mm(bk1[:, hc], ob["A_tm"][:, pr, :], Pc[:, hc])
                p.mm(bk0[:, hc], pb["MAK"][:, hc], Pc[:, hc])
            p.cp(pb["AT"][0:64, :], bk1[0:64, 0:128], eng="act")
            p.cp(pb["AT"][64:128, :], bk1[64:128, 128:256], eng="act")
            p.cp(pb["WT"], bk0[:, 0:256])

        def chunk_level(d, ob, pr, pi):
            pb = pairbuf[d]
            cp_ = slice(64 * pi, 64 * pi + 64)
            ch = pr * 2 + pi
            tcol = slice(pr * 128 + 64 * pi, pr * 128 + 64 * pi + 64)
            Hold = Hbd[d][hcur[d]]
            Hnew = Hbd[d][1 - hcur[d]]
            Hold16 = H16[d][hcur[d]]
            Hnew16 = H16[d][1 - hcur[d]]
            hcur[d] = 1 - hcur[d]
            bu, bh, by = B[4], (B[5] if d == 0 else B[7]), B[6]
            for h in range(2):
                hc = slice(128 * h, 128 * h + 128)
                ic = slice(64 * h, 64 * h + 64)
                p.mm(bu[:, ic], pb["WT"][:, hc], ob["V_tm"][:, pr, ic], start=True, stop=False)
                p.mm(bu[:, ic], pb["AT"], Hold16[:, ic], start=False, stop=True)
            p.cp(pb["U"][cp_, :], bu[cp_, 0:128], eng="act")
            for h in range(2):
                ic = slice(64 * h, 64 * h + 64)
                p.mm(bh[:, ic], ob["K_tm"][cp_, pr, :], ob["V_tm"][cp_, pr, ic], start=True, stop=False)
                p.mm(bh[:, ic], ob["B_tm"][cp_, pr, :], pb["U"][cp_, ic], start=False, stop=True)
            for h in range(2):
                hp = slice(64 * h, 64 * h + 64)
                ic = slice(64 * h, 64 * h + 64)
                p.stt(Hnew[hp, ic], Hold[hp, ic], ob["etot"][hp, ch:ch + 1], bh[hp, ic], ALU.mult, ALU.add)
            p.cp(Hnew16, Hnew, eng="pool")
            for h in range(2):
                ys = slice(64 * h, 64 * h + 64)
                mcol = slice(128 * h + 64 * pi, 128 * h + 64 * pi + 64)
                p.mm(by[:, ys], Hold16, ob["Rt16"][:, tcol], start=True, stop=False)
                p.mm(by[:, ys], ob["V_tm"][:, pr, :], pb["MRK"][:, mcol], start=False, stop=False)
                p.mm(by[:, ys], pb["U"], pb["MRB"][:, mcol], start=False, stop=True)
            p.cp(ob["yb"][0:64, tcol], by[0:64, 0:64], eng="act")
            p.cp(ob["yb"][64:128, tcol], by[64:128, 64:128], eng="act")

        for step in range(NB):
            for d in range(2):
                bi = order[d][step]
                t0, n = blocks[bi]
                ob = ops_[d][step % 2]
                prep(d, bi, ob)
                npair = n // 128
                prs = range(npair) if d == 0 else range(npair - 1, -1, -1)
                for pr in prs:
                    pair_level(d, ob, pr)
                    for pi in ((0, 1) if d == 0 else (1, 0)):
                        chunk_level(d, ob, pr, pi)
                p.dma(yT[d][:, t0:t0 + n], ob["yb"][:, 0:n])

    with p.scope():
        x = {k: p.sb([128, 512], F32, k) for k in ("y0", "y1", "r", "k", "v", "la", "lg", "a0", "a1", "t1", "t2", "t3", "g")}
        ob_ = [p.sb([128, 512], BF16, f"rwo{i}") for i in range(2)]
        for bi, (t0, n) in enumerate(blocks):
            tok = slice(t0, t0 + n)
            N = slice(0, n)
            p.dma(x["y0"][:, N], yT[0][:, tok])
            p.dma(x["y1"][:, N], yT[1][:, tok])
            p.dma(x["r"][:, N], rw_r[:, tok])
            p.dma(x["k"][:, N], rw_k[:, tok])
            p.dma(x["v"][:, N], rw_v[:, tok])
            p.dma(x["la"][:, N], lo_a[:, tok])
            p.dma(x["lg"][:, N], lo_g[:, tok])
            p.tt(x["y0"][:, N], x["y0"][:, N], x["y1"][:, N], ALU.add)
            p.mm(B[0][:, N], ones64, x["y0"][:, N])
            p.tt(x["y0"][:, N], x["y0"][:, N], B[0][:, N], ALU.subtract)
            p.tt(x["t1"][:, N], x["y0"][:, N], x["y0"][:, N], ALU.mult, eng="pool")
            p.mm(B[1][:, N], ones64, x["t1"][:, N])
            p.ts(x["t1"][:, N], B[1][:, N], LN_X_EPS, ALU.add)
            p.act(x["t1"][:, N], x["t1"][:, N], AF.Ln)
            p.act(x["t1"][:, N], x["t1"][:, N], AF.Exp, scale=-0.5)
            p.tt(x["y0"][:, N], x["y0"][:, N], x["t1"][:, N], ALU.mult)
            p.ts(x["y0"][:, N], x["y0"][:, N], LNW, ALU.mult, LNB, ALU.add)
            for d in range(2):
                p.mm(B[2 + d][:, N], a2z[d], x["la"][:, N])
                p.act(x["a%d" % d][:, N], B[2 + d][:, N], AF.Sigmoid, bias=A0[d])
            p.tt(x["a0"][:, N], x["a0"][:, N], x["a1"][:, N], ALU.add, eng="pool")
            p.ts(x["a0"][:, N], x["a0"][:, N], KA_, ALU.mult, omka[:, 1:2], ALU.add)
            p.tt(x["t2"][:, N], x["k"][:, N], x["a0"][:, N], ALU.mult, eng="pool")
            p.stt(x["t2"][:, N], x["t2"][:, N], RK_, x["r"][:, N], ALU.mult, ALU.mult)
            p.mm(B[4][:, N], same, x["t2"][:, N])
            p.tt(x["t3"][:, N], B[4][:, N], x["v"][:, N], ALU.mult)
            p.tt(x["y0"][:, N], x["y0"][:, N], x["t3"][:, N], ALU.add, eng="pool")
            p.act(x["lg"][:, N], x["lg"][:, N], AF.Sigmoid)
            p.mm(B[5][:, N], g2s, x["lg"][:, N])
            o = ob_[bi % 2]
            p.tt(o[:, N], x["y0"][:, N], B[5][:, N], ALU.mult)
            p.dma(rwoT[:, tok], o[:, N])

    _outer.__exit__(None, None, None)
    if do_attn:
        with p.scope():
            c.set_banks(True)
            attention_block(p, c, qT, kT, v_d, oT, 2, 2, 96, 64, S, 96 ** -0.5, qcT_d=qcT, ocT_d=ocT)
    return p


def _make_pos(q, TL):
    NTL = TL // 128
    t = q * TL + np.arange(TL)
    pos = np.stack([t // 64, t % 64], -1).astype(np.float32)
    return np.ascontiguousarray(pos.reshape(NTL, 128, 2).transpose(1, 0, 2))


def _cat(xs, axis):
    return np.ascontiguousarray(np.concatenate(xs, axis=axis))


def kernel(x, c, ctx, c_ctx, ada_w, ada_b, norm1_g, norm2_g, ab_w_in, ab_w_out, rw_mu, rw_w0,
           rw_w2, rw_a0, rw_a2, rw_k_k, rw_k_a, rw_r_k, rw_g2, rw_ln_w, rw_ln_b, mla_g_qa,
           mla_w_q_up, mla_g_kva, mla_w_kv_up, mla_g_q, mla_g_k, gqa_w_in, gqa_w_out, gqa_g_q,
           gqa_g_k, router_w, router_b, moe_w_gate, moe_w_up, moe_w_down, shared_w_gate,
           shared_w_up, shared_w_down):
    f = lambda a: np.ascontiguousarray(np.asarray(a, dtype=np.float32))
    x, c, ctx, c_ctx = f(x), f(c), f(ctx), f(c_ctx)
    S = x.shape[1]
    TL = S // 4
    LT = CTX + S
    R8 = range(8)
    maps = [{"cvec": np.stack([c[r // 4], c_ctx]), "ada_w": f(ada_w), "ada_b": f(ada_b)} for r in R8]
    r0 = run_prog(build_phase0(), maps)
    mods_tm = [f(r0[r]["mods_tm"]) for r in R8]
    mods_fm = [f(r0[r]["mods_fm"]) for r in R8]
    maps = []
    for r in R8:
        b, q = r // 4, r % 4
        halo = np.zeros((128, D), np.float32)
        fl = np.zeros((128, 2), np.float32)
        if q > 0:
            halo[0] = x[b, q * TL - 1]
            fl[:, 0] = 1
        if q < 3:
            halo[1] = x[b, (q + 1) * TL]
            fl[:, 1] = 1
        maps.append({"x": _cat([x[b, q * TL:(q + 1) * TL], ctx[b], halo], 0), "mods_fm": mods_fm[r][0],
                     "norm1_g": f(norm1_g)[0], "w_in": f(ab_w_in)[0], "mu": f(rw_mu)[0], "flags": fl,
                     "g_qa": f(mla_g_qa), "g_kva": f(mla_g_kva), "w_q_up": f(mla_w_q_up)[0],
                     "w_kv_up": f(mla_w_kv_up)[0], "g_q": f(mla_g_q), "g_k": f(mla_g_k), "pos": _make_pos(q, TL)})
    rA = run_prog(build_pre0(S), maps)
    maps = []
    rw_r_k_flat = f(rw_r_k)[0].reshape(512)
    for r in R8:
        b, j = r // 4, r % 4
        cores = [4 * b + qq for qq in range(4)]
        rwT = _cat([rA[4 * b]["rwT"][:, :, TL:]] + [rA[cc]["rwT"][:, :, :TL] for cc in cores], 2)
        cs = slice(128 * j, 128 * j + 128)
        pv = np.stack([f(rw_w0)[0, 0, cs], f(rw_w0)[0, 1, cs], f(rw_a0)[0, 0, cs], f(rw_a0)[0, 1, cs],
                       f(rw_k_k)[0, cs], f(rw_k_a)[0, cs], rw_r_k_flat[cs], f(rw_ln_w)[0, cs], f(rw_ln_b)[0, cs]])
        hs = slice(2 * j, 2 * j + 2)
        qT = _cat([rA[cc]["qT"][hs, :, :TL] for cc in cores], 2)
        qcT = np.ascontiguousarray(rA[4 * b]["qT"][hs, :, TL:])
        kT = _cat([rA[4 * b]["kT"][hs, :, TL:]] + [rA[cc]["kT"][hs, :, :TL] for cc in cores], 2)
        vv = _cat([rA[4 * b]["v"][TL:]] + [rA[cc]["v"][:TL] for cc in cores], 0)
        vv = np.ascontiguousarray(vv.reshape(LT, 8, 64)[:, hs].transpose(1, 0, 2))
        maps.append({"rw_r": np.ascontiguousarray(rwT[j]), "rw_k": np.ascontiguousarray(rwT[4 + j]),
                     "rw_v": np.ascontiguousarray(rwT[8 + j]), "lo_w": np.ascontiguousarray(rwT[12]),
                     "lo_a": np.ascontiguousarray(rwT[13]), "lo_g": np.ascontiguousarray(rwT[14]),
                     "w2": np.ascontiguousarray(f(rw_w2)[0][:, :, cs].reshape(128, 128)),
                     "a2": np.ascontiguousarray(f(rw_a2)[0][:, :, cs].reshape(128, 128)),
                     "g2": np.ascontiguousarray(f(rw_g2)[0][:, cs]), "pv": np.ascontiguousarray(pv),
                     "qT": qT, "qcT": qcT, "kT": kT, "v": vv})
    rB = run_prog(build_mix0(S), maps)
    del rA
    wg0 = _cat([f(moe_w_gate)[0], f(shared_w_gate)[0][None]], 0)
    wu0 = _cat([f(moe_w_up)[0], f(shared_w_up)[0][None]], 0)
    wd0 = _cat([f(moe_w_down)[0], f(shared_w_down)[0][None]], 0)
    maps = []
    for r in R8:
        b, q = r // 4, r % 4
        chunks = []
        for kc in range(4):
            rw = rB[4 * b + kc]["rwoT"]
            chunks.append(_cat([rw[:, CTX + q * TL:CTX + (q + 1) * TL], rw[:, :CTX]], 1))
        for j in range(4):
            o = rB[4 * b + j]["oT"].reshape(128, S)
            oc = rB[4 * b + j]["ocT"].reshape(128, CTX)
            chunks.append(_cat([o[:, q * TL:(q + 1) * TL], oc], 1))
        maps.append({"x": _cat([x[b, q * TL:(q + 1) * TL], ctx[b]], 0), "attT": np.ascontiguousarray(np.stack(chunks, 0)),
                     "mods_tm": mods_tm[r][0], "mods_fm": mods_fm[r][0], "w_out": f(ab_w_out)[0],
                     "norm2_g": f(norm2_g)[0], "router_w": f(router_w), "router_b": f(router_b)[None, :],
                     "wg_all": wg0, "wu_all": wu0, "wd_all": wd0})
    rC = run_prog(build_post(S, True), maps)
    del rB
    x1 = [f(rC[r]["x_out"]) for r in R8]
    maps = []
    for r in R8:
        q = r % 4
        maps.append({"x": x1[r], "mods_fm": mods_fm[r][1], "norm1_g": f(norm1_g)[1], "w_in": f(gqa_w_in)[0],
                     "g_q": f(gqa_g_q), "g_k": f(gqa_g_k), "pos": _make_pos(q, TL)})
    rD = run_prog(build_pre1(S), maps)
    maps = []
    for r in R8:
        b, kvh = r // 4, r % 4
        cores = [4 * b + qq for qq in range(4)]
        qk = _cat([rD[cc]["qkT"][:, :, :TL] for cc in cores], 2).reshape(20, 64, S)
        kc_ = rD[4 * b]["qkT"][:, :, TL:].reshape(20, 64, CTX)
        qT = np.ascontiguousarray(qk[4 * kvh:4 * kvh + 4])
        kT = _cat([kc_[16 + kvh], qk[16 + kvh]], 1)[None]
        vv = _cat([rD[4 * b]["v"][TL:]] + [rD[cc]["v"][:TL] for cc in cores], 0)
        vv = np.ascontiguousarray(vv[:, kvh * 64:(kvh + 1) * 64])[None]
        maps.append({"qT": qT, "kT": np.ascontiguousarray(kT), "v": vv})
    rE = run_prog(build_attn1(S), maps)
    del rD
    wg1 = _cat([f(moe_w_gate)[1], f(shared_w_gate)[1][None]], 0)
    wu1 = _cat([f(moe_w_up)[1], f(shared_w_up)[1][None]], 0)
    wd1 = _cat([f(moe_w_down)[1], f(shared_w_down)[1][None]], 0)
    maps = []
    for r in R8:
        b, q = r // 4, r % 4
        chunks = []
        for kc in range(8):
            o = rE[4 * b + kc // 2]["oT"]
            i0 = (kc % 2) * 2
            chunks.append(o[i0:i0 + 2].reshape(128, S)[:, q * TL:(q + 1) * TL])
        maps.append({"x": np.ascontiguousarray(x1[r][:TL]), "attT": np.ascontiguousarray(np.stack(chunks, 0)),
                     "mods_tm": mods_tm[r][1], "mods_fm": mods_fm[r][1], "w_out": f(gqa_w_out)[0],
                     "norm2_g": f(norm2_g)[1], "router_w": f(router_w), "router_b": f(router_b)[None, :],
                     "wg_all": wg1, "wu_all": wu1, "wd_all": wd1})
    rF = run_prog(build_post(S, False), maps)
    out = np.zeros((2, S, D), np.float32)
    for r in R8:
        b, q = r // 4, r % 4
        out[b, q * TL:(q + 1) * TL] = rF[r]["x_out"]
    return out
```

```python
import numpy as np
import concourse.bass as bass
import concourse.mybir as mybir
from concourse.bass_utils import run_bass_kernel_spmd

F32 = mybir.dt.float32
BF16 = mybir.dt.bfloat16
I32 = mybir.dt.int32
AF = mybir.ActivationFunctionType
ALU = mybir.AluOpType
AX = mybir.AxisListType

EPOCH = 30000
SCHED_WINDOW = 40
NSLOT = {"sp": 24, "pool": 12, "act": 8}


class Buf:
    __slots__ = ("name", "lw", "rd", "excl")

    def __init__(self, name, excl=False):
        self.name = name
        self.lw = None
        self.rd = []
        self.excl = excl


class V:
    __slots__ = ("buf", "ap")

    def __init__(self, buf, ap):
        self.buf = buf
        self.ap = ap

    def __getitem__(self, idx):
        return V(self.buf, self.ap[idx])

    def rr(self, pat, **kw):
        return V(self.buf, self.ap.rearrange(pat, **kw))

    def bc(self, shape):
        return V(self.buf, self.ap.broadcast_to(shape))

    def un(self, axis):
        return V(self.buf, self.ap.unsqueeze(axis))

    def bitcast(self, dt):
        return V(self.buf, self.ap.bitcast(dt))

    @property
    def shape(self):
        return self.ap.shape


class Op:
    __slots__ = ("eng", "fn", "deps", "sig", "cnt", "isdma", "slot", "slotval", "prevval", "cost")

    def __init__(self, eng, fn, isdma):
        self.cost = 300.0
        self.eng = eng
        self.fn = fn
        self.deps = set()
        self.sig = False
        self.cnt = 0
        self.isdma = isdma
        self.slot = None
        self.slotval = 0
        self.prevval = 0


class _Scope:
    def __init__(self, p):
        self.p = p

    def __enter__(self):
        self.p._scopes.append([])
        return self

    def __exit__(self, *a):
        p = self.p
        items = p._scopes.pop()
        ops = set(p._fence)
        for cm, b in items:
            if b.lw is not None:
                ops.add(b.lw)
            ops.update(b.rd)
        best = {}
        keep = []
        for i in ops:
            o = p.ops[i]
            if o.isdma:
                keep.append(i)
            else:
                if o.eng not in best or best[o.eng] < i:
                    best[o.eng] = i
        p._fence = keep + list(best.values())
        for cm, b in reversed(items):
            cm.__exit__(None, None, None)
        return False


class Prog:
    ENGS = ("pe", "act", "dve", "pool", "sp")

    def __init__(self):
        self.nc = bass.Bass("TRN2", target_bir_lowering=False)
        self.ops = []
        self._ctx = []
        self.ntile = 0
        self._scopes = []
        self._fence = []
        self._dcount = {q: 0 for q in NSLOT}

    def dram(self, name, shape, dt, kind):
        t = self.nc.dram_tensor(name, list(shape), dt, kind=kind)
        return V(Buf(name), t.ap())

    def sb(self, shape, dt, name=None):
        self.ntile += 1
        name = name or f"t{self.ntile}"
        cm = self.nc.sbuf_tensor(f"{name}_{self.ntile}", list(shape), dt)
        h = cm.__enter__()
        b = Buf(name)
        b.rd = list(self._fence)
        if self._scopes:
            self._scopes[-1].append((cm, b))
        else:
            self._ctx.append(cm)
        return V(b, h[:])

    def scope(self):
        return _Scope(self)

    def ps(self, shape, dt, name=None):
        self.ntile += 1
        name = name or f"p{self.ntile}"
        cm = self.nc.psum_tensor(f"{name}_{self.ntile}", list(shape), dt)
        h = cm.__enter__()
        b = Buf(name, excl=True)
        b.rd = list(self._fence)
        if self._scopes:
            self._scopes[-1].append((cm, b))
        else:
            self._ctx.append(cm)
        return V(b, h[:])

    def op(self, eng, fn, reads, writes, isdma=False, cost=None):
        i = len(self.ops)
        o = Op(eng, fn, isdma)
        if cost is None:
            w0 = next((v for v in writes if v is not None), None)
            n = 1
            if w0 is not None:
                for d_ in w0.ap.shape[1:]:
                    n *= d_
            if isdma:
                cost = 2000.0 + n * 128 * 4 / 150.0
            elif eng == "act":
                cost = 230.0 + n / 1.2
            elif eng == "dve":
                cost = 70.0 + n / 0.96
            elif eng == "pool":
                cost = 150.0 + n / 0.96
            else:
                cost = 300.0
        o.cost = cost
        rb = {id(v.buf): v.buf for v in reads if v is not None}
        wb = {id(v.buf): v.buf for v in writes if v is not None}
        for b in rb.values():
            if b.lw is not None:
                o.deps.add(b.lw)
            if b.excl:
                for r in b.rd:
                    if self.ops[r].eng != eng:
                        o.deps.add(r)
        for b in wb.values():
            if b.lw is not None:
                o.deps.add(b.lw)
            for r in b.rd:
                o.deps.add(r)
        o.deps.discard(i)
        for b in rb.values():
            if id(b) not in wb:
                b.rd.append(i)
        for b in wb.values():
            b.lw = i
            b.rd = []
        self.ops.append(o)
        return i

    def dma(self, out, in_, q="sp", **kw):
        self.op(q, lambda e: e.dma_start(out=out.ap, in_=in_.ap, **kw), [in_], [out], isdma=True)

    def mm(self, out, lhsT, rhs, start=True, stop=True, **kw):
        n = 1
        for d_ in rhs.ap.shape[1:]:
            n *= d_
        passes = 4 if rhs.ap.dtype == F32 else 1
        self.op("pe", lambda e: e.matmul(out.ap, lhsT.ap, rhs.ap, start=start, stop=stop, **kw),
                [lhsT, rhs], [out], cost=30.0 + max(n, 64) * passes * 0.45)

    def tr(self, out, in_, ident):
        passes = 4 if in_.ap.dtype == F32 else 1
        self.op("pe", lambda e: e.transpose(out.ap, in_.ap, ident.ap), [in_, ident], [out],
                cost=60.0 + 128 * passes * 0.45)

    def act(self, out, in_, func, bias=None, scale=None, accum=None):
        kw = {}
        rd = [in_]
        if bias is not None:
            if isinstance(bias, V):
                kw["bias"] = bias.ap
                rd.append(bias)
            else:
                kw["bias"] = bias
        if scale is not None:
            if isinstance(scale, V):
                kw["scale"] = scale.ap
                rd.append(scale)
            else:
                kw["scale"] = scale
        wr = [out]
        if accum is not None:
            kw["accum_out"] = accum.ap
            wr.append(accum)
        self.op("act", lambda e: e.activation(out.ap, in_.ap, func, **kw), rd, wr)

    def tt(self, out, a, b, op, eng="dve"):
        self.op(eng, lambda e: e.tensor_tensor(out.ap, a.ap, b.ap, op), [a, b], [out])

    def ts(self, out, a, s1, op0, s2=None, op1=None, eng="dve", accum=None):
        rd = [a]
        x1 = s1.ap if isinstance(s1, V) else s1
        x2 = s2.ap if isinstance(s2, V) else s2
        if isinstance(s1, V):
            rd.append(s1)
        if isinstance(s2, V):
            rd.append(s2)
        kw = {}
        wr = [out]
        if op1 is not None:
            kw["op1"] = op1
        if accum is not None:
            kw["accum_out"] = accum.ap
            wr.append(accum)
        self.op(eng, lambda e: e.tensor_scalar(out.ap, a.ap, x1, x2, op0, **kw), rd, wr)

    def stt(self, out, a, s, b, op0, op1, eng="dve"):
        rd = [a, b]
        x = s.ap if isinstance(s, V) else s
        if isinstance(s, V):
            rd.append(s)
        self.op(eng, lambda e: e.scalar_tensor_tensor(out.ap, a.ap, x, b.ap, op0, op1), rd, [out])

    def cp(self, out, in_, eng="dve"):
        if eng == "act":
            self.op("act", lambda e: e.copy(out.ap, in_.ap), [in_], [out])
        else:
            self.op(eng, lambda e: e.tensor_copy(out.ap, in_.ap), [in_], [out])

    def red(self, out, in_, op=ALU.add, axis=AX.X, eng="dve"):
        self.op(eng, lambda e: e.tensor_reduce(out.ap, in_.ap, axis, op), [in_], [out])

    def memset(self, out, val, eng="pool"):
        self.op(eng, lambda e: e.memset(out.ap, val), [], [out])

    def iota(self, out, pattern, base=0, cm=0):
        self.op("pool", lambda e: e.iota(out.ap, pattern, base=base, channel_multiplier=cm,
                                         allow_small_or_imprecise_dtypes=True), [], [out])

    def _schedule(self, ops):
        n = len(ops)
        W = SCHED_WINDOW
        SEM = 120.0
        users = [[] for _ in range(n)]
        nleft = [0] * n
        for i, o in enumerate(ops):
            nleft[i] = len(o.deps)
            for j in o.deps:
                users[j].append(i)
        pend = {e: [i for i, o in enumerate(ops) if o.eng == e] for e in self.ENGS}
        head = {e: 0 for e in self.ENGS}
        done = [False] * n
        finish = [0.0] * n
        ready = [0.0] * n
        tfree = {e: 0.0 for e in self.ENGS}
        order = {e: [] for e in self.ENGS}
        remaining = n
        while remaining:
            best = None
            for e in self.ENGS:
                lst = pend[e]
                h = head[e]
                while h < len(lst) and done[lst[h]]:
                    h += 1
                head[e] = h
                seen_dma = False
                cnt = 0
                k = h
                while k < len(lst) and cnt < W:
                    i = lst[k]
                    k += 1
                    if done[i]:
                        continue
                    cnt += 1
                    o = ops[i]
                    if nleft[i] > 0:
                        continue
                    st = max(tfree[e], ready[i])
                    key = (st + 0.5 * (cnt - 1), i)
                    if best is None or key < best[0]:
                        best = (key, e, i, st)
            if best is None:
                raise RuntimeError("scheduler deadlock")
            _, e, i, st = best
            o = ops[i]
            done[i] = True
            remaining -= 1
            order[e].append(i)
            if o.isdma:
                tfree[e] = st + 60.0
                finish[i] = st + o.cost
            else:
                tfree[e] = st + o.cost
                finish[i] = st + o.cost
            for u in users[i]:
                nleft[u] -= 1
                lat = 0.0 if (o.eng == "pe" and ops[u].eng == "pe" and not o.isdma) else SEM
                r = finish[i] + lat
                if r > ready[u]:
                    ready[u] = r
        self.est_ns = max(finish) if finish else 0.0
        return order

    def finish(self):
        nc = self.nc
        ops = self.ops
        last = {}
        for i, o in enumerate(ops):
            last[o.eng] = i
        fin = Op("sp", None, False)
        for e, i in last.items():
            fin.deps.add(i)
        for i, o in enumerate(ops):
            if o.isdma:
                fin.deps.add(i)
        ops.append(fin)
        order = self._schedule(ops) if SCHED_WINDOW > 0 else {e: [i for i, o in enumerate(ops) if o.eng == e] for e in self.ENGS}
        self._order = order
        dcnt = {q: 0 for q in NSLOT}
        for e_ in self.ENGS:
            for i_ in order[e_]:
                o = ops[i_]
                if o.isdma:
                    k = dcnt[o.eng]
                    dcnt[o.eng] += 1
                    n_ = NSLOT[o.eng]
                    o.slot = (o.eng, k % n_)
                    o.slotval = 16 * (k // n_ + 1)
                    o.prevval = 16 * (k // n_)
        self._dcount = dcnt
        for o in ops:
            for j in o.deps:
                d = ops[j]
                if d.eng == "pe" and o.eng == "pe" and not o.isdma:
                    continue
                d.sig = True
        ccount = {e: 0 for e in self.ENGS}
        dcount = self._dcount
        for e_ in self.ENGS:
            for i_ in order[e_]:
                o = ops[i_]
                if o.isdma:
                    pass
                elif o.sig:
                    ccount[o.eng] += 1
                    o.cnt = ccount[o.eng]
        sems = {}
        cms = []

        def getsem(key):
            if key not in sems:
                cm = nc.semaphore(f"s_{key[0]}_{key[1]}")
                sems[key] = cm.__enter__()
                cms.append(cm)
            return sems[key]

        for e in self.ENGS:
            for ep in range(ccount[e] // EPOCH + 1):
                getsem((e, "c%d" % ep))
        for q, n in NSLOT.items():
            for s in range(min(n, dcount[q])):
                getsem((q, s))

        def compkey(o):
            ep = (o.cnt - 1) // EPOCH
            return (o.eng, "c%d" % ep), o.cnt - ep * EPOCH

        with nc.Block() as block:
            def emit_engine(ename, eng):
                known = {}
                for i in order[ename]:
                    o = ops[i]
                    waits = {}
                    for j in o.deps:
                        d = ops[j]
                        if d.isdma:
                            key, val = d.slot, d.slotval
                        else:
                            if d.eng == "pe" and ename == "pe" and not o.isdma:
                                continue
                            key, val = compkey(d)
                        if waits.get(key, 0) < val:
                            waits[key] = val
                    if o.isdma and o.prevval > 0:
                        key = o.slot
                        if waits.get(key, 0) < o.prevval:
                            waits[key] = o.prevval
                    for key, val in waits.items():
                        if known.get(key, 0) >= val:
                            continue
                        known[key] = val
                        eng.wait_ge(getsem(key), val)
                    if o.fn is None:
                        continue
                    ins = o.fn(eng)
                    if o.isdma:
                        ins.then_inc(getsem(o.slot), 16)
                    elif o.sig:
                        key, _ = compkey(o)
                        ins.then_inc(getsem(key), 1)

            @block.tensor
            def _(e):
                emit_engine("pe", e)

            @block.scalar
            def _(e):
                emit_engine("act", e)

            @block.vector
            def _(e):
                emit_engine("dve", e)

            @block.gpsimd
            def _(e):
                emit_engine("pool", e)

            @block.sync
            def _(e):
                emit_engine("sp", e)
        self._cms = cms
        return nc


D = 1024
CTX = 256
NORM_EPS = 1e-6
THETA = 10000.0
import math


class Ctx:
    def __init__(self, p, banks=True):
        self.p = p
        self.bi = 0
        self.dbanks = []
        if banks:
            self.set_banks(False)
        io = p.sb([128, 128], F32, "io")
        pi = p.sb([128, 1], F32, "pi")
        p.iota(io, [[1, 128]], base=0, cm=0)
        p.iota(pi, [[0, 1]], base=0, cm=1)
        self.pidx = pi
        self.identf = p.sb([128, 128], F32, "identf")
        p.ts(self.identf, io, pi[:, 0:1], ALU.is_equal)
        self.identb = p.sb([128, 128], BF16, "identb")
        p.cp(self.identb, self.identf)
        self.iof = io

    def set_banks(self, dbl):
        p = self.p
        if dbl:
            self.dbanks = [p.ps([128, 1024], F32, f"dbank{i}") for i in range(2)]
            self.banks = [self.dbanks[0][:, 0:512], self.dbanks[0][:, 512:1024],
                          self.dbanks[1][:, 0:512], self.dbanks[1][:, 512:1024]]
            self.banks += [p.ps([128, 512], F32, f"bank{i}") for i in range(4, 8)]
        else:
            self.banks = [p.ps([128, 512], F32, f"bank{i}") for i in range(8)]

    def bank(self):
        b = self.banks[self.bi % 8]
        self.bi += 1
        return b

    def load_fm(self, rows_v, n, eng_out="dve"):
        p = self.p
        tmp = p.sb([n, 128], F32, "lfm_tmp")
        p.dma(tmp, rows_v)
        bk = self.bank()
        p.tr(bk[:, 0:n], tmp, self.identf[0:n, 0:n])
        out = p.sb([128, n], F32, "lfm_out")
        p.cp(out, bk[:, 0:n], eng=eng_out)
        return out

    def rstd(self, ss, n, inv_d, eps):
        p = self.p
        p.ts(ss, ss, inv_d, ALU.mult, eps, ALU.add)
        p.act(ss, ss, AF.Ln)
        p.act(ss, ss, AF.Exp, scale=-0.5)


def run_prog(p, in_maps):
    nc = p.finish()
    res = run_bass_kernel_spmd(nc, in_maps, core_ids=list(range(len(in_maps))))
    return res.results


def build_phase0():
    p = Prog()
    cvec = p.dram("cvec", [2, D], F32, "ExternalInput")
    ada_w = p.dram("ada_w", [2, D, 6 * D], F32, "ExternalInput")
    ada_b = p.dram("ada_b", [2, 6 * D], F32, "ExternalInput")
    mods_tm = p.dram("mods_tm", [2, 2, 6 * D], F32, "ExternalOutput")
    mods_fm = p.dram("mods_fm", [2, 128, 48, 2], F32, "ExternalOutput")
    c = Ctx(p)
    cT = c.load_fm(cvec.rr("j (c p) -> (j c) p", p=128), 16)
    sT = p.sb([128, 16], F32, "sT")
    p.act(sT, cT, AF.Silu)
    sTv = sT.rr("p (j c) -> p c j", j=2)
    wbuf = [p.sb([128, 8, 1536], F32, f"adaw{i}") for i in range(2)]
    it = 0
    for l in range(2):
        bfm = c.load_fm(ada_b[l].rr("(c p) -> c p", p=128), 48)
        ofm = p.sb([128, 48, 2], F32, "ofm")
        for qd in range(4):
            w = wbuf[it % 2]
            it += 1
            for kc in range(8):
                p.dma(w[:, kc, :], ada_w[l, kc * 128:(kc + 1) * 128, qd * 1536:(qd + 1) * 1536],
                      q=("sp" if kc % 2 == 0 else "act"))
            bk = c.bank()
            for cc in range(12):
                for kc in range(8):
                    p.mm(bk[:, cc * 2:cc * 2 + 2], w[:, kc, cc * 128:(cc + 1) * 128], sTv[:, kc, :],
                         start=(kc == 0), stop=(kc == 7))
            p.tt(ofm[:, qd * 12:(qd + 1) * 12, :], bk[:, 0:24].rr("p (c j) -> p c j", j=2),
                 bfm[:, qd * 12:(qd + 1) * 12].un(2).bc([128, 12, 2]), ALU.add)
        p.dma(mods_fm[l], ofm)
        bk = c.bank()
        ofm2 = p.sb([128, 2, 48], F32, "ofm2")
        p.cp(ofm2, ofm.rr("p c j -> p j c"))
        p.tr(bk[0:96, 0:128], ofm2.rr("p j c -> p (j c)"), c.identf)
        otm = p.sb([96, 128], F32, "otm")
        p.cp(otm, bk[0:96, 0:128])
        for j in range(2):
            p.dma(mods_tm[l][j].rr("(c p) -> c p", p=128), otm[48 * j:48 * j + 48, :])
    return p


def build_post(S, has_ctx, stop=99):
    TL = S // 4
    NTL = TL // 128
    NT = NTL + (2 if has_ctx else 0)
    TOK = NT * 128
    p = Prog()
    x_in = p.dram("x", [TOK, D], F32, "ExternalInput")
    attT = p.dram("attT", [8, 128, TOK], BF16, "ExternalInput")
    mods_tm = p.dram("mods_tm", [2, 6 * D], F32, "ExternalInput")
    mods_fm = p.dram("mods_fm", [128, 48, 2], F32, "ExternalInput")
    w_out = p.dram("w_out", [D, D], F32, "ExternalInput")
    n2g = p.dram("norm2_g", [D], F32, "ExternalInput")
    router_w = p.dram("router_w", [D, 16], F32, "ExternalInput")
    router_b = p.dram("router_b", [1, 16], F32, "ExternalInput")
    wg_all = p.dram("wg_all", [17, D, 256], F32, "ExternalInput")
    wu_all = p.dram("wu_all", [17, D, 256], F32, "ExternalInput")
    wd_all = p.dram("wd_all", [17, 256, D], F32, "ExternalInput")
    x_out = p.dram("x_out", [TOK, D], F32, "ExternalOutput")
    c = Ctx(p)
    B = c.banks
    nvar = 2 if has_ctx else 1
    blocks = []
    t = 0
    while t < NTL:
        n = min(4, NTL - t)
        blocks.append((0, t, n))
        t += n
    if has_ctx:
        blocks.append((1, NTL, 2))

    mfm = p.sb([128, 48, 2], F32, "mfm")
    p.dma(mfm, mods_fm)
    g2fm = c.load_fm(n2g.rr("(c p) -> c p", p=128), 8)
    A2 = p.sb([128, 8, 2], F32, "A2")
    p.ts(A2, mfm[:, 32:40, :], 1.0, ALU.add)
    p.tt(A2, A2, g2fm.un(2).bc([128, 8, 2]), ALU.mult)
    SH2 = mfm[:, 24:32, :]
    rw = p.sb([128, 8, 16], F32, "rw")
    p.dma(rw, router_w.rr("(kc p) e -> p kc e", p=128))
    rb = p.sb([128, 16], F32, "rb")
    p.dma(rb, router_b.bc([128, 16]))
    sel = p.sb([16, 16, 128], F32, "sel")
    p.iota(sel, [[1, 16], [0, 128]], base=0, cm=0)
    p.ts(sel, sel, c.pidx[0:16, 0:1], ALU.is_equal)
    xs = p.sb([128, NT, D], F32, "xs")
    for t in range(NT):
        p.dma(xs[:, t, :], x_in[t * 128:(t + 1) * 128, :])
    h2T = p.sb([128, 8, TOK], BF16, "h2T")
    combT = p.sb([16, TOK], F32, "combT")
    lg_all = p.sb([128, NT, 16], F32, "lg_all")
    gabc = [p.sb([128, D], F32, f"gabc{i}") for i in range(2)]

    def load_ga(which, var, dst):
        p.dma(dst, mods_tm[var:var + 1, which * D:(which + 1) * D].bc([128, D]))

    with p.scope():
        if stop >= 2:
            wo = p.sb([128, 8, D], BF16, "wo")
            atb = [p.sb([128, 8, 128], BF16, f"atb{i}") for i in range(2)]
            for var in range(nvar):
                p.dma(wo, w_out.rr("(kc p) d -> p kc d", p=128), q="pool")
                load_ga(2, var, gabc[0])
                p.tt(wo, wo, gabc[0].un(1).bc([128, 8, D]), ALU.mult, eng="pool")
                tiles = range(NTL) if var == 0 else range(NTL, NT)
                for t in tiles:
                    a = atb[t % 2]
                    p.dma(a, attT[:, :, t * 128:(t + 1) * 128].rr("c p t -> p c t"))
                    for half in range(2):
                        bk = B[(t % 2) * 2 + half]
                        for kc in range(8):
                            p.mm(bk[:, :], a[:, kc, :], wo[:, kc, half * 512:(half + 1) * 512],
                                 start=(kc == 0), stop=(kc == 7))
                        xv = xs[:, t, half * 512:(half + 1) * 512]
                        p.tt(xv, xv, bk, ALU.add)

    with p.scope():
        if stop >= 3:
            junk = p.sb([128, D], BF16, "junk")
            xn = [p.sb([128, D], F32, f"xn{i}") for i in range(2)]
            h2f = [p.sb([128, 8, 128], F32, f"h2f{i}") for i in range(2)]
            ssq = p.sb([128, NT], F32, "ssq")
            for t in range(NT):
                var = 0 if t < NTL else 1
                p.act(junk, xs[:, t, :], AF.Square, accum=ssq[:, t:t + 1])
            c.rstd(ssq, NT, 1.0 / D, NORM_EPS)
            for t in range(NT):
                var = 0 if t < NTL else 1
                x_n = xn[t % 2]
                hf = h2f[t % 2]
                p.ts(x_n, xs[:, t, :], ssq[:, t:t + 1], ALU.mult)
                for hb in range(2):
                    bk = B[4 + (t % 2) * 2 + hb]
                    for k4 in range(4):
                        kc = hb * 4 + k4
                        p.tr(bk[:, k4 * 128:(k4 + 1) * 128], x_n[:, kc * 128:(kc + 1) * 128], c.identf)
                    for k4 in range(4):
                        kc = hb * 4 + k4
                        p.act(hf[:, kc, :], bk[:, k4 * 128:(k4 + 1) * 128], AF.Identity,
                              bias=SH2[:, kc, var:var + 1], scale=A2[:, kc, var:var + 1])
                p.cp(h2T[:, :, t * 128:(t + 1) * 128], hf, eng="pool")
                bk = B[t % 2]
                for kc in range(8):
                    p.mm(bk[:, 0:16], hf[:, kc, :], rw[:, kc, :], start=(kc == 0), stop=(kc == 7))
                p.cp(lg_all[:, t, :], bk[:, 0:16])
            N16 = NT * 16
            sc = p.sb([128, NT, 16], F32, "sc")
            bs = p.sb([128, NT, 16], F32, "bs")
            p.act(sc, lg_all, AF.Sigmoid)
            p.tt(bs, sc, rb.un(1).bc([128, NT, 16]), ALU.add)
            bsv = bs.rr("p t (g e) -> p (t g) e", g=4)
            G = NT * 4
            m1 = p.sb([128, G], F32, "m1")
            m2 = p.sb([128, G], F32, "m2")
            p.red(m1, bsv, op=ALU.max)
            eq1 = p.sb([128, G, 4], F32, "eq1")
            p.tt(eq1, bsv, m1.un(2).bc([128, G, 4]), ALU.is_equal)
            p.stt(eq1, eq1, -1e9, bsv, ALU.mult, ALU.add)
            p.red(m2, eq1, op=ALU.max)
            gs = p.sb([128, NT, 4], F32, "gs")
            p.tt(gs.rr("p t g -> p (t g)"), m1, m2, ALU.add)
            gmax = p.sb([128, NT], F32, "gmax")
            p.red(gmax, gs, op=ALU.max)
            gsel = p.sb([128, NT, 4], F32, "gsel")
            p.tt(gsel, gs, gmax.un(2).bc([128, NT, 4]), ALU.is_equal)
            ge2 = p.sb([128, G, 4], F32, "ge2")
            p.tt(ge2, bsv, m2.un(2).bc([128, G, 4]), ALU.is_ge)
            p.tt(ge2, ge2, gsel.rr("p t g -> p (t g)").un(2).bc([128, G, 4]), ALU.mult)
            p.tt(ge2, ge2, sc.rr("p t (g e) -> p (t g) e", g=4), ALU.mult)
            den = p.sb([128, NT], F32, "den")
            p.red(den, ge2.rr("p (t g) e -> p t (g e)", g=4))
            p.op("dve", lambda e: e.reciprocal(den.ap, den.ap), [den], [den])
            comb = p.sb([128, NT, 16], F32, "comb")
            p.tt(comb, ge2.rr("p (t g) e -> p t (g e)", g=4), den.un(2).bc([128, NT, 16]), ALU.mult)
            for t in range(NT):
                bk = B[t % 2]
                p.tr(bk[0:16, 0:128], comb[:, t, :], c.identf)
                p.cp(combT[:, t * 128:(t + 1) * 128], bk[0:16, 0:128], eng="act")

    with p.scope():
        if stop >= 4:
            wg = [p.sb([128, 8, 256], BF16, f"wg{i}") for i in range(2)]
            wu = [p.sb([128, 8, 256], BF16, f"wu{i}") for i in range(2)]
            wd = [p.sb([128, 2, D], BF16, f"wd{i}") for i in range(2)]
            wds = [p.sb([128, 2, D], BF16, f"wds{i}") for i in range(2)]
            comb_sb = [p.sb([128, 512], F32, f"comb_sb{i}") for i in range(2)]
            sg = [p.sb([128, 512], F32, f"sg{i}") for i in range(2)]
            tg = [p.sb([128, 512], F32, f"tg{i}") for i in range(2)]
            actb = [p.sb([128, 2, 512], BF16, f"actb{i}") for i in range(2)]
            for var in range(nvar):
                load_ga(5, var, gabc[var])
            it = 0
            for e in range(17 if stop < 40 or stop == 99 else stop - 40):
                eb = e % 2
                p.dma(wg[eb], wg_all[e].rr("(kc p) f -> p kc f", p=128), q="pool")
                p.dma(wu[eb], wu_all[e].rr("(kc p) f -> p kc f", p=128), q="pool")
                p.dma(wd[eb], wd_all[e].rr("(fc p) d -> p fc d", p=128), q="pool")
                for var in range(nvar):
                    p.tt(wds[eb], wd[eb], gabc[var].un(1).bc([128, 2, D]), ALU.mult, eng="pool")
                    for (bv, t0, nt) in blocks:
                        if bv != var:
                            continue
                        n = nt * 128
                        tok = slice(t0 * 128, t0 * 128 + n)
                        ib = it % 2
                        it += 1
                        if e < 16:
                            p.mm(B[4][:, 0:n], sel[:, e, :], combT[:, tok])
                            p.cp(comb_sb[ib][:, 0:n], B[4][:, 0:n], eng="act")
                        for fc in range(2):
                            gb = B[fc * 2]
                            ub = B[fc * 2 + 1]
                            for kc in range(8):
                                p.mm(gb[:, 0:n], wg[eb][:, kc, fc * 128:(fc + 1) * 128], h2T[:, kc, tok],
                                     start=(kc == 0), stop=(kc == 7))
                            for kc in range(8):
                                p.mm(ub[:, 0:n], wu[eb][:, kc, fc * 128:(fc + 1) * 128], h2T[:, kc, tok],
                                     start=(kc == 0), stop=(kc == 7))
                            p.act(sg[fc][:, 0:n], gb[:, 0:n], AF.Silu)
                            if e < 16:
                                p.tt(tg[fc][:, 0:n], ub[:, 0:n], comb_sb[ib][:, 0:n], ALU.mult)
                                p.tt(actb[ib][:, fc, 0:n], sg[fc][:, 0:n], tg[fc][:, 0:n], ALU.mult, eng="pool")
                            else:
                                p.tt(actb[ib][:, fc, 0:n], sg[fc][:, 0:n], ub[:, 0:n], ALU.mult)
                        for ti in range(nt):
                            t = t0 + ti
                            for half in range(2):
                                db = B[5 + (ti * 2 + half) % 3]
                                for fc in range(2):
                                    p.mm(db, actb[ib][:, fc, ti * 128:(ti + 1) * 128],
                                         wds[eb][:, fc, half * 512:(half + 1) * 512],
                                         start=(fc == 0), stop=(fc == 1))
                                xv = xs[:, t, half * 512:(half + 1) * 512]
                                p.tt(xv, xv, db, ALU.add)
    for t in range(NT):
        p.dma(x_out[t * 128:(t + 1) * 128, :], xs[:, t, :])
    return p


def rope_tables(p, c, pos, NTL, half):
    inv = p.sb([128, half], F32, "inv")
    for i in range(half):
        p.memset(inv[:, i:i + 1], float(THETA ** (-i / half)) / (2.0 * math.pi))
    Y = p.sb([128, NTL, 2, half], F32, "ropeY")
    p.tt(Y.rr("p t r h -> p (t r) h"), pos.rr("p t r -> p (t r)").un(2).bc([128, NTL * 2, half]),
         inv.un(1).bc([128, NTL * 2, half]), ALU.mult)
    COS = p.sb([128, NTL, 2, 2 * half], F32, "COS")
    SINS = p.sb([128, NTL, 2, 2 * half], F32, "SINS")
    Yi = p.sb([128, NTL, 2, half], I32, "ropeYi")
    Yf = p.sb([128, NTL, 2, half], F32, "ropeYf")
    T = p.sb([128, NTL, 2, half], F32, "ropeT")
    R = p.sb([128, NTL, 2, half], F32, "ropeR")
    for which in range(2):
        if which == 1:
            p.ts(Y, Y, 0.25, ALU.add)
        p.cp(Yi, Y)
        p.cp(Yf, Yi)
        p.tt(R, Y, Yf, ALU.subtract)
        p.ts(T, R, 0.5, ALU.is_gt)
        p.tt(R, R, T, ALU.subtract)
        p.ts(T, R, -0.5, ALU.is_lt)
        p.tt(R, R, T, ALU.add)
        if which == 0:
            p.act(SINS[:, :, :, half:2 * half], R, AF.Sin, scale=2.0 * math.pi * (1 - 1e-6))
            p.ts(SINS[:, :, :, 0:half], SINS[:, :, :, half:2 * half], -1.0, ALU.mult)
        else:
            p.act(COS[:, :, :, 0:half], R, AF.Sin, scale=2.0 * math.pi * (1 - 1e-6))
            p.cp(COS[:, :, :, half:2 * half], COS[:, :, :, 0:half])
    return COS, SINS


def qk_post(p, c, pieces, nh, hd, gains_bc, dst, scratch, rope=None):
    sq, qk, ss = scratch["sq"], scratch["qk"], scratch["ss"]
    off = 0
    for (v, n) in pieces:
        p.act(sq[:, off:off + n * hd], v, AF.Square)
        off += n * hd
    p.red(ss, sq.rr("p (h d) -> p h d", d=hd))
    c.rstd(ss, nh, 1.0 / hd, NORM_EPS)
    off = 0
    h0 = 0
    for (v, n) in pieces:
        p.tt(qk[:, off:off + n * hd].rr("p (h d) -> p h d", d=hd), v.rr("p (h d) -> p h d", d=hd),
             ss[:, h0:h0 + n].un(2).bc([128, n, hd]), ALU.mult)
        off += n * hd
        h0 += n
    if rope is None:
        p.tt(dst, qk, gains_bc, ALU.mult, eng="pool")
        return
    p.tt(qk, qk, gains_bc, ALU.mult, eng="pool")
    roff, half, COS_t, SINS_t = rope
    qv = qk.rr("p (h d) -> p h d", d=hd)
    dv = dst.rr("p (h d) -> p h d", d=hd)
    if roff > 0:
        p.cp(dv[:, :, 0:roff], qv[:, :, 0:roff], eng="pool")
    t1, t2 = scratch["t1"], scratch["t2"]
    w = 2 * half
    for rc in range(2):
        xs = qv[:, :, roff + rc * w: roff + (rc + 1) * w]
        a = t1[:, 0:nh * w].rr("p (h d) -> p h d", d=w)
        b = t2[:, 0:nh * w].rr("p (h d) -> p h d", d=w)
        eng = "dve" if rc == 0 else "pool"
        p.tt(a, xs, COS_t[:, rc, :].un(1).bc([128, nh, w]), ALU.mult, eng=eng)
        p.tt(b[:, :, 0:half], xs[:, :, half:w], SINS_t[:, rc, 0:half].un(1).bc([128, nh, half]), ALU.mult, eng=eng)
        p.tt(b[:, :, half:w], xs[:, :, 0:half], SINS_t[:, rc, half:w].un(1).bc([128, nh, half]), ALU.mult, eng=eng)
        p.tt(dv[:, :, roff + rc * w: roff + (rc + 1) * w], a, b, ALU.add, eng=eng)


def norm1_hT(p, c, xt, A, SH, var, hT_dst, xnb, junk, ssq, banks):
    p.act(junk, xt, AF.Square, accum=ssq)
    c.rstd(ssq, 1, 1.0 / D, NORM_EPS)
    p.ts(xnb, xt, ssq[:, 0:1], ALU.mult)
    for hb in range(2):
        bk = banks[hb].bitcast(BF16)
        for k4 in range(4):
            kc = hb * 4 + k4
            p.tr(bk[:, k4 * 128:(k4 + 1) * 128], xnb[:, kc * 128:(kc + 1) * 128], c.identb)
        for k4 in range(4):
            kc = hb * 4 + k4
            p.act(hT_dst[:, kc, :], bk[:, k4 * 128:(k4 + 1) * 128], AF.Identity,
                  bias=SH[:, kc, var:var + 1], scale=A[:, kc, var:var + 1])


def mod_consts(p, c, mods_fm, norm_g, which_sh, which_sc):
    mfm = p.sb([128, 48, 2], F32, "mfm")
    p.dma(mfm, mods_fm)
    gfm = c.load_fm(norm_g.rr("(c p) -> c p", p=128), 8)
    A = p.sb([128, 8, 2], F32, "Amod")
    p.ts(A, mfm[:, which_sc * 8:(which_sc + 1) * 8, :], 1.0, ALU.add)
    p.tt(A, A, gfm.un(2).bc([128, 8, 2]), ALU.mult)
    SH = mfm[:, which_sh * 8:(which_sh + 1) * 8, :]
    return A, SH


def build_pre1(S):
    TL = S // 4
    NTL = TL // 128
    NT = NTL + 2
    TOK = NT * 128
    p = Prog()
    x_in = p.dram("x", [TOK, D], F32, "ExternalInput")
    mods_fm = p.dram("mods_fm", [128, 48, 2], F32, "ExternalInput")
    n1g = p.dram("norm1_g", [D], F32, "ExternalInput")
    w_in = p.dram("w_in", [D, 1536], F32, "ExternalInput")
    g_q = p.dram("g_q", [1, 64], F32, "ExternalInput")
    g_k = p.dram("g_k", [1, 64], F32, "ExternalInput")
    pos_in = p.dram("pos", [128, NTL, 2], F32, "ExternalInput")
    qkT = p.dram("qkT", [10, 128, TOK], BF16, "ExternalOutput")
    v_out = p.dram("v", [TOK, 256], BF16, "ExternalOutput")
    c = Ctx(p)
    B = c.banks
    A, SH = mod_consts(p, c, mods_fm, n1g, 0, 1)
    pos = p.sb([128, NTL, 2], F32, "pos")
    p.dma(pos, pos_in)
    COS, SINS = rope_tables(p, c, pos, NTL, 16)
    gains = p.sb([128, 20, 64], F32, "gains")
    gq = p.sb([128, 64], F32, "gq")
    gk = p.sb([128, 64], F32, "gk")
    p.dma(gq, g_q.bc([128, 64]))
    p.dma(gk, g_k.bc([128, 64]))
    p.cp(gains[:, 0:16, :], gq.un(1).bc([128, 16, 64]))
    p.cp(gains[:, 16:20, :], gk.un(1).bc([128, 4, 64]))
    gains_f = gains.rr("p h d -> p (h d)")
    wi = p.sb([128, 8, 1536], BF16, "wi")
    p.dma(wi, w_in.rr("(kc p) n -> p kc n", p=128), q="pool")
    xt = [p.sb([128, D], F32, f"xt{i}") for i in range(2)]
    xnb = [p.sb([128, D], BF16, f"xnb{i}") for i in range(2)]
    junk = p.sb([128, D], BF16, "junk")
    hT = [p.sb([128, 8, 128], BF16, f"hT{i}") for i in range(2)]
    ssq = [p.sb([128, 1], F32, f"ssq{i}") for i in range(2)]
    scrs = [dict(sq=p.sb([128, 1280], F32, "sq"), qk=p.sb([128, 1280], F32, "qk"), ss=p.sb([128, 20], F32, "ss"),
                 t1=p.sb([128, 1280], F32, "t1"), t2=p.sb([128, 1280], F32, "t2")) for _ in range(2)]
    qkb = [p.sb([128, 1280], BF16, f"qkb{i}") for i in range(2)]
    vb = [p.sb([128, 256], BF16, f"vb{i}") for i in range(2)]
    qkTs = [p.sb([128, 10, 128], BF16, f"qkTs{i}") for i in range(2)]
    for t in range(NT):
        i2 = t % 2
        var = 0 if t < NTL else 1
        p.dma(xt[i2], x_in[t * 128:(t + 1) * 128, :])
        norm1_hT(p, c, xt[i2], A, SH, var, hT[i2], xnb[i2], junk, ssq[i2], (B[0], B[1]))
        for blk in range(3):
            bk = B[2 + blk]
            for kc in range(8):
                p.mm(bk, hT[i2][:, kc, :], wi[:, kc, blk * 512:(blk + 1) * 512], start=(kc == 0), stop=(kc == 7))
        rope = None if var == 1 else (0, 16, COS[:, t], SINS[:, t])
        qk_post(p, c, [(B[2], 8), (B[3], 8), (B[4][:, 0:256], 4)], 20, 64, gains_f, qkb[i2], scrs[i2], rope)
        p.cp(vb[i2], B[4][:, 256:512], eng="act")
        p.dma(v_out[t * 128:(t + 1) * 128, :], vb[i2])
        bA = B[5].bitcast(BF16)
        bB = B[6].bitcast(BF16)
        for pr in range(10):
            dstb = bA[:, pr * 128:(pr + 1) * 128] if pr < 8 else bB[:, (pr - 8) * 128:(pr - 7) * 128]
            p.tr(dstb, qkb[i2][:, pr * 128:(pr + 1) * 128], c.identb)
        p.cp(qkTs[i2][:, 0:8, :].rr("p a t -> p (a t)"), bA, eng="act")
        p.cp(qkTs[i2][:, 8:10, :].rr("p a t -> p (a t)"), bB[:, 0:256])
        p.dma(qkT[:, :, t * 128:(t + 1) * 128].rr("a p t -> p a t"), qkTs[i2])
    return p


def attention_gen(p, c, qT_d, kT_d, v_d, oT_d, NH, NKV, dk, dv, S, scale, qcT_d=None, ocT_d=None, banks=None):
    LK = CTX + S
    NKT = LK // 128
    B = banks if banks is not None else c.banks
    kT = p.sb([128, NKV, LK], BF16, "kT")
    if dk < 128:
        p.memset(kT, 0.0)
    for kv in range(NKV):
        p.dma(kT[0:dk, kv, :], kT_d[kv])
    vp = p.sb([128, NKV, NKT, dv + 1], BF16, "vp")
    p.memset(vp[:, :, :, dv:dv + 1], 1.0)
    for kv in range(NKV):
        p.dma(vp[:, kv, :, 0:dv], v_d[kv].rr("(t p) d -> p t d", p=128))
    selden = p.sb([dv + 1, dv], F32, "selden")
    p.memset(selden, 0.0)
    p.memset(selden[dv:dv + 1, :], 1.0)
    qTs = [p.sb([128, NH, 512], BF16, f"qTs{i}") for i in range(2)]
    if dk < 128:
        for t_ in qTs:
            p.memset(t_, 0.0)
    pT = [p.sb([128, 512], BF16, f"pT{i}") for i in range(4)]
    accs = [p.sb([dv + 1, 512], F32, f"accs{i}") for i in range(2)]
    rec = [p.sb([dv, 512], F32, f"rec{i}") for i in range(2)]
    ob = [p.sb([dv, 512], BF16, f"ob{i}") for i in range(2)]
    jobs = []
    for qb in range(S // 512):
        jobs.append((qT_d, oT_d, qb * 512, 512, NKT))
    if qcT_d is not None:
        jobs.append((qcT_d, ocT_d, 0, CTX, CTX // 128))
    DB = c.dbanks
    items = []
    for ji, (qd, od, q0, n, nkt) in enumerate(jobs):
        for h in range(NH):
            for kp in range(nkt // 2):
                items.append((ji, h, kp))
    pT2 = [p.sb([128, 2, 512], BF16, f"pT2_{i}") for i in range(3)]
    loaded = set()

    def load_q(ji):
        if ji in loaded or ji >= len(jobs):
            return
        loaded.add(ji)
        qd, od, q0, n, nkt = jobs[ji]
        qs = qTs[ji % 2]
        for h in range(NH):
            p.dma(qs[0:dk, h, 0:n], qd[h, :, q0:q0 + n])

    def issue_s(i):
        ji, h, kp = items[i]
        qd, od, q0, n, nkt = jobs[ji]
        load_q(ji)
        kv = h * NKV // NH
        db = DB[i % 2]
        for a_ in range(2):
            kt = 2 * kp + a_
            p.mm(db[:, a_ * 512:a_ * 512 + n], kT[:, kv, kt * 128:(kt + 1) * 128], qTs[ji % 2][:, h, 0:n])

    issue_s(0)
    ih = 0
    for i, (ji, h, kp) in enumerate(items):
        qd, od, q0, n, nkt = jobs[ji]
        kv = h * NKV // NH
        if h == 0 and kp == 0:
            load_q(ji + 1)
        if i + 1 < len(items):
            issue_s(i + 1)
        acc = B[4 + ih % 2]
        pt = pT2[i % 3]
        dbv = DB[i % 2].rr("p (a t) -> p a t", a=2)
        p.act(pt[:, :, 0:n], dbv[:, :, 0:n], AF.Exp, scale=scale)
        for a_ in range(2):
            kt = 2 * kp + a_
            p.mm(acc[0:dv + 1, 0:n], vp[:, kv, kt, :], pt[:, a_, 0:n], start=(kt == 0), stop=(kt == nkt - 1))
        if kp == nkt // 2 - 1:
            a = accs[ih % 2]
            p.cp(a[:, 0:n], acc[0:dv + 1, 0:n])
            p.mm(B[6][0:dv, 0:n], selden, a[:, 0:n])
            r = rec[ih % 2]
            p.op("dve", lambda e, r=r, n=n: e.reciprocal(r.ap[:, 0:n], B[6].ap[0:dv, 0:n]), [B[6]], [r])
            o = ob[ih % 2]
            p.tt(o[:, 0:n], a[0:dv, 0:n], r[:, 0:n], ALU.mult, eng="pool")
            p.dma(od[h, :, q0:q0 + n], o[:, 0:n])
            ih += 1
            yield ih


def attention_block(*a, **kw):
    for _ in attention_gen(*a, **kw):
        pass


def build_attn1(S):
    p = Prog()
    LK = CTX + S
    qT = p.dram("qT", [4, 64, S], BF16, "ExternalInput")
    kT = p.dram("kT", [1, 64, LK], BF16, "ExternalInput")
    v = p.dram("v", [1, LK, 64], BF16, "ExternalInput")
    oT = p.dram("oT", [4, 64, S], BF16, "ExternalOutput")
    c = Ctx(p, banks=False)
    c.set_banks(True)
    attention_block(p, c, qT, kT, v, oT, 4, 1, 64, 64, S, 64 ** -0.5)
    return p


RW_COLS = 1920


def build_pre0(S, dbg=0):
    TL = S // 4
    NTL = TL // 128
    NTM = NTL + 2
    NTA = NTL + 3
    TOKM = NTM * 128
    p = Prog()
    x_in = p.dram("x", [NTA * 128, D], F32, "ExternalInput")
    mods_fm = p.dram("mods_fm", [128, 48, 2], F32, "ExternalInput")
    n1g = p.dram("norm1_g", [D], F32, "ExternalInput")
    w_in = p.dram("w_in", [D, 2592], F32, "ExternalInput")
    mu_d = p.dram("mu", [RW_COLS], F32, "ExternalInput")
    flags_d = p.dram("flags", [128, 2], F32, "ExternalInput")
    g_qa = p.dram("g_qa", [1, 384], F32, "ExternalInput")
    g_kva = p.dram("g_kva", [1, 256], F32, "ExternalInput")
    w_q_up = p.dram("w_q_up", [384, 768], F32, "ExternalInput")
    w_kv_up = p.dram("w_kv_up", [256, 1024], F32, "ExternalInput")
    g_q = p.dram("g_q", [1, 96], F32, "ExternalInput")
    g_k = p.dram("g_k", [1, 96], F32, "ExternalInput")
    pos_in = p.dram("pos", [128, NTL, 2], F32, "ExternalInput")
    rwT = p.dram("rwT", [15, 128, TL + CTX], F32, "ExternalOutput")
    qT_o = p.dram("qT", [8, 96, TOKM], BF16, "ExternalOutput")
    kT_o = p.dram("kT", [8, 96, TOKM], BF16, "ExternalOutput")
    v_o = p.dram("v", [TOKM, 512], BF16, "ExternalOutput")
    c = Ctx(p)
    B = c.banks
    A, SH = mod_consts(p, c, mods_fm, n1g, 0, 1)
    pos = p.sb([128, NTL, 2], F32, "pos")
    p.dma(pos, pos_in)
    COS, SINS = rope_tables(p, c, pos, NTL, 8)
    hT_all = p.sb([128, 8, NTA * 128], BF16, "hT_all")

    with p.scope():
        gq_bc = p.sb([128, 8, 96], F32, "gq_bc")
        gk_bc = p.sb([128, 8, 96], F32, "gk_bc")
        g96 = p.sb([128, 96], F32, "g96")
        p.dma(g96, g_q.bc([128, 96]))
        p.cp(gq_bc, g96.un(1).bc([128, 8, 96]))
        g96b = p.sb([128, 96], F32, "g96b")
        p.dma(g96b, g_k.bc([128, 96]))
        p.cp(gk_bc, g96b.un(1).bc([128, 8, 96]))
        gqa_bc = p.sb([128, 384], F32, "gqa_bc")
        gkva_bc = p.sb([128, 256], F32, "gkva_bc")
        p.dma(gqa_bc, g_qa.bc([128, 384]))
        p.dma(gkva_bc, g_kva.bc([128, 256]))
        wim = p.sb([128, 8, 672], BF16, "wim")
        p.dma(wim, w_in[:, RW_COLS:2592].rr("(kc p) n -> p kc n", p=128), q="pool")
        wq = p.sb([128, 3, 768], BF16, "wq")
        p.dma(wq, w_q_up.rr("(kc p) n -> p kc n", p=128), q="pool")
        wkv = p.sb([128, 2, 1024], BF16, "wkv")
        p.dma(wkv, w_kv_up.rr("(kc p) n -> p kc n", p=128), q="pool")
        xt = [p.sb([128, D], F32, f"xt{i}") for i in range(2)]
        xnb = [p.sb([128, D], BF16, f"xnb{i}") for i in range(2)]
        junk = p.sb([128, D], BF16, "junk")
        ssq = [p.sb([128, 1], F32, f"ssq{i}") for i in range(2)]
        ssa = [p.sb([128, 2], F32, f"ssa{i}") for i in range(2)]
        anb = [p.sb([128, 640], BF16, f"anb{i}") for i in range(2)]
        anT = [p.sb([128, 5, 128], BF16, f"anT{i}") for i in range(2)]
        krs = [p.sb([128, 32], F32, f"krs{i}") for i in range(2)]
        Ksb = [p.sb([128, 8, 96], F32, f"Ksb{i}") for i in range(2)]
        vb = [p.sb([128, 8, 64], BF16, f"vb{i}") for i in range(2)]
        scrs = [dict(sq=p.sb([128, 768], F32, "sq"), qk=p.sb([128, 768], F32, "qk"), ss=p.sb([128, 8], F32, "ss"),
                     t1=p.sb([128, 768], F32, "t1"), t2=p.sb([128, 768], F32, "t2")) for _ in range(2)]
        qb_ = [p.sb([128, 768], BF16, f"qb{i}") for i in range(2)]
        kb_ = [p.sb([128, 768], BF16, f"kb{i}") for i in range(2)]
        qTs = [p.sb([96, 8, 128], BF16, f"qTs{i}") for i in range(2)]
        kTs = [p.sb([96, 8, 128], BF16, f"kTs{i}") for i in range(2)]
        for t in range(NTA):
            i2 = t % 2
            var = 1 if (NTL <= t < NTL + 2) else 0
            p.dma(xt[i2], x_in[t * 128:(t + 1) * 128, :])
            hT = hT_all[:, :, t * 128:(t + 1) * 128]
            norm1_hT(p, c, xt[i2], A, SH, var, hT, xnb[i2], junk, ssq[i2], (B[0], B[1]))
            if t >= NTM or dbg == 1:
                continue
            for kc in range(8):
                p.mm(B[2][:, 0:384], hT[:, kc, :], wim[:, kc, 0:384], start=(kc == 0), stop=(kc == 7))
            for kc in range(8):
                p.mm(B[3][:, 0:288], hT[:, kc, :], wim[:, kc, 384:672], start=(kc == 0), stop=(kc == 7))
            sa = ssa[i2]
            p.act(junk[:, 0:384], B[2][:, 0:384], AF.Square, accum=sa[:, 0:1])
            p.act(junk[:, 384:640], B[3][:, 0:256], AF.Square, accum=sa[:, 1:2])
            p.ts(sa[:, 0:1], sa[:, 0:1], 256.0 / 384.0, ALU.mult)
            c.rstd(sa, 2, 1.0 / 256.0, NORM_EPS)
            p.stt(anb[i2][:, 0:384], B[2][:, 0:384], sa[:, 0:1], gqa_bc, ALU.mult, ALU.mult)
            p.stt(anb[i2][:, 384:640], B[3][:, 0:256], sa[:, 1:2], gkva_bc, ALU.mult, ALU.mult)
            p.cp(krs[i2], B[3][:, 256:288], eng="act")
            if dbg == 3:
                continue
            b4 = B[4].bitcast(BF16)
            for k5 in range(5):
                p.tr(b4[:, k5 * 128:(k5 + 1) * 128], anb[i2][:, k5 * 128:(k5 + 1) * 128], c.identb)
            p.cp(anT[i2].rr("p a t -> p (a t)"), b4[:, 0:640], eng="act")
            if dbg == 4:
                continue
            for (bk, c0, c1) in ((B[5], 0, 480), (B[6], 480, 768)):
                for kc in range(3):
                    p.mm(bk[:, 0:c1 - c0], anT[i2][:, kc, :], wq[:, kc, c0:c1], start=(kc == 0), stop=(kc == 2))
            if dbg == 51:
                continue
            for (bk, c0) in ((B[7], 0), (B[2], 512)):
                for kc in range(2):
                    p.mm(bk, anT[i2][:, 3 + kc, :], wkv[:, kc, c0:c0 + 512], start=(kc == 0), stop=(kc == 1))
            if dbg == 52:
                continue
            K = Ksb[i2]
            for (bk, h0) in ((B[7], 0), (B[2], 4)):
                kvv = bk.rr("p (h d) -> p h d", d=128)
                p.cp(K[:, h0:h0 + 4, 0:64], kvv[:, :, 0:64])
                if dbg != 531:
                    p.cp(vb[i2][:, h0:h0 + 4, :], kvv[:, :, 64:128], eng=("dve" if dbg == 532 else "act"))
            if dbg != 533:
                p.cp(K[:, :, 64:96], krs[i2].un(1).bc([128, 8, 32]), eng="pool")
            if dbg in (53, 531, 532, 533):
                continue
            p.dma(v_o[t * 128:(t + 1) * 128, :], vb[i2].rr("p h d -> p (h d)"))
            if dbg == 5:
                continue
            rope = None if var == 1 else (64, 8, COS[:, t], SINS[:, t])
            qk_post(p, c, [(B[5][:, 0:480], 5), (B[6][:, 0:288], 3)], 8, 96, gq_bc.rr("p h d -> p (h d)"),
                    qb_[i2], scrs[0], rope)
            if dbg == 6:
                continue
            qk_post(p, c, [(K.rr("p h d -> p (h d)"), 8)], 8, 96, gk_bc.rr("p h d -> p (h d)"), kb_[i2], scrs[1], rope)
            if dbg == 7:
                continue
            for (src, dstT, bk, out_d, eng) in ((qb_[i2], qTs[i2], B[0], qT_o, "act"), (kb_[i2], kTs[i2], B[1], kT_o, "dve")):
                bb = bk.bitcast(BF16)
                for h in range(8):
                    p.tr(bb[0:96, h * 128:(h + 1) * 128], src[:, h * 96:(h + 1) * 96], c.identb)
                p.cp(dstT.rr("p a t -> p (a t)"), bb[0:96, :], eng=eng)
                p.dma(out_d[:, :, t * 128:(t + 1) * 128].rr("a p t -> p a t"), dstT)

    with p.scope():
        mufm = c.load_fm(mu_d.rr("(c p) -> c p", p=128), 15)
        om = p.sb([128, 15], F32, "om")
        hm = p.sb([128, 15], F32, "hm")
        p.ts(om, mufm, -1.0, ALU.mult, 1.0, ALU.add)
        p.ts(hm, mufm, 0.5, ALU.mult)
        flags = p.sb([128, 2], F32, "flags")
        p.dma(flags, flags_d)
        W = TL + CTX + 4
        E = [p.sb([128, W], F32, f"E{i}") for i in range(2)]
        for i in range(2):
            p.memset(E[i][:, TL + 2:TL + 3], 0.0)
            p.memset(E[i][:, W - 1:W], 0.0)
        hal = [p.sb([128, 2], F32, f"hal{i}") for i in range(2)]
        sm = [p.sb([128, TL + CTX], F32, f"sm{i}") for i in range(2)]
        tm = [p.sb([128, TL + CTX], F32, f"tm{i}") for i in range(2)]
        wc = [p.sb([128, 8, 128], BF16, f"wc{i}") for i in range(3)]
        blks = []
        t0 = 0
        while t0 < TL:
            n = min(512, TL - t0)
            blks.append((t0, n, 1 + t0))
            t0 += n
        blks.append((TL, CTX, TL + 3))
        ib = 0
        for cc in range(15 if dbg < 2 else 0):
            w = wc[cc % 3]
            e = E[cc % 2]
            p.dma(w, w_in[:, cc * 128:(cc + 1) * 128].rr("(kc p) n -> p kc n", p=128), q="pool")
            for (t0, n, e0) in blks:
                bk = B[ib % 6]
                ib += 1
                for kc in range(8):
                    p.mm(bk[:, 0:n], w[:, kc, :], hT_all[:, kc, t0:t0 + n], start=(kc == 0), stop=(kc == 7))
                p.cp(e[:, e0:e0 + n], bk[:, 0:n], eng=("act" if ib % 2 else "dve"))
            bk = B[6 + cc % 2]
            hoff = (NTL + 2) * 128
            for kc in range(8):
                p.mm(bk[:, 0:2], w[:, kc, :], hT_all[:, kc, hoff:hoff + 2], start=(kc == 0), stop=(kc == 7))
            p.tt(hal[cc % 2], bk[:, 0:2], flags, ALU.mult)
            p.cp(e[:, 0:1], hal[cc % 2][:, 0:1], eng="pool")
            p.cp(e[:, TL + 1:TL + 2], hal[cc % 2][:, 1:2], eng="pool")
            s_, t_ = sm[cc % 2], tm[cc % 2]
            for (o0, n, e0) in ((0, TL, 1), (TL, CTX, TL + 3)):
                p.tt(s_[:, o0:o0 + n], e[:, e0 - 1:e0 - 1 + n], e[:, e0 + 1:e0 + 1 + n], ALU.add, eng="pool")
                p.ts(t_[:, o0:o0 + n], e[:, e0:e0 + n], om[:, cc:cc + 1], ALU.mult)
                p.stt(s_[:, o0:o0 + n], s_[:, o0:o0 + n], hm[:, cc:cc + 1], t_[:, o0:o0 + n], ALU.mult, ALU.add)
            p.dma(rwT[cc], s_)
    return p


LN_X_EPS = 64e-5
LAM = math.exp(-0.5)


def build_mix0(S, do_attn=True, RWP=BF16):
    LT = CTX + S
    NPAIR = LT // 128
    p = Prog()
    rw_r = p.dram("rw_r", [128, LT], F32, "ExternalInput")
    rw_k = p.dram("rw_k", [128, LT], F32, "ExternalInput")
    rw_v = p.dram("rw_v", [128, LT], F32, "ExternalInput")
    lo_w = p.dram("lo_w", [128, LT], F32, "ExternalInput")
    lo_a = p.dram("lo_a", [128, LT], F32, "ExternalInput")
    lo_g = p.dram("lo_g", [128, LT], F32, "ExternalInput")
    w2_d = p.dram("w2", [128, 128], F32, "ExternalInput")
    a2_d = p.dram("a2", [128, 128], F32, "ExternalInput")
    g2_d = p.dram("g2", [128, 128], F32, "ExternalInput")
    pv_d = p.dram("pv", [9, 128], F32, "ExternalInput")
    yT = p.dram("yT", [2, 128, LT], F32, "ExternalOutput")
    rwoT = p.dram("rwoT", [128, LT], BF16, "ExternalOutput")
    if do_attn:
        qT = p.dram("qT", [2, 96, S], BF16, "ExternalInput")
        qcT = p.dram("qcT", [2, 96, CTX], BF16, "ExternalInput")
        kT = p.dram("kT", [2, 96, LT], BF16, "ExternalInput")
        v_d = p.dram("v", [2, LT, 64], BF16, "ExternalInput")
        oT = p.dram("oT", [2, 64, S], BF16, "ExternalOutput")
        ocT = p.dram("ocT", [2, 64, CTX], BF16, "ExternalOutput")
    c = Ctx(p, banks=False)
    _outer = p.scope()
    _outer.__enter__()
    c.set_banks(False)
    B = c.banks
    pv = c.load_fm(pv_d, 9)
    W0 = [pv[:, 0:1], pv[:, 1:2]]
    A0 = [pv[:, 2:3], pv[:, 3:4]]
    KK_, KA_, RK_, LNW, LNB = pv[:, 4:5], pv[:, 5:6], pv[:, 6:7], pv[:, 7:8], pv[:, 8:9]
    omka = p.sb([128, 2], F32, "omka")
    p.ts(omka[:, 0:1], KA_, -1.0, ALU.mult, 1.0, ALU.add)
    p.ts(omka[:, 1:2], KA_, -2.0, ALU.mult, 2.0, ALU.add)
    w2s = p.sb([128, 128], F32, "w2s")
    a2s = p.sb([128, 128], F32, "a2s")
    g2s = p.sb([128, 128], F32, "g2s")
    p.dma(w2s, w2_d)
    p.dma(a2s, a2_d)
    p.dma(g2s, g2_d)
    w2z, a2z = [], []
    for d in range(2):
        for (src_, lst_) in ((w2s, w2z), (a2s, a2z)):
            z = p.sb([128, 128], F32, "loraz")
            p.memset(z, 0.0)
            dp_ = slice(64 * d, 64 * d + 64)
            p.cp(z[dp_, :], src_[dp_, :], eng="pool")
            lst_.append(z)
    p64 = p.sb([128, 1], F32, "p64")
    p.ts(p64, c.pidx, 63.5, ALU.is_gt)
    c64 = p.sb([128, 128], F32, "c64")
    p.ts(c64, c.iof, 63.5, ALU.is_gt)
    same = p.sb([128, 128], F32, "same")
    p.ts(same, c64, p64[:, 0:1], ALU.is_equal)
    masks = {}
    for nm, op_ in (("SU", ALU.is_gt), ("SL", ALU.is_lt), ("IU", ALU.is_ge), ("IL", ALU.is_le)):
        m = p.sb([128, 2, 128], F32, "mask" + nm)
        p.ts(m[:, 0, :], c.iof, c.pidx[:, 0:1], op_)
        p.tt(m[:, 0, :], m[:, 0, :], same, ALU.mult)
        p.cp(m[:, 1, :], m[:, 0, :])
        masks[nm] = m.rr("p a t -> p (a t)")
    ones64 = p.sb([128, 128], F32, "ones64")
    p.ts(ones64, same, 1.0 / 64.0, ALU.mult)
    ident2 = p.sb([128, 2, 128], F32, "ident2")
    p.cp(ident2[:, 0, :], c.identf)
    p.cp(ident2[:, 1, :], c.identf)
    ident2 = ident2.rr("p a t -> p (a t)")
    rmask = p.sb([128, 512], F32, "rmask")
    p.memset(rmask, 1.0)
    p.memset(rmask.rr("p (a t) -> p a t", t=64)[:, :, 0:1], 0.0)

    blocks = [(0, CTX)]
    t0 = CTX
    while t0 < LT:
        blocks.append((t0, 512))
        t0 += 512
    NB = len(blocks)
    order = [list(range(NB)), [0] + list(range(NB - 1, 0, -1))]

    with p.scope():
        def T(shape, nm, dt=F32):
            return p.sb(shape, dt, nm)
        ops_ = []
        for d in range(2):
            two = []
            for i in range(2):
                two.append(dict(Rt=T([128, 512], "Rt"), Rt16=T([128, 512], "Rt16", RWP), Kt=T([128, 512], "Kt", RWP),
                                Bt=T([128, 512], "Bt", RWP), At=T([128, 512], "At", RWP),
                                A_tm=T([128, 4, 128], "A_tm", RWP), K_tm=T([128, 4, 128], "K_tm", RWP), B_tm=T([128, 4, 128], "B_tm", RWP),
                                V_tm=T([128, 4, 128], "V_tm", RWP), etot=T([128, 8], "etot"), yb=T([128, 512], "yb")))
            ops_.append(two)
        tmp = {k: T([128, 512], k) for k in ("r", "k", "v", "lw", "la", "th", "sg", "a", "kk", "t1", "t2", "c", "E", "kd", "b", "Kh", "Bh")}
        tot = T([128, 8], "tot")
        Hbd = [[T([128, 128], f"H{d}{i}") for i in range(2)] for d in range(2)]
        H16 = [[T([128, 128], f"H16{d}{i}", RWP) for i in range(2)] for d in range(2)]
        for d in range(2):
            for i in range(2):
                p.memset(Hbd[d][i], 0.0)
                p.memset(H16[d][i], 0.0)
        hcur = [0, 0]
        pairbuf = []
        for d in range(2):
            pairbuf.append({k: T([128, 256], f"{k}{d}", RWP) for k in ("X", "XT", "X2", "XT2", "P", "P2", "MAK")}
                           | {k: T([128, 256], f"{k}{d}", RWP) for k in ("MRK", "MRB", "WT")}
                           | {"AT": T([128, 128], f"AT{d}", RWP), "U": T([128, 128], f"U{d}", RWP)})
            p.memset(pairbuf[d]["U"], 0.0)

        def prep(d, bi, ob):
            t0, n = blocks[bi]
            tok = slice(t0, t0 + n)
            nch = n // 64
            dp = slice(64 * d, 64 * d + 64)
            x = tmp
            p.dma(x["r"][:, 0:n], rw_r[:, tok])
            p.dma(x["k"][:, 0:n], rw_k[:, tok])
            p.dma(x["v"][:, 0:n], rw_v[:, tok])
            p.dma(x["lw"][:, 0:n], lo_w[:, tok])
            p.dma(x["la"][:, 0:n], lo_a[:, tok])
            N = slice(0, n)
            p.act(x["th"][:, N], x["lw"][:, N], AF.Tanh)
            p.mm(B[6][:, N], w2z[d], x["th"][:, N])
            p.act(x["sg"][:, N], B[6][:, N], AF.Sigmoid, bias=W0[d])
            p.mm(B[4][:, N], a2z[d], x["la"][:, N])
            p.act(x["a"][:, N], B[4][:, N], AF.Sigmoid, bias=A0[d])
            p.ts(x["kk"][:, N], x["k"][:, N], KK_, ALU.mult)
            p.tt(x["t1"][:, N], x["kk"][:, N], x["kk"][:, N], ALU.mult, eng="pool")
            p.mm(B[6][:, N], same, x["t1"][:, N])
            p.ts(x["t2"][:, N], B[6][:, N], 1e-12, ALU.add)
            p.act(x["t2"][:, N], x["t2"][:, N], AF.Ln)
            p.act(x["t2"][:, N], x["t2"][:, N], AF.Exp, scale=-0.5)
            p.tt(x["kk"][:, N], x["kk"][:, N], x["t2"][:, N], ALU.mult)
            p.ts(x["t1"][:, N], x["a"][:, N], KA_, ALU.mult, omka[:, 0:1], ALU.add)
            p.tt(x["kd"][:, N], x["k"][:, N], x["t1"][:, N], ALU.mult, eng="pool")
            p.tt(x["b"][:, N], x["kk"][:, N], x["a"][:, N], ALU.mult, eng="pool")
            p.op("dve", lambda e: e.tensor_tensor_scan(x["c"].ap[:, N], rmask.ap[:, N], x["sg"].ap[:, N], 0.0,
                                                       ALU.mult, ALU.add), [rmask, x["sg"]], [x["c"]])
            cv = x["c"][:, N].rr("p (a t) -> p a t", t=64)
            p.cp(tot[:, 0:nch], cv[:, :, 63])
            if d == 1:
                p.tt(x["c"][:, N], x["sg"][:, N], x["c"][:, N], ALU.subtract)
                p.tt(cv, cv, tot[:, 0:nch].un(2).bc([128, nch, 64]), ALU.add)
            p.act(ob["etot"][:, 0:nch], tot[:, 0:nch], AF.Exp, scale=-LAM)
            p.act(x["E"][:, N], x["c"][:, N], AF.Exp, scale=-LAM)
            p.tt(ob["Rt"][:, N], x["r"][:, N], x["E"][:, N], ALU.mult)
            p.cp(ob["Rt16"][:, N], ob["Rt"][:, N], eng="pool")
            p.act(x["E"][:, N], x["c"][:, N], AF.Exp, scale=LAM)
            p.tt(ob["Kt"][:, N], x["kd"][:, N], x["E"][:, N], ALU.mult)
            p.tt(ob["Bt"][:, N], x["b"][:, N], x["E"][:, N], ALU.mult, eng="pool")
            p.tt(x["t1"][:, N], x["c"][:, N], x["sg"][:, N], ALU.subtract)
            p.act(x["E"][:, N], x["t1"][:, N], AF.Exp, scale=-LAM)
            p.stt(ob["At"][:, N], x["kk"][:, N], -1.0, x["E"][:, N], ALU.mult, ALU.mult)
            p.tt(x["t2"][:, N].rr("p (a t) -> p a t", t=64), tot[:, 0:nch].un(2).bc([128, nch, 64]), cv, ALU.subtract)
            p.act(x["E"][:, N], x["t2"][:, N], AF.Exp, scale=-LAM)
            p.tt(x["Kh"][:, N], x["kd"][:, N], x["E"][:, N], ALU.mult)
            p.tt(x["Bh"][:, N], x["b"][:, N], x["E"][:, N], ALU.mult, eng="pool")
            npair = n // 128
            for (src, dst, bk, eng) in ((ob["At"], ob["A_tm"], B[4], "act"), (x["Kh"], ob["K_tm"], B[6], "dve"),
                                        (x["Bh"], ob["B_tm"], B[4], "act"), (x["v"], ob["V_tm"], B[6], "dve")):
                is16 = (src is ob["At"]) and RWP == BF16
                bkv = bk.bitcast(BF16) if is16 else bk
                for pr in range(npair):
                    p.tr(bkv[:, pr * 128:(pr + 1) * 128], src[:, pr * 128:(pr + 1) * 128], c.identb if is16 else c.identf)
                p.cp(dst.rr("p a t -> p (a t)")[:, 0:n], bkv[:, 0:n], eng=eng)

        def pair_level(d, ob, pr):
            pb = pairbuf[d]
            Tk = slice(pr * 128, (pr + 1) * 128)
            mN, mM, mI = (("SU", "SL", "IU") if d == 0 else ("SL", "SU", "IL"))
            bk0, bk1 = B[2], B[3]
            HB = ((B[0], B[2]), (B[1], B[3]))

            def prod(slot, lhs, rhs):
                for h in range(2):
                    hp = slice(64 * h, 64 * h + 64)
                    p.mm(HB[h][slot][:, 0:128], ob[lhs][hp, Tk], ob[rhs][hp, Tk])

            def evac(slot, dst, mk):
                for h in range(2):
                    hc = slice(128 * h, 128 * h + 128)
                    p.tt(pb[dst][:, hc], HB[h][slot][:, 0:128], masks[mk][:, 0:128], ALU.mult)

            prod(0, "Bt", "At")
            prod(1, "At", "Bt")
            evac(0, "X", mN)
            evac(1, "XT", mM)
            prod(0, "At", "Kt")
            prod(1, "Kt", "Rt16")
            evac(0, "MAK", mM)
            evac(1, "MRK", mI)
            prod(0, "Bt", "Rt16")
            evac(0, "MRB", mI)
            p.tt(pb["P"], pb["X"], ident2, ALU.add, eng="pool")
            X, XT, Pc = pb["X"], pb["XT"], pb["P"]
            X2, XT2, P2 = pb["X2"], pb["XT2"], pb["P2"]
            for k in range(1, 6):
                for h in range(2):
                    hc = slice(128 * h, 128 * h + 128)
                    p.mm(bk1[:, hc], X[:, hc], XT[:, hc])
                    if k < 5:
                        p.mm(bk0[:, hc], XT[:, hc], X[:, hc])
                p.cp(XT2, bk1[:, 0:256], eng="act")
                if k < 5:
                    p.cp(X2, bk0[:, 0:256])
                for h in range(2):
                    hc = slice(128 * h, 128 * h + 128)
                    p.mm(bk0[:, hc], XT2[:, hc], Pc[:, hc])
                p.tt(P2, bk0[:, 0:256], Pc, ALU.add)
                X, X2 = X2, X
                XT, XT2 = XT2, XT
                Pc, P2 = P2, Pc
            for h in range(2):
                hc = slice(128 * h, 128 * h + 128)
                p.mm(bk1[:, hc], ob["A_tm"][:, pr, :], Pc[:, hc])
                p.mm(bk0[:, hc], pb["MAK"][:, hc], Pc[:, hc])
            p.cp(pb["AT"][0:64, :], bk1[0:64, 0:128], eng="act")
            p.cp(pb["AT"][64:128, :], bk1[64:128, 128:256], eng="act")
            p.cp(pb["WT"], bk0[:, 0:256])

        def chunk_level(d, ob, pr, pi):
            pb = pairbuf[d]
            cp_ = slice(64 * pi, 64 * pi + 64)
            ch = pr * 2 + pi
            tcol = slice(pr * 128 + 64 * pi, pr * 128 + 64 * pi + 64)
            Hold = Hbd[d][hcur[d]]
            Hnew = Hbd[d][1 - hcur[d]]
            Hold16 = H16[d][hcur[d]]
            Hnew16 = H16[d][1 - hcur[d]]
            hcur[d] = 1 - hcur[d]
            bu, bh, by = B[4], (B[5] if d == 0 else B[7]), B[6]
            for h in range(2):
                hc = slice(128 * h, 128 * h + 128)
                ic = slice(64 * h, 64 * h + 64)
                p.mm(bu[:, ic], pb["WT"][:, hc], ob["V_tm"][:, pr, ic], start=True, stop=False)
                p.mm(bu[:, ic], pb["AT"], Hold16[:, ic], start=False, stop=True)
            p.cp(pb["U"][cp_, :], bu[cp_, 0:128], eng="act")
            for h in range(2):
                ic = slice(64 * h, 64 * h + 64)
                p.mm(bh[:, ic], ob["K_tm"][cp_, pr, :], ob["V_tm"][cp_, pr, ic], start=True, stop=False)
                p.mm(bh[:, ic], ob["B_tm"][cp_, pr, :], pb["U"][cp_, ic], start=False, stop=True)
            for h in range(2):
                hp = slice(64 * h, 64 * h + 64)
                ic = slice(64 * h, 64 * h + 64)
                p.stt(Hnew[hp, ic], Hold[hp, ic], ob["etot"][hp, ch:ch + 1], bh[hp, ic], ALU.mult, ALU.add)
            p.cp(Hnew16, Hnew, eng="pool")
            for h in range(2):
                ys = slice(64 * h, 64 * h + 64)
                mcol = slice(128 * h + 64 * pi, 128 * h + 64 * pi + 64)
                p.mm(by[:, ys], Hold16, ob["Rt16"][:, tcol], start=True, stop=False)
                p.mm(by[:, ys], ob["V_tm"][:, pr, :], pb["MRK"][:, mcol], start=False, stop=False)
                p.mm(by[:, ys], pb["U"], pb["MRB"][:, mcol], start=False, stop=True)
            p.cp(ob["yb"][0:64, tcol], by[0:64, 0:64], eng="act")
            p.cp(ob["yb"][64:128, tcol], by[64:128, 64:128], eng="act")

        for step in range(NB):
            for d in range(2):
                bi = order[d][step]
                t0, n = blocks[bi]
                ob = ops_[d][step % 2]
                prep(d, bi, ob)
                npair = n // 128
                prs = range(npair) if d == 0 else range(npair - 1, -1, -1)
                for pr in prs:
                    pair_level(d, ob, pr)
                    for pi in ((0, 1) if d == 0 else (1, 0)):
                        chunk_level(d, ob, pr, pi)
                p.dma(yT[d][:, t0:t0 + n], ob["yb"][:, 0:n])

    with p.scope():
        x = {k: p.sb([128, 512], F32, k) for k in ("y0", "y1", "r", "k", "v", "la", "lg", "a0", "a1", "t1", "t2", "t3", "g")}
        ob_ = [p.sb([128, 512], BF16, f"rwo{i}") for i in range(2)]
        for bi, (t0, n) in enumerate(blocks):
            tok = slice(t0, t0 + n)
            N = slice(0, n)
            p.dma(x["y0"][:, N], yT[0][:, tok])
            p.dma(x["y1"][:, N], yT[1][:, tok])
            p.dma(x["r"][:, N], rw_r[:, tok])
            p.dma(x["k"][:, N], rw_k[:, tok])
            p.dma(x["v"][:, N], rw_v[:, tok])
            p.dma(x["la"][:, N], lo_a[:, tok])
            p.dma(x["lg"][:, N], lo_g[:, tok])
            p.tt(x["y0"][:, N], x["y0"][:, N], x["y1"][:, N], ALU.add)
            p.mm(B[0][:, N], ones64, x["y0"][:, N])
            p.tt(x["y0"][:, N], x["y0"][:, N], B[0][:, N], ALU.subtract)
            p.tt(x["t1"][:, N], x["y0"][:, N], x["y0"][:, N], ALU.mult, eng="pool")
            p.mm(B[1][:, N], ones64, x["t1"][:, N])
            p.ts(x["t1"][:, N], B[1][:, N], LN_X_EPS, ALU.add)
            p.act(x["t1"][:, N], x["t1"][:, N], AF.Ln)
            p.act(x["t1"][:, N], x["t1"][:, N], AF.Exp, scale=-0.5)
            p.tt(x["y0"][:, N], x["y0"][:, N], x["t1"][:, N], ALU.mult)
            p.ts(x["y0"][:, N], x["y0"][:, N], LNW, ALU.mult, LNB, ALU.add)
            for d in range(2):
                p.mm(B[2 + d][:, N], a2z[d], x["la"][:, N])
                p.act(x["a%d" % d][:, N], B[2 + d][:, N], AF.Sigmoid, bias=A0[d])
            p.tt(x["a0"][:, N], x["a0"][:, N], x["a1"][:, N], ALU.add, eng="pool")
            p.ts(x["a0"][:, N], x["a0"][:, N], KA_, ALU.mult, omka[:, 1:2], ALU.add)
            p.tt(x["t2"][:, N], x["k"][:, N], x["a0"][:, N], ALU.mult, eng="pool")
            p.stt(x["t2"][:, N], x["t2"][:, N], RK_, x["r"][:, N], ALU.mult, ALU.mult)
            p.mm(B[4][:, N], same, x["t2"][:, N])
            p.tt(x["t3"][:, N], B[4][:, N], x["v"][:, N], ALU.mult)
            p.tt(x["y0"][:, N], x["y0"][:, N], x["t3"][:, N], ALU.add, eng="pool")
            p.act(x["lg"][:, N], x["lg"][:, N], AF.Sigmoid)
            p.mm(B[5][:, N], g2s, x["lg"][:, N])
            o = ob_[bi % 2]
            p.tt(o[:, N], x["y0"][:, N], B[5][:, N], ALU.mult)
            p.dma(rwoT[:, tok], o[:, N])

    _outer.__exit__(None, None, None)
    if do_attn:
        with p.scope():
            c.set_banks(True)
            attention_block(p, c, qT, kT, v_d, oT, 2, 2, 96, 64, S, 96 ** -0.5, qcT_d=qcT, ocT_d=ocT)
    return p


def _make_pos(q, TL):
    NTL = TL // 128
    t = q * TL + np.arange(TL)
    pos = np.stack([t // 64, t % 64], -1).astype(np.float32)
    return np.ascontiguousarray(pos.reshape(NTL, 128, 2).transpose(1, 0, 2))


def _cat(xs, axis):
    return np.ascontiguousarray(np.concatenate(xs, axis=axis))


def kernel(x, c, ctx, c_ctx, ada_w, ada_b, norm1_g, norm2_g, ab_w_in, ab_w_out, rw_mu, rw_w0,
           rw_w2, rw_a0, rw_a2, rw_k_k, rw_k_a, rw_r_k, rw_g2, rw_ln_w, rw_ln_b, mla_g_qa,
           mla_w_q_up, mla_g_kva, mla_w_kv_up, mla_g_q, mla_g_k, gqa_w_in, gqa_w_out, gqa_g_q,
           gqa_g_k, router_w, router_b, moe_w_gate, moe_w_up, moe_w_down, shared_w_gate,
           shared_w_up, shared_w_down):
    f = lambda a: np.ascontiguousarray(np.asarray(a, dtype=np.float32))
    x, c, ctx, c_ctx = f(x), f(c), f(ctx), f(c_ctx)
    S = x.shape[1]
    TL = S // 4
    LT = CTX + S
    R8 = range(8)
    maps = [{"cvec": np.stack([c[r // 4], c_ctx]), "ada_w": f(ada_w), "ada_b": f(ada_b)} for r in R8]
    r0 = run_prog(build_phase0(), maps)
    mods_tm = [f(r0[r]["mods_tm"]) for r in R8]
    mods_fm = [f(r0[r]["mods_fm"]) for r in R8]
    maps = []
    for r in R8:
        b, q = r // 4, r % 4
        halo = np.zeros((128, D), np.float32)
        fl = np.zeros((128, 2), np.float32)
        if q > 0:
            halo[0] = x[b, q * TL - 1]
            fl[:, 0] = 1
        if q < 3:
            halo[1] = x[b, (q + 1) * TL]
            fl[:, 1] = 1
        maps.append({"x": _cat([x[b, q * TL:(q + 1) * TL], ctx[b], halo], 0), "mods_fm": mods_fm[r][0],
                     "norm1_g": f(norm1_g)[0], "w_in": f(ab_w_in)[0], "mu": f(rw_mu)[0], "flags": fl,
                     "g_qa": f(mla_g_qa), "g_kva": f(mla_g_kva), "w_q_up": f(mla_w_q_up)[0],
                     "w_kv_up": f(mla_w_kv_up)[0], "g_q": f(mla_g_q), "g_k": f(mla_g_k), "pos": _make_pos(q, TL)})
    rA = run_prog(build_pre0(S), maps)
    maps = []
    rw_r_k_flat = f(rw_r_k)[0].reshape(512)
    for r in R8:
        b, j = r // 4, r % 4
        cores = [4 * b + qq for qq in range(4)]
        rwT = _cat([rA[4 * b]["rwT"][:, :, TL:]] + [rA[cc]["rwT"][:, :, :TL] for cc in cores], 2)
        cs = slice(128 * j, 128 * j + 128)
        pv = np.stack([f(rw_w0)[0, 0, cs], f(rw_w0)[0, 1, cs], f(rw_a0)[0, 0, cs], f(rw_a0)[0, 1, cs],
                       f(rw_k_k)[0, cs], f(rw_k_a)[0, cs], rw_r_k_flat[cs], f(rw_ln_w)[0, cs], f(rw_ln_b)[0, cs]])
        hs = slice(2 * j, 2 * j + 2)
        qT = _cat([rA[cc]["qT"][hs, :, :TL] for cc in cores], 2)
        qcT = np.ascontiguousarray(rA[4 * b]["qT"][hs, :, TL:])
        kT = _cat([rA[4 * b]["kT"][hs, :, TL:]] + [rA[cc]["kT"][hs, :, :TL] for cc in cores], 2)
        vv = _cat([rA[4 * b]["v"][TL:]] + [rA[cc]["v"][:TL] for cc in cores], 0)
        vv = np.ascontiguousarray(vv.reshape(LT, 8, 64)[:, hs].transpose(1, 0, 2))
        maps.append({"rw_r": np.ascontiguousarray(rwT[j]), "rw_k": np.ascontiguousarray(rwT[4 + j]),
                     "rw_v": np.ascontiguousarray(rwT[8 + j]), "lo_w": np.ascontiguousarray(rwT[12]),
                     "lo_a": np.ascontiguousarray(rwT[13]), "lo_g": np.ascontiguousarray(rwT[14]),
                     "w2": np.ascontiguousarray(f(rw_w2)[0][:, :, cs].reshape(128, 128)),
                     "a2": np.ascontiguousarray(f(rw_a2)[0][:, :, cs].reshape(128, 128)),
                     "g2": np.ascontiguousarray(f(rw_g2)[0][:, cs]), "pv": np.ascontiguousarray(pv),
                     "qT": qT, "qcT": qcT, "kT": kT, "v": vv})
    rB = run_prog(build_mix0(S), maps)
    del rA
    wg0 = _cat([f(moe_w_gate)[0], f(shared_w_gate)[0][None]], 0)
    wu0 = _cat([f(moe_w_up)[0], f(shared_w_up)[0][None]], 0)
    wd0 = _cat([f(moe_w_down)[0], f(shared_w_down)[0][None]], 0)
    maps = []
    for r in R8:
        b, q = r // 4, r % 4
        chunks = []
        for kc in range(4):
            rw = rB[4 * b + kc]["rwoT"]
            chunks.append(_cat([rw[:, CTX + q * TL:CTX + (q + 1) * TL], rw[:, :CTX]], 1))
        for j in range(4):
            o = rB[4 * b + j]["oT"].reshape(128, S)
            oc = rB[4 * b + j]["ocT"].reshape(128, CTX)
            chunks.append(_cat([o[:, q * TL:(q + 1) * TL], oc], 1))
        maps.append({"x": _cat([x[b, q * TL:(q + 1) * TL], ctx[b]], 0), "attT": np.ascontiguousarray(np.stack(chunks, 0)),
                     "mods_tm": mods_tm[r][0], "mods_fm": mods_fm[r][0], "w_out": f(ab_w_out)[0],
                     "norm2_g": f(norm2_g)[0], "router_w": f(router_w), "router_b": f(router_b)[None, :],
                     "wg_all": wg0, "wu_all": wu0, "wd_all": wd0})
    rC = run_prog(build_post(S, True), maps)
    del rB
    x1 = [f(rC[r]["x_out"]) for r in R8]
    maps = []
    for r in R8:
        q = r % 4
        maps.append({"x": x1[r], "mods_fm": mods_fm[r][1], "norm1_g": f(norm1_g)[1], "w_in": f(gqa_w_in)[0],
                     "g_q": f(gqa_g_q), "g_k": f(gqa_g_k), "pos": _make_pos(q, TL)})
    rD = run_prog(build_pre1(S), maps)
    maps = []
    for r in R8:
        b, kvh = r // 4, r % 4
        cores = [4 * b + qq for qq in range(4)]
        qk = _cat([rD[cc]["qkT"][:, :, :TL] for cc in cores], 2).reshape(20, 64, S)
        kc_ = rD[4 * b]["qkT"][:, :, TL:].reshape(20, 64, CTX)
        qT = np.ascontiguousarray(qk[4 * kvh:4 * kvh + 4])
        kT = _cat([kc_[16 + kvh], qk[16 + kvh]], 1)[None]
        vv = _cat([rD[4 * b]["v"][TL:]] + [rD[cc]["v"][:TL] for cc in cores], 0)
        vv = np.ascontiguousarray(vv[:, kvh * 64:(kvh + 1) * 64])[None]
        maps.append({"qT": qT, "kT": np.ascontiguousarray(kT), "v": vv})
    rE = run_prog(build_attn1(S), maps)
    del rD
    wg1 = _cat([f(moe_w_gate)[1], f(shared_w_gate)[1][None]], 0)
    wu1 = _cat([f(moe_w_up)[1], f(shared_w_up)[1][None]], 0)
    wd1 = _cat([f(moe_w_down)[1], f(shared_w_down)[1][None]], 0)
    maps = []
    for r in R8:
        b, q = r // 4, r % 4
        chunks = []
        for kc in range(8):
            o = rE[4 * b + kc // 2]["oT"]
            i0 = (kc % 2) * 2
            chunks.append(o[i0:i0 + 2].reshape(128, S)[:, q * TL:(q + 1) * TL])
        maps.append({"x": np.ascontiguousarray(x1[r][:TL]), "attT": np.ascontiguousarray(np.stack(chunks, 0)),
                     "mods_tm": mods_tm[r][1], "mods_fm": mods_fm[r][1], "w_out": f(gqa_w_out)[0],
                     "norm2_g": f(norm2_g)[1], "router_w": f(router_w), "router_b": f(router_b)[None, :],
                     "wg_all": wg1, "wu_all": wu1, "wd_all": wd1})
    rF = run_prog(build_post(S, False), maps)
    out = np.zeros((2, S, D), np.float32)
    for r in R8:
        b, q = r // 4, r % 4
        out[b, q * TL:(q + 1) * TL] = rF[r]["x_out"]
    return out
```

```python
import numpy as np
import concourse.bass as bass
import concourse.mybir as mybir
from concourse.bass_utils import run_bass_kernel_spmd

F32 = mybir.dt.float32
BF16 = mybir.dt.bfloat16
I32 = mybir.dt.int32
AF = mybir.ActivationFunctionType
ALU = mybir.AluOpType
AX = mybir.AxisListType

EPOCH = 30000
SCHED_WINDOW = 40
NSLOT = {"sp": 24, "pool": 12, "act": 8}


class Buf:
    __slots__ = ("name", "lw", "rd", "excl")

    def __init__(self, name, excl=False):
        self.name = name
        self.lw = None
        self.rd = []
        self.excl = excl


class V:
    __slots__ = ("buf", "ap")

    def __init__(self, buf, ap):
        self.buf = buf
        self.ap = ap

    def __getitem__(self, idx):
        return V(self.buf, self.ap[idx])

    def rr(self, pat, **kw):
        return V(self.buf, self.ap.rearrange(pat, **kw))

    def bc(self, shape):
        return V(self.buf, self.ap.broadcast_to(shape))

    def un(self, axis):
        return V(self.buf, self.ap.unsqueeze(axis))

    def bitcast(self, dt):
        return V(self.buf, self.ap.bitcast(dt))

    @property
    def shape(self):
        return self.ap.shape


class Op:
    __slots__ = ("eng", "fn", "deps", "sig", "cnt", "isdma", "slot", "slotval", "prevval", "cost")

    def __init__(self, eng, fn, isdma):
        self.cost = 300.0
        self.eng = eng
        self.fn = fn
        self.deps = set()
        self.sig = False
        self.cnt = 0
        self.isdma = isdma
        self.slot = None
        self.slotval = 0
        self.prevval = 0


class _Scope:
    def __init__(self, p):
        self.p = p

    def __enter__(self):
        self.p._scopes.append([])
        return self

    def __exit__(self, *a):
        p = self.p
        items = p._scopes.pop()
        ops = set(p._fence)
        for cm, b in items:
            if b.lw is not None:
                ops.add(b.lw)
            ops.update(b.rd)
        best = {}
        keep = []
        for i in ops:
            o = p.ops[i]
            if o.isdma:
                keep.append(i)
            else:
                if o.eng not in best or best[o.eng] < i:
                    best[o.eng] = i
        p._fence = keep + list(best.values())
        for cm, b in reversed(items):
            cm.__exit__(None, None, None)
        return False


class Prog:
    ENGS = ("pe", "act", "dve", "pool", "sp")

    def __init__(self):
        self.nc = bass.Bass("TRN2", target_bir_lowering=False)
        self.ops = []
        self._ctx = []
        self.ntile = 0
        self._scopes = []
        self._fence = []
        self._dcount = {q: 0 for q in NSLOT}

    def dram(self, name, shape, dt, kind):
        t = self.nc.dram_tensor(name, list(shape), dt, kind=kind)
        return V(Buf(name), t.ap())

    def sb(self, shape, dt, name=None):
        self.ntile += 1
        name = name or f"t{self.ntile}"
        cm = self.nc.sbuf_tensor(f"{name}_{self.ntile}", list(shape), dt)
        h = cm.__enter__()
        b = Buf(name)
        b.rd = list(self._fence)
        if self._scopes:
            self._scopes[-1].append((cm, b))
        else:
            self._ctx.append(cm)
        return V(b, h[:])

    def scope(self):
        return _Scope(self)

    def ps(self, shape, dt, name=None):
        self.ntile += 1
        name = name or f"p{self.ntile}"
        cm = self.nc.psum_tensor(f"{name}_{self.ntile}", list(shape), dt)
        h = cm.__enter__()
        b = Buf(name, excl=True)
        b.rd = list(self._fence)
        if self._scopes:
            self._scopes[-1].append((cm, b))
        else:
            self._ctx.append(cm)
        return V(b, h[:])

    def op(self, eng, fn, reads, writes, isdma=False, cost=None):
        i = len(self.ops)
        o = Op(eng, fn, isdma)
        if cost is None:
            w0 = next((v for v in writes if v is not None), None)
            n = 1
            if w0 is not None:
                for d_ in w0.ap.shape[1:]:
                    n *= d_
            if isdma:
                cost = 2000.0 + n * 128 * 4 / 150.0
            elif eng == "act":
                cost = 230.0 + n / 1.2
            elif eng == "dve":
                cost = 70.0 + n / 0.96
            elif eng == "pool":
                cost = 150.0 + n / 0.96
            else:
                cost = 300.0
        o.cost = cost
        rb = {id(v.buf): v.buf for v in reads if v is not None}
        wb = {id(v.buf): v.buf for v in writes if v is not None}
        for b in rb.values():
            if b.lw is not None:
                o.deps.add(b.lw)
            if b.excl:
                for r in b.rd:
                    if self.ops[r].eng != eng:
                        o.deps.add(r)
        for b in wb.values():
            if b.lw is not None:
                o.deps.add(b.lw)
            for r in b.rd:
                o.deps.add(r)
        o.deps.discard(i)
        for b in rb.values():
            if id(b) not in wb:
                b.rd.append(i)
        for b in wb.values():
            b.lw = i
            b.rd = []
        self.ops.append(o)
        return i

    def dma(self, out, in_, q="sp", **kw):
        self.op(q, lambda e: e.dma_start(out=out.ap, in_=in_.ap, **kw), [in_], [out], isdma=True)

    def mm(self, out, lhsT, rhs, start=True, stop=True, **kw):
        n = 1
        for d_ in rhs.ap.shape[1:]:
            n *= d_
        passes = 4 if rhs.ap.dtype == F32 else 1
        self.op("pe", lambda e: e.matmul(out.ap, lhsT.ap, rhs.ap, start=start, stop=stop, **kw),
                [lhsT, rhs], [out], cost=30.0 + max(n, 64) * passes * 0.45)

    def tr(self, out, in_, ident):
        passes = 4 if in_.ap.dtype == F32 else 1
        self.op("pe", lambda e: e.transpose(out.ap, in_.ap, ident.ap), [in_, ident], [out],
                cost=60.0 + 128 * passes * 0.45)

    def act(self, out, in_, func, bias=None, scale=None, accum=None):
        kw = {}
        rd = [in_]
        if bias is not None:
            if isinstance(bias, V):
                kw["bias"] = bias.ap
                rd.append(bias)
            else:
                kw["bias"] = bias
        if scale is not None:
            if isinstance(scale, V):
                kw["scale"] = scale.ap
                rd.append(scale)
            else:
                kw["scale"] = scale
        wr = [out]
        if accum is not None:
            kw["accum_out"] = accum.ap
            wr.append(accum)
        self.op("act", lambda e: e.activation(out.ap, in_.ap, func, **kw), rd, wr)

    def tt(self, out, a, b, op, eng="dve"):
        self.op(eng, lambda e: e.tensor_tensor(out.ap, a.ap, b.ap, op), [a, b], [out])

    def ts(self, out, a, s1, op0, s2=None, op1=None, eng="dve", accum=None):
        rd = [a]
        x1 = s1.ap if isinstance(s1, V) else s1
        x2 = s2.ap if isinstance(s2, V) else s2
        if isinstance(s1, V):
            rd.append(s1)
        if isinstance(s2, V):
            rd.append(s2)
        kw = {}
        wr = [out]
        if op1 is not None:
            kw["op1"] = op1
        if accum is not None:
            kw["accum_out"] = accum.ap
            wr.append(accum)
        self.op(eng, lambda e: e.tensor_scalar(out.ap, a.ap, x1, x2, op0, **kw), rd, wr)

    def stt(self, out, a, s, b, op0, op1, eng="dve"):
        rd = [a, b]
        x = s.ap if isinstance(s, V) else s
        if isinstance(s, V):
            rd.append(s)
        self.op(eng, lambda e: e.scalar_tensor_tensor(out.ap, a.ap, x, b.ap, op0, op1), rd, [out])

    def cp(self, out, in_, eng="dve"):
        if eng == "act":
            self.op("act", lambda e: e.copy(out.ap, in_.ap), [in_], [out])
        else:
            self.op(eng, lambda e: e.tensor_copy(out.ap, in_.ap), [in_], [out])

    def red(self, out, in_, op=ALU.add, axis=AX.X, eng="dve"):
        self.op(eng, lambda e: e.tensor_reduce(out.ap, in_.ap, axis, op), [in_], [out])

    def memset(self, out, val, eng="pool"):
        self.op(eng, lambda e: e.memset(out.ap, val), [], [out])

    def iota(self, out, pattern, base=0, cm=0):
        self.op("pool", lambda e: e.iota(out.ap, pattern, base=base, channel_multiplier=cm,
                                         allow_small_or_imprecise_dtypes=True), [], [out])

    def _schedule(self, ops):
        n = len(ops)
        W = SCHED_WINDOW
        SEM = 120.0
        users = [[] for _ in range(n)]
        nleft = [0] * n
        for i, o in enumerate(ops):
            nleft[i] = len(o.deps)
            for j in o.deps:
                users[j].append(i)
        pend = {e: [i for i, o in enumerate(ops) if o.eng == e] for e in self.ENGS}
        head = {e: 0 for e in self.ENGS}
        done = [False] * n
        finish = [0.0] * n
        ready = [0.0] * n
        tfree = {e: 0.0 for e in self.ENGS}
        order = {e: [] for e in self.ENGS}
        remaining = n
        while remaining:
            best = None
            for e in self.ENGS:
                lst = pend[e]
                h = head[e]
                while h < len(lst) and done[lst[h]]:
                    h += 1
                head[e] = h
                seen_dma = False
                cnt = 0
                k = h
                while k < len(lst) and cnt < W:
                    i = lst[k]
                    k += 1
                    if done[i]:
                        continue
                    cnt += 1
                    o = ops[i]
                    if nleft[i] > 0:
                        continue
                    st = max(tfree[e], ready[i])
                    key = (st + 0.5 * (cnt - 1), i)
                    if best is None or key < best[0]:
                        best = (key, e, i, st)
            if best is None:
                raise RuntimeError("scheduler deadlock")
            _, e, i, st = best
            o = ops[i]
            done[i] = True
            remaining -= 1
            order[e].append(i)
            if o.isdma:
                tfree[e] = st + 60.0
                finish[i] = st + o.cost
            else:
                tfree[e] = st + o.cost
                finish[i] = st + o.cost
            for u in users[i]:
                nleft[u] -= 1
                lat = 0.0 if (o.eng == "pe" and ops[u].eng == "pe" and not o.isdma) else SEM
                r = finish[i] + lat
                if r > ready[u]:
                    ready[u] = r
        self.est_ns = max(finish) if finish else 0.0
        return order

    def finish(self):
        nc = self.nc
        ops = self.ops
        last = {}
        for i, o in enumerate(ops):
            last[o.eng] = i
        fin = Op("sp", None, False)
        for e, i in last.items():
            fin.deps.add(i)
        for i, o in enumerate(ops):
            if o.isdma:
                fin.deps.add(i)
        ops.append(fin)
        order = self._schedule(ops) if SCHED_WINDOW > 0 else {e: [i for i, o in enumerate(ops) if o.eng == e] for e in self.ENGS}
        self._order = order
        dcnt = {q: 0 for q in NSLOT}
        for e_ in self.ENGS:
            for i_ in order[e_]:
                o = ops[i_]
                if o.isdma:
                    k = dcnt[o.eng]
                    dcnt[o.eng] += 1
                    n_ = NSLOT[o.eng]
                    o.slot = (o.eng, k % n_)
                    o.slotval = 16 * (k // n_ + 1)
                    o.prevval = 16 * (k // n_)
        self._dcount = dcnt
        for o in ops:
            for j in o.deps:
                d = ops[j]
                if d.eng == "pe" and o.eng == "pe" and not o.isdma:
                    continue
                d.sig = True
        ccount = {e: 0 for e in self.ENGS}
        dcount = self._dcount
        for e_ in self.ENGS:
            for i_ in order[e_]:
                o = ops[i_]
                if o.isdma:
                    pass
                elif o.sig:
                    ccount[o.eng] += 1
                    o.cnt = ccount[o.eng]
        sems = {}
        cms = []

        def getsem(key):
            if key not in sems:
                cm = nc.semaphore(f"s_{key[0]}_{key[1]}")
                sems[key] = cm.__enter__()
                cms.append(cm)
            return sems[key]

        for e in self.ENGS:
            for ep in range(ccount[e] // EPOCH + 1):
                getsem((e, "c%d" % ep))
        for q, n in NSLOT.items():
            for s in range(min(n, dcount[q])):
                getsem((q, s))

        def compkey(o):
            ep = (o.cnt - 1) // EPOCH
            return (o.eng, "c%d" % ep), o.cnt - ep * EPOCH

        with nc.Block() as block:
            def emit_engine(ename, eng):
                known = {}
                for i in order[ename]:
                    o = ops[i]
                    waits = {}
                    for j in o.deps:
                        d = ops[j]
                        if d.isdma:
                            key, val = d.slot, d.slotval
                        else:
                            if d.eng == "pe" and ename == "pe" and not o.isdma:
                                continue
                            key, val = compkey(d)
                        if waits.get(key, 0) < val:
                            waits[key] = val
                    if o.isdma and o.prevval > 0:
                        key = o.slot
                        if waits.get(key, 0) < o.prevval:
                            waits[key] = o.prevval
                    for key, val in waits.items():
                        if known.get(key, 0) >= val:
                            continue
                        known[key] = val
                        eng.wait_ge(getsem(key), val)
                    if o.fn is None:
                        continue
                    ins = o.fn(eng)
                    if o.isdma:
                        ins.then_inc(getsem(o.slot), 16)
                    elif o.sig:
                        key, _ = compkey(o)
                        ins.then_inc(getsem(key), 1)

            @block.tensor
            def _(e):
                emit_engine("pe", e)

            @block.scalar
            def _(e):
                emit_engine("act", e)

            @block.vector
            def _(e):
                emit_engine("dve", e)

            @block.gpsimd
            def _(e):
                emit_engine("pool", e)

            @block.sync
            def _(e):
                emit_engine("sp", e)
        self._cms = cms
        return nc


D = 1024
CTX = 256
NORM_EPS = 1e-6
THETA = 10000.0
import math


class Ctx:
    def __init__(self, p, banks=True):
        self.p = p
        self.bi = 0
        self.dbanks = []
        if banks:
            self.set_banks(False)
        io = p.sb([128, 128], F32, "io")
        pi = p.sb([128, 1], F32, "pi")
        p.iota(io, [[1, 128]], base=0, cm=0)
        p.iota(pi, [[0, 1]], base=0, cm=1)
        self.pidx = pi
        self.identf = p.sb([128, 128], F32, "identf")
        p.ts(self.identf, io, pi[:, 0:1], ALU.is_equal)
        self.identb = p.sb([128, 128], BF16, "identb")
        p.cp(self.identb, self.identf)
        self.iof = io

    def set_banks(self, dbl):
        p = self.p
        if dbl == 3:
            self.dbanks = [p.ps([128, 1536], F32, f"tbank{i}") for i in range(2)]
            self.banks = [None] * 6 + [p.ps([128, 512], F32, f"bank{i}") for i in range(6, 8)]
            return
        if dbl:
            self.dbanks = [p.ps([128, 1024], F32, f"dbank{i}") for i in range(2)]
            self.banks = [self.dbanks[0][:, 0:512], self.dbanks[0][:, 512:1024],
                          self.dbanks[1][:, 0:512], self.dbanks[1][:, 512:1024]]
            self.banks += [p.ps([128, 512], F32, f"bank{i}") for i in range(4, 8)]
        else:
            self.banks = [p.ps([128, 512], F32, f"bank{i}") for i in range(8)]

    def bank(self):
        b = self.banks[self.bi % 8]
        self.bi += 1
        return b

    def load_fm(self, rows_v, n, eng_out="dve"):
        p = self.p
        tmp = p.sb([n, 128], F32, "lfm_tmp")
        p.dma(tmp, rows_v)
        bk = self.bank()
        p.tr(bk[:, 0:n], tmp, self.identf[0:n, 0:n])
        out = p.sb([128, n], F32, "lfm_out")
        p.cp(out, bk[:, 0:n], eng=eng_out)
        return out

    def rstd(self, ss, n, inv_d, eps):
        p = self.p
        p.ts(ss, ss, inv_d, ALU.mult, eps, ALU.add)
        p.act(ss, ss, AF.Ln)
        p.act(ss, ss, AF.Exp, scale=-0.5)


def run_prog(p, in_maps):
    nc = p.finish()
    res = run_bass_kernel_spmd(nc, in_maps, core_ids=list(range(len(in_maps))))
    return res.results


def build_phase0():
    p = Prog()
    cvec = p.dram("cvec", [2, D], F32, "ExternalInput")
    ada_w = p.dram("ada_w", [2, D, 6 * D], F32, "ExternalInput")
    ada_b = p.dram("ada_b", [2, 6 * D], F32, "ExternalInput")
    mods_tm = p.dram("mods_tm", [2, 2, 6 * D], F32, "ExternalOutput")
    mods_fm = p.dram("mods_fm", [2, 128, 48, 2], F32, "ExternalOutput")
    c = Ctx(p)
    cT = c.load_fm(cvec.rr("j (c p) -> (j c) p", p=128), 16)
    sT = p.sb([128, 16], F32, "sT")
    p.act(sT, cT, AF.Silu)
    sTv = sT.rr("p (j c) -> p c j", j=2)
    wbuf = [p.sb([128, 8, 1536], F32, f"adaw{i}") for i in range(2)]
    it = 0
    for l in range(2):
        bfm = c.load_fm(ada_b[l].rr("(c p) -> c p", p=128), 48)
        ofm = p.sb([128, 48, 2], F32, "ofm")
        for qd in range(4):
            w = wbuf[it % 2]
            it += 1
            for kc in range(8):
                p.dma(w[:, kc, :], ada_w[l, kc * 128:(kc + 1) * 128, qd * 1536:(qd + 1) * 1536],
                      q=("sp" if kc % 2 == 0 else "act"))
            bk = c.bank()
            for cc in range(12):
                for kc in range(8):
                    p.mm(bk[:, cc * 2:cc * 2 + 2], w[:, kc, cc * 128:(cc + 1) * 128], sTv[:, kc, :],
                         start=(kc == 0), stop=(kc == 7))
            p.tt(ofm[:, qd * 12:(qd + 1) * 12, :], bk[:, 0:24].rr("p (c j) -> p c j", j=2),
                 bfm[:, qd * 12:(qd + 1) * 12].un(2).bc([128, 12, 2]), ALU.add)
        p.dma(mods_fm[l], ofm)
        bk = c.bank()
        ofm2 = p.sb([128, 2, 48], F32, "ofm2")
        p.cp(ofm2, ofm.rr("p c j -> p j c"))
        p.tr(bk[0:96, 0:128], ofm2.rr("p j c -> p (j c)"), c.identf)
        otm = p.sb([96, 128], F32, "otm")
        p.cp(otm, bk[0:96, 0:128])
        for j in range(2):
            p.dma(mods_tm[l][j].rr("(c p) -> c p", p=128), otm[48 * j:48 * j + 48, :])
    return p


def build_post(S, has_ctx, stop=99):
    TL = S // 4
    NTL = TL // 128
    NT = NTL + (2 if has_ctx else 0)
    TOK = NT * 128
    p = Prog()
    x_in = p.dram("x", [TOK, D], F32, "ExternalInput")
    attT = p.dram("attT", [8, 128, TOK], BF16, "ExternalInput")
    mods_tm = p.dram("mods_tm", [2, 6 * D], F32, "ExternalInput")
    mods_fm = p.dram("mods_fm", [128, 48, 2], F32, "ExternalInput")
    w_out = p.dram("w_out", [D, D], F32, "ExternalInput")
    n2g = p.dram("norm2_g", [D], F32, "ExternalInput")
    router_w = p.dram("router_w", [D, 16], F32, "ExternalInput")
    router_b = p.dram("router_b", [1, 16], F32, "ExternalInput")
    wg_all = p.dram("wg_all", [17, D, 256], F32, "ExternalInput")
    wu_all = p.dram("wu_all", [17, D, 256], F32, "ExternalInput")
    wd_all = p.dram("wd_all", [17, 256, D], F32, "ExternalInput")
    x_out = p.dram("x_out", [TOK, D], F32, "ExternalOutput")
    c = Ctx(p)
    B = c.banks
    nvar = 2 if has_ctx else 1
    blocks = []
    t = 0
    while t < NTL:
        n = min(4, NTL - t)
        blocks.append((0, t, n))
        t += n
    if has_ctx:
        blocks.append((1, NTL, 2))

    mfm = p.sb([128, 48, 2], F32, "mfm")
    p.dma(mfm, mods_fm)
    g2fm = c.load_fm(n2g.rr("(c p) -> c p", p=128), 8)
    A2 = p.sb([128, 8, 2], F32, "A2")
    p.ts(A2, mfm[:, 32:40, :], 1.0, ALU.add)
    p.tt(A2, A2, g2fm.un(2).bc([128, 8, 2]), ALU.mult)
    SH2 = mfm[:, 24:32, :]
    rw = p.sb([128, 8, 16], F32, "rw")
    p.dma(rw, router_w.rr("(kc p) e -> p kc e", p=128))
    rb = p.sb([128, 16], F32, "rb")
    p.dma(rb, router_b.bc([128, 16]))
    sel = p.sb([16, 16, 128], F32, "sel")
    p.iota(sel, [[1, 16], [0, 128]], base=0, cm=0)
    p.ts(sel, sel, c.pidx[0:16, 0:1], ALU.is_equal)
    xs = p.sb([128, NT, D], F32, "xs")
    for t in range(NT):
        p.dma(xs[:, t, :], x_in[t * 128:(t + 1) * 128, :])
    h2T = p.sb([128, 8, TOK], BF16, "h2T")
    combT = p.sb([16, TOK], F32, "combT")
    lg_all = p.sb([128, NT, 16], F32, "lg_all")
    gabc = [p.sb([128, D], F32, f"gabc{i}") for i in range(2)]

    def load_ga(which, var, dst):
        p.dma(dst, mods_tm[var:var + 1, which * D:(which + 1) * D].bc([128, D]))

    with p.scope():
        if stop >= 2:
            wo = p.sb([128, 8, D], BF16, "wo")
            atb = [p.sb([128, 8, 128], BF16, f"atb{i}") for i in range(2)]
            for var in range(nvar):
                p.dma(wo, w_out.rr("(kc p) d -> p kc d", p=128), q="pool")
                load_ga(2, var, gabc[0])
                p.tt(wo, wo, gabc[0].un(1).bc([128, 8, D]), ALU.mult, eng="pool")
                tiles = range(NTL) if var == 0 else range(NTL, NT)
                for t in tiles:
                    a = atb[t % 2]
                    p.dma(a, attT[:, :, t * 128:(t + 1) * 128].rr("c p t -> p c t"))
                    for half in range(2):
                        bk = B[(t % 2) * 2 + half]
                        for kc in range(8):
                            p.mm(bk[:, :], a[:, kc, :], wo[:, kc, half * 512:(half + 1) * 512],
                                 start=(kc == 0), stop=(kc == 7))
                        xv = xs[:, t, half * 512:(half + 1) * 512]
                        p.tt(xv, xv, bk, ALU.add)

    with p.scope():
        if stop >= 3:
            junk = p.sb([128, D], BF16, "junk")
            xn = [p.sb([128, D], F32, f"xn{i}") for i in range(2)]
            h2f = [p.sb([128, 8, 128], F32, f"h2f{i}") for i in range(2)]
            ssq = p.sb([128, NT], F32, "ssq")
            for t in range(NT):
                var = 0 if t < NTL else 1
                p.act(junk, xs[:, t, :], AF.Square, accum=ssq[:, t:t + 1])
            c.rstd(ssq, NT, 1.0 / D, NORM_EPS)
            for t in range(NT):
                var = 0 if t < NTL else 1
                x_n = xn[t % 2]
                hf = h2f[t % 2]
                p.ts(x_n, xs[:, t, :], ssq[:, t:t + 1], ALU.mult)
                for hb in range(2):
                    bk = B[4 + (t % 2) * 2 + hb]
                    for k4 in range(4):
                        kc = hb * 4 + k4
                        p.tr(bk[:, k4 * 128:(k4 + 1) * 128], x_n[:, kc * 128:(kc + 1) * 128], c.identf)
                    for k4 in range(4):
                        kc = hb * 4 + k4
                        p.act(hf[:, kc, :], bk[:, k4 * 128:(k4 + 1) * 128], AF.Identity,
                              bias=SH2[:, kc, var:var + 1], scale=A2[:, kc, var:var + 1])
                p.cp(h2T[:, :, t * 128:(t + 1) * 128], hf, eng="pool")
                bk = B[t % 2]
                for kc in range(8):
                    p.mm(bk[:, 0:16], hf[:, kc, :], rw[:, kc, :], start=(kc == 0), stop=(kc == 7))
                p.cp(lg_all[:, t, :], bk[:, 0:16])
            N16 = NT * 16
            sc = p.sb([128, NT, 16], F32, "sc")
            bs = p.sb([128, NT, 16], F32, "bs")
            p.act(sc, lg_all, AF.Sigmoid)
            p.tt(bs, sc, rb.un(1).bc([128, NT, 16]), ALU.add)
            bsv = bs.rr("p t (g e) -> p (t g) e", g=4)
            G = NT * 4
            m1 = p.sb([128, G], F32, "m1")
            m2 = p.sb([128, G], F32, "m2")
            p.red(m1, bsv, op=ALU.max)
            eq1 = p.sb([128, G, 4], F32, "eq1")
            p.tt(eq1, bsv, m1.un(2).bc([128, G, 4]), ALU.is_equal)
            p.stt(eq1, eq1, -1e9, bsv, ALU.mult, ALU.add)
            p.red(m2, eq1, op=ALU.max)
            gs = p.sb([128, NT, 4], F32, "gs")
            p.tt(gs.rr("p t g -> p (t g)"), m1, m2, ALU.add)
            gmax = p.sb([128, NT], F32, "gmax")
            p.red(gmax, gs, op=ALU.max)
            gsel = p.sb([128, NT, 4], F32, "gsel")
            p.tt(gsel, gs, gmax.un(2).bc([128, NT, 4]), ALU.is_equal)
            ge2 = p.sb([128, G, 4], F32, "ge2")
            p.tt(ge2, bsv, m2.un(2).bc([128, G, 4]), ALU.is_ge)
            p.tt(ge2, ge2, gsel.rr("p t g -> p (t g)").un(2).bc([128, G, 4]), ALU.mult)
            p.tt(ge2, ge2, sc.rr("p t (g e) -> p (t g) e", g=4), ALU.mult)
            den = p.sb([128, NT], F32, "den")
            p.red(den, ge2.rr("p (t g) e -> p t (g e)", g=4))
            p.op("dve", lambda e: e.reciprocal(den.ap, den.ap), [den], [den])
            comb = p.sb([128, NT, 16], F32, "comb")
            p.tt(comb, ge2.rr("p (t g) e -> p t (g e)", g=4), den.un(2).bc([128, NT, 16]), ALU.mult)
            for t in range(NT):
                bk = B[t % 2]
                p.tr(bk[0:16, 0:128], comb[:, t, :], c.identf)
                p.cp(combT[:, t * 128:(t + 1) * 128], bk[0:16, 0:128], eng="act")

    with p.scope():
        if stop >= 4:
            wg = [p.sb([128, 8, 256], BF16, f"wg{i}") for i in range(2)]
            wu = [p.sb([128, 8, 256], BF16, f"wu{i}") for i in range(2)]
            wd = [p.sb([128, 2, D], BF16, f"wd{i}") for i in range(2)]
            wds = [p.sb([128, 2, D], BF16, f"wds{i}") for i in range(2)]
            comb_sb = [p.sb([128, 512], F32, f"comb_sb{i}") for i in range(2)]
            sg = [p.sb([128, 512], F32, f"sg{i}") for i in range(2)]
            tg = [p.sb([128, 512], F32, f"tg{i}") for i in range(2)]
            actb = [p.sb([128, 2, 512], BF16, f"actb{i}") for i in range(2)]
            for var in range(nvar):
                load_ga(5, var, gabc[var])
            it = 0
            for e in range(17 if stop < 40 or stop == 99 else stop - 40):
                eb = e % 2
                p.dma(wg[eb], wg_all[e].rr("(kc p) f -> p kc f", p=128), q="pool")
                p.dma(wu[eb], wu_all[e].rr("(kc p) f -> p kc f", p=128), q="pool")
                p.dma(wd[eb], wd_all[e].rr("(fc p) d -> p fc d", p=128), q="pool")
                for var in range(nvar):
                    p.tt(wds[eb], wd[eb], gabc[var].un(1).bc([128, 2, D]), ALU.mult, eng="pool")
                    for (bv, t0, nt) in blocks:
                        if bv != var:
                            continue
                        n = nt * 128
                        tok = slice(t0 * 128, t0 * 128 + n)
                        ib = it % 2
                        it += 1
                        if e < 16:
                            p.mm(B[4][:, 0:n], sel[:, e, :], combT[:, tok])
                            p.cp(comb_sb[ib][:, 0:n], B[4][:, 0:n], eng="act")
                        for fc in range(2):
                            gb = B[fc * 2]
                            ub = B[fc * 2 + 1]
                            for kc in range(8):
                                p.mm(gb[:, 0:n], wg[eb][:, kc, fc * 128:(fc + 1) * 128], h2T[:, kc, tok],
                                     start=(kc == 0), stop=(kc == 7))
                            for kc in range(8):
                                p.mm(ub[:, 0:n], wu[eb][:, kc, fc * 128:(fc + 1) * 128], h2T[:, kc, tok],
                                     start=(kc == 0), stop=(kc == 7))
                            p.act(sg[fc][:, 0:n], gb[:, 0:n], AF.Silu)
                            if e < 16:
                                p.tt(tg[fc][:, 0:n], ub[:, 0:n], comb_sb[ib][:, 0:n], ALU.mult)
                                p.tt(actb[ib][:, fc, 0:n], sg[fc][:, 0:n], tg[fc][:, 0:n], ALU.mult, eng="pool")
                            else:
                                p.tt(actb[ib][:, fc, 0:n], sg[fc][:, 0:n], ub[:, 0:n], ALU.mult)
                        for ti in range(nt):
                            t = t0 + ti
                            for half in range(2):
                                db = B[5 + (ti * 2 + half) % 3]
                                for fc in range(2):
                                    p.mm(db, actb[ib][:, fc, ti * 128:(ti + 1) * 128],
                                         wds[eb][:, fc, half * 512:(half + 1) * 512],
                                         start=(fc == 0), stop=(fc == 1))
                                xv = xs[:, t, half * 512:(half + 1) * 512]
                                p.tt(xv, xv, db, ALU.add)
    for t in range(NT):
        p.dma(x_out[t * 128:(t + 1) * 128, :], xs[:, t, :])
    return p


def rope_tables(p, c, pos, NTL, half):
    inv = p.sb([128, half], F32, "inv")
    for i in range(half):
        p.memset(inv[:, i:i + 1], float(THETA ** (-i / half)) / (2.0 * math.pi))
    Y = p.sb([128, NTL, 2, half], F32, "ropeY")
    p.tt(Y.rr("p t r h -> p (t r) h"), pos.rr("p t r -> p (t r)").un(2).bc([128, NTL * 2, half]),
         inv.un(1).bc([128, NTL * 2, half]), ALU.mult)
    COS = p.sb([128, NTL, 2, 2 * half], F32, "COS")
    SINS = p.sb([128, NTL, 2, 2 * half], F32, "SINS")
    Yi = p.sb([128, NTL, 2, half], I32, "ropeYi")
    Yf = p.sb([128, NTL, 2, half], F32, "ropeYf")
    T = p.sb([128, NTL, 2, half], F32, "ropeT")
    R = p.sb([128, NTL, 2, half], F32, "ropeR")
    for which in range(2):
        if which == 1:
            p.ts(Y, Y, 0.25, ALU.add)
        p.cp(Yi, Y)
        p.cp(Yf, Yi)
        p.tt(R, Y, Yf, ALU.subtract)
        p.ts(T, R, 0.5, ALU.is_gt)
        p.tt(R, R, T, ALU.subtract)
        p.ts(T, R, -0.5, ALU.is_lt)
        p.tt(R, R, T, ALU.add)
        if which == 0:
            p.act(SINS[:, :, :, half:2 * half], R, AF.Sin, scale=2.0 * math.pi * (1 - 1e-6))
            p.ts(SINS[:, :, :, 0:half], SINS[:, :, :, half:2 * half], -1.0, ALU.mult)
        else:
            p.act(COS[:, :, :, 0:half], R, AF.Sin, scale=2.0 * math.pi * (1 - 1e-6))
            p.cp(COS[:, :, :, half:2 * half], COS[:, :, :, 0:half])
    return COS, SINS


def qk_post(p, c, pieces, nh, hd, gains_bc, dst, scratch, rope=None):
    sq, qk, ss = scratch["sq"], scratch["qk"], scratch["ss"]
    off = 0
    for (v, n) in pieces:
        p.act(sq[:, off:off + n * hd], v, AF.Square)
        off += n * hd
    p.red(ss, sq.rr("p (h d) -> p h d", d=hd))
    c.rstd(ss, nh, 1.0 / hd, NORM_EPS)
    off = 0
    h0 = 0
    for (v, n) in pieces:
        p.tt(qk[:, off:off + n * hd].rr("p (h d) -> p h d", d=hd), v.rr("p (h d) -> p h d", d=hd),
             ss[:, h0:h0 + n].un(2).bc([128, n, hd]), ALU.mult)
        off += n * hd
        h0 += n
    if rope is None:
        p.tt(dst, qk, gains_bc, ALU.mult, eng="pool")
        return
    p.tt(qk, qk, gains_bc, ALU.mult, eng="pool")
    roff, half, COS_t, SINS_t = rope
    qv = qk.rr("p (h d) -> p h d", d=hd)
    dv = dst.rr("p (h d) -> p h d", d=hd)
    if roff > 0:
        p.cp(dv[:, :, 0:roff], qv[:, :, 0:roff], eng="pool")
    t1, t2 = scratch["t1"], scratch["t2"]
    w = 2 * half
    for rc in range(2):
        xs = qv[:, :, roff + rc * w: roff + (rc + 1) * w]
        a = t1[:, 0:nh * w].rr("p (h d) -> p h d", d=w)
        b = t2[:, 0:nh * w].rr("p (h d) -> p h d", d=w)
        eng = "dve" if rc == 0 else "pool"
        p.tt(a, xs, COS_t[:, rc, :].un(1).bc([128, nh, w]), ALU.mult, eng=eng)
        p.tt(b[:, :, 0:half], xs[:, :, half:w], SINS_t[:, rc, 0:half].un(1).bc([128, nh, half]), ALU.mult, eng=eng)
        p.tt(b[:, :, half:w], xs[:, :, 0:half], SINS_t[:, rc, half:w].un(1).bc([128, nh, half]), ALU.mult, eng=eng)
        p.tt(dv[:, :, roff + rc * w: roff + (rc + 1) * w], a, b, ALU.add, eng=eng)


def norm1_hT(p, c, xt, A, SH, var, hT_dst, xnb, junk, ssq, banks):
    p.act(junk, xt, AF.Square, accum=ssq)
    c.rstd(ssq, 1, 1.0 / D, NORM_EPS)
    p.ts(xnb, xt, ssq[:, 0:1], ALU.mult)
    for hb in range(2):
        bk = banks[hb].bitcast(BF16)
        for k4 in range(4):
            kc = hb * 4 + k4
            p.tr(bk[:, k4 * 128:(k4 + 1) * 128], xnb[:, kc * 128:(kc + 1) * 128], c.identb)
        for k4 in range(4):
            kc = hb * 4 + k4
            p.act(hT_dst[:, kc, :], bk[:, k4 * 128:(k4 + 1) * 128], AF.Identity,
                  bias=SH[:, kc, var:var + 1], scale=A[:, kc, var:var + 1])


def mod_consts(p, c, mods_fm, norm_g, which_sh, which_sc):
    mfm = p.sb([128, 48, 2], F32, "mfm")
    p.dma(mfm, mods_fm)
    gfm = c.load_fm(norm_g.rr("(c p) -> c p", p=128), 8)
    A = p.sb([128, 8, 2], F32, "Amod")
    p.ts(A, mfm[:, which_sc * 8:(which_sc + 1) * 8, :], 1.0, ALU.add)
    p.tt(A, A, gfm.un(2).bc([128, 8, 2]), ALU.mult)
    SH = mfm[:, which_sh * 8:(which_sh + 1) * 8, :]
    return A, SH


def build_pre1(S):
    TL = S // 4
    NTL = TL // 128
    NT = NTL + 2
    TOK = NT * 128
    p = Prog()
    x_in = p.dram("x", [TOK, D], F32, "ExternalInput")
    mods_fm = p.dram("mods_fm", [128, 48, 2], F32, "ExternalInput")
    n1g = p.dram("norm1_g", [D], F32, "ExternalInput")
    w_in = p.dram("w_in", [D, 1536], F32, "ExternalInput")
    g_q = p.dram("g_q", [1, 64], F32, "ExternalInput")
    g_k = p.dram("g_k", [1, 64], F32, "ExternalInput")
    pos_in = p.dram("pos", [128, NTL, 2], F32, "ExternalInput")
    qkT = p.dram("qkT", [10, 128, TOK], BF16, "ExternalOutput")
    v_out = p.dram("v", [TOK, 256], BF16, "ExternalOutput")
    c = Ctx(p)
    B = c.banks
    A, SH = mod_consts(p, c, mods_fm, n1g, 0, 1)
    pos = p.sb([128, NTL, 2], F32, "pos")
    p.dma(pos, pos_in)
    COS, SINS = rope_tables(p, c, pos, NTL, 16)
    gains = p.sb([128, 20, 64], F32, "gains")
    gq = p.sb([128, 64], F32, "gq")
    gk = p.sb([128, 64], F32, "gk")
    p.dma(gq, g_q.bc([128, 64]))
    p.dma(gk, g_k.bc([128, 64]))
    p.cp(gains[:, 0:16, :], gq.un(1).bc([128, 16, 64]))
    p.cp(gains[:, 16:20, :], gk.un(1).bc([128, 4, 64]))
    gains_f = gains.rr("p h d -> p (h d)")
    wi = p.sb([128, 8, 1536], BF16, "wi")
    p.dma(wi, w_in.rr("(kc p) n -> p kc n", p=128), q="pool")
    xt = [p.sb([128, D], F32, f"xt{i}") for i in range(2)]
    xnb = [p.sb([128, D], BF16, f"xnb{i}") for i in range(2)]
    junk = p.sb([128, D], BF16, "junk")
    hT = [p.sb([128, 8, 128], BF16, f"hT{i}") for i in range(2)]
    ssq = [p.sb([128, 1], F32, f"ssq{i}") for i in range(2)]
    scrs = [dict(sq=p.sb([128, 1280], F32, "sq"), qk=p.sb([128, 1280], F32, "qk"), ss=p.sb([128, 20], F32, "ss"),
                 t1=p.sb([128, 1280], F32, "t1"), t2=p.sb([128, 1280], F32, "t2")) for _ in range(2)]
    qkb = [p.sb([128, 1280], BF16, f"qkb{i}") for i in range(2)]
    vb = [p.sb([128, 256], BF16, f"vb{i}") for i in range(2)]
    qkTs = [p.sb([128, 10, 128], BF16, f"qkTs{i}") for i in range(2)]
    for t in range(NT):
        i2 = t % 2
        var = 0 if t < NTL else 1
        p.dma(xt[i2], x_in[t * 128:(t + 1) * 128, :])
        norm1_hT(p, c, xt[i2], A, SH, var, hT[i2], xnb[i2], junk, ssq[i2], (B[0], B[1]))
        for blk in range(3):
            bk = B[2 + blk]
            for kc in range(8):
                p.mm(bk, hT[i2][:, kc, :], wi[:, kc, blk * 512:(blk + 1) * 512], start=(kc == 0), stop=(kc == 7))
        rope = None if var == 1 else (0, 16, COS[:, t], SINS[:, t])
        qk_post(p, c, [(B[2], 8), (B[3], 8), (B[4][:, 0:256], 4)], 20, 64, gains_f, qkb[i2], scrs[i2], rope)
        p.cp(vb[i2], B[4][:, 256:512], eng="act")
        p.dma(v_out[t * 128:(t + 1) * 128, :], vb[i2])
        bA = B[5].bitcast(BF16)
        bB = B[6].bitcast(BF16)
        for pr in range(10):
            dstb = bA[:, pr * 128:(pr + 1) * 128] if pr < 8 else bB[:, (pr - 8) * 128:(pr - 7) * 128]
            p.tr(dstb, qkb[i2][:, pr * 128:(pr + 1) * 128], c.identb)
        p.cp(qkTs[i2][:, 0:8, :].rr("p a t -> p (a t)"), bA, eng="act")
        p.cp(qkTs[i2][:, 8:10, :].rr("p a t -> p (a t)"), bB[:, 0:256])
        p.dma(qkT[:, :, t * 128:(t + 1) * 128].rr("a p t -> p a t"), qkTs[i2])
    return p


def attention_gen(p, c, qT_d, kT_d, v_d, oT_d, NH, NKV, dk, dv, S, scale, qcT_d=None, ocT_d=None, banks=None, ndb=2):
    LK = CTX + S
    NKT = LK // 128
    B = banks if banks is not None else c.banks
    kT = p.sb([128, NKV, LK], BF16, "kT")
    if dk < 128:
        p.memset(kT, 0.0)
    for kv in range(NKV):
        p.dma(kT[0:dk, kv, :], kT_d[kv])
    vp = p.sb([128, NKV, NKT, dv + 1], BF16, "vp")
    p.memset(vp[:, :, :, dv:dv + 1], 1.0)
    for kv in range(NKV):
        p.dma(vp[:, kv, :, 0:dv], v_d[kv].rr("(t p) d -> p t d", p=128))
    selden = p.sb([dv + 1, dv], F32, "selden")
    p.memset(selden, 0.0)
    p.memset(selden[dv:dv + 1, :], 1.0)
    qTs = [p.sb([128, NH, 512], BF16, f"qTs{i}") for i in range(2)]
    if dk < 128:
        for t_ in qTs:
            p.memset(t_, 0.0)
    pT = [p.sb([128, 512], BF16, f"pT{i}") for i in range(4)]
    accs = [p.sb([dv + 1, 512], F32, f"accs{i}") for i in range(2)]
    rec = [p.sb([dv, 512], F32, f"rec{i}") for i in range(2)]
    ob = [p.sb([dv, 512], BF16, f"ob{i}") for i in range(2)]
    jobs = []
    for qb in range(S // 512):
        jobs.append((qT_d, oT_d, qb * 512, 512, NKT))
    if qcT_d is not None:
        jobs.append((qcT_d, ocT_d, 0, CTX, CTX // 128))
    DB = c.dbanks
    G3 = (DB[0].shape[1] == 1536)
    items = []
    for ji, (qd, od, q0, n, nkt) in enumerate(jobs):
        G = 3 if (G3 and nkt % 3 == 0) else 2
        for h in range(NH):
            for kg in range(nkt // G):
                items.append((ji, h, kg, G))
    pT2 = [p.sb([128, 3, 512], BF16, f"pT2_{i}") for i in range(3)]
    loaded = set()

    def load_q(ji):
        if ji in loaded or ji >= len(jobs):
            return
        loaded.add(ji)
        qd, od, q0, n, nkt = jobs[ji]
        qs = qTs[ji % 2]
        for h in range(NH):
            p.dma(qs[0:dk, h, 0:n], qd[h, :, q0:q0 + n])

    def issue_s(i):
        ji, h, kg, G = items[i]
        qd, od, q0, n, nkt = jobs[ji]
        load_q(ji)
        kv = h * NKV // NH
        db = DB[i % ndb]
        for a_ in range(G):
            kt = G * kg + a_
            p.mm(db[:, a_ * 512:a_ * 512 + n], kT[:, kv, kt * 128:(kt + 1) * 128], qTs[ji % 2][:, h, 0:n])

    issue_s(0)
    ih = 0
    for i, (ji, h, kg, G) in enumerate(items):
        qd, od, q0, n, nkt = jobs[ji]
        kv = h * NKV // NH
        if h == 0 and kg == 0:
            load_q(ji + 1)
        if ndb == 2 and i + 1 < len(items):
            issue_s(i + 1)
        if G3:
            acc = B[6]
        else:
            acc = B[4 + ih % 2] if ndb == 2 else B[6]
        pt = pT2[i % 3]
        dbv = DB[i % ndb].rr("p (a t) -> p a t", t=512)
        p.act(pt[:, 0:G, 0:n], dbv[:, 0:G, 0:n], AF.Exp, scale=scale)
        if ndb == 1 and i + 1 < len(items):
            issue_s(i + 1)
        for a_ in range(G):
            kt = G * kg + a_
            p.mm(acc[0:dv + 1, 0:n], vp[:, kv, kt, :], pt[:, a_, 0:n], start=(kt == 0), stop=(kt == nkt - 1))
        if kg == nkt // G - 1:
            a = accs[ih % 2]
            p.cp(a[:, 0:n], acc[0:dv + 1, 0:n])
            denb = B[7] if (G3 or ndb == 1) else B[6]
            p.mm(denb[0:dv, 0:n], selden, a[:, 0:n])
            r = rec[ih % 2]
            p.op("dve", lambda e, r=r, n=n, denb=denb: e.reciprocal(r.ap[:, 0:n], denb.ap[0:dv, 0:n]), [denb], [r])
            o = ob[ih % 2]
            p.tt(o[:, 0:n], a[0:dv, 0:n], r[:, 0:n], ALU.mult, eng="pool")
            p.dma(od[h, :, q0:q0 + n], o[:, 0:n])
            ih += 1
            yield ih


def attention_block(*a, **kw):
    for _ in attention_gen(*a, **kw):
        pass


def build_attn1(S):
    p = Prog()
    LK = CTX + S
    qT = p.dram("qT", [4, 64, S], BF16, "ExternalInput")
    kT = p.dram("kT", [1, 64, LK], BF16, "ExternalInput")
    v = p.dram("v", [1, LK, 64], BF16, "ExternalInput")
    oT = p.dram("oT", [4, 64, S], BF16, "ExternalOutput")
    c = Ctx(p, banks=False)
    c.set_banks(3)
    attention_block(p, c, qT, kT, v, oT, 4, 1, 64, 64, S, 64 ** -0.5)
    return p


RW_COLS = 1920


def build_pre0(S, dbg=0):
    TL = S // 4
    NTL = TL // 128
    NTM = NTL + 2
    NTA = NTL + 3
    TOKM = NTM * 128
    p = Prog()
    x_in = p.dram("x", [NTA * 128, D], F32, "ExternalInput")
    mods_fm = p.dram("mods_fm", [128, 48, 2], F32, "ExternalInput")
    n1g = p.dram("norm1_g", [D], F32, "ExternalInput")
    w_in = p.dram("w_in", [D, 2592], F32, "ExternalInput")
    mu_d = p.dram("mu", [RW_COLS], F32, "ExternalInput")
    flags_d = p.dram("flags", [128, 2], F32, "ExternalInput")
    g_qa = p.dram("g_qa", [1, 384], F32, "ExternalInput")
    g_kva = p.dram("g_kva", [1, 256], F32, "ExternalInput")
    w_q_up = p.dram("w_q_up", [384, 768], F32, "ExternalInput")
    w_kv_up = p.dram("w_kv_up", [256, 1024], F32, "ExternalInput")
    g_q = p.dram("g_q", [1, 96], F32, "ExternalInput")
    g_k = p.dram("g_k", [1, 96], F32, "ExternalInput")
    pos_in = p.dram("pos", [128, NTL, 2], F32, "ExternalInput")
    rwT = p.dram("rwT", [15, 128, TL + CTX], F32, "ExternalOutput")
    qT_o = p.dram("qT", [8, 96, TOKM], BF16, "ExternalOutput")
    kT_o = p.dram("kT", [8, 96, TOKM], BF16, "ExternalOutput")
    v_o = p.dram("v", [TOKM, 512], BF16, "ExternalOutput")
    c = Ctx(p)
    B = c.banks
    A, SH = mod_consts(p, c, mods_fm, n1g, 0, 1)
    pos = p.sb([128, NTL, 2], F32, "pos")
    p.dma(pos, pos_in)
    COS, SINS = rope_tables(p, c, pos, NTL, 8)
    hT_all = p.sb([128, 8, NTA * 128], BF16, "hT_all")

    with p.scope():
        gq_bc = p.sb([128, 8, 96], F32, "gq_bc")
        gk_bc = p.sb([128, 8, 96], F32, "gk_bc")
        g96 = p.sb([128, 96], F32, "g96")
        p.dma(g96, g_q.bc([128, 96]))
        p.cp(gq_bc, g96.un(1).bc([128, 8, 96]))
        g96b = p.sb([128, 96], F32, "g96b")
        p.dma(g96b, g_k.bc([128, 96]))
        p.cp(gk_bc, g96b.un(1).bc([128, 8, 96]))
        gqa_bc = p.sb([128, 384], F32, "gqa_bc")
        gkva_bc = p.sb([128, 256], F32, "gkva_bc")
        p.dma(gqa_bc, g_qa.bc([128, 384]))
        p.dma(gkva_bc, g_kva.bc([128, 256]))
        wim = p.sb([128, 8, 672], BF16, "wim")
        p.dma(wim, w_in[:, RW_COLS:2592].rr("(kc p) n -> p kc n", p=128), q="pool")
        wq = p.sb([128, 3, 768], BF16, "wq")
        p.dma(wq, w_q_up.rr("(kc p) n -> p kc n", p=128), q="pool")
        wkv = p.sb([128, 2, 1024], BF16, "wkv")
        p.dma(wkv, w_kv_up.rr("(kc p) n -> p kc n", p=128), q="pool")
        xt = [p.sb([128, D], F32, f"xt{i}") for i in range(2)]
        xnb = [p.sb([128, D], BF16, f"xnb{i}") for i in range(2)]
        junk = p.sb([128, D], BF16, "junk")
        ssq = [p.sb([128, 1], F32, f"ssq{i}") for i in range(2)]
        ssa = [p.sb([128, 2], F32, f"ssa{i}") for i in range(2)]
        anb = [p.sb([128, 640], BF16, f"anb{i}") for i in range(2)]
        anT = [p.sb([128, 5, 128], BF16, f"anT{i}") for i in range(2)]
        krs = [p.sb([128, 32], F32, f"krs{i}") for i in range(2)]
        Ksb = [p.sb([128, 8, 96], F32, f"Ksb{i}") for i in range(2)]
        vb = [p.sb([128, 8, 64], BF16, f"vb{i}") for i in range(2)]
        scrs = [dict(sq=p.sb([128, 768], F32, "sq"), qk=p.sb([128, 768], F32, "qk"), ss=p.sb([128, 8], F32, "ss"),
                     t1=p.sb([128, 768], F32, "t1"), t2=p.sb([128, 768], F32, "t2")) for _ in range(2)]
        qb_ = [p.sb([128, 768], BF16, f"qb{i}") for i in range(2)]
        kb_ = [p.sb([128, 768], BF16, f"kb{i}") for i in range(2)]
        qTs = [p.sb([96, 8, 128], BF16, f"qTs{i}") for i in range(2)]
        kTs = [p.sb([96, 8, 128], BF16, f"kTs{i}") for i in range(2)]
        for t in range(NTA):
            i2 = t % 2
            var = 1 if (NTL <= t < NTL + 2) else 0
            p.dma(xt[i2], x_in[t * 128:(t + 1) * 128, :])
            hT = hT_all[:, :, t * 128:(t + 1) * 128]
            norm1_hT(p, c, xt[i2], A, SH, var, hT, xnb[i2], junk, ssq[i2], (B[0], B[1]))
            if t >= NTM or dbg == 1:
                continue
            for kc in range(8):
                p.mm(B[2][:, 0:384], hT[:, kc, :], wim[:, kc, 0:384], start=(kc == 0), stop=(kc == 7))
            for kc in range(8):
                p.mm(B[3][:, 0:288], hT[:, kc, :], wim[:, kc, 384:672], start=(kc == 0), stop=(kc == 7))
            sa = ssa[i2]
            p.act(junk[:, 0:384], B[2][:, 0:384], AF.Square, accum=sa[:, 0:1])
            p.act(junk[:, 384:640], B[3][:, 0:256], AF.Square, accum=sa[:, 1:2])
            p.ts(sa[:, 0:1], sa[:, 0:1], 256.0 / 384.0, ALU.mult)
            c.rstd(sa, 2, 1.0 / 256.0, NORM_EPS)
            p.stt(anb[i2][:, 0:384], B[2][:, 0:384], sa[:, 0:1], gqa_bc, ALU.mult, ALU.mult)
            p.stt(anb[i2][:, 384:640], B[3][:, 0:256], sa[:, 1:2], gkva_bc, ALU.mult, ALU.mult)
            p.cp(krs[i2], B[3][:, 256:288], eng="act")
            if dbg == 3:
                continue
            b4 = B[4].bitcast(BF16)
            for k5 in range(5):
                p.tr(b4[:, k5 * 128:(k5 + 1) * 128], anb[i2][:, k5 * 128:(k5 + 1) * 128], c.identb)
            p.cp(anT[i2].rr("p a t -> p (a t)"), b4[:, 0:640], eng="act")
            if dbg == 4:
                continue
            for (bk, c0, c1) in ((B[5], 0, 480), (B[6], 480, 768)):
                for kc in range(3):
                    p.mm(bk[:, 0:c1 - c0], anT[i2][:, kc, :], wq[:, kc, c0:c1], start=(kc == 0), stop=(kc == 2))
            if dbg == 51:
                continue
            for (bk, c0) in ((B[7], 0), (B[2], 512)):
                for kc in range(2):
                    p.mm(bk, anT[i2][:, 3 + kc, :], wkv[:, kc, c0:c0 + 512], start=(kc == 0), stop=(kc == 1))
            if dbg == 52:
                continue
            K = Ksb[i2]
            for (bk, h0) in ((B[7], 0), (B[2], 4)):
                kvv = bk.rr("p (h d) -> p h d", d=128)
                p.cp(K[:, h0:h0 + 4, 0:64], kvv[:, :, 0:64])
                if dbg != 531:
                    p.cp(vb[i2][:, h0:h0 + 4, :], kvv[:, :, 64:128], eng=("dve" if dbg == 532 else "act"))
            if dbg != 533:
                p.cp(K[:, :, 64:96], krs[i2].un(1).bc([128, 8, 32]), eng="pool")
            if dbg in (53, 531, 532, 533):
                continue
            p.dma(v_o[t * 128:(t + 1) * 128, :], vb[i2].rr("p h d -> p (h d)"))
            if dbg == 5:
                continue
            rope = None if var == 1 else (64, 8, COS[:, t], SINS[:, t])
            qk_post(p, c, [(B[5][:, 0:480], 5), (B[6][:, 0:288], 3)], 8, 96, gq_bc.rr("p h d -> p (h d)"),
                    qb_[i2], scrs[0], rope)
            if dbg == 6:
                continue
            qk_post(p, c, [(K.rr("p h d -> p (h d)"), 8)], 8, 96, gk_bc.rr("p h d -> p (h d)"), kb_[i2], scrs[1], rope)
            if dbg == 7:
                continue
            for (src, dstT, bk, out_d, eng) in ((qb_[i2], qTs[i2], B[0], qT_o, "act"), (kb_[i2], kTs[i2], B[1], kT_o, "dve")):
                bb = bk.bitcast(BF16)
                for h in range(8):
                    p.tr(bb[0:96, h * 128:(h + 1) * 128], src[:, h * 96:(h + 1) * 96], c.identb)
                p.cp(dstT.rr("p a t -> p (a t)"), bb[0:96, :], eng=eng)
                p.dma(out_d[:, :, t * 128:(t + 1) * 128].rr("a p t -> p a t"), dstT)

    with p.scope():
        mufm = c.load_fm(mu_d.rr("(c p) -> c p", p=128), 15)
        om = p.sb([128, 15], F32, "om")
        hm = p.sb([128, 15], F32, "hm")
        p.ts(om, mufm, -1.0, ALU.mult, 1.0, ALU.add)
        p.ts(hm, mufm, 0.5, ALU.mult)
        flags = p.sb([128, 2], F32, "flags")
        p.dma(flags, flags_d)
        W = TL + CTX + 4
        E = [p.sb([128, W], F32, f"E{i}") for i in range(2)]
        for i in range(2):
            p.memset(E[i][:, TL + 2:TL + 3], 0.0)
            p.memset(E[i][:, W - 1:W], 0.0)
        hal = [p.sb([128, 2], F32, f"hal{i}") for i in range(2)]
        sm = [p.sb([128, TL + CTX], F32, f"sm{i}") for i in range(2)]
        tm = [p.sb([128, TL + CTX], F32, f"tm{i}") for i in range(2)]
        wc = [p.sb([128, 8, 128], BF16, f"wc{i}") for i in range(3)]
        blks = []
        t0 = 0
        while t0 < TL:
            n = min(512, TL - t0)
            blks.append((t0, n, 1 + t0))
            t0 += n
        blks.append((TL, CTX, TL + 3))
        ib = 0
        for cc in range(15 if dbg < 2 else 0):
            w = wc[cc % 3]
            e = E[cc % 2]
            p.dma(w, w_in[:, cc * 128:(cc + 1) * 128].rr("(kc p) n -> p kc n", p=128), q="pool")
            for (t0, n, e0) in blks:
                bk = B[ib % 6]
                ib += 1
                for kc in range(8):
                    p.mm(bk[:, 0:n], w[:, kc, :], hT_all[:, kc, t0:t0 + n], start=(kc == 0), stop=(kc == 7))
                p.cp(e[:, e0:e0 + n], bk[:, 0:n], eng=("act" if ib % 2 else "dve"))
            bk = B[6 + cc % 2]
            hoff = (NTL + 2) * 128
            for kc in range(8):
                p.mm(bk[:, 0:2], w[:, kc, :], hT_all[:, kc, hoff:hoff + 2], start=(kc == 0), stop=(kc == 7))
            p.tt(hal[cc % 2], bk[:, 0:2], flags, ALU.mult)
            p.cp(e[:, 0:1], hal[cc % 2][:, 0:1], eng="pool")
            p.cp(e[:, TL + 1:TL + 2], hal[cc % 2][:, 1:2], eng="pool")
            s_, t_ = sm[cc % 2], tm[cc % 2]
            for (o0, n, e0) in ((0, TL, 1), (TL, CTX, TL + 3)):
                p.tt(s_[:, o0:o0 + n], e[:, e0 - 1:e0 - 1 + n], e[:, e0 + 1:e0 + 1 + n], ALU.add, eng="pool")
                p.ts(t_[:, o0:o0 + n], e[:, e0:e0 + n], om[:, cc:cc + 1], ALU.mult)
                p.stt(s_[:, o0:o0 + n], s_[:, o0:o0 + n], hm[:, cc:cc + 1], t_[:, o0:o0 + n], ALU.mult, ALU.add)
            p.dma(rwT[cc], s_)
    return p


LN_X_EPS = 64e-5
LAM = math.exp(-0.5)


def build_mix0(S, do_attn=True, RWP=BF16):
    LT = CTX + S
    NPAIR = LT // 128
    p = Prog()
    rw_r = p.dram("rw_r", [128, LT], F32, "ExternalInput")
    rw_k = p.dram("rw_k", [128, LT], F32, "ExternalInput")
    rw_v = p.dram("rw_v", [128, LT], F32, "ExternalInput")
    lo_w = p.dram("lo_w", [128, LT], F32, "ExternalInput")
    lo_a = p.dram("lo_a", [128, LT], F32, "ExternalInput")
    lo_g = p.dram("lo_g", [128, LT], F32, "ExternalInput")
    w2_d = p.dram("w2", [128, 128], F32, "ExternalInput")
    a2_d = p.dram("a2", [128, 128], F32, "ExternalInput")
    g2_d = p.dram("g2", [128, 128], F32, "ExternalInput")
    pv_d = p.dram("pv", [9, 128], F32, "ExternalInput")
    yT = p.dram("yT", [2, 128, LT], F32, "ExternalOutput")
    rwoT = p.dram("rwoT", [128, LT], BF16, "ExternalOutput")
    if do_attn:
        qT = p.dram("qT", [2, 96, S], BF16, "ExternalInput")
        qcT = p.dram("qcT", [2, 96, CTX], BF16, "ExternalInput")
        kT = p.dram("kT", [2, 96, LT], BF16, "ExternalInput")
        v_d = p.dram("v", [2, LT, 64], BF16, "ExternalInput")
        oT = p.dram("oT", [2, 64, S], BF16, "ExternalOutput")
        ocT = p.dram("ocT", [2, 64, CTX], BF16, "ExternalOutput")
    c = Ctx(p, banks=False)
    _outer = p.scope()
    _outer.__enter__()
    c.set_banks(False)
    B = c.banks
    pv = c.load_fm(pv_d, 9)
    W0 = [pv[:, 0:1], pv[:, 1:2]]
    A0 = [pv[:, 2:3], pv[:, 3:4]]
    KK_, KA_, RK_, LNW, LNB = pv[:, 4:5], pv[:, 5:6], pv[:, 6:7], pv[:, 7:8], pv[:, 8:9]
    omka = p.sb([128, 2], F32, "omka")
    p.ts(omka[:, 0:1], KA_, -1.0, ALU.mult, 1.0, ALU.add)
    p.ts(omka[:, 1:2], KA_, -2.0, ALU.mult, 2.0, ALU.add)
    w2s = p.sb([128, 128], F32, "w2s")
    a2s = p.sb([128, 128], F32, "a2s")
    g2s = p.sb([128, 128], F32, "g2s")
    p.dma(w2s, w2_d)
    p.dma(a2s, a2_d)
    p.dma(g2s, g2_d)
    w2z, a2z = [], []
    for d in range(2):
        for (src_, lst_) in ((w2s, w2z), (a2s, a2z)):
            z = p.sb([128, 128], F32, "loraz")
            p.memset(z, 0.0)
            dp_ = slice(64 * d, 64 * d + 64)
            p.cp(z[dp_, :], src_[dp_, :], eng="pool")
            lst_.append(z)
    p64 = p.sb([128, 1], F32, "p64")
    p.ts(p64, c.pidx, 63.5, ALU.is_gt)
    c64 = p.sb([128, 128], F32, "c64")
    p.ts(c64, c.iof, 63.5, ALU.is_gt)
    same = p.sb([128, 128], F32, "same")
    p.ts(same, c64, p64[:, 0:1], ALU.is_equal)
    masks = {}
    for nm, op_ in (("SU", ALU.is_gt), ("SL", ALU.is_lt), ("IU", ALU.is_ge), ("IL", ALU.is_le)):
        m = p.sb([128, 2, 128], F32, "mask" + nm)
        p.ts(m[:, 0, :], c.iof, c.pidx[:, 0:1], op_)
        p.tt(m[:, 0, :], m[:, 0, :], same, ALU.mult)
        p.cp(m[:, 1, :], m[:, 0, :])
        masks[nm] = m.rr("p a t -> p (a t)")
    ones64 = p.sb([128, 128], F32, "ones64")
    p.ts(ones64, same, 1.0 / 64.0, ALU.mult)
    ident2 = p.sb([128, 2, 128], F32, "ident2")
    p.cp(ident2[:, 0, :], c.identf)
    p.cp(ident2[:, 1, :], c.identf)
    ident2 = ident2.rr("p a t -> p (a t)")
    rmask = p.sb([128, 512], F32, "rmask")
    p.memset(rmask, 1.0)
    p.memset(rmask.rr("p (a t) -> p a t", t=64)[:, :, 0:1], 0.0)

    blocks = [(0, CTX)]
    t0 = CTX
    while t0 < LT:
        blocks.append((t0, 512))
        t0 += 512
    NB = len(blocks)
    order = [list(range(NB)), [0] + list(range(NB - 1, 0, -1))]

    with p.scope():
        def T(shape, nm, dt=F32):
            return p.sb(shape, dt, nm)
        ops_ = []
        for d in range(2):
            two = []
            for i in range(2):
                two.append(dict(Rt=T([128, 512], "Rt"), Rt16=T([128, 512], "Rt16", RWP), Kt=T([128, 512], "Kt", RWP),
                                Bt=T([128, 512], "Bt", RWP), At=T([128, 512], "At", RWP),
                                A_tm=T([128, 4, 128], "A_tm", RWP), K_tm=T([128, 4, 128], "K_tm", RWP), B_tm=T([128, 4, 128], "B_tm", RWP),
                                V_tm=T([128, 4, 128], "V_tm", RWP), etot=T([128, 8], "etot"), yb=T([128, 512], "yb")))
            ops_.append(two)
        tmp = {k: T([128, 512], k) for k in ("r", "k", "v", "lw", "la", "th", "sg", "a", "kk", "t1", "t2", "c", "E", "kd", "b", "Kh", "Bh")}
        tot = T([128, 8], "tot")
        Hbd = [[T([128, 128], f"H{d}{i}") for i in range(2)] for d in range(2)]
        H16 = [[T([128, 128], f"H16{d}{i}", RWP) for i in range(2)] for d in range(2)]
        for d in range(2):
            for i in range(2):
                p.memset(Hbd[d][i], 0.0)
                p.memset(H16[d][i], 0.0)
        hcur = [0, 0]
        pairbuf = []
        for d in range(2):
            pairbuf.append({k: T([128, 256], f"{k}{d}", RWP) for k in ("X", "XT", "X2", "XT2", "P", "P2", "MAK")}
                           | {k: T([128, 256], f"{k}{d}", RWP) for k in ("MRK", "MRB", "WT")}
                           | {"AT": T([128, 128], f"AT{d}", RWP), "U": T([128, 128], f"U{d}", RWP)})
            p.memset(pairbuf[d]["U"], 0.0)

        def prep(d, bi, ob):
            t0, n = blocks[bi]
            tok = slice(t0, t0 + n)
            nch = n // 64
            dp = slice(64 * d, 64 * d + 64)
            x = tmp
            p.dma(x["r"][:, 0:n], rw_r[:, tok])
            p.dma(x["k"][:, 0:n], rw_k[:, tok])
            p.dma(x["v"][:, 0:n], rw_v[:, tok])
            p.dma(x["lw"][:, 0:n], lo_w[:, tok])
            p.dma(x["la"][:, 0:n], lo_a[:, tok])
            N = slice(0, n)
            p.act(x["th"][:, N], x["lw"][:, N], AF.Tanh)
            p.mm(B[6][:, N], w2z[d], x["th"][:, N])
            p.act(x["sg"][:, N], B[6][:, N], AF.Sigmoid, bias=W0[d])
            p.mm(B[4][:, N], a2z[d], x["la"][:, N])
            p.act(x["a"][:, N], B[4][:, N], AF.Sigmoid, bias=A0[d])
            p.ts(x["kk"][:, N], x["k"][:, N], KK_, ALU.mult)
            p.tt(x["t1"][:, N], x["kk"][:, N], x["kk"][:, N], ALU.mult, eng="pool")
            p.mm(B[6][:, N], same, x["t1"][:, N])
            p.ts(x["t2"][:, N], B[6][:, N], 1e-12, ALU.add)
            p.act(x["t2"][:, N], x["t2"][:, N], AF.Ln)
            p.act(x["t2"][:, N], x["t2"][:, N], AF.Exp, scale=-0.5)
            p.tt(x["kk"][:, N], x["kk"][:, N], x["t2"][:, N], ALU.mult)
            p.ts(x["t1"][:, N], x["a"][:, N], KA_, ALU.mult, omka[:, 0:1], ALU.add)
            p.tt(x["kd"][:, N], x["k"][:, N], x["t1"][:, N], ALU.mult, eng="pool")
            p.tt(x["b"][:, N], x["kk"][:, N], x["a"][:, N], ALU.mult, eng="pool")
            p.op("dve", lambda e: e.tensor_tensor_scan(x["c"].ap[:, N], rmask.ap[:, N], x["sg"].ap[:, N], 0.0,
                                                       ALU.mult, ALU.add), [rmask, x["sg"]], [x["c"]])
            cv = x["c"][:, N].rr("p (a t) -> p a t", t=64)
            p.cp(tot[:, 0:nch], cv[:, :, 63])
            if d == 1:
                p.tt(x["c"][:, N], x["sg"][:, N], x["c"][:, N], ALU.subtract)
                p.tt(cv, cv, tot[:, 0:nch].un(2).bc([128, nch, 64]), ALU.add)
            p.act(ob["etot"][:, 0:nch], tot[:, 0:nch], AF.Exp, scale=-LAM)
            p.act(x["E"][:, N], x["c"][:, N], AF.Exp, scale=-LAM)
            p.tt(ob["Rt"][:, N], x["r"][:, N], x["E"][:, N], ALU.mult)
            p.cp(ob["Rt16"][:, N], ob["Rt"][:, N], eng="pool")
            p.act(x["E"][:, N], x["c"][:, N], AF.Exp, scale=LAM)
            p.tt(ob["Kt"][:, N], x["kd"][:, N], x["E"][:, N], ALU.mult)
            p.tt(ob["Bt"][:, N], x["b"][:, N], x["E"][:, N], ALU.mult, eng="pool")
            p.tt(x["t1"][:, N], x["c"][:, N], x["sg"][:, N], ALU.subtract)
            p.act(x["E"][:, N], x["t1"][:, N], AF.Exp, scale=-LAM)
            p.stt(ob["At"][:, N], x["kk"][:, N], -1.0, x["E"][:, N], ALU.mult, ALU.mult)
            p.tt(x["t2"][:, N].rr("p (a t) -> p a t", t=64), tot[:, 0:nch].un(2).bc([128, nch, 64]), cv, ALU.subtract)
            p.act(x["E"][:, N], x["t2"][:, N], AF.Exp, scale=-LAM)
            p.tt(x["Kh"][:, N], x["kd"][:, N], x["E"][:, N], ALU.mult)
            p.tt(x["Bh"][:, N], x["b"][:, N], x["E"][:, N], ALU.mult, eng="pool")
            npair = n // 128
            for (src, dst, bk, eng) in ((ob["At"], ob["A_tm"], B[4], "act"), (x["Kh"], ob["K_tm"], B[6], "dve"),
                                        (x["Bh"], ob["B_tm"], B[4], "act"), (x["v"], ob["V_tm"], B[6], "dve")):
                is16 = (src is ob["At"]) and RWP == BF16
                bkv = bk.bitcast(BF16) if is16 else bk
                for pr in range(npair):
                    p.tr(bkv[:, pr * 128:(pr + 1) * 128], src[:, pr * 128:(pr + 1) * 128], c.identb if is16 else c.identf)
                p.cp(dst.rr("p a t -> p (a t)")[:, 0:n], bkv[:, 0:n], eng=eng)

        def pair_level(d, ob, pr):
            pb = pairbuf[d]
            Tk = slice(pr * 128, (pr + 1) * 128)
            mN, mM, mI = (("SU", "SL", "IU") if d == 0 else ("SL", "SU", "IL"))
            bk0, bk1 = B[2], B[3]
            HB = ((B[0], B[2]), (B[1], B[3]))

            def prod(slot, lhs, rhs):
                for h in range(2):
                    hp = slice(64 * h, 64 * h + 64)
                    p.mm(HB[h][slot][:, 0:128], ob[lhs][hp, Tk], ob[rhs][hp, Tk])

            def evac(slot, dst, mk):
                for h in range(2):
                    hc = slice(128 * h, 128 * h + 128)
                    p.tt(pb[dst][:, hc], HB[h][slot][:, 0:128], masks[mk][:, 0:128], ALU.mult)

            prod(0, "Bt", "At")
            prod(1, "At", "Bt")
            evac(0, "X", mN)
            evac(1, "XT", mM)
            prod(0, "At", "Kt")
            prod(1, "Kt", "Rt16")
            evac(0, "MAK", mM)
            evac(1, "MRK", mI)
            prod(0, "Bt", "Rt16")
            evac(0, "MRB", mI)
            p.tt(pb["P"], pb["X"], ident2, ALU.add, eng="pool")
            X, XT, Pc = pb["X"], pb["XT"], pb["P"]
            X2, XT2, P2 = pb["X2"], pb["XT2"], pb["P2"]
            for k in range(1, 6):
                for h in range(2):
                    hc = slice(128 * h, 128 * h + 128)
                    p.mm(bk1[:, hc], X[:, hc], XT[:, hc])
                    if k < 5:
                        p.mm(bk0[:, hc], XT[:, hc], X[:, hc])
                p.cp(XT2, bk1[:, 0:256], eng="act")
                if k < 5:
                    p.cp(X2, bk0[:, 0:256])
                for h in range(2):
                    hc = slice(128 * h, 128 * h + 128)
                    p.mm(bk0[:, hc], XT2[:, hc], Pc[:, hc])
                p.tt(P2, bk0[:, 0:256], Pc, ALU.add)
                X, X2 = X2, X
                XT, XT2 = XT2, XT
                Pc, P2 = P2, Pc
            for h in range(2):
                hc = slice(128 * h, 128 * h + 128)
                p.mm(bk1[:, hc], ob["A_tm"][:, pr, :], Pc[:, hc])
                p.mm(bk0[:, hc], pb["MAK"][:, hc], Pc[:, hc])
            p.cp(pb["AT"][0:64, :], bk1[0:64, 0:128], eng="act")
            p.cp(pb["AT"][64:128, :], bk1[64:128, 128:256], eng="act")
            p.cp(pb["WT"], bk0[:, 0:256])

        def chunk_level(d, ob, pr, pi):
            pb = pairbuf[d]
            cp_ = slice(64 * pi, 64 * pi + 64)
            ch = pr * 2 + pi
            tcol = slice(pr * 128 + 64 * pi, pr * 128 + 64 * pi + 64)
            Hold = Hbd[d][hcur[d]]
            Hnew = Hbd[d][1 - hcur[d]]
            Hold16 = H16[d][hcur[d]]
            Hnew16 = H16[d][1 - hcur[d]]
            hcur[d] = 1 - hcur[d]
            bu, bh, by = B[4], (B[5] if d == 0 else B[7]), B[6]
            for h in range(2):
                hc = slice(128 * h, 128 * h + 128)
                ic = slice(64 * h, 64 * h + 64)
                p.mm(bu[:, ic], pb["WT"][:, hc], ob["V_tm"][:, pr, ic], start=True, stop=False)
                p.mm(bu[:, ic], pb["AT"], Hold16[:, ic], start=False, stop=True)
            p.cp(pb["U"][cp_, :], bu[cp_, 0:128], eng="act")
            for h in range(2):
                ic = slice(64 * h, 64 * h + 64)
                p.mm(bh[:, ic], ob["K_tm"][cp_, pr, :], ob["V_tm"][cp_, pr, ic], start=True, stop=False)
                p.mm(bh[:, ic], ob["B_tm"][cp_, pr, :], pb["U"][cp_, ic], start=False, stop=True)
            for h in range(2):
                hp = slice(64 * h, 64 * h + 64)
                ic = slice(64 * h, 64 * h + 64)
                p.stt(Hnew[hp, ic], Hold[hp, ic], ob["etot"][hp, ch:ch + 1], bh[hp, ic], ALU.mult, ALU.add)
            p.cp(Hnew16, Hnew, eng="pool")
            for h in range(2):
                ys = slice(64 * h, 64 * h + 64)
                mcol = slice(128 * h + 64 * pi, 128 * h + 64 * pi + 64)
                p.mm(by[:, ys], Hold16, ob["Rt16"][:, tcol], start=True, stop=False)
                p.mm(by[:, ys], ob["V_tm"][:, pr, :], pb["MRK"][:, mcol], start=False, stop=False)
                p.mm(by[:, ys], pb["U"], pb["MRB"][:, mcol], start=False, stop=True)
            p.cp(ob["yb"][0:64, tcol], by[0:64, 0:64], eng="act")
            p.cp(ob["yb"][64:128, tcol], by[64:128, 64:128], eng="act")

        for step in range(NB):
            for d in range(2):
                bi = order[d][step]
                t0, n = blocks[bi]
                ob = ops_[d][step % 2]
                prep(d, bi, ob)
                npair = n // 128
                prs = range(npair) if d == 0 else range(npair - 1, -1, -1)
                for pr in prs:
                    pair_level(d, ob, pr)
                    for pi in ((0, 1) if d == 0 else (1, 0)):
                        chunk_level(d, ob, pr, pi)
                p.dma(yT[d][:, t0:t0 + n], ob["yb"][:, 0:n])

    with p.scope():
        x = {k: p.sb([128, 512], F32, k) for k in ("y0", "y1", "r", "k", "v", "la", "lg", "a0", "a1", "t1", "t2", "t3", "g")}
        ob_ = [p.sb([128, 512], BF16, f"rwo{i}") for i in range(2)]
        for bi, (t0, n) in enumerate(blocks):
            tok = slice(t0, t0 + n)
            N = slice(0, n)
            p.dma(x["y0"][:, N], yT[0][:, tok])
            p.dma(x["y1"][:, N], yT[1][:, tok])
            p.dma(x["r"][:, N], rw_r[:, tok])
            p.dma(x["k"][:, N], rw_k[:, tok])
            p.dma(x["v"][:, N], rw_v[:, tok])
            p.dma(x["la"][:, N], lo_a[:, tok])
            p.dma(x["lg"][:, N], lo_g[:, tok])
            p.tt(x["y0"][:, N], x["y0"][:, N], x["y1"][:, N], ALU.add)
            p.mm(B[0][:, N], ones64, x["y0"][:, N])
            p.tt(x["y0"][:, N], x["y0"][:, N], B[0][:, N], ALU.subtract)
            p.tt(x["t1"][:, N], x["y0"][:, N], x["y0"][:, N], ALU.mult, eng="pool")
            p.mm(B[1][:, N], ones64, x["t1"][:, N])
            p.ts(x["t1"][:, N], B[1][:, N], LN_X_EPS, ALU.add)
            p.act(x["t1"][:, N], x["t1"][:, N], AF.Ln)
            p.act(x["t1"][:, N], x["t1"][:, N], AF.Exp, scale=-0.5)
            p.tt(x["y0"][:, N], x["y0"][:, N], x["t1"][:, N], ALU.mult)
            p.ts(x["y0"][:, N], x["y0"][:, N], LNW, ALU.mult, LNB, ALU.add)
            for d in range(2):
                p.mm(B[2 + d][:, N], a2z[d], x["la"][:, N])
                p.act(x["a%d" % d][:, N], B[2 + d][:, N], AF.Sigmoid, bias=A0[d])
            p.tt(x["a0"][:, N], x["a0"][:, N], x["a1"][:, N], ALU.add, eng="pool")
            p.ts(x["a0"][:, N], x["a0"][:, N], KA_, ALU.mult, omka[:, 1:2], ALU.add)
            p.tt(x["t2"][:, N], x["k"][:, N], x["a0"][:, N], ALU.mult, eng="pool")
            p.stt(x["t2"][:, N], x["t2"][:, N], RK_, x["r"][:, N], ALU.mult, ALU.mult)
            p.mm(B[4][:, N], same, x["t2"][:, N])
            p.tt(x["t3"][:, N], B[4][:, N], x["v"][:, N], ALU.mult)
            p.tt(x["y0"][:, N], x["y0"][:, N], x["t3"][:, N], ALU.add, eng="pool")
            p.act(x["lg"][:, N], x["lg"][:, N], AF.Sigmoid)
            p.mm(B[5][:, N], g2s, x["lg"][:, N])
            o = ob_[bi % 2]
            p.tt(o[:, N], x["y0"][:, N], B[5][:, N], ALU.mult)
            p.dma(rwoT[:, tok], o[:, N])

    _outer.__exit__(None, None, None)
    if do_attn:
        with p.scope():
            c.set_banks(3)
            attention_block(p, c, qT, kT, v_d, oT, 2, 2, 96, 64, S, 96 ** -0.5, qcT_d=qcT, ocT_d=ocT)
    return p


def _make_pos(q, TL):
    NTL = TL // 128
    t = q * TL + np.arange(TL)
    pos = np.stack([t // 64, t % 64], -1).astype(np.float32)
    return np.ascontiguousarray(pos.reshape(NTL, 128, 2).transpose(1, 0, 2))


def _cat(xs, axis):
    return np.ascontiguousarray(np.concatenate(xs, axis=axis))


def kernel(x, c, ctx, c_ctx, ada_w, ada_b, norm1_g, norm2_g, ab_w_in, ab_w_out, rw_mu, rw_w0,
           rw_w2, rw_a0, rw_a2, rw_k_k, rw_k_a, rw_r_k, rw_g2, rw_ln_w, rw_ln_b, mla_g_qa,
           mla_w_q_up, mla_g_kva, mla_w_kv_up, mla_g_q, mla_g_k, gqa_w_in, gqa_w_out, gqa_g_q,
           gqa_g_k, router_w, router_b, moe_w_gate, moe_w_up, moe_w_down, shared_w_gate,
           shared_w_up, shared_w_down):
    f = lambda a: np.ascontiguousarray(np.asarray(a, dtype=np.float32))
    x, c, ctx, c_ctx = f(x), f(c), f(ctx), f(c_ctx)
    S = x.shape[1]
    TL = S // 4
    LT = CTX + S
    R8 = range(8)
    maps = [{"cvec": np.stack([c[r // 4], c_ctx]), "ada_w": f(ada_w), "ada_b": f(ada_b)} for r in R8]
    r0 = run_prog(build_phase0(), maps)
    mods_tm = [f(r0[r]["mods_tm"]) for r in R8]
    mods_fm = [f(r0[r]["mods_fm"]) for r in R8]
    maps = []
    for r in R8:
        b, q = r // 4, r % 4
        halo = np.zeros((128, D), np.float32)
        fl = np.zeros((128, 2), np.float32)
        if q > 0:
            halo[0] = x[b, q * TL - 1]
            fl[:, 0] = 1
        if q < 3:
            halo[1] = x[b, (q + 1) * TL]
            fl[:, 1] = 1
        maps.append({"x": _cat([x[b, q * TL:(q + 1) * TL], ctx[b], halo], 0), "mods_fm": mods_fm[r][0],
                     "norm1_g": f(norm1_g)[0], "w_in": f(ab_w_in)[0], "mu": f(rw_mu)[0], "flags": fl,
                     "g_qa": f(mla_g_qa), "g_kva": f(mla_g_kva), "w_q_up": f(mla_w_q_up)[0],
                     "w_kv_up": f(mla_w_kv_up)[0], "g_q": f(mla_g_q), "g_k": f(mla_g_k), "pos": _make_pos(q, TL)})
    rA = run_prog(build_pre0(S), maps)
    maps = []
    rw_r_k_flat = f(rw_r_k)[0].reshape(512)
    for r in R8:
        b, j = r // 4, r % 4
        cores = [4 * b + qq for qq in range(4)]
        rwT = _cat([rA[4 * b]["rwT"][:, :, TL:]] + [rA[cc]["rwT"][:, :, :TL] for cc in cores], 2)
        cs = slice(128 * j, 128 * j + 128)
        pv = np.stack([f(rw_w0)[0, 0, cs], f(rw_w0)[0, 1, cs], f(rw_a0)[0, 0, cs], f(rw_a0)[0, 1, cs],
                       f(rw_k_k)[0, cs], f(rw_k_a)[0, cs], rw_r_k_flat[cs], f(rw_ln_w)[0, cs], f(rw_ln_b)[0, cs]])
        hs = slice(2 * j, 2 * j + 2)
        qT = _cat([rA[cc]["qT"][hs, :, :TL] for cc in cores], 2)
        qcT = np.ascontiguousarray(rA[4 * b]["qT"][hs, :, TL:])
        kT = _cat([rA[4 * b]["kT"][hs, :, TL:]] + [rA[cc]["kT"][hs, :, :TL] for cc in cores], 2)
        vv = _cat([rA[4 * b]["v"][TL:]] + [rA[cc]["v"][:TL] for cc in cores], 0)
        vv = np.ascontiguousarray(vv.reshape(LT, 8, 64)[:, hs].transpose(1, 0, 2))
        maps.append({"rw_r": np.ascontiguousarray(rwT[j]), "rw_k": np.ascontiguousarray(rwT[4 + j]),
                     "rw_v": np.ascontiguousarray(rwT[8 + j]), "lo_w": np.ascontiguousarray(rwT[12]),
                     "lo_a": np.ascontiguousarray(rwT[13]), "lo_g": np.ascontiguousarray(rwT[14]),
                     "w2": np.ascontiguousarray(f(rw_w2)[0][:, :, cs].reshape(128, 128)),
                     "a2": np.ascontiguousarray(f(rw_a2)[0][:, :, cs].reshape(128, 128)),
                     "g2": np.ascontiguousarray(f(rw_g2)[0][:, cs]), "pv": np.ascontiguousarray(pv),
                     "qT": qT, "qcT": qcT, "kT": kT, "v": vv})
    rB = run_prog(build_mix0(S), maps)
    del rA
    wg0 = _cat([f(moe_w_gate)[0], f(shared_w_gate)[0][None]], 0)
    wu0 = _cat([f(moe_w_up)[0], f(shared_w_up)[0][None]], 0)
    wd0 = _cat([f(moe_w_down)[0], f(shared_w_down)[0][None]], 0)
    maps = []
    for r in R8:
        b, q = r // 4, r % 4
        chunks = []
        for kc in range(4):
            rw = rB[4 * b + kc]["rwoT"]
            chunks.append(_cat([rw[:, CTX + q * TL:CTX + (q + 1) * TL], rw[:, :CTX]], 1))
        for j in range(4):
            o = rB[4 * b + j]["oT"].reshape(128, S)
            oc = rB[4 * b + j]["ocT"].reshape(128, CTX)
            chunks.append(_cat([o[:, q * TL:(q + 1) * TL], oc], 1))
        maps.append({"x": _cat([x[b, q * TL:(q + 1) * TL], ctx[b]], 0), "attT": np.ascontiguousarray(np.stack(chunks, 0)),
                     "mods_tm": mods_tm[r][0], "mods_fm": mods_fm[r][0], "w_out": f(ab_w_out)[0],
                     "norm2_g": f(norm2_g)[0], "router_w": f(router_w), "router_b": f(router_b)[None, :],
                     "wg_all": wg0, "wu_all": wu0, "wd_all": wd0})
    rC = run_prog(build_post(S, True), maps)
    del rB
    x1 = [f(rC[r]["x_out"]) for r in R8]
    maps = []
    for r in R8:
        q = r % 4
        maps.append({"x": x1[r], "mods_fm": mods_fm[r][1], "norm1_g": f(norm1_g)[1], "w_in": f(gqa_w_in)[0],
                     "g_q": f(gqa_g_q), "g_k": f(gqa_g_k), "pos": _make_pos(q, TL)})
    rD = run_prog(build_pre1(S), maps)
    maps = []
    for r in R8:
        b, kvh = r // 4, r % 4
        cores = [4 * b + qq for qq in range(4)]
        qk = _cat([rD[cc]["qkT"][:, :, :TL] for cc in cores], 2).reshape(20, 64, S)
        kc_ = rD[4 * b]["qkT"][:, :, TL:].reshape(20, 64, CTX)
        qT = np.ascontiguousarray(qk[4 * kvh:4 * kvh + 4])
        kT = _cat([kc_[16 + kvh], qk[16 + kvh]], 1)[None]
        vv = _cat([rD[4 * b]["v"][TL:]] + [rD[cc]["v"][:TL] for cc in cores], 0)
        vv = np.ascontiguousarray(vv[:, kvh * 64:(kvh + 1) * 64])[None]
        maps.append({"qT": qT, "kT": np.ascontiguousarray(kT), "v": vv})
    rE = run_prog(build_attn1(S), maps)
    del rD
    wg1 = _cat([f(moe_w_gate)[1], f(shared_w_gate)[1][None]], 0)
    wu1 = _cat([f(moe_w_up)[1], f(shared_w_up)[1][None]], 0)
    wd1 = _cat([f(moe_w_down)[1], f(shared_w_down)[1][None]], 0)
    maps = []
    for r in R8:
        b, q = r // 4, r % 4
        chunks = []
        for kc in range(8):
            o = rE[4 * b + kc // 2]["oT"]
            i0 = (kc % 2) * 2
            chunks.append(o[i0:i0 + 2].reshape(128, S)[:, q * TL:(q + 1) * TL])
        maps.append({"x": np.ascontiguousarray(x1[r][:TL]), "attT": np.ascontiguousarray(np.stack(chunks, 0)),
                     "mods_tm": mods_tm[r][1], "mods_fm": mods_fm[r][1], "w_out": f(gqa_w_out)[0],
                     "norm2_g": f(norm2_g)[1], "router_w": f(router_w), "router_b": f(router_b)[None, :],
                     "wg_all": wg1, "wu_all": wu1, "wd_all": wd1})
    rF = run_prog(build_post(S, False), maps)
    out = np.zeros((2, S, D), np.float32)
    for r in R8:
        b, q = r // 4, r % 4
        out[b, q * TL:(q + 1) * TL] = rF[r]["x_out"]
    return out
```
